# Optimizing a Trainium2 kernel written in Bass

```python
import math
import numpy as np
import jax
import jax.numpy as jnp
from jax import lax

D_MODEL = 1024
BATCH = 8
SEQ = 4096
DEPTH = 1

HEAD_DIM = 64
N_HEADS_NSA = 8
N_KV_NSA = 2
GROUP_NSA = N_HEADS_NSA // N_KV_NSA
N_HEADS_FOX = 8
MIX_WIDTH = (N_HEADS_NSA + N_HEADS_FOX) * HEAD_DIM
NSA_Q_W = N_HEADS_NSA * HEAD_DIM
NSA_KV_W = N_KV_NSA * HEAD_DIM
FOX_W = N_HEADS_FOX * HEAD_DIM
PROJ_SIZES = (NSA_Q_W, NSA_KV_W, NSA_KV_W, NSA_KV_W, NSA_KV_W, NSA_KV_W, NSA_KV_W,
              3 * N_HEADS_NSA, FOX_W, FOX_W, FOX_W, N_HEADS_FOX)
IN_WIDTH = sum(PROJ_SIZES)
D_FF = 2816
D_PLE = 256
ROPE_THETA = 500000.0
ROT_DIM = HEAD_DIM // 4
CMP_BLOCK = 32
CMP_STRIDE = 16
CMP_HIDDEN = 256
SEL_BLOCK = 64
SEL_TOPK = 16
WINDOW = 512
NSA_Q_BLOCK = 32
FOX_Q_BLOCK = 128
FORCED_SCORE = 1e6
EPS = 1e-6
NEG = -1e30

kernel_name = 'hybrid_nsa_fox_macaron_ple'


def rmsnorm(x, g):
    xf = x.astype(jnp.float32)
    y = xf * lax.rsqrt(jnp.mean(xf * xf, axis=-1, keepdims=True) + EPS)
    return (y * g.astype(jnp.float32)).astype(x.dtype)


def swiglu(x, wg, wu, wd):
    return (jax.nn.silu(x @ wg) * (x @ wu)) @ wd


def split_heads(t, n):
    b, s, _ = t.shape
    return t.reshape(b, s, n, HEAD_DIM).transpose(0, 2, 1, 3)


def merge_heads(t):
    b, n, s, d = t.shape
    return t.transpose(0, 2, 1, 3).reshape(b, s, n * d)


def rope_tables(seq):
    pos = jnp.arange(seq, dtype=jnp.float32)
    inv = ROPE_THETA ** (-jnp.arange(0, ROT_DIM, 2, dtype=jnp.float32) / ROT_DIM)
    ang = pos[:, None] * inv[None, :]
    return jnp.cos(ang), jnp.sin(ang)


def partial_rope(x, cos, sin):
    half = ROT_DIM // 2
    xf = x.astype(jnp.float32)
    x1, x2, rest = xf[..., :half], xf[..., half:ROT_DIM], xf[..., ROT_DIM:]
    out = jnp.concatenate([x1 * cos - x2 * sin, x2 * cos + x1 * sin, rest], axis=-1)
    return out.astype(x.dtype)


def masked_softmax(s, mask):
    return jax.nn.softmax(jnp.where(mask, s.astype(jnp.float32), NEG), axis=-1)


def compress_blocks(tok, pos_emb, w1, w2):
    s = tok.shape[2]
    n_cmp = (s - CMP_BLOCK) // CMP_STRIDE + 1
    idx = np.arange(n_cmp)[:, None] * CMP_STRIDE + np.arange(CMP_BLOCK)[None, :]
    blk = tok[:, :, idx, :] + pos_emb
    blk = blk.reshape(blk.shape[0], blk.shape[1], n_cmp, CMP_BLOCK * HEAD_DIM)
    return jax.nn.silu(blk @ w1) @ w2


def nsa_attention(q, k_cmp, v_cmp, k_slc, v_slc, k_win, v_win, gates):
    b, h, s, d = q.shape
    qg = q.reshape(b, N_KV_NSA, GROUP_NSA, s, d)
    gg_all = gates.reshape(b, N_KV_NSA, GROUP_NSA, s, 3)
    n_cmp = k_cmp.shape[2]
    n_sel = s // SEL_BLOCK
    top = min(SEL_TOPK, n_sel)
    cmp_start = np.arange(n_cmp) * CMP_STRIDE
    cmp_end = jnp.asarray(cmp_start + CMP_BLOCK - 1)
    sel_start = np.arange(n_sel) * SEL_BLOCK
    overlap = jnp.asarray(((cmp_start[:, None] <= sel_start[None, :] + SEL_BLOCK - 1)
                           & (cmp_start[:, None] + CMP_BLOCK - 1 >= sel_start[None, :])).astype(np.float32))
    ks_blocks = k_slc.reshape(b, N_KV_NSA, n_sel, SEL_BLOCK, d)
    vs_blocks = v_slc.reshape(b, N_KV_NSA, n_sel, SEL_BLOCK, d)
    pad = ((0, 0), (0, 0), (WINDOW, 0), (0, 0))
    kw_pad = jnp.pad(k_win, pad)
    vw_pad = jnp.pad(v_win, pad)
    bi = np.arange(b)[:, None, None, None]
    hi = np.arange(N_KV_NSA)[None, :, None, None]
    blk_j = jnp.arange(n_sel)
    scale = HEAD_DIM ** -0.5
    qn = NSA_Q_BLOCK

    def block(qb):
        t0 = qb * qn
        tpos = t0 + jnp.arange(qn)
        qq = lax.dynamic_slice_in_dim(qg, t0, qn, axis=3)
        gg = lax.dynamic_slice_in_dim(gg_all, t0, qn, axis=3)
        m_c = cmp_end[None, :] <= tpos[:, None]
        s_c = jnp.einsum('bkgqd,bknd->bkgqn', qq, k_cmp) * scale
        p_c = masked_softmax(s_c, m_c) * m_c
        o_c = jnp.einsum('bkgqn,bknd->bkgqd', p_c.astype(v_cmp.dtype), v_cmp)
        imp = jnp.einsum('bkgqn,nj->bkqj', p_c, overlap)
        cur = tpos // SEL_BLOCK
        valid = blk_j[None, :] <= cur[:, None]
        forced = (blk_j[None, :] == 0) | (blk_j[None, :] == cur[:, None]) | (blk_j[None, :] == cur[:, None] - 1)
        imp = jnp.where(valid, jnp.where(forced, FORCED_SCORE, imp), -1.0)
        vals, sel = lax.top_k(imp, top)
        kg = ks_blocks[bi, hi, sel].reshape(b, N_KV_NSA, qn, top * SEL_BLOCK, d)
        vg = vs_blocks[bi, hi, sel].reshape(b, N_KV_NSA, qn, top * SEL_BLOCK, d)
        kpos = (sel[..., None] * SEL_BLOCK + jnp.arange(SEL_BLOCK)).reshape(b, N_KV_NSA, qn, top * SEL_BLOCK)
        m_s = (kpos <= tpos[:, None]) & jnp.repeat(vals >= 0, SEL_BLOCK, axis=-1)
        s_s = jnp.einsum('bkgqd,bkqnd->bkgqn', qq, kg) * scale
        p_s = masked_softmax(s_s, m_s[:, :, None])
        o_s = jnp.einsum('bkgqn,bkqnd->bkgqd', p_s.astype(vg.dtype), vg)
        kw = lax.dynamic_slice_in_dim(kw_pad, t0, qn + WINDOW, axis=2)
        vw = lax.dynamic_slice_in_dim(vw_pad, t0, qn + WINDOW, axis=2)
        kpos_w = t0 - WINDOW + jnp.arange(qn + WINDOW)
        diff = tpos[:, None] - kpos_w[None, :]
        m_w = (kpos_w[None, :] >= 0) & (diff >= 0) & (diff < WINDOW)
        s_w = jnp.einsum('bkgqd,bknd->bkgqn', qq, kw) * scale
        p_w = masked_softmax(s_w, m_w)
        o_w = jnp.einsum('bkgqn,bknd->bkgqd', p_w.astype(vw.dtype), vw)
        return gg[..., 0:1] * o_c + gg[..., 1:2] * o_s + gg[..., 2:3] * o_w

    out = lax.map(block, jnp.arange(s // qn))
    return jnp.moveaxis(out, 0, 3).reshape(b, h, s, d)


def forgetting_attention(q, k, v, log_f):
    b, h, s, d = q.shape
    c = jnp.cumsum(log_f, axis=-1)
    kpos = jnp.arange(s)
    scale = HEAD_DIM ** -0.5
    qn = FOX_Q_BLOCK

    def block(qb):
        t0 = qb * qn
        tpos = t0 + jnp.arange(qn)
        qq = lax.dynamic_slice_in_dim(q, t0, qn, axis=2)
        cq = lax.dynamic_slice_in_dim(c, t0, qn, axis=2)
        logits = (jnp.einsum('bhqd,bhsd->bhqs', qq, k).astype(jnp.float32) * scale
                  + cq[..., :, None] - c[:, :, None, :])
        p = masked_softmax(logits, kpos[None, :] <= tpos[:, None])
        return jnp.einsum('bhqs,bhsd->bhqd', p.astype(v.dtype), v)

    out = lax.map(block, jnp.arange(s // qn))
    return jnp.moveaxis(out, 0, 2).reshape(b, h, s, d)


def setup_inputs(seed: int = 0) -> dict:
    key = jax.random.key(seed)
    ks = iter(jax.random.split(key, 40))
    nrm = lambda shape, sc: jax.random.normal(next(ks), shape, jnp.float32) * sc
    gain = lambda n: 1.0 + nrm((DEPTH, n), 0.05)
    L = DEPTH
    return {
        'x': nrm((BATCH, SEQ, D_MODEL), 1.0),
        'p': nrm((DEPTH, BATCH, SEQ, D_PLE), 1.0),
        'ffn1_norm': gain(D_MODEL),
        'ffn1_wg': nrm((L, D_MODEL, D_FF), D_MODEL ** -0.5),
        'ffn1_wu': nrm((L, D_MODEL, D_FF), D_MODEL ** -0.5),
        'ffn1_wd': nrm((L, D_FF, D_MODEL), D_FF ** -0.5),
        'mix_norm': gain(D_MODEL),
        'w_in': nrm((L, D_MODEL, IN_WIDTH), D_MODEL ** -0.5),
        'b_forget': 4.0 + nrm((L, N_HEADS_FOX), 0.5),
        'q_norm_nsa': gain(HEAD_DIM),
        'k_norm_cmp': gain(HEAD_DIM),
        'k_norm_slc': gain(HEAD_DIM),
        'k_norm_win': gain(HEAD_DIM),
        'cmp_pos_k': nrm((L, CMP_BLOCK, HEAD_DIM), 0.02),
        'cmp_pos_v': nrm((L, CMP_BLOCK, HEAD_DIM), 0.02),
        'cmp_k_w1': nrm((L, CMP_BLOCK * HEAD_DIM, CMP_HIDDEN), (CMP_BLOCK * HEAD_DIM) ** -0.5),
        'cmp_k_w2': nrm((L, CMP_HIDDEN, HEAD_DIM), CMP_HIDDEN ** -0.5),
        'cmp_v_w1': nrm((L, CMP_BLOCK * HEAD_DIM, CMP_HIDDEN), (CMP_BLOCK * HEAD_DIM) ** -0.5),
        'cmp_v_w2': nrm((L, CMP_HIDDEN, HEAD_DIM), CMP_HIDDEN ** -0.5),
        'q_norm_fox': gain(HEAD_DIM),
        'k_norm_fox': gain(HEAD_DIM),
        'out_norm_nsa': gain(NSA_Q_W),
        'out_norm_fox': gain(FOX_W),
        'w_out': nrm((L, MIX_WIDTH, D_MODEL), MIX_WIDTH ** -0.5),
        'ffn2_norm': gain(D_MODEL),
        'ffn2_wg': nrm((L, D_MODEL, D_FF), D_MODEL ** -0.5),
        'ffn2_wu': nrm((L, D_MODEL, D_FF), D_MODEL ** -0.5),
        'ffn2_wd': nrm((L, D_FF, D_MODEL), D_FF ** -0.5),
        'ple_gate_norm': gain(D_MODEL),
        'ple_w_gate': nrm((L, D_MODEL, D_MODEL), D_MODEL ** -0.5),
        'ple_w_proj': nrm((L, D_PLE, D_MODEL), D_PLE ** -0.5),
        'ple_norm': gain(D_MODEL),
    }


def reference(x, p, ffn1_norm, ffn1_wg, ffn1_wu, ffn1_wd, mix_norm, w_in, b_forget,
              q_norm_nsa, k_norm_cmp, k_norm_slc, k_norm_win, cmp_pos_k, cmp_pos_v,
              cmp_k_w1, cmp_k_w2, cmp_v_w1, cmp_v_w2, q_norm_fox, k_norm_fox,
              out_norm_nsa, out_norm_fox, w_out, ffn2_norm, ffn2_wg, ffn2_wu, ffn2_wd,
              ple_gate_norm, ple_w_gate, ple_w_proj, ple_norm):
    b, s, _ = x.shape
    cos, sin = rope_tables(s)
    split_points = [int(v) for v in np.cumsum(PROJ_SIZES)[:-1]]
    h = x
    for i in range(DEPTH):
        h = h + 0.5 * swiglu(rmsnorm(h, ffn1_norm[i]), ffn1_wg[i], ffn1_wu[i], ffn1_wd[i])
        a = rmsnorm(h, mix_norm[i])
        u = a @ w_in[i]
        (qa, kc, vc, ksl, vsl, kwn, vwn, ga, qf, kf, vf, fl) = jnp.split(u, split_points, axis=-1)
        q_a = partial_rope(rmsnorm(split_heads(qa, N_HEADS_NSA), q_norm_nsa[i]), cos, sin)
        kc_tok = partial_rope(split_heads(kc, N_KV_NSA), cos, sin)
        k_cmp = rmsnorm(compress_blocks(kc_tok, cmp_pos_k[i], cmp_k_w1[i], cmp_k_w2[i]), k_norm_cmp[i])
        v_cmp = compress_blocks(split_heads(vc, N_KV_NSA), cmp_pos_v[i], cmp_v_w1[i], cmp_v_w2[i])
        k_slc = partial_rope(rmsnorm(split_heads(ksl, N_KV_NSA), k_norm_slc[i]), cos, sin)
        k_win = partial_rope(rmsnorm(split_heads(kwn, N_KV_NSA), k_norm_win[i]), cos, sin)
        gates = jax.nn.sigmoid(ga).reshape(b, s, N_HEADS_NSA, 3).transpose(0, 2, 1, 3)
        o_a = nsa_attention(q_a, k_cmp, v_cmp, k_slc, split_heads(vsl, N_KV_NSA),
                            k_win, split_heads(vwn, N_KV_NSA), gates)
        q_b = rmsnorm(split_heads(qf, N_HEADS_FOX), q_norm_fox[i])
        k_b = rmsnorm(split_heads(kf, N_HEADS_FOX), k_norm_fox[i])
        log_f = jax.nn.log_sigmoid((fl + b_forget[i]).astype(jnp.float32)).transpose(0, 2, 1)
        o_b = forgetting_attention(q_b, k_b, split_heads(vf, N_HEADS_FOX), log_f)
        mixed = jnp.concatenate([rmsnorm(merge_heads(o_a), out_norm_nsa[i]),
                                 rmsnorm(merge_heads(o_b), out_norm_fox[i])], axis=-1)
        h = h + mixed @ w_out[i]
        h = h + 0.5 * swiglu(rmsnorm(h, ffn2_norm[i]), ffn2_wg[i], ffn2_wu[i], ffn2_wd[i])
        gate = jax.nn.sigmoid(rmsnorm(h, ple_gate_norm[i]) @ ple_w_gate[i])
        e = rmsnorm(p[i] @ ple_w_proj[i], ple_norm[i])
        h = h + gate * e
    return h
```

```python
import numpy as np
from contextlib import ExitStack
import concourse.bass as bass
import concourse.mybir as mybir
from concourse.bass_utils import run_bass_kernel_spmd

F32 = mybir.dt.float32
BF16 = mybir.dt.bfloat16
AF = mybir.ActivationFunctionType
ALU = mybir.AluOpType
AX = mybir.AxisListType

S = 4096
D = 1024
DFF = 2816
NT = S // 128
NG = S // 512
EPS = 1e-6
SAME_ENGINE_SYNC = True
DBG = dict(kh=2, ng=NG, stage=5)
_UID = [0]


def _u(name):
    _UID[0] += 1
    return "%s_%d" % (name, _UID[0])

_OFF = dict(qa=0, kc=512, vc=640, ksl=768, vsl=896, kwn=1024, vwn=1152, ga=1280, qf=1304, kf=1816, vf=2328, fl=2840)
_SZ = dict(qa=512, kc=128, vc=128, ksl=128, vsl=128, kwn=128, vwn=128, ga=24, qf=512, kf=512, vf=512, fl=8)
_ORDER = ['kc', 'qa', 'ksl', 'kwn', 'qf', 'kf', 'vc', 'vsl', 'vwn', 'vf', 'ga', 'fl']
W_IN_PERM = np.concatenate([np.arange(_OFF[k], _OFF[k] + _SZ[k]) for k in _ORDER])


class Chan:
    def __init__(self, sem):
        self.sem = sem
        self.count = 0
        self.ops = []

    def seal(self):
        for op in self.ops:
            op.chanval = self.count


class Op:
    __slots__ = ("eng", "fn", "deps", "sig", "sigval", "chan", "chanval", "idx")


class Prog:
    ENGS = ("pe", "act", "dve", "pool", "sp")

    def __init__(self, nc, es):
        self.nc = nc
        self.es = es
        self.ops = {e: [] for e in self.ENGS}
        self.last_w = {}
        self.readers = {}
        self.engsem = {e: es.enter_context(nc.semaphore(_u("s_" + e))) for e in self.ENGS}
        self.chans = []

    def chan(self):
        c = Chan(self.es.enter_context(self.nc.semaphore(_u("c"))))
        self.chans.append(c)
        return c

    def add(self, eng, fn, reads=(), writes=(), chan=None):
        op = Op()
        op.eng = eng
        op.fn = fn
        op.sig = False
        op.sigval = 0
        op.chan = chan
        deps = []
        for r in reads:
            w = self.last_w.get(r)
            if w is not None:
                deps.append(w)
        for w in writes:
            lw = self.last_w.get(w)
            if lw is not None:
                deps.append(lw)
            deps.extend(self.readers.get(w, ()))
        best = {}
        for d in deps:
            k = ("c", id(d.chan)) if d.chan is not None else ("e", d.eng)
            if k not in best or best[k].idx < d.idx:
                best[k] = d
        op.deps = list(best.values())
        op.idx = len(self.ops[eng])
        for r in reads:
            self.readers.setdefault(r, []).append(op)
        for w in writes:
            self.last_w[w] = op
            self.readers[w] = []
        if chan is not None:
            chan.count += 16
            op.chanval = chan.count
            chan.ops.append(op)
        self.ops[eng].append(op)
        return op

    def emit(self, block):
        for e in self.ENGS:
            for op in self.ops[e]:
                for d in op.deps:
                    if d.chan is None and (d.eng != op.eng or SAME_ENGINE_SYNC):
                        d.sig = True
        for e in self.ENGS:
            c = 0
            for op in self.ops[e]:
                if op.sig and op.chan is None:
                    c += 1
                    op.sigval = c
        final = [(c.sem, c.count) for c in self.chans if c.count > 0]

        def mk(ename):
            ops = self.ops[ename]

            def body(eng):
                waited = {}
                if ename == "pool":
                    _FILL.clear()
                for op in ops:
                    need = []
                    for d in op.deps:
                        if d.chan is not None:
                            sem, val = d.chan.sem, d.chanval
                        elif d.eng != ename or SAME_ENGINE_SYNC:
                            sem, val = self.engsem[d.eng], d.sigval
                        else:
                            continue
                        k = id(sem)
                        if waited.get(k, 0) >= val:
                            continue
                        need.append((sem, val))
                        waited[k] = val
                    for sem, val in need[:-1]:
                        eng.wait_ge(sem, val)
                    ins = op.fn(eng)
                    if need:
                        ins._wait_ge(need[-1][0], need[-1][1])
                    if op.chan is not None:
                        ins.then_inc(op.chan.sem, 16)
                    elif op.sig:
                        ins.then_inc(self.engsem[ename], 1)
                if ename == "sp":
                    for sem, val in final:
                        eng.wait_ge(sem, val)
                if ename == "pool":
                    for r in _FILL.values():
                        eng.free_register(r)
                    _FILL.clear()
            return body

        block.tensor(mk("pe"))
        block.scalar(mk("act"))
        block.vector(mk("dve"))
        block.gpsimd(mk("pool"))
        block.sync(mk("sp"))


def run_phase(nc, build):
    with ExitStack() as es:
        P = Prog(nc, es)
        build(P, es)
        sems = list(P.engsem.values()) + [c.sem for c in P.chans]
        with nc.Block() as b0:
            def clr(e):
                for sm in sems:
                    e.sem_clear(sm)
            b0.sync(clr)
        with nc.Block() as block:
            P.emit(block)


def sb(nc, es, name, shape, dt):
    return es.enter_context(nc.sbuf_tensor(_u(name), shape, dt))


def ps(nc, es, name, shape, dt):
    return es.enter_context(nc.psum_tensor(_u(name), shape, dt))


def load_cast_rows(P, nc, es, dst, src_rows, ncols, chans, stage, key):
    n = len(src_rows)
    for k in range(n):
        s = k % len(stage)
        st = stage[s]
        ch = chans[s]
        src = src_rows[k]
        P.add("sp", lambda e, st=st, src=src: e.dma_start(out=st[:, 0:ncols], in_=src),
              writes=[("stage", s)], chan=ch)
        eng = "dve" if k % 2 == 0 else "pool"
        P.add(eng, lambda e, st=st, k=k: e.tensor_copy(out=dst[:, k, 0:ncols], in_=st[:, 0:ncols]),
              reads=[("stage", s)], writes=[(key, k)])


def ffn_half_phase(nc, T, src_norm, src_res, dst, gain, wg, wu, wd, f0, nfc, tagp):
    def build(P, es):
        FW = nfc * 128
        wg_s = sb(nc, es, "wg_s", [128, 8, FW], BF16)
        wu_s = sb(nc, es, "wu_s", [128, 8, FW], BF16)
        wd_s = sb(nc, es, "wd_s", [128, nfc, D], BF16)
        stage = [sb(nc, es, "stg%d" % i, [128, FW], F32) for i in range(3)]
        stch = [P.chan() for _ in range(3)]
        gB = sb(nc, es, "gB", [128, D], F32)
        ident = sb(nc, es, "ident", [128, 128], BF16)
        identf = sb(nc, es, "identf", [128, 128], F32)
        epst = sb(nc, es, "epst", [128, 1], F32)
        xn = [sb(nc, es, "xn%d" % i, [128, D], F32) for i in range(2)]
        xr = [sb(nc, es, "xr%d" % i, [128, D], F32) for i in range(2)]
        nb = [sb(nc, es, "nb%d" % i, [128, D], BF16) for i in range(4)]
        junk = sb(nc, es, "junk", [128, D], BF16)
        ssq = [sb(nc, es, "ssq%d" % i, [128, 4], F32) for i in range(2)]
        rs = [sb(nc, es, "rs%d" % i, [128, 4], F32) for i in range(2)]
        nT = [sb(nc, es, "nT%d" % i, [128, 8, 512], BF16) for i in range(2)]
        hT = sb(nc, es, "hT", [128, nfc, 512], BF16)
        sg = [sb(nc, es, "sg%d" % i, [128, 512], F32) for i in range(2)]
        psT = [ps(nc, es, "psT%d" % i, [128, D], BF16) for i in range(2)]
        psg = [ps(nc, es, "psg%d" % i, [128, 512], F32) for i in range(2)]
        psu = [ps(nc, es, "psu%d" % i, [128, 512], F32) for i in range(2)]
        pso = [ps(nc, es, "pso%d" % i, [128, 512], F32) for i in range(2)]
        cx = [P.chan() for _ in range(2)]
        cr = [P.chan() for _ in range(2)]
        co = [P.chan() for _ in range(2)]
        cg = P.chan()

        P.add("sp", lambda e: e.dma_start(out=gB[:, :], in_=gain.partition_broadcast(128)),
              writes=["gB"], chan=cg)
        P.add("pool", lambda e: e.memset(identf[:, :], 0.0), writes=["identf"])
        P.add("pool", lambda e: asel(e, out=identf[:, :], in_=identf[:, :], pattern=[[-1, 128]],
                                                compare_op=ALU.not_equal, fill=1.0, base=0,
                                                channel_multiplier=1),
              reads=["identf"], writes=["identf"])
        P.add("pool", lambda e: e.tensor_copy(out=ident[:, :], in_=identf[:, :]), reads=["identf"], writes=["ident"])
        P.add("pool", lambda e: e.memset(epst[:, :], EPS), writes=["eps"])

        wgv = wg.rearrange("(c p) f -> p c f", p=128)
        wuv = wu.rearrange("(c p) f -> p c f", p=128)
        load_cast_rows(P, nc, es, wg_s, [wgv[:, c, f0 * 128:f0 * 128 + FW] for c in range(8)], FW, stch, stage, "wg")
        load_cast_rows(P, nc, es, wu_s, [wuv[:, c, f0 * 128:f0 * 128 + FW] for c in range(8)], FW, stch, stage, "wu")
        load_cast_rows(P, nc, es, wd_s, [wd[(f0 + c) * 128:(f0 + c + 1) * 128, :] for c in range(nfc)], D, stch, stage, "wd")
        wkeys = [("wg", c) for c in range(8)] + [("wu", c) for c in range(8)]
        wdkeys = [("wd", c) for c in range(nfc)]

        def prep_group(g):
            sl = g % 2
            for k in range(4):
                t = 4 * g + k
                xs = t % 2
                P.add("sp", lambda e, xs=xs, t=t: e.dma_start(out=xn[xs][:, :], in_=src_norm[t * 128:(t + 1) * 128, :]),
                      writes=[("xn", xs)], chan=cx[xs])
                P.add("act", lambda e, xs=xs, sl=sl, k=k: e.activation(
                    out=junk[:, :], in_=xn[xs][:, :], func=AF.Square, accum_out=ssq[sl][:, k:k + 1]),
                    reads=[("xn", xs)], writes=["junk", ("ssq", sl, k)])
                P.add("act", lambda e, sl=sl, k=k: e.activation(out=rs[sl][:, k:k + 1], in_=ssq[sl][:, k:k + 1],
                                                                func=AF.Sqrt, scale=1.0 / D, bias=epst[:, 0:1]),
                      reads=[("ssq", sl, k), "eps"], writes=[("rs", sl, k)])
                P.add("dve", lambda e, sl=sl, k=k: e.reciprocal(out=rs[sl][:, k:k + 1], in_=rs[sl][:, k:k + 1]),
                      reads=[("rs", sl, k)], writes=[("rs", sl, k)])
                P.add("dve", lambda e, xs=xs, sl=sl, k=k: e.scalar_tensor_tensor(
                    out=nb[k][:, :], in0=xn[xs][:, :], scalar=rs[sl][:, k:k + 1], in1=gB[:, :],
                    op0=ALU.mult, op1=ALU.mult),
                    reads=[("xn", xs), ("rs", sl, k), "gB"], writes=[("nb", k)])

        def transposes(g):
            sl = g % 2
            for k in range(4):
                pb = k % 2
                for c in range(8):
                    P.add("pe", lambda e, pb=pb, k=k, c=c: e.transpose(
                        out=psT[pb][:, c * 128:(c + 1) * 128], in_=nb[k][:, c * 128:(c + 1) * 128], identity=ident[:, :]),
                        reads=[("nb", k), "ident"], writes=[("psT", pb)] if c == 0 else [])
                P.last_w[("psT", pb)] = P.ops["pe"][-1]
                P.add("act", lambda e, pb=pb, sl=sl, k=k: e.copy(
                    out=nT[sl][:, :, k * 128:(k + 1) * 128],
                    in_=psT[pb][:, :].rearrange("p (c t) -> p c t", c=8)),
                    reads=[("psT", pb)], writes=[("nT", sl, k)])

        def upgate(g):
            sl = g % 2
            for fc in range(nfc):
                b = fc % 2
                for (wt, pst, nm) in ((wg_s, psg, "psg"), (wu_s, psu, "psu")):
                    for c in range(8):
                        P.add("pe", lambda e, wt=wt, pst=pst, b=b, c=c, fc=fc, sl=sl: e.matmul(
                            out=pst[b][:, :], lhsT=wt[:, c, fc * 128:(fc + 1) * 128], rhs=nT[sl][:, c, :],
                            start=(c == 0), stop=(c == 7)),
                            reads=[("nT", sl, 0), ("nT", sl, 1), ("nT", sl, 2), ("nT", sl, 3)] + (wkeys if g == 0 else []),
                            writes=[(nm, b)] if c == 0 else [])
                    P.last_w[(nm, b)] = P.ops["pe"][-1]
                P.add("act", lambda e, b=b: e.activation(out=sg[b][:, :], in_=psg[b][:, :], func=AF.Silu),
                      reads=[("psg", b)], writes=[("sg", b)])
                P.add("dve", lambda e, b=b, fc=fc: e.tensor_tensor(out=hT[:, fc, :], in0=psu[b][:, :], in1=sg[b][:, :],
                                                                  op=ALU.mult),
                      reads=[("psu", b), ("sg", b)], writes=[("hT", fc)])

        def down(g):
            for k in range(4):
                t = 4 * g + k
                rsl = t % 2
                P.add("sp", lambda e, rsl=rsl, t=t: e.dma_start(out=xr[rsl][:, :], in_=src_res[t * 128:(t + 1) * 128, :]),
                      reads=[("dram", t)], writes=[("xr", rsl)], chan=cr[rsl])
                for half in range(2):
                    b = half
                    for fc in range(nfc):
                        P.add("pe", lambda e, b=b, fc=fc, k=k, half=half: e.matmul(
                            out=pso[b][:, :], lhsT=hT[:, fc, k * 128:(k + 1) * 128],
                            rhs=wd_s[:, fc, half * 512:(half + 1) * 512], start=(fc == 0), stop=(fc == nfc - 1)),
                            reads=[("hT", fc)] + (wdkeys if g == 0 else []),
                            writes=[("pso", b)] if fc == 0 else [])
                    P.last_w[("pso", b)] = P.ops["pe"][-1]
                    P.add("dve", lambda e, b=b, rsl=rsl, half=half: e.scalar_tensor_tensor(
                        out=xr[rsl][:, half * 512:(half + 1) * 512], in0=pso[b][:, :], scalar=0.5,
                        in1=xr[rsl][:, half * 512:(half + 1) * 512], op0=ALU.mult, op1=ALU.add),
                        reads=[("pso", b), ("xr", rsl)], writes=[("xr", rsl)])
                P.add("sp", lambda e, rsl=rsl, t=t: e.dma_start(out=dst[t * 128:(t + 1) * 128, :], in_=xr[rsl][:, :]),
                      reads=[("xr", rsl)], writes=[("dram", t)], chan=co[rsl])

        prep_group(0)
        transposes(0)
        for g in range(NG):
            if g + 1 < NG:
                prep_group(g + 1)
            upgate(g)
            if g + 1 < NG:
                transposes(g + 1)
            down(g)

    run_phase(nc, build)


def proj_phase(nc, T):
    def build(P, es):
        h1 = T["h1"]
        WIN = 2848
        win_s = sb(nc, es, "win_s", [128, 8, WIN], BF16)
        HW_ = WIN // 2
        stage = [sb(nc, es, "pstg%d" % i, [128, HW_], F32) for i in range(3)]
        stch = [P.chan() for _ in range(3)]
        gB = sb(nc, es, "gB", [128, D], F32)
        ident = sb(nc, es, "ident", [128, 128], BF16)
        identf = sb(nc, es, "identf", [128, 128], F32)
        epst = sb(nc, es, "epst", [128, 1], F32)
        g5 = sb(nc, es, "g5", [128, 5, 64], F32)
        GQ = sb(nc, es, "GQ", [128, 28, 64], F32)
        bfg = sb(nc, es, "bfg", [128, 8], F32)
        cosT = sb(nc, es, "cosT", [128, NT, 8], F32)
        sinT = sb(nc, es, "sinT", [128, NT, 8], F32)
        Gall = sb(nc, es, "Gall", [128, NT, 24], F32)
        LFall = sb(nc, es, "LFall", [128, NT, 8], F32)
        xn = [sb(nc, es, "xn%d" % i, [128, D], F32) for i in range(2)]
        nb = [sb(nc, es, "nb%d" % i, [128, D], BF16) for i in range(2)]
        junk = sb(nc, es, "junk", [128, D], BF16)
        ssq = sb(nc, es, "ssq", [128, 2], F32)
        rs = sb(nc, es, "rs", [128, 2], F32)
        aT = [sb(nc, es, "aT%d" % i, [128, 8, 128], BF16) for i in range(2)]
        qk = [sb(nc, es, "qk%d" % i, [128, 32, 64], F32) for i in range(2)]
        sq = sb(nc, es, "sq", [128, 32, 64], F32)
        hs = [sb(nc, es, "hs%d" % i, [128, 32], F32) for i in range(2)]
        rt = [sb(nc, es, "rt%d" % i, [128, 14, 8], F32) for i in range(4)]
        qkb = [sb(nc, es, "qkb%d" % i, [128, 2048], BF16) for i in range(2)]
        qkT = sb(nc, es, "qkT", [128, 16, 512], BF16)
        vst = [sb(nc, es, "vst%d" % i, [128, 768], BF16) for i in range(2)]
        psT = [ps(nc, es, "psT%d" % i, [128, D], BF16) for i in range(2)]
        pq = [ps(nc, es, "pq%d" % i, [128, 512], F32) for i in range(4)]
        psQ = [ps(nc, es, "psQ%d" % i, [128, 8, 128], BF16) for i in range(2)]
        cx = [P.chan() for _ in range(2)]
        cg = P.chan()
        cq = P.chan()
        cv = [P.chan() for _ in range(2)]
        cf = P.chan()

        P.add("sp", lambda e: e.dma_start(out=gB[:, :], in_=T["mix_norm"].partition_broadcast(128)), writes=["gB"], chan=cg)
        for i, nm in enumerate(("q_norm_nsa", "k_norm_slc", "k_norm_win", "q_norm_fox", "k_norm_fox")):
            P.add("sp", lambda e, i=i, nm=nm: e.dma_start(out=g5[:, i, :], in_=T[nm].partition_broadcast(128)),
                  writes=[("g5", i)], chan=cg)
        P.add("sp", lambda e: e.dma_start(out=bfg[:, :], in_=T["b_forget"].partition_broadcast(128)), writes=["bfg"], chan=cg)
        P.add("sp", lambda e: e.dma_start(out=cosT[:, :, :], in_=T["rope_cos"].rearrange("(t p) c -> p t c", p=128)),
              writes=["cosT"], chan=cg)
        P.add("sp", lambda e: e.dma_start(out=sinT[:, :, :], in_=T["rope_sin"].rearrange("(t p) c -> p t c", p=128)),
              writes=["sinT"], chan=cg)
        cg.seal()
        for (i, h0, nh) in ((0, 0, 8), (1, 8, 2), (2, 10, 2), (3, 12, 8), (4, 20, 8)):
            P.add("dve", lambda e, i=i, h0=h0, nh=nh: e.tensor_copy(
                out=GQ[:, h0:h0 + nh, :], in_=g5[:, i, :].unsqueeze(1).to_broadcast([128, nh, 64])),
                reads=[("g5", i)], writes=[("GQ", i)])
        gqk = [("GQ", i) for i in range(5)]
        P.add("pool", lambda e: e.memset(identf[:, :], 0.0), writes=["identf"])
        P.add("pool", lambda e: asel(e, out=identf[:, :], in_=identf[:, :], pattern=[[-1, 128]],
                                                compare_op=ALU.not_equal, fill=1.0, base=0, channel_multiplier=1),
              reads=["identf"], writes=["identf"])
        P.add("pool", lambda e: e.tensor_copy(out=ident[:, :], in_=identf[:, :]), reads=["identf"], writes=["ident"])
        P.add("pool", lambda e: e.memset(epst[:, :], EPS), writes=["eps"])
        wv = T["w_in"].rearrange("(c p) f -> p c f", p=128)
        for hf in range(2):
            for c in range(8):
                k = hf * 8 + c
                s = k % 3
                P.add("sp", lambda e, s=s, c=c, hf=hf: e.dma_start(out=stage[s][:, :], in_=wv[:, c, hf * HW_:(hf + 1) * HW_]),
                      writes=[("stage", s)], chan=stch[s])
                P.add("dve" if k % 2 == 0 else "pool", lambda e, s=s, c=c, hf=hf: e.tensor_copy(
                    out=win_s[:, c, hf * HW_:(hf + 1) * HW_], in_=stage[s][:, :]),
                    reads=[("stage", s)], writes=[("win", c, hf)])
        wkeys = [("win", c, hf) for c in range(8) for hf in range(2)]
        QKTv = T["QKT"].rearrange("(pr two) d s -> (two d) pr s", two=2)
        CH = [(0, 512), (512, 512), (1024, 512), (1536, 512), (2048, 512), (2560, 288)]

        for t in range(NT):
            xs = t % 2
            g, k = t // 4, t % 4
            P.add("sp", lambda e, xs=xs, t=t: e.dma_start(out=xn[xs][:, :], in_=h1[t * 128:(t + 1) * 128, :]),
                  writes=[("xn", xs)], chan=cx[xs])
            P.add("act", lambda e, xs=xs: e.activation(out=junk[:, :], in_=xn[xs][:, :], func=AF.Square,
                                                       accum_out=ssq[:, xs:xs + 1]),
                  reads=[("xn", xs)], writes=["junk", ("ssq", xs)])
            P.add("act", lambda e, xs=xs: e.activation(out=rs[:, xs:xs + 1], in_=ssq[:, xs:xs + 1], func=AF.Sqrt,
                                                       scale=1.0 / D, bias=epst[:, 0:1]),
                  reads=[("ssq", xs), "eps"], writes=[("rs", xs)])
            P.add("dve", lambda e, xs=xs: e.reciprocal(out=rs[:, xs:xs + 1], in_=rs[:, xs:xs + 1]),
                  reads=[("rs", xs)], writes=[("rs", xs)])
            P.add("dve", lambda e, xs=xs: e.scalar_tensor_tensor(
                out=nb[xs][:, :], in0=xn[xs][:, :], scalar=rs[:, xs:xs + 1], in1=gB[:, :], op0=ALU.mult, op1=ALU.mult),
                reads=[("xn", xs), ("rs", xs), "gB"], writes=[("nb", xs)])
            for c in range(8):
                P.add("pe", lambda e, xs=xs, c=c: e.transpose(
                    out=psT[xs][:, c * 128:(c + 1) * 128], in_=nb[xs][:, c * 128:(c + 1) * 128], identity=ident[:, :]),
                    reads=[("nb", xs), "ident"], writes=[("psT", xs)] if c == 0 else [])
            P.last_w[("psT", xs)] = P.ops["pe"][-1]
            P.add("act", lambda e, xs=xs: e.copy(out=aT[xs][:, :, :], in_=psT[xs][:, :].rearrange("p (c t) -> p c t", c=8)),
                  reads=[("psT", xs)], writes=[("aT", xs)])
            for ci, (c0, cw) in enumerate(CH):
                pb = (t * 6 + ci) % 4
                for c in range(8):
                    P.add("pe", lambda e, pb=pb, c=c, c0=c0, cw=cw, xs=xs: e.matmul(
                        out=pq[pb][:, 0:cw], lhsT=aT[xs][:, c, :], rhs=win_s[:, c, c0:c0 + cw],
                        start=(c == 0), stop=(c == 7)),
                        reads=[("aT", xs)] + (wkeys if t == 0 else []), writes=[("pq", pb)] if c == 0 else [])
                P.last_w[("pq", pb)] = P.ops["pe"][-1]
                if ci < 4:
                    P.add("act", lambda e, pb=pb, ci=ci, xs=xs: e.copy(
                        out=qk[xs][:, ci * 8:(ci + 1) * 8, :], in_=pq[pb][:, :].rearrange("p (h d) -> p h d", h=8)),
                        reads=[("pq", pb)], writes=[("qk", xs, ci)])
                    P.add("act", lambda e, pb=pb, ci=ci: e.activation(
                        out=sq[:, ci * 8:(ci + 1) * 8, :], in_=pq[pb][:, :].rearrange("p (h d) -> p h d", h=8), func=AF.Square),
                        reads=[("pq", pb)], writes=[("sq", ci)])
                elif ci == 4:
                    P.add("dve", lambda e, pb=pb, xs=xs: e.tensor_copy(out=vst[xs][:, 0:512], in_=pq[pb][:, 0:512]),
                          reads=[("pq", pb)], writes=[("vst", xs, 0)])
                else:
                    P.add("dve", lambda e, pb=pb, xs=xs: e.tensor_copy(out=vst[xs][:, 512:768], in_=pq[pb][:, 0:256]),
                          reads=[("pq", pb)], writes=[("vst", xs, 1)])
                    P.add("dve", lambda e, pb=pb, t=t: e.tensor_copy(out=Gall[:, t, :], in_=pq[pb][:, 256:280]),
                          reads=[("pq", pb)], writes=[("Gall", t)])
                    P.add("dve", lambda e, pb=pb, t=t: e.tensor_tensor(out=LFall[:, t, :], in0=pq[pb][:, 280:288], in1=bfg[:, :],
                                                                      op=ALU.add),
                          reads=[("pq", pb), "bfg"], writes=[("LFall", t)])
            P.add("sp", lambda e, xs=xs, t=t: e.dma_start(out=T["V"][t * 128:(t + 1) * 128, :], in_=vst[xs][:, :]),
                  reads=[("vst", xs, 0), ("vst", xs, 1)], chan=cv[xs])
            P.add("dve", lambda e, xs=xs: e.tensor_reduce(out=hs[xs][:, :], in_=sq[:, :, :], axis=AX.X, op=ALU.add),
                  reads=[("sq", i) for i in range(4)], writes=[("hs", xs)])
            P.add("act", lambda e, xs=xs: e.activation(out=hs[xs][:, :], in_=hs[xs][:, :], func=AF.Sqrt,
                                                       scale=1.0 / 64, bias=epst[:, 0:1]),
                  reads=[("hs", xs), "eps"], writes=[("hs", xs)])
            P.add("dve", lambda e, xs=xs: e.reciprocal(out=hs[xs][:, :], in_=hs[xs][:, :]),
                  reads=[("hs", xs)], writes=[("hs", xs)])
            qkk = [("qk", xs, i) for i in range(4)]
            P.add("dve", lambda e, xs=xs: e.tensor_tensor(
                out=qk[xs][:, 2:30, :], in0=qk[xs][:, 2:30, :], in1=hs[xs][:, 2:30].unsqueeze(2).to_broadcast([128, 28, 64]),
                op=ALU.mult), reads=qkk + [("hs", xs)], writes=qkk)
            P.add("dve", lambda e, xs=xs: e.tensor_tensor(
                out=qk[xs][:, 2:30, :], in0=qk[xs][:, 2:30, :], in1=GQ[:, :, :], op=ALU.mult),
                reads=qkk + gqk, writes=qkk)
            cb = lambda tab, t=t: tab[:, t, :].unsqueeze(1).to_broadcast([128, 14, 8])
            x1 = lambda xs=xs: qk[xs][:, 0:14, 0:8]
            x2 = lambda xs=xs: qk[xs][:, 0:14, 8:16]
            for j, (src, tab) in enumerate(((x1, cosT), (x2, sinT), (x2, cosT), (x1, sinT))):
                P.add("pool", lambda e, j=j, src=src, tab=tab, cb=cb: e.tensor_tensor(
                    out=rt[j][:, :, :], in0=src(), in1=cb(tab), op=ALU.mult),
                    reads=qkk + ["cosT", "sinT"], writes=[("rt", j)])
            P.add("pool", lambda e, x1=x1: e.tensor_tensor(out=x1(), in0=rt[0][:, :, :], in1=rt[1][:, :, :], op=ALU.subtract),
                  reads=[("rt", 0), ("rt", 1)], writes=qkk)
            P.add("pool", lambda e, x2=x2: e.tensor_tensor(out=x2(), in0=rt[2][:, :, :], in1=rt[3][:, :, :], op=ALU.add),
                  reads=[("rt", 2), ("rt", 3)], writes=qkk)
            P.add("pool", lambda e, xs=xs: e.tensor_copy(out=qkb[xs][:, :], in_=qk[xs][:, :, :].rearrange("p h d -> p (h d)")),
                  reads=qkk, writes=[("qkb", xs)])
            for pr in range(16):
                hb = pr // 8
                P.add("pe", lambda e, pr=pr, hb=hb, xs=xs: e.transpose(
                    out=psQ[hb][:, pr % 8, :], in_=qkb[xs][:, pr * 128:(pr + 1) * 128], identity=ident[:, :]),
                    reads=[("qkb", xs), "ident"], writes=[("psQ", hb)] if pr % 8 == 0 else [])
                if pr % 8 == 7:
                    P.last_w[("psQ", hb)] = P.ops["pe"][-1]
                    P.add("act" if hb == 0 else "dve", (lambda e, hb=hb, k=k: e.copy(
                        out=qkT[:, hb * 8:(hb + 1) * 8, k * 128:(k + 1) * 128], in_=psQ[hb][:, :, :])) if hb == 0 else
                        (lambda e, hb=hb, k=k: e.tensor_copy(
                            out=qkT[:, hb * 8:(hb + 1) * 8, k * 128:(k + 1) * 128], in_=psQ[hb][:, :, :])),
                        reads=[("psQ", hb)], writes=[("qkT", k, hb)])
            if k == 3:
                P.add("sp", lambda e, g=g: e.dma_start(out=QKTv[:, :, g * 512:(g + 1) * 512], in_=qkT[:, :, :]),
                      reads=[("qkT", kk, hb) for kk in range(4) for hb in range(2)], chan=cq)
        P.add("act", lambda e: e.activation(out=Gall[:, :, :], in_=Gall[:, :, :], func=AF.Sigmoid),
              reads=[("Gall", t) for t in range(NT)], writes=["GallF"])
        P.add("sp", lambda e: e.dma_start(out=T["G"].rearrange("(t p) c -> p t c", p=128), in_=Gall[:, :, :]),
              reads=["GallF"], chan=cf)
        P.add("act", lambda e: e.activation(out=LFall[:, :, :], in_=LFall[:, :, :], func=AF.Exp, scale=-1.0),
              reads=[("LFall", t) for t in range(NT)], writes=["LF1"])
        P.add("act", lambda e: e.activation(out=LFall[:, :, :], in_=LFall[:, :, :], func=AF.Ln, bias=1.0),
              reads=["LF1"], writes=["LF2"])
        P.add("dve", lambda e: e.tensor_scalar(out=LFall[:, :, :], in0=LFall[:, :, :], scalar1=-1.0, scalar2=None, op0=ALU.mult),
              reads=["LF2"], writes=["LF3"])
        P.add("sp", lambda e: e.dma_start(out=T["LF"].rearrange("(t p) c -> p t c", p=128), in_=LFall[:, :, :]),
              reads=["LF3"], chan=cf)

    run_phase(nc, build)


_FILL = {}


def asel(e, **kw):
    v = float(kw.pop("fill"))
    r = _FILL.get(v)
    if r is None:
        r = e.alloc_register()
        e.reg_mov(r, v)
        _FILL[v] = r
    return e.affine_select(fill=r, **kw)


def make_ident(P, nc, es):
    ident = sb(nc, es, "ident", [128, 128], BF16)
    identf = sb(nc, es, "identf", [128, 128], F32)
    P.add("pool", lambda e: e.memset(identf[:, :], 0.0), writes=["identf"])
    P.add("pool", lambda e: asel(e, out=identf[:, :], in_=identf[:, :], pattern=[[-1, 128]],
                                            compare_op=ALU.not_equal, fill=1.0, base=0, channel_multiplier=1),
          reads=["identf"], writes=["identf"])
    P.add("pool", lambda e: e.tensor_copy(out=ident[:, :], in_=identf[:, :]), reads=["identf"], writes=["ident"])
    return ident, identf


def cmp_phase(nc, T):
    def build(P, es):
        ident, identf = make_ident(P, nc, es)
        epst = sb(nc, es, "epst", [128, 1], F32)
        P.add("pool", lambda e: e.memset(epst[:, :], EPS), writes=["eps"])
        tok = sb(nc, es, "tok", [64, 4, S], BF16)
        w1s = [sb(nc, es, "w1s%d" % i, [64, 32, 256], BF16) for i in range(2)]
        stg = [sb(nc, es, "cstg%d" % i, [64, 32, 256], F32) for i in range(2)]
        w1f = [sb(nc, es, "w1f%d" % i, [128, 16, 256], F32) for i in range(2)]
        posr = sb(nc, es, "posr", [16, 2, 128], F32)
        posc = sb(nc, es, "posc", [128, 2, 16], F32)
        w2f = sb(nc, es, "w2f", [128, 2, 2, 64], F32)
        w2s = sb(nc, es, "w2s", [128, 2, 2, 64], BF16)
        biasT = sb(nc, es, "biasT", [128, 4], F32)
        gk = sb(nc, es, "gk", [128, 64], F32)
        hidT = [sb(nc, es, "hidT%d" % i, [128, 2, 256], BF16) for i in range(2)]
        ssq = sb(nc, es, "ssq", [128, 4], F32)
        junk = sb(nc, es, "junk", [128, 64], F32)
        kcb = [sb(nc, es, "kcb%d" % i, [128, 64], BF16) for i in range(2)]
        kcT = [sb(nc, es, "kcT%d" % i, [64, 256], BF16) for i in range(2)]
        vce = [sb(nc, es, "vce%d" % i, [128, 2, 65], BF16) for i in range(2)]
        psH = [ps(nc, es, "psH%d" % i, [128, 256], F32) for i in range(2)]
        psO = [ps(nc, es, "psO%d" % i, [128, 64], F32) for i in range(2)]
        psB = ps(nc, es, "psB", [128, 4], F32)
        psP = ps(nc, es, "psP", [128, 2, 16], F32)
        psK = ps(nc, es, "psK", [64, 128], BF16)
        c0 = P.chan()
        c1 = [P.chan() for _ in range(2)]
        co = P.chan()

        for j, h in enumerate((0, 1, 30, 31)):
            P.add("sp", lambda e, j=j, h=h: e.dma_start(out=tok[:, j, :], in_=T["QKT"][h, :, :]), writes=[("tok", j)], chan=c0)
        P.add("sp", lambda e: e.dma_start(out=gk[:, :], in_=T["k_norm_cmp"].partition_broadcast(128)), writes=["gk"], chan=c0)
        for kv, nm in enumerate(("cmp_pos_k", "cmp_pos_v")):
            P.add("sp", lambda e, kv=kv, nm=nm: e.dma_start(
                out=posr[:, kv, :], in_=T[nm].rearrange("(c a) d -> c (a d)", a=2)), writes=[("posr", kv)], chan=c0)
        for kv, nm in enumerate(("cmp_k_w2", "cmp_v_w2")):
            P.add("sp", lambda e, kv=kv, nm=nm: e.dma_start(
                out=w2f[:, kv, :, :], in_=T[nm].rearrange("(c p) d -> p c d", p=128)), writes=[("w2f", kv)], chan=c0)
        for kv, nm in enumerate(("cmp_k_w1", "cmp_v_w1")):
            P.add("sp", lambda e, kv=kv, nm=nm: e.dma_start(
                out=w1f[kv][:, :, :], in_=T[nm].rearrange("(c p) h -> p c h", p=128)), writes=[("w1f", kv)], chan=c0)
        c0.seal()
        for kv, nm in enumerate(("cmp_k_w1", "cmp_v_w1")):
            P.add("sp", lambda e, kv=kv, nm=nm: e.dma_start(
                out=stg[kv][:, :, :], in_=T[nm].rearrange("(l d) h -> d l h", d=64)), writes=[("stg", kv)], chan=c1[kv])
            P.add("dve" if kv == 0 else "pool", lambda e, kv=kv: e.tensor_copy(out=w1s[kv][:, :, :], in_=stg[kv][:, :, :]),
                  reads=[("stg", kv)], writes=[("w1s", kv)])
        P.add("dve", lambda e: e.tensor_copy(out=w2s[:, :, :, :], in_=w2f[:, :, :, :]),
              reads=[("w2f", 0), ("w2f", 1)], writes=["w2s"])
        for kv in range(2):
            P.add("pe", lambda e, kv=kv: e.transpose(out=psP[:, kv, :], in_=posr[:, kv, :], identity=identf[0:16, 0:16]),
                  reads=[("posr", kv), "identf"], writes=[("psP", kv)])
        P.add("dve", lambda e: e.tensor_copy(out=posc[:, :, :], in_=psP[:, :, :]),
              reads=[("psP", 0), ("psP", 1)], writes=["posc"])
        for kv in range(2):
            for hc in range(2):
                for c in range(16):
                    P.add("pe", lambda e, kv=kv, hc=hc, c=c: e.matmul(
                        out=psB[:, kv * 2 + hc:kv * 2 + hc + 1], lhsT=w1f[kv][:, c, hc * 128:(hc + 1) * 128],
                        rhs=posc[:, kv, c:c + 1], start=(c == 0), stop=(c == 15)),
                        reads=[("w1f", kv), "posc"], writes=["psB"] if (c == 0 and kv == 0 and hc == 0) else [])
        P.last_w["psB"] = P.ops["pe"][-1]
        P.add("dve", lambda e: e.tensor_copy(out=biasT[:, :], in_=psB[:, :]), reads=["psB"], writes=["biasT"])
        for i in range(2):
            P.add("pool", lambda e, i=i: e.memset(kcb[i][:, :], 0.0), writes=[("kcb", i)])
            P.add("pool", lambda e, i=i: e.memset(vce[i][:, :, :], 0.0), writes=[("vce", i)])
            P.add("pool", lambda e, i=i: e.memset(vce[i][:, :, 64:65], 1.0), reads=[("vce", i)], writes=[("vce", i)])
            P.add("pool", lambda e, i=i: e.memset(hidT[i][:, :, :], 0.0), writes=[("hidT", i, 0), ("hidT", i, 1)])
        VCv = T["VC"].rearrange("h (c p) e -> h p c e", p=128)
        it = 0
        for kv in range(2):
            for head in range(2):
                sl = it % 2
                it += 1
                tv = tok[:, kv * 2 + head, :].rearrange("p (n r) -> p n r", r=16)
                for hc in range(2):
                    for l in range(32):
                        q, r = l // 16, l % 16
                        P.add("pe", lambda e, kv=kv, hc=hc, l=l, q=q, r=r, tv=tv: e.matmul(
                            out=psH[hc][:, 0:255], lhsT=w1s[kv][:, l, hc * 128:(hc + 1) * 128], rhs=tv[:, q:q + 255, r],
                            start=(l == 0), stop=(l == 31)),
                            reads=[("tok", kv * 2 + head), ("w1s", kv)], writes=[("psH", hc)] if l == 0 else [])
                    P.last_w[("psH", hc)] = P.ops["pe"][-1]
                    P.add("act", lambda e, kv=kv, hc=hc, sl=sl: e.activation(
                        out=hidT[sl][:, hc, 0:255], in_=psH[hc][:, 0:255], func=AF.Silu,
                        bias=biasT[:, kv * 2 + hc:kv * 2 + hc + 1]),
                        reads=[("psH", hc), "biasT"], writes=[("hidT", sl, hc)])
                for ci, (n0, nn) in enumerate(((0, 128), (128, 127))):
                    for hc in range(2):
                        P.add("pe", lambda e, kv=kv, hc=hc, sl=sl, ci=ci, n0=n0, nn=nn: e.matmul(
                            out=psO[ci][0:nn, :], lhsT=hidT[sl][:, hc, n0:n0 + nn], rhs=w2s[:, kv, hc, :],
                            start=(hc == 0), stop=(hc == 1)),
                            reads=[("hidT", sl, 0), ("hidT", sl, 1), "w2s"], writes=[("psO", ci)] if hc == 0 else [])
                    P.last_w[("psO", ci)] = P.ops["pe"][-1]
                    if kv == 0:
                        col = head * 2 + ci
                        P.add("act", lambda e, ci=ci, nn=nn, col=col: e.activation(
                            out=junk[0:nn, :], in_=psO[ci][0:nn, :], func=AF.Square, accum_out=ssq[0:nn, col:col + 1]),
                            reads=[("psO", ci)], writes=["junk", ("ssq", col)])
                        P.add("act", lambda e, nn=nn, col=col: e.activation(
                            out=ssq[0:nn, col:col + 1], in_=ssq[0:nn, col:col + 1], func=AF.Sqrt, scale=1.0 / 64,
                            bias=epst[0:nn, 0:1]), reads=[("ssq", col), "eps"], writes=[("ssq", col)])
                        P.add("dve", lambda e, nn=nn, col=col: e.reciprocal(out=ssq[0:nn, col:col + 1], in_=ssq[0:nn, col:col + 1]),
                              reads=[("ssq", col)], writes=[("ssq", col)])
                        P.add("dve", lambda e, ci=ci, nn=nn, col=col: e.scalar_tensor_tensor(
                            out=kcb[ci][0:nn, :], in0=psO[ci][0:nn, :], scalar=ssq[0:nn, col:col + 1], in1=gk[0:nn, :],
                            op0=ALU.mult, op1=ALU.mult), reads=[("psO", ci), ("ssq", col), "gk"], writes=[("kcb", ci)])
                        P.add("pe", lambda e, ci=ci: e.transpose(out=psK[:, :], in_=kcb[ci][:, :], identity=ident[:, :]),
                              reads=[("kcb", ci), "ident"], writes=["psK"])
                        P.add("act", lambda e, head=head, n0=n0: e.copy(out=kcT[head][:, n0:n0 + 128], in_=psK[:, :]),
                              reads=["psK"], writes=[("kcT", head, n0)])
                    else:
                        P.add("dve", lambda e, ci=ci, nn=nn, head=head: e.tensor_copy(
                            out=vce[head][0:nn, ci, 0:64], in_=psO[ci][0:nn, :]), reads=[("psO", ci)], writes=[("vce", head)])
                if kv == 0:
                    P.add("sp", lambda e, head=head: e.dma_start(out=T["KCT"][head, :, :], in_=kcT[head][:, :]),
                          reads=[("kcT", head, 0), ("kcT", head, 128)], chan=co)
                else:
                    P.add("sp", lambda e, head=head: e.dma_start(out=VCv[head], in_=vce[head][:, :, :]),
                          reads=[("vce", head)], chan=co)

    run_phase(nc, build)


def nsa_phase(nc, T):
    BIG = 2048.0
    TINY = 1e-30

    def build(P, es):
        ident, identf = make_ident(P, nc, es)
        QB = sb(nc, es, "QB", [128, 4, S], BF16)
        KE = sb(nc, es, "KE", [128, S], BF16)
        KW = sb(nc, es, "KW", [64, S], BF16)
        KC = sb(nc, es, "KC", [64, 256], BF16)
        Vs = sb(nc, es, "Vs", [128, NT, 72], BF16)
        Vw = sb(nc, es, "Vw", [128, NT, 72], BF16)
        VCs = sb(nc, es, "VCs", [128, 2, 72], BF16)
        OVf = sb(nc, es, "OVf", [128, 2, 72], F32)
        OV = sb(nc, es, "OV", [128, 2, 72], BF16)
        Gs = sb(nc, es, "Gs", [128, NT, 24], F32)
        ET = [sb(nc, es, "ET%d" % i, [128, 512], BF16) for i in range(2)]
        PT = [sb(nc, es, "PT%d" % i, [128, 512], BF16) for i in range(4)]
        OCs = sb(nc, es, "OCs", [65, 4, 512], F32)
        OWs = sb(nc, es, "OWs", [65, 4, 512], F32)
        OSs = [sb(nc, es, "OSs%d" % i, [65, 512], F32) for i in range(2)]
        imp = sb(nc, es, "imp", [128, 4, 64], F32)
        impt = sb(nc, es, "impt", [128, 4, 64], F32)
        impm = [sb(nc, es, "impm%d" % i, [128, 64], F32) for i in range(2)]
        rd4 = sb(nc, es, "rd4", [128, 4], F32)
        m1 = sb(nc, es, "m1", [128, 8], F32)
        m2 = sb(nc, es, "m2", [128, 8], F32)
        tmp = sb(nc, es, "tmp", [128, 64], F32)
        thr = sb(nc, es, "thr", [128, 1], F32)
        BN = [sb(nc, es, "BN%d" % i, [128, 128], BF16) for i in range(4)]
        dn = [sb(nc, es, "dn%d" % i, [128, 3], F32) for i in range(2)]
        ost = [sb(nc, es, "ost%d" % i, [128, 4, 256], F32) for i in range(2)]
        psS = [ps(nc, es, "psS%d" % i, [128, 512], F32) for i in range(2)]
        psOC = ps(nc, es, "psOC", [128, 512], F32)
        psOS = ps(nc, es, "psOS", [128, 512], F32)
        psOW = ps(nc, es, "psOW", [128, 512], F32)
        psI = ps(nc, es, "psI", [128, 4, 65], F32)
        psF = ps(nc, es, "psF", [128, 3, 65], F32)
        psBT = ps(nc, es, "psBT", [128, 128], BF16)
        c0 = P.chan()
        cks = [P.chan() for _ in range(2)]
        cst = [P.chan() for _ in range(2)]

        P.add("sp", lambda e: e.dma_start(out=Gs[:, :, :], in_=T["G"].rearrange("(t p) c -> p t c", p=128)), writes=["Gs"], chan=c0)
        P.add("pool", lambda e: e.memset(KE[64:128, :], BIG), writes=["KEm"])
        P.add("pool", lambda e: asel(e, out=KE[64:128, :], in_=KE[64:128, :], pattern=[[1, S]], compare_op=ALU.is_ge,
                                                fill=0.0, base=0, channel_multiplier=-64), reads=["KEm"], writes=["KEm"])
        P.add("pool", lambda e: asel(e, out=KE[64:128, :], in_=KE[64:128, :], pattern=[[-1, S]], compare_op=ALU.is_ge,
                                                fill=0.0, base=63, channel_multiplier=64), reads=["KEm"], writes=["KEm"])
        P.add("pool", lambda e: e.memset(OVf[:, :, :], 1.0), writes=["OVf"])
        for nt in range(2):
            P.add("pool", lambda e, nt=nt: asel(e,
                out=OVf[:, nt, 0:64], in_=OVf[:, nt, 0:64], pattern=[[64, 64]], compare_op=ALU.is_ge, fill=0.0,
                base=63 - 2048 * nt, channel_multiplier=-16), reads=["OVf"], writes=["OVf"])
            P.add("pool", lambda e, nt=nt: asel(e,
                out=OVf[:, nt, 0:64], in_=OVf[:, nt, 0:64], pattern=[[-64, 64]], compare_op=ALU.is_ge, fill=0.0,
                base=2048 * nt + 31, channel_multiplier=16), reads=["OVf"], writes=["OVf"])
        P.add("pool", lambda e: e.tensor_copy(out=OV[:, :, :], in_=OVf[:, :, :]), reads=["OVf"], writes=["OV"])
        for i in range(4):
            P.add("pool", lambda e, i=i: e.memset(BN[i][:, 0:64], 0.0), writes=[("BN0", i)])
        P.add("pool", lambda e: e.memset(Vs[:, :, 64:65], 1.0), writes=["Vs1"])
        P.add("pool", lambda e: e.memset(Vw[:, :, 64:65], 1.0), writes=["Vw1"])
        Vv = T["V"].rearrange("(t p) c -> p t c", p=128)
        OAv = T["OA"].rearrange("(t p) c -> p t c", p=128)

        def mask_ge(tile, base, cm, step):
            return lambda e: asel(e, out=tile[:, :], in_=tile[:, :], pattern=[[step, 512]], compare_op=ALU.is_ge,
                                             fill=0.0, base=base, channel_multiplier=cm)

        ucount = [0]

        def unit(lhsT, rhs, vlhsT, pacc, acc_key, first, last, maskspec, kdeps):
            u = ucount[0]
            ucount[0] += 1
            sb_, pb = u % 2, u % 4
            P.add("pe", lambda e: e.matmul(out=psS[sb_][:, :], lhsT=lhsT, rhs=rhs, start=True, stop=True),
                  reads=kdeps, writes=[("psS", sb_)])
            P.add("act", lambda e: e.activation(out=PT[pb][:, :], in_=psS[sb_][:, :], func=AF.Exp, scale=0.125),
                  reads=[("psS", sb_)], writes=[("PT", pb)])
            if maskspec is not None:
                P.add("pool", mask_ge(PT[pb], *maskspec), reads=[("PT", pb)], writes=[("PT", pb)])
            P.add("pe", lambda e: e.matmul(out=pacc[0:65, :], lhsT=vlhsT, rhs=PT[pb][:, :], start=first, stop=last),
                  reads=[("PT", pb)] + kdeps, writes=[acc_key] if first else [])
            if last:
                P.last_w[acc_key] = P.ops["pe"][-1]

        for kh in range(DBG['kh']):
            ck = cks[kh]
            for g in range(4):
                P.add("sp", lambda e, g=g, kh=kh: e.dma_start(out=QB[0:64, g, :], in_=T["QKT"][2 + 4 * kh + g, :, :]),
                      writes=[("QBq", g)], chan=ck)
            P.add("sp", lambda e, kh=kh: e.dma_start(out=KE[0:64, :], in_=T["QKT"][10 + kh, :, :]), writes=["KEk"], chan=ck)
            P.add("sp", lambda e, kh=kh: e.dma_start(out=KW[:, :], in_=T["QKT"][12 + kh, :, :]), writes=["KW"], chan=ck)
            P.add("sp", lambda e, kh=kh: e.dma_start(out=KC[:, :], in_=T["KCT"][kh, :, :]), writes=["KC"], chan=ck)
            P.add("sp", lambda e, kh=kh: e.dma_start(out=Vs[:, :, 0:64], in_=Vv[:, :, kh * 64:(kh + 1) * 64]),
                  reads=["Vs1"], writes=["Vs"], chan=ck)
            P.add("sp", lambda e, kh=kh: e.dma_start(out=Vw[:, :, 0:64], in_=Vv[:, :, 128 + kh * 64:128 + (kh + 1) * 64]),
                  reads=["Vw1"], writes=["Vw"], chan=ck)
            P.add("sp", lambda e, kh=kh: e.dma_start(out=VCs[:, :, 0:65], in_=T["VC"][kh].rearrange("(c p) e -> p c e", p=128)),
                  writes=["VCs"], chan=ck)
            ck.seal()
            for i in range(DBG['ng']):
                qsl = slice(i * 512, (i + 1) * 512)
                nts = [0] if i < 4 else [0, 1]
                for g in range(4):
                    for nt in nts:
                        P.add("pe", lambda e, nt=nt, g=g, qsl=qsl: e.matmul(
                            out=psS[nt][:, :], lhsT=KC[:, nt * 128:(nt + 1) * 128], rhs=QB[0:64, g, qsl], start=True, stop=True),
                            reads=["KC", ("QBq", g)], writes=[("psS", nt)])
                        P.add("act", lambda e, nt=nt: e.activation(out=ET[nt][:, :], in_=psS[nt][:, :], func=AF.Exp, scale=0.125),
                              reads=[("psS", nt)], writes=[("ET", nt)])
                        P.add("pool", mask_ge(ET[nt], 512 * i - 2048 * nt - 31, -16, 1), reads=[("ET", nt)], writes=[("ET", nt)])
                    for j, nt in enumerate(nts):
                        P.add("pe", lambda e, nt=nt, j=j: e.matmul(
                            out=psOC[0:65, :], lhsT=VCs[:, nt, 0:65], rhs=ET[nt][:, :], start=(j == 0), stop=(j == len(nts) - 1)),
                            reads=[("ET", nt), "VCs"], writes=["psOC"] if j == 0 else [])
                    P.last_w["psOC"] = P.ops["pe"][-1]
                    P.add("act", lambda e, g=g: e.copy(out=OCs[:, g, :], in_=psOC[0:65, :]), reads=["psOC"], writes=[("OCs", g)])
                    for tt in range(4):
                        for j, nt in enumerate(nts):
                            P.add("pe", lambda e, nt=nt, j=j, tt=tt: e.matmul(
                                out=psI[:, tt, :], lhsT=ET[nt][:, tt * 128:(tt + 1) * 128], rhs=OV[:, nt, 0:65],
                                start=(j == 0), stop=(j == len(nts) - 1)),
                                reads=[("ET", nt), "OV"], writes=["psI"] if (j == 0 and tt == 0) else [])
                    P.last_w["psI"] = P.ops["pe"][-1]
                    P.add("dve", lambda e: e.tensor_scalar(out=rd4[:, :], in0=psI[:, :, 64], scalar1=TINY, scalar2=None, op0=ALU.max),
                          reads=["psI"], writes=["rd4"])
                    P.add("dve", lambda e: e.reciprocal(out=rd4[:, :], in_=rd4[:, :]), reads=["rd4"], writes=["rd4"])
                    if g == 0:
                        P.add("dve", lambda e: e.tensor_tensor(
                            out=imp[:, :, :], in0=psI[:, :, 0:64], in1=rd4[:, :].unsqueeze(2).to_broadcast([128, 4, 64]), op=ALU.mult),
                            reads=["psI", "rd4"], writes=["imp"])
                    else:
                        P.add("dve", lambda e: e.tensor_tensor(
                            out=impt[:, :, :], in0=psI[:, :, 0:64], in1=rd4[:, :].unsqueeze(2).to_broadcast([128, 4, 64]), op=ALU.mult),
                            reads=["psI", "rd4"], writes=["impt"])
                        P.add("dve", lambda e: e.tensor_tensor(out=imp[:, :, :], in0=imp[:, :, :], in1=impt[:, :, :], op=ALU.add),
                              reads=["imp", "impt"], writes=["imp"])
                if DBG['stage'] < 2:
                    continue
                for tt in range(4):
                    bs = tt % 2
                    t0 = 512 * i + 128 * tt
                    P.add("pool", lambda e, tt=tt, bs=bs, t0=t0: asel(e,
                        out=impm[bs][:, :], in_=imp[:, tt, :], pattern=[[-64, 64]], compare_op=ALU.is_ge, fill=1.0e6,
                        base=t0 - 128, channel_multiplier=1), reads=["imp"], writes=[("impm", bs)])
                    P.add("pool", lambda e, bs=bs, t0=t0: asel(e,
                        out=impm[bs][:, :], in_=impm[bs][:, :], pattern=[[-64, 64]], compare_op=ALU.is_ge, fill=-1.0,
                        base=t0, channel_multiplier=1), reads=[("impm", bs)], writes=[("impm", bs)])
                    P.add("pool", lambda e, bs=bs: e.memset(impm[bs][:, 0:1], 1.0e6), reads=[("impm", bs)], writes=[("impm", bs)])
                    P.add("dve", lambda e, bs=bs: e.max(out=m1[:, :], in_=impm[bs][:, :]), reads=[("impm", bs)], writes=["m1"])
                    P.add("dve", lambda e, bs=bs: e.match_replace(out=tmp[:, :], in_to_replace=m1[:, :], in_values=impm[bs][:, :],
                                                                  imm_value=-2.0), reads=[("impm", bs), "m1"], writes=["tmp"])
                    P.add("dve", lambda e: e.max(out=m2[:, :], in_=tmp[:, :]), reads=["tmp"], writes=["m2"])
                    P.add("dve", lambda e: e.tensor_scalar(out=thr[:, :], in0=m2[:, 7:8], scalar1=0.0, scalar2=None, op0=ALU.max),
                          reads=["m2"], writes=["thr"])
                    P.add("dve", lambda e, bs=bs, tt=tt: e.tensor_scalar(
                        out=BN[tt][:, 64:128], in0=impm[bs][:, :], scalar1=thr[:, 0:1], scalar2=1.0, op0=ALU.is_ge, op1=ALU.subtract),
                        reads=[("impm", bs), "thr", ("BN0", tt)], writes=[("BN", tt)])
                if DBG['stage'] < 3:
                    continue
                for g in range(4):
                    kts = list(range(max(0, 4 * i - 4), 4 * i + 4))
                    for j, kt in enumerate(kts):
                        if kt >= 4 * i:
                            ms = (-128 * (kt - 4 * i), -1, 1)
                        else:
                            ms = (128 * (kt - 4 * i + 4) - 1, 1, -1)
                        unit(KW[:, kt * 128:(kt + 1) * 128], QB[0:64, g, qsl], Vw[:, kt, 0:65], psOW, "psOW",
                             j == 0, j == len(kts) - 1, ms, ["KW", ("QBq", g), "Vw"])
                    P.add("dve", lambda e, g=g: e.tensor_copy(out=OWs[:, g, :], in_=psOW[0:65, :]), reads=["psOW"], writes=[("OWs", g)])
                if DBG['stage'] < 4:
                    continue
                for tt in range(4):
                    bs = tt % 2
                    t0 = 512 * i + 128 * tt
                    P.add("pe", lambda e, tt=tt: e.transpose(out=psBT[:, :], in_=BN[tt][:, :], identity=ident[:, :]),
                          reads=[("BN", tt), ("BN0", tt), "ident"], writes=["psBT"])
                    P.add("act", lambda e, t0=t0: e.copy(out=QB[64:128, :, t0:t0 + 128],
                                                         in_=psBT[64:128, :].unsqueeze(1).to_broadcast([64, 4, 128])),
                          reads=["psBT"], writes=[("QBm", tt)])
                if DBG['stage'] < 5:
                    continue
                for g in range(4):
                    kts = list(range(0, 4 * i + 4))
                    for j, kt in enumerate(kts):
                        ms = (-128 * (kt - 4 * i), -1, 1) if kt >= 4 * i else None
                        unit(KE[:, kt * 128:(kt + 1) * 128], QB[:, g, qsl], Vs[:, kt, 0:65], psOS, "psOS",
                             j == 0, j == len(kts) - 1, ms,
                             ["KEk", "KEm", ("QBq", g), "Vs"] + [("QBm", tt) for tt in range(4)])
                    osl = g % 2
                    P.add("dve", lambda e, osl=osl: e.tensor_copy(out=OSs[osl][:, :], in_=psOS[0:65, :]), reads=["psOS"], writes=[("OSs", osl)])
                    if "DBGT" in T and kh == 0 and i == DBG.get("dbg_i", 0) and g == 0:
                        P.add("sp", lambda e, g=g: e.dma_start(out=T["DBGT"][0], in_=OCs[:, g, :]), reads=[("OCs", g)], chan=c0)
                        P.add("sp", lambda e, osl=osl: e.dma_start(out=T["DBGT"][1], in_=OSs[osl][:, :]), reads=[("OSs", osl)], chan=c0)
                        P.add("sp", lambda e, g=g: e.dma_start(out=T["DBGT"][2], in_=OWs[:, g, :]), reads=[("OWs", g)], chan=c0)
                    hd = kh * 4 + g
                    for tt in range(4):
                        tile_i = 4 * i + tt
                        ds = tt % 2
                        tsl = slice(tt * 128, (tt + 1) * 128)
                        for b, (src, key) in enumerate(((OCs[:, g, tsl], ("OCs", g)), (OSs[osl][:, tsl], ("OSs", osl)),
                                                         (OWs[:, g, tsl], ("OWs", g)))):
                            P.add("pe", lambda e, b=b, src=src: e.transpose(out=psF[:, b, :], in_=src, identity=identf[0:65, 0:65]),
                                  reads=[key, "identf"], writes=["psF"] if b == 0 else [])
                        P.last_w["psF"] = P.ops["pe"][-1]
                        P.add("dve", lambda e, ds=ds: e.tensor_scalar(out=dn[ds][:, :], in0=psF[:, :, 64], scalar1=TINY, scalar2=None,
                                                                      op0=ALU.max), reads=["psF"], writes=[("dn", ds)])
                        P.add("dve", lambda e, ds=ds: e.reciprocal(out=dn[ds][:, :], in_=dn[ds][:, :]), reads=[("dn", ds)], writes=[("dn", ds)])
                        P.add("dve", lambda e, ds=ds, tile_i=tile_i, hd=hd: e.tensor_tensor(
                            out=dn[ds][:, :], in0=dn[ds][:, :], in1=Gs[:, tile_i, hd * 3:hd * 3 + 3], op=ALU.mult),
                            reads=[("dn", ds), "Gs"], writes=[("dn", ds)])
                        oo = ost[i % 2][:, tt, g * 64:(g + 1) * 64]
                        P.add("dve", lambda e, ds=ds, oo=oo: e.tensor_scalar(out=oo, in0=psF[:, 0, 0:64], scalar1=dn[ds][:, 0:1],
                                                                             scalar2=None, op0=ALU.mult),
                              reads=["psF", ("dn", ds)], writes=[("ost", i % 2, tt, g)])
                        for b in (1, 2):
                            P.add("dve", lambda e, ds=ds, oo=oo, b=b: e.scalar_tensor_tensor(
                                out=oo, in0=psF[:, b, 0:64], scalar=dn[ds][:, b:b + 1], in1=oo, op0=ALU.mult, op1=ALU.add),
                                reads=["psF", ("dn", ds), ("ost", i % 2, tt, g)], writes=[("ost", i % 2, tt, g)])
                if "DBGB" in T and kh == 0:
                    P.add("sp", lambda e, qsl=qsl: e.dma_start(out=T["DBGB"][:, qsl], in_=QB[64:128, 0, qsl]),
                          reads=[("QBm", tt) for tt in range(4)], chan=c0)
                P.add("sp", lambda e, i=i, kh=kh: e.dma_start(out=OAv[:, 4 * i:4 * i + 4, kh * 256:(kh + 1) * 256], in_=ost[i % 2][:, :, :]),
                      reads=[("ost", i % 2, tt, g) for tt in range(4) for g in range(4)], chan=cst[i % 2])

    run_phase(nc, build)


def fox_phase(nc, T):
    TINY = 1e-30

    def build(P, es):
        ident, identf = make_ident(P, nc, es)
        QT = [sb(nc, es, "QT%d" % i, [64, S], BF16) for i in range(2)]
        KT = [sb(nc, es, "KT%d" % i, [64, S], BF16) for i in range(2)]
        Vf = [sb(nc, es, "Vf%d" % i, [128, NT, 72], BF16) for i in range(2)]
        lf = sb(nc, es, "lf", [128, NT, 8], F32)
        U = sb(nc, es, "U", [128, 128], F32)
        ONES = sb(nc, es, "ONES", [128, 128], F32)
        ones32 = sb(nc, es, "ones32", [128, NT], F32)
        cin = sb(nc, es, "cin", [128, NT, 8], F32)
        tot = sb(nc, es, "tot", [128, NT, 8], F32)
        incl = sb(nc, es, "incl", [128, NT, 8], F32)
        call = sb(nc, es, "call", [128, NT, 8], F32)
        biasT = sb(nc, es, "biasT", [128, NG, 8, NT], F32)
        PT = [sb(nc, es, "PT%d" % i, [128, 512], BF16) for i in range(4)]
        OFs = [sb(nc, es, "OFs%d" % i, [65, 512], F32) for i in range(2)]
        dn = [sb(nc, es, "dn%d" % i, [128, 1], F32) for i in range(2)]
        ostf = [sb(nc, es, "ostf%d" % i, [128, 4, 64], F32) for i in range(2)]
        psS = [ps(nc, es, "psS%d" % i, [128, 512], F32) for i in range(2)]
        psO = [ps(nc, es, "psO%d" % i, [128, 512], F32) for i in range(2)]
        psF = [ps(nc, es, "psF%d" % i, [128, 65], F32) for i in range(2)]
        psC = ps(nc, es, "psC", [128, NT * 8], F32)
        psTt = ps(nc, es, "psTt", [128, NT * 8], F32)
        c0 = P.chan()
        ckh = [P.chan() for _ in range(2)]
        cst = [P.chan() for _ in range(2)]
        Vv = T["V"].rearrange("(t p) c -> p t c", p=128)
        OAv = T["OA"].rearrange("(t p) c -> p t c", p=128)

        P.add("sp", lambda e: e.dma_start(out=lf[:, :, :], in_=T["LF"].rearrange("(t p) c -> p t c", p=128)), writes=["lf"], chan=c0)
        P.add("pool", lambda e: e.memset(U[:, :], 1.0), writes=["U"])
        P.add("pool", lambda e: asel(e, out=U[:, :], in_=U[:, :], pattern=[[1, 128]], compare_op=ALU.is_ge, fill=0.0,
                                     base=0, channel_multiplier=-1), reads=["U"], writes=["U"])
        P.add("pool", lambda e: e.memset(ONES[:, :], 1.0), writes=["ONES"])
        P.add("pool", lambda e: e.memset(ones32[:, :], 1.0), writes=["ones32"])
        for i in range(2):
            P.add("pool", lambda e, i=i: e.memset(Vf[i][:, :, 64:65], 1.0), writes=[("Vf1", i)])
        lff = lf[:, :, :].rearrange("p t c -> p (t c)")
        P.add("pe", lambda e: e.matmul(out=psC[:, :], lhsT=U[:, :], rhs=lff, start=True, stop=True), reads=["U", "lf"], writes=["psC"])
        P.add("pe", lambda e: e.matmul(out=psTt[:, :], lhsT=ONES[:, :], rhs=lff, start=True, stop=True), reads=["ONES", "lf"], writes=["psTt"])
        P.add("dve", lambda e: e.tensor_copy(out=cin[:, :, :].rearrange("p t c -> p (t c)"), in_=psC[:, :]), reads=["psC"], writes=["cin"])
        P.add("dve", lambda e: e.tensor_copy(out=tot[:, :, :].rearrange("p t c -> p (t c)"), in_=psTt[:, :]), reads=["psTt"], writes=["tot"])
        for h in range(8):
            P.add("dve", lambda e, h=h: e.tensor_tensor_scan(out=incl[:, :, h], data0=ones32[:, :], data1=tot[:, :, h], initial=0.0,
                                                             op0=ALU.mult, op1=ALU.add), reads=["tot", "ones32"], writes=[("incl", h)])
        inck = [("incl", h) for h in range(8)]
        P.add("dve", lambda e: e.tensor_tensor(out=call[:, :, :], in0=incl[:, :, :], in1=tot[:, :, :], op=ALU.subtract),
              reads=inck + ["tot"], writes=["call"])
        P.add("dve", lambda e: e.tensor_tensor(out=call[:, :, :], in0=call[:, :, :], in1=cin[:, :, :], op=ALU.add),
              reads=["call", "cin"], writes=["call"])
        for i in range(NG):
            for h in range(8):
                P.add("dve", lambda e, i=i, h=h: e.tensor_scalar(
                    out=biasT[:, i, h, :], in0=call[:, :, h], scalar1=-1.0, scalar2=incl[:, 4 * i + 1, h:h + 1],
                    op0=ALU.mult, op1=ALU.add), reads=["call"] + inck, writes=[("biasT", i, h)])
        u = 0
        fi = 0
        for h in range(DBG.get('fh', 8)):
            hs_ = h % 2
            ck = ckh[hs_]
            P.add("sp", lambda e, h=h, hs_=hs_: e.dma_start(out=QT[hs_][:, :], in_=T["QKT"][14 + h, :, :]), writes=[("QT", hs_)], chan=ck)
            P.add("sp", lambda e, h=h, hs_=hs_: e.dma_start(out=KT[hs_][:, :], in_=T["QKT"][22 + h, :, :]), writes=[("KT", hs_)], chan=ck)
            P.add("sp", lambda e, h=h, hs_=hs_: e.dma_start(out=Vf[hs_][:, :, 0:64], in_=Vv[:, :, 256 + 64 * h:256 + 64 * (h + 1)]),
                  reads=[("Vf1", hs_)], writes=[("Vf", hs_)], chan=ck)
            for op in ck.ops[-3:]:
                op.chanval = ck.count
            for i in range(NG):
                qsl = slice(i * 512, (i + 1) * 512)
                ob = fi % 2
                fi += 1
                nk = 4 * i + 4
                for kt in range(nk):
                    sb_, pb = u % 2, u % 4
                    u += 1
                    P.add("pe", lambda e, sb_=sb_, hs_=hs_, kt=kt, qsl=qsl: e.matmul(
                        out=psS[sb_][:, :], lhsT=KT[hs_][:, kt * 128:(kt + 1) * 128], rhs=QT[hs_][:, qsl], start=True, stop=True),
                        reads=[("KT", hs_), ("QT", hs_)], writes=[("psS", sb_)])
                    P.add("act", lambda e, sb_=sb_, pb=pb, i=i, h=h, kt=kt: e.activation(
                        out=PT[pb][:, :], in_=psS[sb_][:, :], func=AF.Exp, scale=0.125, bias=biasT[:, i, h, kt:kt + 1]),
                        reads=[("psS", sb_), ("biasT", i, h)], writes=[("PT", pb)])
                    if kt >= 4 * i:
                        P.add("pool", lambda e, pb=pb, kt=kt, i=i: asel(
                            e, out=PT[pb][:, :], in_=PT[pb][:, :], pattern=[[1, 512]], compare_op=ALU.is_ge, fill=0.0,
                            base=-128 * (kt - 4 * i), channel_multiplier=-1), reads=[("PT", pb)], writes=[("PT", pb)])
                    P.add("pe", lambda e, pb=pb, ob=ob, hs_=hs_, kt=kt, nk=nk: e.matmul(
                        out=psO[ob][0:65, :], lhsT=Vf[hs_][:, kt, 0:65], rhs=PT[pb][:, :], start=(kt == 0), stop=(kt == nk - 1)),
                        reads=[("PT", pb), ("Vf", hs_)], writes=[("psO", ob)] if kt == 0 else [])
                P.last_w[("psO", ob)] = P.ops["pe"][-1]
                P.add("dve", lambda e, ob=ob: e.tensor_copy(out=OFs[ob][:, :], in_=psO[ob][0:65, :]), reads=[("psO", ob)], writes=[("OFs", ob)])
                for tt in range(4):
                    fb = tt % 2
                    P.add("pe", lambda e, fb=fb, ob=ob, tt=tt: e.transpose(
                        out=psF[fb][:, :], in_=OFs[ob][:, tt * 128:(tt + 1) * 128], identity=identf[0:65, 0:65]),
                        reads=[("OFs", ob), "identf"], writes=[("psF", fb)])
                    P.add("dve", lambda e, fb=fb: e.tensor_scalar(out=dn[fb][:, :], in0=psF[fb][:, 64:65], scalar1=TINY, scalar2=None,
                                                                  op0=ALU.max), reads=[("psF", fb)], writes=[("dn", fb)])
                    P.add("dve", lambda e, fb=fb: e.reciprocal(out=dn[fb][:, :], in_=dn[fb][:, :]), reads=[("dn", fb)], writes=[("dn", fb)])
                    P.add("dve", lambda e, fb=fb, ob=ob, tt=tt: e.tensor_scalar(
                        out=ostf[ob][:, tt, :], in0=psF[fb][:, 0:64], scalar1=dn[fb][:, 0:1], scalar2=None, op0=ALU.mult),
                        reads=[("psF", fb), ("dn", fb)], writes=[("ostf", ob, tt)])
                P.add("sp", lambda e, i=i, h=h, ob=ob: e.dma_start(
                    out=OAv[:, 4 * i:4 * i + 4, 512 + 64 * h:512 + 64 * (h + 1)], in_=ostf[ob][:, :, :]),
                    reads=[("ostf", ob, tt) for tt in range(4)], chan=cst[ob])

    run_phase(nc, build)


def norm_rows(P, src, junk, ssq, rs, epst, nb, gB, nparts, width, key):
    for j in range(nparts):
        cs = slice(j * width, (j + 1) * width)
        P.add("act", lambda e, cs=cs, j=j: e.activation(out=junk[:, cs], in_=src[:, cs], func=AF.Square, accum_out=ssq[:, j:j + 1]),
              reads=[key], writes=["junk", ("ssq", key, j)])
    P.add("act", lambda e: e.activation(out=rs[:, 0:nparts], in_=ssq[:, 0:nparts], func=AF.Sqrt, scale=1.0 / width, bias=epst[:, 0:1]),
          reads=[("ssq", key, j) for j in range(nparts)] + ["eps"], writes=[("rs", key)])
    P.add("dve", lambda e: e.reciprocal(out=rs[:, 0:nparts], in_=rs[:, 0:nparts]), reads=[("rs", key)], writes=[("rs", key)])
    for j in range(nparts):
        cs = slice(j * width, (j + 1) * width)
        P.add("dve", lambda e, cs=cs, j=j: e.scalar_tensor_tensor(out=nb[:, cs], in0=src[:, cs], scalar=rs[:, j:j + 1], in1=gB[:, cs],
                                                                  op0=ALU.mult, op1=ALU.mult),
              reads=[key, ("rs", key), "gB", "gB2"], writes=[("nb", key)])


def out_phase(nc, T):
    def build(P, es):
        ident, identf = make_ident(P, nc, es)
        epst = sb(nc, es, "epst", [128, 1], F32)
        P.add("pool", lambda e: e.memset(epst[:, :], EPS), writes=["eps"])
        wo = sb(nc, es, "wo", [128, 8, D], BF16)
        stage = [sb(nc, es, "ostg%d" % i, [128, D], F32) for i in range(2)]
        stch = [P.chan() for _ in range(2)]
        gB = sb(nc, es, "gB", [128, D], F32)
        xn = [sb(nc, es, "xn%d" % i, [128, D], F32) for i in range(2)]
        xr = [sb(nc, es, "xr%d" % i, [128, D], F32) for i in range(2)]
        nb = [sb(nc, es, "nb%d" % i, [128, D], BF16) for i in range(2)]
        junk = sb(nc, es, "junk", [128, D], BF16)
        ssq = [sb(nc, es, "ssq%d" % i, [128, 2], F32) for i in range(2)]
        rs = [sb(nc, es, "rs%d" % i, [128, 2], F32) for i in range(2)]
        mT = [sb(nc, es, "mT%d" % i, [128, 8, 128], BF16) for i in range(2)]
        psT = [ps(nc, es, "psT%d" % i, [128, D], BF16) for i in range(2)]
        pso = [ps(nc, es, "pso%d" % i, [128, 512], F32) for i in range(4)]
        cg = P.chan()
        cx = [P.chan() for _ in range(2)]
        cr = [P.chan() for _ in range(2)]
        co = [P.chan() for _ in range(2)]
        P.add("sp", lambda e: e.dma_start(out=gB[:, 0:512], in_=T["out_norm_nsa"].partition_broadcast(128)), writes=["gB"], chan=cg)
        P.add("sp", lambda e: e.dma_start(out=gB[:, 512:1024], in_=T["out_norm_fox"].partition_broadcast(128)), writes=["gB2"], chan=cg)
        cg.seal()
        wv = T["w_out"].rearrange("(c p) f -> p c f", p=128)
        for c in range(8):
            s_ = c % 2
            P.add("sp", lambda e, s_=s_, c=c: e.dma_start(out=stage[s_][:, :], in_=wv[:, c, :]), writes=[("stage", s_)], chan=stch[s_])
            P.add("dve" if c % 2 == 0 else "pool", lambda e, s_=s_, c=c: e.tensor_copy(out=wo[:, c, :], in_=stage[s_][:, :]),
                  reads=[("stage", s_)], writes=[("wo", c)])
        wkeys = [("wo", c) for c in range(8)]
        for t in range(NT):
            xs = t % 2
            P.add("sp", lambda e, xs=xs, t=t: e.dma_start(out=xn[xs][:, :], in_=T["OA"][t * 128:(t + 1) * 128, :]), writes=[("xn", xs)], chan=cx[xs])
            P.add("sp", lambda e, xs=xs, t=t: e.dma_start(out=xr[xs][:, :], in_=T["h1"][t * 128:(t + 1) * 128, :]), writes=[("xr", xs)], chan=cr[xs])
            norm_rows(P, xn[xs], junk, ssq[xs], rs[xs], epst, nb[xs], gB, 2, 512, ("xn", xs))
            for c in range(8):
                P.add("pe", lambda e, xs=xs, c=c: e.transpose(out=psT[xs][:, c * 128:(c + 1) * 128], in_=nb[xs][:, c * 128:(c + 1) * 128],
                                                              identity=ident[:, :]),
                      reads=[("nb", ("xn", xs)), "ident"], writes=[("psT", xs)] if c == 0 else [])
            P.last_w[("psT", xs)] = P.ops["pe"][-1]
            P.add("act", lambda e, xs=xs: e.copy(out=mT[xs][:, :, :], in_=psT[xs][:, :].rearrange("p (c t) -> p c t", c=8)),
                  reads=[("psT", xs)], writes=[("mT", xs)])
            for half in range(2):
                pb = (t * 2 + half) % 4
                for c in range(8):
                    P.add("pe", lambda e, pb=pb, c=c, xs=xs, half=half: e.matmul(
                        out=pso[pb][:, :], lhsT=mT[xs][:, c, :], rhs=wo[:, c, half * 512:(half + 1) * 512], start=(c == 0), stop=(c == 7)),
                        reads=[("mT", xs)] + (wkeys if t == 0 else []), writes=[("pso", pb)] if c == 0 else [])
                P.last_w[("pso", pb)] = P.ops["pe"][-1]
                P.add("dve", lambda e, pb=pb, xs=xs, half=half: e.tensor_tensor(
                    out=xr[xs][:, half * 512:(half + 1) * 512], in0=pso[pb][:, :], in1=xr[xs][:, half * 512:(half + 1) * 512], op=ALU.add),
                    reads=[("pso", pb), ("xr", xs)], writes=[("xr", xs)])
            P.add("sp", lambda e, xs=xs, t=t: e.dma_start(out=T["h1"][t * 128:(t + 1) * 128, :], in_=xr[xs][:, :]),
                  reads=[("xr", xs)], chan=co[xs])

    run_phase(nc, build)


def ple_phase(nc, T):
    def build(P, es):
        ident, identf = make_ident(P, nc, es)
        epst = sb(nc, es, "epst", [128, 1], F32)
        P.add("pool", lambda e: e.memset(epst[:, :], EPS), writes=["eps"])
        wg = sb(nc, es, "wg", [128, 8, D], BF16)
        wp = sb(nc, es, "wp", [128, 2, D], BF16)
        stage = [sb(nc, es, "lstg%d" % i, [128, D], F32) for i in range(2)]
        stch = [P.chan() for _ in range(2)]
        gB = sb(nc, es, "gB", [128, D], F32)
        gE = sb(nc, es, "gE", [128, D], F32)
        xn = [sb(nc, es, "xn%d" % i, [128, D], F32) for i in range(2)]
        pt = [sb(nc, es, "pt%d" % i, [128, 256], F32) for i in range(2)]
        pb16 = [sb(nc, es, "pb%d" % i, [128, 256], BF16) for i in range(2)]
        nb = [sb(nc, es, "nb%d" % i, [128, D], BF16) for i in range(2)]
        junk = sb(nc, es, "junk", [128, D], BF16)
        ssq = [sb(nc, es, "ssq%d" % i, [128, 4], F32) for i in range(2)]
        rs = [sb(nc, es, "rs%d" % i, [128, 4], F32) for i in range(2)]
        mT = [sb(nc, es, "mT%d" % i, [128, 10, 128], BF16) for i in range(2)]
        gate = [sb(nc, es, "gate%d" % i, [128, D], F32) for i in range(2)]
        ev = [sb(nc, es, "ev%d" % i, [128, D], F32) for i in range(2)]
        psT = [ps(nc, es, "psT%d" % i, [128, 10, 128], BF16) for i in range(1)]
        psg = [ps(nc, es, "psg%d" % i, [128, 512], F32) for i in range(2)]
        pse = [ps(nc, es, "pse%d" % i, [128, 512], F32) for i in range(2)]
        cg = P.chan()
        cx = [P.chan() for _ in range(2)]
        cp = [P.chan() for _ in range(2)]
        co = [P.chan() for _ in range(2)]
        P.add("sp", lambda e: e.dma_start(out=gB[:, :], in_=T["ple_gate_norm"].partition_broadcast(128)), writes=["gB"], chan=cg)
        P.add("sp", lambda e: e.dma_start(out=gE[:, :], in_=T["ple_norm"].partition_broadcast(128)), writes=["gE"], chan=cg)
        cg.seal()
        wv = T["ple_w_gate"].rearrange("(c p) f -> p c f", p=128)
        wpv = T["ple_w_proj"].rearrange("(c p) f -> p c f", p=128)
        for c in range(10):
            s_ = c % 2
            src = wv[:, c, :] if c < 8 else wpv[:, c - 8, :]
            dstw = wg[:, c, :] if c < 8 else wp[:, c - 8, :]
            P.add("sp", lambda e, s_=s_, src=src: e.dma_start(out=stage[s_][:, :], in_=src), writes=[("stage", s_)], chan=stch[s_])
            P.add("dve" if c % 2 == 0 else "pool", lambda e, s_=s_, dstw=dstw: e.tensor_copy(out=dstw, in_=stage[s_][:, :]),
                  reads=[("stage", s_)], writes=[("w", c)])
        wkeys = [("w", c) for c in range(10)]
        for t in range(NT):
            xs = t % 2
            P.add("sp", lambda e, xs=xs, t=t: e.dma_start(out=xn[xs][:, :], in_=T["h3"][t * 128:(t + 1) * 128, :]), writes=[("xn", xs)], chan=cx[xs])
            P.add("sp", lambda e, xs=xs, t=t: e.dma_start(out=pt[xs][:, :], in_=T["p"][t * 128:(t + 1) * 128, :]), writes=[("pt", xs)], chan=cp[xs])
            norm_rows(P, xn[xs], junk, ssq[xs], rs[xs], epst, nb[xs], gB, 1, D, ("xn", xs))
            P.add("pool", lambda e, xs=xs: e.tensor_copy(out=pb16[xs][:, :], in_=pt[xs][:, :]), reads=[("pt", xs)], writes=[("pb16", xs)])
            for c in range(10):
                src = nb[xs][:, c * 128:(c + 1) * 128] if c < 8 else pb16[xs][:, (c - 8) * 128:(c - 7) * 128]
                P.add("pe", lambda e, c=c, src=src: e.transpose(out=psT[0][:, c, :], in_=src, identity=ident[:, :]),
                      reads=[("nb", ("xn", xs)), ("pb16", xs), "ident"], writes=["psT"] if c == 0 else [])
            P.last_w["psT"] = P.ops["pe"][-1]
            P.add("act", lambda e, xs=xs: e.copy(out=mT[xs][:, :, :], in_=psT[0][:, :, :]), reads=["psT"], writes=[("mT", xs)])
            for half in range(2):
                hsl = slice(half * 512, (half + 1) * 512)
                for c in range(8):
                    P.add("pe", lambda e, c=c, xs=xs, half=half, hsl=hsl: e.matmul(
                        out=psg[half][:, :], lhsT=mT[xs][:, c, :], rhs=wg[:, c, hsl], start=(c == 0), stop=(c == 7)),
                        reads=[("mT", xs)] + (wkeys if t == 0 else []), writes=[("psg", half)] if c == 0 else [])
                P.last_w[("psg", half)] = P.ops["pe"][-1]
                P.add("act", lambda e, xs=xs, half=half, hsl=hsl: e.activation(out=gate[xs][:, hsl], in_=psg[half][:, :], func=AF.Sigmoid),
                      reads=[("psg", half)], writes=[("gate", xs, half)])
                for c in range(2):
                    P.add("pe", lambda e, c=c, xs=xs, half=half, hsl=hsl: e.matmul(
                        out=pse[half][:, :], lhsT=mT[xs][:, 8 + c, :], rhs=wp[:, c, hsl], start=(c == 0), stop=(c == 1)),
                        reads=[("mT", xs)] + (wkeys if t == 0 else []), writes=[("pse", half)] if c == 0 else [])
                P.last_w[("pse", half)] = P.ops["pe"][-1]
                P.add("act", lambda e, xs=xs, half=half, hsl=hsl: e.activation(
                    out=junk[:, hsl], in_=pse[half][:, :], func=AF.Square, accum_out=ssq[xs][:, 2 + half:3 + half]),
                    reads=[("pse", half)], writes=[("junk2", half), ("ssqe", xs, half)])
            P.add("dve", lambda e, xs=xs: e.tensor_tensor(out=rs[xs][:, 2:3], in0=ssq[xs][:, 2:3], in1=ssq[xs][:, 3:4], op=ALU.add),
                  reads=[("ssqe", xs, 0), ("ssqe", xs, 1)], writes=[("rse", xs)])
            P.add("act", lambda e, xs=xs: e.activation(out=rs[xs][:, 2:3], in_=rs[xs][:, 2:3], func=AF.Sqrt, scale=1.0 / D, bias=epst[:, 0:1]),
                  reads=[("rse", xs), "eps"], writes=[("rse", xs)])
            P.add("dve", lambda e, xs=xs: e.reciprocal(out=rs[xs][:, 2:3], in_=rs[xs][:, 2:3]), reads=[("rse", xs)], writes=[("rse", xs)])
            for half in range(2):
                hsl = slice(half * 512, (half + 1) * 512)
                P.add("dve", lambda e, xs=xs, half=half, hsl=hsl: e.scalar_tensor_tensor(
                    out=ev[xs][:, hsl], in0=pse[half][:, :], scalar=rs[xs][:, 2:3], in1=gE[:, hsl], op0=ALU.mult, op1=ALU.mult),
                    reads=[("pse", half), ("rse", xs), "gE"], writes=[("ev", xs, half)])
                P.add("pool", lambda e, xs=xs, hsl=hsl: e.tensor_tensor(out=ev[xs][:, hsl], in0=ev[xs][:, hsl], in1=gate[xs][:, hsl], op=ALU.mult),
                      reads=[("ev", xs, half), ("gate", xs, half)], writes=[("ev", xs, half)])
                P.add("pool", lambda e, xs=xs, hsl=hsl: e.tensor_tensor(out=ev[xs][:, hsl], in0=ev[xs][:, hsl], in1=xn[xs][:, hsl], op=ALU.add),
                      reads=[("ev", xs, half), ("xn", xs)], writes=[("ev", xs, half)])
            P.add("sp", lambda e, xs=xs, t=t: e.dma_start(out=T["out"][t * 128:(t + 1) * 128, :], in_=ev[xs][:, :]),
                  reads=[("ev", xs, 0), ("ev", xs, 1)], chan=co[xs])

    run_phase(nc, build)


def rope_tables_np():
    pos = np.arange(S, dtype=np.float32)
    inv = (np.float32(500000.0) ** (-np.arange(0, 16, 2, dtype=np.float32) / np.float32(16))).astype(np.float32)
    ang = (pos[:, None] * inv[None, :]).astype(np.float32)
    return np.cos(ang).astype(np.float32), np.sin(ang).astype(np.float32)


IN_SHAPES = dict(
    x=[S, D], p=[S, 256], ffn1_norm=[D], ffn1_wg=[D, DFF], ffn1_wu=[D, DFF], ffn1_wd=[DFF, D],
    mix_norm=[D], w_in=[D, 2848], b_forget=[8], q_norm_nsa=[64], k_norm_cmp=[64], k_norm_slc=[64], k_norm_win=[64],
    cmp_pos_k=[32, 64], cmp_pos_v=[32, 64], cmp_k_w1=[2048, 256], cmp_k_w2=[256, 64], cmp_v_w1=[2048, 256],
    cmp_v_w2=[256, 64], q_norm_fox=[64], k_norm_fox=[64], out_norm_nsa=[512], out_norm_fox=[512], w_out=[D, D],
    ffn2_norm=[D], ffn2_wg=[D, DFF], ffn2_wu=[D, DFF], ffn2_wd=[DFF, D], ple_gate_norm=[D], ple_w_gate=[D, D],
    ple_w_proj=[256, D], ple_norm=[D], rope_cos=[S, 8], rope_sin=[S, 8])


def build_nc(nph=99, debug=(), skip=()):
    nc = bass.Bass("TRN2", target_bir_lowering=False)
    T = {}
    for name, shape in IN_SHAPES.items():
        T[name] = nc.dram_tensor(name, shape, F32, kind="ExternalInput").ap()

    def scratch(name, shape, dt):
        kind = "ExternalOutput" if name in debug else "Internal"
        T[name] = nc.dram_tensor(name, shape, dt, kind=kind).ap()

    T["out"] = nc.dram_tensor("out", [S, D], F32, kind="ExternalOutput").ap()
    scratch("h1", [S, D], F32)
    scratch("QKT", [32, 64, S], BF16)
    scratch("V", [S, 768], BF16)
    scratch("G", [S, 24], F32)
    scratch("LF", [S, 8], F32)
    scratch("KCT", [2, 64, 256], BF16)
    scratch("VC", [2, 256, 65], BF16)
    scratch("OA", [S, D], F32)
    scratch("h3", [S, D], F32)
    if "DBGB" in debug:
        scratch("DBGB", [64, S], BF16)
    if "DBGT" in debug:
        scratch("DBGT", [3, 65, 512], F32)
    phases = [
        lambda: ffn_half_phase(nc, T, T["x"], T["x"], T["h1"], T["ffn1_norm"], T["ffn1_wg"], T["ffn1_wu"], T["ffn1_wd"], 0, 11, "f1a"),
        lambda: ffn_half_phase(nc, T, T["x"], T["h1"], T["h1"], T["ffn1_norm"], T["ffn1_wg"], T["ffn1_wu"], T["ffn1_wd"], 11, 11, "f1b"),
        lambda: proj_phase(nc, T),
        lambda: cmp_phase(nc, T),
        lambda: nsa_phase(nc, T),
        lambda: fox_phase(nc, T),
        lambda: out_phase(nc, T),
        lambda: ffn_half_phase(nc, T, T["h1"], T["h1"], T["h3"], T["ffn2_norm"], T["ffn2_wg"], T["ffn2_wu"], T["ffn2_wd"], 0, 11, "f2a"),
        lambda: ffn_half_phase(nc, T, T["h1"], T["h3"], T["h3"], T["ffn2_norm"], T["ffn2_wg"], T["ffn2_wu"], T["ffn2_wd"], 11, 11, "f2b"),
        lambda: ple_phase(nc, T),
    ]
    for k, ph in enumerate(phases[:nph]):
        if k not in skip:
            ph()
    return nc


def make_in_maps(inputs, cores=range(8)):
    cos, sin = rope_tables_np()
    in_maps = []
    for b in cores:
        m = {}
        for name in IN_SHAPES:
            if name == "x":
                a = inputs["x"][b]
            elif name == "p":
                a = inputs["p"][0, b]
            elif name == "rope_cos":
                a = cos
            elif name == "rope_sin":
                a = sin
            elif name == "w_in":
                a = inputs["w_in"][0][:, W_IN_PERM]
            else:
                a = inputs[name][0]
            m[name] = np.ascontiguousarray(a, dtype=np.float32)
        in_maps.append(m)
    return in_maps


def kernel(**inputs):
    nc = build_nc()
    res = run_bass_kernel_spmd(nc, make_in_maps(inputs), core_ids=list(range(8)))
    return np.stack([np.asarray(r["out"]) for r in res.results], axis=0)
```

```python
import numpy as np
from contextlib import ExitStack
import concourse.bass as bass
import concourse.mybir as mybir
from concourse.bass_utils import run_bass_kernel_spmd

F32 = mybir.dt.float32
BF16 = mybir.dt.bfloat16
AF = mybir.ActivationFunctionType
ALU = mybir.AluOpType
AX = mybir.AxisListType

S = 4096
D = 1024
DFF = 2816
NT = S // 128
NG = S // 512
EPS = 1e-6
SAME_ENGINE_SYNC = True
DBG = dict(kh=2, ng=NG, stage=5)
_UID = [0]


def _u(name):
    _UID[0] += 1
    return "%s_%d" % (name, _UID[0])

_OFF = dict(qa=0, kc=512, vc=640, ksl=768, vsl=896, kwn=1024, vwn=1152, ga=1280, qf=1304, kf=1816, vf=2328, fl=2840)
_SZ = dict(qa=512, kc=128, vc=128, ksl=128, vsl=128, kwn=128, vwn=128, ga=24, qf=512, kf=512, vf=512, fl=8)
_ORDER = ['kc', 'qa', 'ksl', 'kwn', 'qf', 'kf', 'vc', 'vsl', 'vwn', 'vf', 'ga', 'fl']
W_IN_PERM = np.concatenate([np.arange(_OFF[k], _OFF[k] + _SZ[k]) for k in _ORDER])


class Chan:
    def __init__(self, sem):
        self.sem = sem
        self.count = 0
        self.ops = []

    def seal(self):
        for op in self.ops:
            op.chanval = self.count


class Op:
    __slots__ = ("eng", "fn", "deps", "sig", "sigval", "chan", "chanval", "idx")


class Prog:
    ENGS = ("pe", "act", "dve", "pool", "sp")

    def __init__(self, nc, es):
        self.nc = nc
        self.es = es
        self.ops = {e: [] for e in self.ENGS}
        self.last_w = {}
        self.readers = {}
        self.engsem = {e: es.enter_context(nc.semaphore(_u("s_" + e))) for e in self.ENGS}
        self.chans = []

    def chan(self):
        c = Chan(self.es.enter_context(self.nc.semaphore(_u("c"))))
        self.chans.append(c)
        return c

    def add(self, eng, fn, reads=(), writes=(), chan=None):
        op = Op()
        op.eng = eng
        op.fn = fn
        op.sig = False
        op.sigval = 0
        op.chan = chan
        deps = []
        for r in reads:
            w = self.last_w.get(r)
            if w is not None:
                deps.append(w)
        for w in writes:
            lw = self.last_w.get(w)
            if lw is not None:
                deps.append(lw)
            deps.extend(self.readers.get(w, ()))
        best = {}
        for d in deps:
            k = ("c", id(d.chan)) if d.chan is not None else ("e", d.eng)
            if k not in best or best[k].idx < d.idx:
                best[k] = d
        op.deps = list(best.values())
        op.idx = len(self.ops[eng])
        for r in reads:
            self.readers.setdefault(r, []).append(op)
        for w in writes:
            self.last_w[w] = op
            self.readers[w] = []
        if chan is not None:
            chan.count += 16
            op.chanval = chan.count
            chan.ops.append(op)
        self.ops[eng].append(op)
        return op

    def emit(self, block):
        for e in self.ENGS:
            for op in self.ops[e]:
                for d in op.deps:
                    if d.chan is None and (d.eng != op.eng or SAME_ENGINE_SYNC):
                        d.sig = True
        for e in self.ENGS:
            c = 0
            for op in self.ops[e]:
                if op.sig and op.chan is None:
                    c += 1
                    op.sigval = c
        final = [(c.sem, c.count) for c in self.chans if c.count > 0]

        def mk(ename):
            ops = self.ops[ename]

            def body(eng):
                waited = {}
                if ename == "pool":
                    _FILL.clear()
                for op in ops:
                    need = []
                    for d in op.deps:
                        if d.chan is not None:
                            sem, val = d.chan.sem, d.chanval
                        elif d.eng != ename or SAME_ENGINE_SYNC:
                            sem, val = self.engsem[d.eng], d.sigval
                        else:
                            continue
                        k = id(sem)
                        if waited.get(k, 0) >= val:
                            continue
                        need.append((sem, val))
                        waited[k] = val
                    for sem, val in need[:-1]:
                        eng.wait_ge(sem, val)
                    ins = op.fn(eng)
                    if need:
                        ins._wait_ge(need[-1][0], need[-1][1])
                    if op.chan is not None:
                        ins.then_inc(op.chan.sem, 16)
                    elif op.sig:
                        ins.then_inc(self.engsem[ename], 1)
                if ename == "sp":
                    for sem, val in final:
                        eng.wait_ge(sem, val)
                if ename == "pool":
                    for r in _FILL.values():
                        eng.free_register(r)
                    _FILL.clear()
            return body

        block.tensor(mk("pe"))
        block.scalar(mk("act"))
        block.vector(mk("dve"))
        block.gpsimd(mk("pool"))
        block.sync(mk("sp"))


def run_phase(nc, build):
    with ExitStack() as es:
        P = Prog(nc, es)
        build(P, es)
        sems = list(P.engsem.values()) + [c.sem for c in P.chans]
        with nc.Block() as b0:
            def clr(e):
                for sm in sems:
                    e.sem_clear(sm)
            b0.sync(clr)
        with nc.Block() as block:
            P.emit(block)


def sb(nc, es, name, shape, dt):
    return es.enter_context(nc.sbuf_tensor(_u(name), shape, dt))


def ps(nc, es, name, shape, dt):
    return es.enter_context(nc.psum_tensor(_u(name), shape, dt))


def load_cast_rows(P, nc, es, dst, src_rows, ncols, chans, stage, key):
    n = len(src_rows)
    for k in range(n):
        s = k % len(stage)
        st = stage[s]
        ch = chans[s]
        src = src_rows[k]
        P.add("sp", lambda e, st=st, src=src: e.dma_start(out=st[:, 0:ncols], in_=src),
              writes=[("stage", s)], chan=ch)
        eng = "dve" if k % 2 == 0 else "pool"
        P.add(eng, lambda e, st=st, k=k: e.tensor_copy(out=dst[:, k, 0:ncols], in_=st[:, 0:ncols]),
              reads=[("stage", s)], writes=[(key, k)])


def ffn_half_phase(nc, T, src_norm, src_res, dst, gain, wg, wu, wd, f0, nfc, tagp):
    def build(P, es):
        FW = nfc * 128
        wg_s = sb(nc, es, "wg_s", [128, 8, FW], BF16)
        wu_s = sb(nc, es, "wu_s", [128, 8, FW], BF16)
        wd_s = sb(nc, es, "wd_s", [128, nfc, D], BF16)
        stage = [sb(nc, es, "stg%d" % i, [128, FW], F32) for i in range(3)]
        stch = [P.chan() for _ in range(3)]
        gB = sb(nc, es, "gB", [128, D], F32)
        ident = sb(nc, es, "ident", [128, 128], BF16)
        identf = sb(nc, es, "identf", [128, 128], F32)
        epst = sb(nc, es, "epst", [128, 1], F32)
        xn = [sb(nc, es, "xn%d" % i, [128, D], F32) for i in range(2)]
        xr = [sb(nc, es, "xr%d" % i, [128, D], F32) for i in range(2)]
        nb = [sb(nc, es, "nb%d" % i, [128, D], BF16) for i in range(4)]
        junk = sb(nc, es, "junk", [128, D], BF16)
        ssq = [sb(nc, es, "ssq%d" % i, [128, 4], F32) for i in range(2)]
        rs = [sb(nc, es, "rs%d" % i, [128, 4], F32) for i in range(2)]
        nT = [sb(nc, es, "nT%d" % i, [128, 8, 512], BF16) for i in range(2)]
        hT = sb(nc, es, "hT", [128, nfc, 512], BF16)
        sg = [sb(nc, es, "sg%d" % i, [128, 512], F32) for i in range(2)]
        psT = [ps(nc, es, "psT%d" % i, [128, D], BF16) for i in range(2)]
        psg = [ps(nc, es, "psg%d" % i, [128, 512], F32) for i in range(2)]
        psu = [ps(nc, es, "psu%d" % i, [128, 512], F32) for i in range(2)]
        pso = [ps(nc, es, "pso%d" % i, [128, 512], F32) for i in range(2)]
        cx = [P.chan() for _ in range(2)]
        cr = [P.chan() for _ in range(2)]
        co = [P.chan() for _ in range(2)]
        cg = P.chan()

        P.add("sp", lambda e: e.dma_start(out=gB[:, :], in_=gain.partition_broadcast(128)),
              writes=["gB"], chan=cg)
        P.add("pool", lambda e: e.memset(identf[:, :], 0.0), writes=["identf"])
        P.add("pool", lambda e: asel(e, out=identf[:, :], in_=identf[:, :], pattern=[[-1, 128]],
                                                compare_op=ALU.not_equal, fill=1.0, base=0,
                                                channel_multiplier=1),
              reads=["identf"], writes=["identf"])
        P.add("pool", lambda e: e.tensor_copy(out=ident[:, :], in_=identf[:, :]), reads=["identf"], writes=["ident"])
        P.add("pool", lambda e: e.memset(epst[:, :], EPS), writes=["eps"])

        wgv = wg.rearrange("(c p) f -> p c f", p=128)
        wuv = wu.rearrange("(c p) f -> p c f", p=128)
        load_cast_rows(P, nc, es, wg_s, [wgv[:, c, f0 * 128:f0 * 128 + FW] for c in range(8)], FW, stch, stage, "wg")
        load_cast_rows(P, nc, es, wu_s, [wuv[:, c, f0 * 128:f0 * 128 + FW] for c in range(8)], FW, stch, stage, "wu")
        load_cast_rows(P, nc, es, wd_s, [wd[(f0 + c) * 128:(f0 + c + 1) * 128, :] for c in range(nfc)], D, stch, stage, "wd")
        wkeys = [("wg", c) for c in range(8)] + [("wu", c) for c in range(8)]
        wdkeys = [("wd", c) for c in range(nfc)]

        def prep_group(g):
            sl = g % 2
            for k in range(4):
                t = 4 * g + k
                xs = t % 2
                P.add("sp", lambda e, xs=xs, t=t: e.dma_start(out=xn[xs][:, :], in_=src_norm[t * 128:(t + 1) * 128, :]),
                      writes=[("xn", xs)], chan=cx[xs])
                P.add("act", lambda e, xs=xs, sl=sl, k=k: e.activation(
                    out=junk[:, :], in_=xn[xs][:, :], func=AF.Square, accum_out=ssq[sl][:, k:k + 1]),
                    reads=[("xn", xs)], writes=["junk", ("ssq", sl, k)])
                P.add("act", lambda e, sl=sl, k=k: e.activation(out=rs[sl][:, k:k + 1], in_=ssq[sl][:, k:k + 1],
                                                                func=AF.Sqrt, scale=1.0 / D, bias=epst[:, 0:1]),
                      reads=[("ssq", sl, k), "eps"], writes=[("rs", sl, k)])
                P.add("dve", lambda e, sl=sl, k=k: e.reciprocal(out=rs[sl][:, k:k + 1], in_=rs[sl][:, k:k + 1]),
                      reads=[("rs", sl, k)], writes=[("rs", sl, k)])
                P.add("dve", lambda e, xs=xs, sl=sl, k=k: e.scalar_tensor_tensor(
                    out=nb[k][:, :], in0=xn[xs][:, :], scalar=rs[sl][:, k:k + 1], in1=gB[:, :],
                    op0=ALU.mult, op1=ALU.mult),
                    reads=[("xn", xs), ("rs", sl, k), "gB"], writes=[("nb", k)])

        def transposes(g):
            sl = g % 2
            for k in range(4):
                pb = k % 2
                for c in range(8):
                    P.add("pe", lambda e, pb=pb, k=k, c=c: e.transpose(
                        out=psT[pb][:, c * 128:(c + 1) * 128], in_=nb[k][:, c * 128:(c + 1) * 128], identity=ident[:, :]),
                        reads=[("nb", k), "ident"], writes=[("psT", pb)] if c == 0 else [])
                P.last_w[("psT", pb)] = P.ops["pe"][-1]
                P.add("act", lambda e, pb=pb, sl=sl, k=k: e.copy(
                    out=nT[sl][:, :, k * 128:(k + 1) * 128],
                    in_=psT[pb][:, :].rearrange("p (c t) -> p c t", c=8)),
                    reads=[("psT", pb)], writes=[("nT", sl, k)])

        def upgate(g):
            sl = g % 2
            for fc in range(nfc):
                b = fc % 2
                for (wt, pst, nm) in ((wg_s, psg, "psg"), (wu_s, psu, "psu")):
                    for c in range(8):
                        P.add("pe", lambda e, wt=wt, pst=pst, b=b, c=c, fc=fc, sl=sl: e.matmul(
                            out=pst[b][:, :], lhsT=wt[:, c, fc * 128:(fc + 1) * 128], rhs=nT[sl][:, c, :],
                            start=(c == 0), stop=(c == 7)),
                            reads=[("nT", sl, 0), ("nT", sl, 1), ("nT", sl, 2), ("nT", sl, 3)] + (wkeys if g == 0 else []),
                            writes=[(nm, b)] if c == 0 else [])
                    P.last_w[(nm, b)] = P.ops["pe"][-1]
                P.add("act", lambda e, b=b: e.activation(out=sg[b][:, :], in_=psg[b][:, :], func=AF.Silu),
                      reads=[("psg", b)], writes=[("sg", b)])
                P.add("dve", lambda e, b=b, fc=fc: e.tensor_tensor(out=hT[:, fc, :], in0=psu[b][:, :], in1=sg[b][:, :],
                                                                  op=ALU.mult),
                      reads=[("psu", b), ("sg", b)], writes=[("hT", fc)])

        def down(g):
            for k in range(4):
                t = 4 * g + k
                rsl = t % 2
                P.add("sp", lambda e, rsl=rsl, t=t: e.dma_start(out=xr[rsl][:, :], in_=src_res[t * 128:(t + 1) * 128, :]),
                      reads=[("dram", t)], writes=[("xr", rsl)], chan=cr[rsl])
                for half in range(2):
                    b = half
                    for fc in range(nfc):
                        P.add("pe", lambda e, b=b, fc=fc, k=k, half=half: e.matmul(
                            out=pso[b][:, :], lhsT=hT[:, fc, k * 128:(k + 1) * 128],
                            rhs=wd_s[:, fc, half * 512:(half + 1) * 512], start=(fc == 0), stop=(fc == nfc - 1)),
                            reads=[("hT", fc)] + (wdkeys if g == 0 else []),
                            writes=[("pso", b)] if fc == 0 else [])
                    P.last_w[("pso", b)] = P.ops["pe"][-1]
                    P.add("dve", lambda e, b=b, rsl=rsl, half=half: e.scalar_tensor_tensor(
                        out=xr[rsl][:, half * 512:(half + 1) * 512], in0=pso[b][:, :], scalar=0.5,
                        in1=xr[rsl][:, half * 512:(half + 1) * 512], op0=ALU.mult, op1=ALU.add),
                        reads=[("pso", b), ("xr", rsl)], writes=[("xr", rsl)])
                P.add("sp", lambda e, rsl=rsl, t=t: e.dma_start(out=dst[t * 128:(t + 1) * 128, :], in_=xr[rsl][:, :]),
                      reads=[("xr", rsl)], writes=[("dram", t)], chan=co[rsl])

        prep_group(0)
        transposes(0)
        for g in range(NG):
            if g + 1 < NG:
                prep_group(g + 1)
            upgate(g)
            if g + 1 < NG:
                transposes(g + 1)
            down(g)

    run_phase(nc, build)


def proj_phase(nc, T):
    def build(P, es):
        h1 = T["h1"]
        WIN = 2848
        win_s = sb(nc, es, "win_s", [128, 8, WIN], BF16)
        HW_ = WIN // 2
        stage = [sb(nc, es, "pstg%d" % i, [128, HW_], F32) for i in range(3)]
        stch = [P.chan() for _ in range(3)]
        gB = sb(nc, es, "gB", [128, D], F32)
        ident = sb(nc, es, "ident", [128, 128], BF16)
        identf = sb(nc, es, "identf", [128, 128], F32)
        epst = sb(nc, es, "epst", [128, 1], F32)
        g5 = sb(nc, es, "g5", [128, 5, 64], F32)
        GQ = sb(nc, es, "GQ", [128, 28, 64], F32)
        bfg = sb(nc, es, "bfg", [128, 8], F32)
        cosT = sb(nc, es, "cosT", [128, NT, 8], F32)
        sinT = sb(nc, es, "sinT", [128, NT, 8], F32)
        Gall = sb(nc, es, "Gall", [128, NT, 24], F32)
        LFall = sb(nc, es, "LFall", [128, NT, 8], F32)
        xn = [sb(nc, es, "xn%d" % i, [128, D], F32) for i in range(2)]
        nb = [sb(nc, es, "nb%d" % i, [128, D], BF16) for i in range(2)]
        junk = sb(nc, es, "junk", [128, D], BF16)
        ssq = sb(nc, es, "ssq", [128, 2], F32)
        rs = sb(nc, es, "rs", [128, 2], F32)
        aT = [sb(nc, es, "aT%d" % i, [128, 8, 128], BF16) for i in range(2)]
        qk = [sb(nc, es, "qk%d" % i, [128, 32, 64], F32) for i in range(2)]
        sq = sb(nc, es, "sq", [128, 32, 64], F32)
        hs = [sb(nc, es, "hs%d" % i, [128, 32], F32) for i in range(2)]
        rt = [sb(nc, es, "rt%d" % i, [128, 14, 8], F32) for i in range(4)]
        qkb = [sb(nc, es, "qkb%d" % i, [128, 2048], BF16) for i in range(2)]
        qkT = sb(nc, es, "qkT", [128, 16, 512], BF16)
        vst = [sb(nc, es, "vst%d" % i, [128, 768], BF16) for i in range(2)]
        psT = [ps(nc, es, "psT%d" % i, [128, D], BF16) for i in range(2)]
        pq = [ps(nc, es, "pq%d" % i, [128, 512], F32) for i in range(4)]
        psQ = [ps(nc, es, "psQ%d" % i, [128, 8, 128], BF16) for i in range(2)]
        cx = [P.chan() for _ in range(2)]
        cg = P.chan()
        cq = P.chan()
        cv = [P.chan() for _ in range(2)]
        cf = P.chan()

        P.add("sp", lambda e: e.dma_start(out=gB[:, :], in_=T["mix_norm"].partition_broadcast(128)), writes=["gB"], chan=cg)
        for i, nm in enumerate(("q_norm_nsa", "k_norm_slc", "k_norm_win", "q_norm_fox", "k_norm_fox")):
            P.add("sp", lambda e, i=i, nm=nm: e.dma_start(out=g5[:, i, :], in_=T[nm].partition_broadcast(128)),
                  writes=[("g5", i)], chan=cg)
        P.add("sp", lambda e: e.dma_start(out=bfg[:, :], in_=T["b_forget"].partition_broadcast(128)), writes=["bfg"], chan=cg)
        P.add("sp", lambda e: e.dma_start(out=cosT[:, :, :], in_=T["rope_cos"].rearrange("(t p) c -> p t c", p=128)),
              writes=["cosT"], chan=cg)
        P.add("sp", lambda e: e.dma_start(out=sinT[:, :, :], in_=T["rope_sin"].rearrange("(t p) c -> p t c", p=128)),
              writes=["sinT"], chan=cg)
        cg.seal()
        for (i, h0, nh) in ((0, 0, 8), (1, 8, 2), (2, 10, 2), (3, 12, 8), (4, 20, 8)):
            P.add("dve", lambda e, i=i, h0=h0, nh=nh: e.tensor_copy(
                out=GQ[:, h0:h0 + nh, :], in_=g5[:, i, :].unsqueeze(1).to_broadcast([128, nh, 64])),
                reads=[("g5", i)], writes=[("GQ", i)])
        gqk = [("GQ", i) for i in range(5)]
        P.add("pool", lambda e: e.memset(identf[:, :], 0.0), writes=["identf"])
        P.add("pool", lambda e: asel(e, out=identf[:, :], in_=identf[:, :], pattern=[[-1, 128]],
                                                compare_op=ALU.not_equal, fill=1.0, base=0, channel_multiplier=1),
              reads=["identf"], writes=["identf"])
        P.add("pool", lambda e: e.tensor_copy(out=ident[:, :], in_=identf[:, :]), reads=["identf"], writes=["ident"])
        P.add("pool", lambda e: e.memset(epst[:, :], EPS), writes=["eps"])
        wv = T["w_in"].rearrange("(c p) f -> p c f", p=128)
        for hf in range(2):
            for c in range(8):
                k = hf * 8 + c
                s = k % 3
                P.add("sp", lambda e, s=s, c=c, hf=hf: e.dma_start(out=stage[s][:, :], in_=wv[:, c, hf * HW_:(hf + 1) * HW_]),
                      writes=[("stage", s)], chan=stch[s])
                P.add("dve" if k % 2 == 0 else "pool", lambda e, s=s, c=c, hf=hf: e.tensor_copy(
                    out=win_s[:, c, hf * HW_:(hf + 1) * HW_], in_=stage[s][:, :]),
                    reads=[("stage", s)], writes=[("win", c, hf)])
        wkeys = [("win", c, hf) for c in range(8) for hf in range(2)]
        QKTv = T["QKT"].rearrange("(pr two) d s -> (two d) pr s", two=2)
        CH = [(0, 512), (512, 512), (1024, 512), (1536, 512), (2048, 512), (2560, 288)]

        for t in range(NT):
            xs = t % 2
            g, k = t // 4, t % 4
            P.add("sp", lambda e, xs=xs, t=t: e.dma_start(out=xn[xs][:, :], in_=h1[t * 128:(t + 1) * 128, :]),
                  writes=[("xn", xs)], chan=cx[xs])
            P.add("act", lambda e, xs=xs: e.activation(out=junk[:, :], in_=xn[xs][:, :], func=AF.Square,
                                                       accum_out=ssq[:, xs:xs + 1]),
                  reads=[("xn", xs)], writes=["junk", ("ssq", xs)])
            P.add("act", lambda e, xs=xs: e.activation(out=rs[:, xs:xs + 1], in_=ssq[:, xs:xs + 1], func=AF.Sqrt,
                                                       scale=1.0 / D, bias=epst[:, 0:1]),
                  reads=[("ssq", xs), "eps"], writes=[("rs", xs)])
            P.add("dve", lambda e, xs=xs: e.reciprocal(out=rs[:, xs:xs + 1], in_=rs[:, xs:xs + 1]),
                  reads=[("rs", xs)], writes=[("rs", xs)])
            P.add("dve", lambda e, xs=xs: e.scalar_tensor_tensor(
                out=nb[xs][:, :], in0=xn[xs][:, :], scalar=rs[:, xs:xs + 1], in1=gB[:, :], op0=ALU.mult, op1=ALU.mult),
                reads=[("xn", xs), ("rs", xs), "gB"], writes=[("nb", xs)])
            for c in range(8):
                P.add("pe", lambda e, xs=xs, c=c: e.transpose(
                    out=psT[xs][:, c * 128:(c + 1) * 128], in_=nb[xs][:, c * 128:(c + 1) * 128], identity=ident[:, :]),
                    reads=[("nb", xs), "ident"], writes=[("psT", xs)] if c == 0 else [])
            P.last_w[("psT", xs)] = P.ops["pe"][-1]
            P.add("act", lambda e, xs=xs: e.copy(out=aT[xs][:, :, :], in_=psT[xs][:, :].rearrange("p (c t) -> p c t", c=8)),
                  reads=[("psT", xs)], writes=[("aT", xs)])
            for ci, (c0, cw) in enumerate(CH):
                pb = (t * 6 + ci) % 4
                for c in range(8):
                    P.add("pe", lambda e, pb=pb, c=c, c0=c0, cw=cw, xs=xs: e.matmul(
                        out=pq[pb][:, 0:cw], lhsT=aT[xs][:, c, :], rhs=win_s[:, c, c0:c0 + cw],
                        start=(c == 0), stop=(c == 7)),
                        reads=[("aT", xs)] + (wkeys if t == 0 else []), writes=[("pq", pb)] if c == 0 else [])
                P.last_w[("pq", pb)] = P.ops["pe"][-1]
                if ci < 4:
                    P.add("act", lambda e, pb=pb, ci=ci, xs=xs: e.copy(
                        out=qk[xs][:, ci * 8:(ci + 1) * 8, :], in_=pq[pb][:, :].rearrange("p (h d) -> p h d", h=8)),
                        reads=[("pq", pb)], writes=[("qk", xs, ci)])
                    P.add("act", lambda e, pb=pb, ci=ci: e.activation(
                        out=sq[:, ci * 8:(ci + 1) * 8, :], in_=pq[pb][:, :].rearrange("p (h d) -> p h d", h=8), func=AF.Square),
                        reads=[("pq", pb)], writes=[("sq", ci)])
                elif ci == 4:
                    P.add("dve", lambda e, pb=pb, xs=xs: e.tensor_copy(out=vst[xs][:, 0:512], in_=pq[pb][:, 0:512]),
                          reads=[("pq", pb)], writes=[("vst", xs, 0)])
                else:
                    P.add("dve", lambda e, pb=pb, xs=xs: e.tensor_copy(out=vst[xs][:, 512:768], in_=pq[pb][:, 0:256]),
                          reads=[("pq", pb)], writes=[("vst", xs, 1)])
                    P.add("dve", lambda e, pb=pb, t=t: e.tensor_copy(out=Gall[:, t, :], in_=pq[pb][:, 256:280]),
                          reads=[("pq", pb)], writes=[("Gall", t)])
                    P.add("dve", lambda e, pb=pb, t=t: e.tensor_tensor(out=LFall[:, t, :], in0=pq[pb][:, 280:288], in1=bfg[:, :],
                                                                      op=ALU.add),
                          reads=[("pq", pb), "bfg"], writes=[("LFall", t)])
            P.add("sp", lambda e, xs=xs, t=t: e.dma_start(out=T["V"][t * 128:(t + 1) * 128, :], in_=vst[xs][:, :]),
                  reads=[("vst", xs, 0), ("vst", xs, 1)], chan=cv[xs])
            P.add("dve", lambda e, xs=xs: e.tensor_reduce(out=hs[xs][:, :], in_=sq[:, :, :], axis=AX.X, op=ALU.add),
                  reads=[("sq", i) for i in range(4)], writes=[("hs", xs)])
            P.add("act", lambda e, xs=xs: e.activation(out=hs[xs][:, :], in_=hs[xs][:, :], func=AF.Sqrt,
                                                       scale=1.0 / 64, bias=epst[:, 0:1]),
                  reads=[("hs", xs), "eps"], writes=[("hs", xs)])
            P.add("dve", lambda e, xs=xs: e.reciprocal(out=hs[xs][:, :], in_=hs[xs][:, :]),
                  reads=[("hs", xs)], writes=[("hs", xs)])
            qkk = [("qk", xs, i) for i in range(4)]
            P.add("dve", lambda e, xs=xs: e.tensor_tensor(
                out=qk[xs][:, 2:30, :], in0=qk[xs][:, 2:30, :], in1=hs[xs][:, 2:30].unsqueeze(2).to_broadcast([128, 28, 64]),
                op=ALU.mult), reads=qkk + [("hs", xs)], writes=qkk)
            P.add("dve", lambda e, xs=xs: e.tensor_tensor(
                out=qk[xs][:, 2:30, :], in0=qk[xs][:, 2:30, :], in1=GQ[:, :, :], op=ALU.mult),
                reads=qkk + gqk, writes=qkk)
            cb = lambda tab, t=t: tab[:, t, :].unsqueeze(1).to_broadcast([128, 14, 8])
            x1 = lambda xs=xs: qk[xs][:, 0:14, 0:8]
            x2 = lambda xs=xs: qk[xs][:, 0:14, 8:16]
            for j, (src, tab) in enumerate(((x1, cosT), (x2, sinT), (x2, cosT), (x1, sinT))):
                P.add("pool", lambda e, j=j, src=src, tab=tab, cb=cb: e.tensor_tensor(
                    out=rt[j][:, :, :], in0=src(), in1=cb(tab), op=ALU.mult),
                    reads=qkk + ["cosT", "sinT"], writes=[("rt", j)])
            P.add("pool", lambda e, x1=x1: e.tensor_tensor(out=x1(), in0=rt[0][:, :, :], in1=rt[1][:, :, :], op=ALU.subtract),
                  reads=[("rt", 0), ("rt", 1)], writes=qkk)
            P.add("pool", lambda e, x2=x2: e.tensor_tensor(out=x2(), in0=rt[2][:, :, :], in1=rt[3][:, :, :], op=ALU.add),
                  reads=[("rt", 2), ("rt", 3)], writes=qkk)
            P.add("pool", lambda e, xs=xs: e.tensor_copy(out=qkb[xs][:, :], in_=qk[xs][:, :, :].rearrange("p h d -> p (h d)")),
                  reads=qkk, writes=[("qkb", xs)])
            for pr in range(16):
                hb = pr // 8
                P.add("pe", lambda e, pr=pr, hb=hb, xs=xs: e.transpose(
                    out=psQ[hb][:, pr % 8, :], in_=qkb[xs][:, pr * 128:(pr + 1) * 128], identity=ident[:, :]),
                    reads=[("qkb", xs), "ident"], writes=[("psQ", hb)] if pr % 8 == 0 else [])
                if pr % 8 == 7:
                    P.last_w[("psQ", hb)] = P.ops["pe"][-1]
                    P.add("act" if hb == 0 else "dve", (lambda e, hb=hb, k=k: e.copy(
                        out=qkT[:, hb * 8:(hb + 1) * 8, k * 128:(k + 1) * 128], in_=psQ[hb][:, :, :])) if hb == 0 else
                        (lambda e, hb=hb, k=k: e.tensor_copy(
                            out=qkT[:, hb * 8:(hb + 1) * 8, k * 128:(k + 1) * 128], in_=psQ[hb][:, :, :])),
                        reads=[("psQ", hb)], writes=[("qkT", k, hb)])
            if k == 3:
                P.add("sp", lambda e, g=g: e.dma_start(out=QKTv[:, :, g * 512:(g + 1) * 512], in_=qkT[:, :, :]),
                      reads=[("qkT", kk, hb) for kk in range(4) for hb in range(2)], chan=cq)
        P.add("act", lambda e: e.activation(out=Gall[:, :, :], in_=Gall[:, :, :], func=AF.Sigmoid),
              reads=[("Gall", t) for t in range(NT)], writes=["GallF"])
        P.add("sp", lambda e: e.dma_start(out=T["G"].rearrange("(t p) c -> p t c", p=128), in_=Gall[:, :, :]),
              reads=["GallF"], chan=cf)
        P.add("act", lambda e: e.activation(out=LFall[:, :, :], in_=LFall[:, :, :], func=AF.Exp, scale=-1.0),
              reads=[("LFall", t) for t in range(NT)], writes=["LF1"])
        P.add("act", lambda e: e.activation(out=LFall[:, :, :], in_=LFall[:, :, :], func=AF.Ln, bias=1.0),
              reads=["LF1"], writes=["LF2"])
        P.add("dve", lambda e: e.tensor_scalar(out=LFall[:, :, :], in0=LFall[:, :, :], scalar1=-1.0, scalar2=None, op0=ALU.mult),
              reads=["LF2"], writes=["LF3"])
        P.add("sp", lambda e: e.dma_start(out=T["LF"].rearrange("(t p) c -> p t c", p=128), in_=LFall[:, :, :]),
              reads=["LF3"], chan=cf)

    run_phase(nc, build)


_FILL = {}


def asel(e, **kw):
    v = float(kw.pop("fill"))
    r = _FILL.get(v)
    if r is None:
        r = e.alloc_register()
        e.reg_mov(r, v)
        _FILL[v] = r
    return e.affine_select(fill=r, **kw)


def make_ident(P, nc, es):
    ident = sb(nc, es, "ident", [128, 128], BF16)
    identf = sb(nc, es, "identf", [128, 128], F32)
    P.add("pool", lambda e: e.memset(identf[:, :], 0.0), writes=["identf"])
    P.add("pool", lambda e: asel(e, out=identf[:, :], in_=identf[:, :], pattern=[[-1, 128]],
                                            compare_op=ALU.not_equal, fill=1.0, base=0, channel_multiplier=1),
          reads=["identf"], writes=["identf"])
    P.add("pool", lambda e: e.tensor_copy(out=ident[:, :], in_=identf[:, :]), reads=["identf"], writes=["ident"])
    return ident, identf


def cmp_phase(nc, T):
    def build(P, es):
        ident, identf = make_ident(P, nc, es)
        epst = sb(nc, es, "epst", [128, 1], F32)
        P.add("pool", lambda e: e.memset(epst[:, :], EPS), writes=["eps"])
        tok = sb(nc, es, "tok", [64, 4, S], BF16)
        w1s = [sb(nc, es, "w1s%d" % i, [64, 32, 256], BF16) for i in range(2)]
        stg = [sb(nc, es, "cstg%d" % i, [64, 32, 256], F32) for i in range(2)]
        w1f = [sb(nc, es, "w1f%d" % i, [128, 16, 256], F32) for i in range(2)]
        posr = sb(nc, es, "posr", [16, 2, 128], F32)
        posc = sb(nc, es, "posc", [128, 2, 16], F32)
        w2f = sb(nc, es, "w2f", [128, 2, 2, 64], F32)
        w2s = sb(nc, es, "w2s", [128, 2, 2, 64], BF16)
        biasT = sb(nc, es, "biasT", [128, 4], F32)
        gk = sb(nc, es, "gk", [128, 64], F32)
        hidT = [sb(nc, es, "hidT%d" % i, [128, 2, 256], BF16) for i in range(2)]
        ssq = sb(nc, es, "ssq", [128, 4], F32)
        junk = sb(nc, es, "junk", [128, 64], F32)
        kcb = [sb(nc, es, "kcb%d" % i, [128, 64], BF16) for i in range(2)]
        kcT = [sb(nc, es, "kcT%d" % i, [64, 256], BF16) for i in range(2)]
        vce = [sb(nc, es, "vce%d" % i, [128, 2, 65], BF16) for i in range(2)]
        psH = [ps(nc, es, "psH%d" % i, [128, 256], F32) for i in range(2)]
        psO = [ps(nc, es, "psO%d" % i, [128, 64], F32) for i in range(2)]
        psB = ps(nc, es, "psB", [128, 4], F32)
        psP = ps(nc, es, "psP", [128, 2, 16], F32)
        psK = ps(nc, es, "psK", [64, 128], BF16)
        c0 = P.chan()
        c1 = [P.chan() for _ in range(2)]
        co = P.chan()

        for j, h in enumerate((0, 1, 30, 31)):
            P.add("sp", lambda e, j=j, h=h: e.dma_start(out=tok[:, j, :], in_=T["QKT"][h, :, :]), writes=[("tok", j)], chan=c0)
        P.add("sp", lambda e: e.dma_start(out=gk[:, :], in_=T["k_norm_cmp"].partition_broadcast(128)), writes=["gk"], chan=c0)
        for kv, nm in enumerate(("cmp_pos_k", "cmp_pos_v")):
            P.add("sp", lambda e, kv=kv, nm=nm: e.dma_start(
                out=posr[:, kv, :], in_=T[nm].rearrange("(c a) d -> c (a d)", a=2)), writes=[("posr", kv)], chan=c0)
        for kv, nm in enumerate(("cmp_k_w2", "cmp_v_w2")):
            P.add("sp", lambda e, kv=kv, nm=nm: e.dma_start(
                out=w2f[:, kv, :, :], in_=T[nm].rearrange("(c p) d -> p c d", p=128)), writes=[("w2f", kv)], chan=c0)
        for kv, nm in enumerate(("cmp_k_w1", "cmp_v_w1")):
            P.add("sp", lambda e, kv=kv, nm=nm: e.dma_start(
                out=w1f[kv][:, :, :], in_=T[nm].rearrange("(c p) h -> p c h", p=128)), writes=[("w1f", kv)], chan=c0)
        c0.seal()
        for kv, nm in enumerate(("cmp_k_w1", "cmp_v_w1")):
            P.add("sp", lambda e, kv=kv, nm=nm: e.dma_start(
                out=stg[kv][:, :, :], in_=T[nm].rearrange("(l d) h -> d l h", d=64)), writes=[("stg", kv)], chan=c1[kv])
            P.add("dve" if kv == 0 else "pool", lambda e, kv=kv: e.tensor_copy(out=w1s[kv][:, :, :], in_=stg[kv][:, :, :]),
                  reads=[("stg", kv)], writes=[("w1s", kv)])
        P.add("dve", lambda e: e.tensor_copy(out=w2s[:, :, :, :], in_=w2f[:, :, :, :]),
              reads=[("w2f", 0), ("w2f", 1)], writes=["w2s"])
        for kv in range(2):
            P.add("pe", lambda e, kv=kv: e.transpose(out=psP[:, kv, :], in_=posr[:, kv, :], identity=identf[0:16, 0:16]),
                  reads=[("posr", kv), "identf"], writes=[("psP", kv)])
        P.add("dve", lambda e: e.tensor_copy(out=posc[:, :, :], in_=psP[:, :, :]),
              reads=[("psP", 0), ("psP", 1)], writes=["posc"])
        for kv in range(2):
            for hc in range(2):
                for c in range(16):
                    P.add("pe", lambda e, kv=kv, hc=hc, c=c: e.matmul(
                        out=psB[:, kv * 2 + hc:kv * 2 + hc + 1], lhsT=w1f[kv][:, c, hc * 128:(hc + 1) * 128],
                        rhs=posc[:, kv, c:c + 1], start=(c == 0), stop=(c == 15)),
                        reads=[("w1f", kv), "posc"], writes=["psB"] if (c == 0 and kv == 0 and hc == 0) else [])
        P.last_w["psB"] = P.ops["pe"][-1]
        P.add("dve", lambda e: e.tensor_copy(out=biasT[:, :], in_=psB[:, :]), reads=["psB"], writes=["biasT"])
        for i in range(2):
            P.add("pool", lambda e, i=i: e.memset(kcb[i][:, :], 0.0), writes=[("kcb", i)])
            P.add("pool", lambda e, i=i: e.memset(vce[i][:, :, :], 0.0), writes=[("vce", i)])
            P.add("pool", lambda e, i=i: e.memset(vce[i][:, :, 64:65], 1.0), reads=[("vce", i)], writes=[("vce", i)])
            P.add("pool", lambda e, i=i: e.memset(hidT[i][:, :, :], 0.0), writes=[("hidT", i, 0), ("hidT", i, 1)])
        VCv = T["VC"].rearrange("h (c p) e -> h p c e", p=128)
        it = 0
        for kv in range(2):
            for head in range(2):
                sl = it % 2
                it += 1
                tv = tok[:, kv * 2 + head, :].rearrange("p (n r) -> p n r", r=16)
                for hc in range(2):
                    for l in range(32):
                        q, r = l // 16, l % 16
                        P.add("pe", lambda e, kv=kv, hc=hc, l=l, q=q, r=r, tv=tv: e.matmul(
                            out=psH[hc][:, 0:255], lhsT=w1s[kv][:, l, hc * 128:(hc + 1) * 128], rhs=tv[:, q:q + 255, r],
                            start=(l == 0), stop=(l == 31)),
                            reads=[("tok", kv * 2 + head), ("w1s", kv)], writes=[("psH", hc)] if l == 0 else [])
                    P.last_w[("psH", hc)] = P.ops["pe"][-1]
                    P.add("act", lambda e, kv=kv, hc=hc, sl=sl: e.activation(
                        out=hidT[sl][:, hc, 0:255], in_=psH[hc][:, 0:255], func=AF.Silu,
                        bias=biasT[:, kv * 2 + hc:kv * 2 + hc + 1]),
                        reads=[("psH", hc), "biasT"], writes=[("hidT", sl, hc)])
                for ci, (n0, nn) in enumerate(((0, 128), (128, 127))):
                    for hc in range(2):
                        P.add("pe", lambda e, kv=kv, hc=hc, sl=sl, ci=ci, n0=n0, nn=nn: e.matmul(
                            out=psO[ci][0:nn, :], lhsT=hidT[sl][:, hc, n0:n0 + nn], rhs=w2s[:, kv, hc, :],
                            start=(hc == 0), stop=(hc == 1)),
                            reads=[("hidT", sl, 0), ("hidT", sl, 1), "w2s"], writes=[("psO", ci)] if hc == 0 else [])
                    P.last_w[("psO", ci)] = P.ops["pe"][-1]
                    if kv == 0:
                        col = head * 2 + ci
                        P.add("act", lambda e, ci=ci, nn=nn, col=col: e.activation(
                            out=junk[0:nn, :], in_=psO[ci][0:nn, :], func=AF.Square, accum_out=ssq[0:nn, col:col + 1]),
                            reads=[("psO", ci)], writes=["junk", ("ssq", col)])
                        P.add("act", lambda e, nn=nn, col=col: e.activation(
                            out=ssq[0:nn, col:col + 1], in_=ssq[0:nn, col:col + 1], func=AF.Sqrt, scale=1.0 / 64,
                            bias=epst[0:nn, 0:1]), reads=[("ssq", col), "eps"], writes=[("ssq", col)])
                        P.add("dve", lambda e, nn=nn, col=col: e.reciprocal(out=ssq[0:nn, col:col + 1], in_=ssq[0:nn, col:col + 1]),
                              reads=[("ssq", col)], writes=[("ssq", col)])
                        P.add("dve", lambda e, ci=ci, nn=nn, col=col: e.scalar_tensor_tensor(
                            out=kcb[ci][0:nn, :], in0=psO[ci][0:nn, :], scalar=ssq[0:nn, col:col + 1], in1=gk[0:nn, :],
                            op0=ALU.mult, op1=ALU.mult), reads=[("psO", ci), ("ssq", col), "gk"], writes=[("kcb", ci)])
                        P.add("pe", lambda e, ci=ci: e.transpose(out=psK[:, :], in_=kcb[ci][:, :], identity=ident[:, :]),
                              reads=[("kcb", ci), "ident"], writes=["psK"])
                        P.add("act", lambda e, head=head, n0=n0: e.copy(out=kcT[head][:, n0:n0 + 128], in_=psK[:, :]),
                              reads=["psK"], writes=[("kcT", head, n0)])
                    else:
                        P.add("dve", lambda e, ci=ci, nn=nn, head=head: e.tensor_copy(
                            out=vce[head][0:nn, ci, 0:64], in_=psO[ci][0:nn, :]), reads=[("psO", ci)], writes=[("vce", head)])
                if kv == 0:
                    P.add("sp", lambda e, head=head: e.dma_start(out=T["KCT"][head, :, :], in_=kcT[head][:, :]),
                          reads=[("kcT", head, 0), ("kcT", head, 128)], chan=co)
                else:
                    P.add("sp", lambda e, head=head: e.dma_start(out=VCv[head], in_=vce[head][:, :, :]),
                          reads=[("vce", head)], chan=co)

    run_phase(nc, build)


class UnitPipe:
    def __init__(self, P, psS, PT, depth=2):
        self.P, self.psS, self.PT, self.depth = P, psS, PT, depth
        self.q = []
        self.u = 0

    def push(self, lhsT, rhs, vlhsT, pacc, acc_key, first, last, mask, kdeps, bias=None, bkeys=(), post=None):
        P = self.P
        u = self.u
        self.u += 1
        sb_, pb = u % len(self.psS), u % len(self.PT)
        psS, PT = self.psS[sb_], self.PT[pb]
        P.add("pe", lambda e: e.matmul(out=psS[:, :], lhsT=lhsT, rhs=rhs, start=True, stop=True),
              reads=kdeps, writes=[("psS", sb_)])
        if bias is None:
            P.add("act", lambda e: e.activation(out=PT[:, :], in_=psS[:, :], func=AF.Exp, scale=0.125),
                  reads=[("psS", sb_)], writes=[("PT", pb)])
        else:
            P.add("act", lambda e: e.activation(out=PT[:, :], in_=psS[:, :], func=AF.Exp, scale=0.125, bias=bias),
                  reads=[("psS", sb_)] + list(bkeys), writes=[("PT", pb)])
        if mask is not None:
            base, cm, step = mask
            P.add("pool", lambda e: asel(e, out=PT[:, :], in_=PT[:, :], pattern=[[step, 512]], compare_op=ALU.is_ge,
                                         fill=0.0, base=base, channel_multiplier=cm), reads=[("PT", pb)], writes=[("PT", pb)])
        self.q.append((PT, pb, vlhsT, pacc, acc_key, first, last, kdeps, post))
        if len(self.q) > self.depth:
            self._pv()

    def _pv(self):
        P = self.P
        PT, pb, vlhsT, pacc, acc_key, first, last, kdeps, post = self.q.pop(0)
        P.add("pe", lambda e: e.matmul(out=pacc[0:65, :], lhsT=vlhsT, rhs=PT[:, :], start=first, stop=last),
              reads=[("PT", pb)] + list(kdeps), writes=[acc_key] if first else [])
        if last:
            P.last_w[acc_key] = P.ops["pe"][-1]
            if post is not None:
                post()

    def flush(self):
        while self.q:
            self._pv()


def nsa_phase(nc, T):
    BIG = 2048.0
    TINY = 1e-30

    def build(P, es):
        ident, identf = make_ident(P, nc, es)
        QB = sb(nc, es, "QB", [128, 4, S], BF16)
        KE = sb(nc, es, "KE", [128, S], BF16)
        KW = sb(nc, es, "KW", [64, S], BF16)
        KC = sb(nc, es, "KC", [64, 256], BF16)
        Vs = sb(nc, es, "Vs", [128, NT, 72], BF16)
        Vw = sb(nc, es, "Vw", [128, NT, 72], BF16)
        VCs = sb(nc, es, "VCs", [128, 2, 72], BF16)
        OVf = sb(nc, es, "OVf", [128, 2, 72], F32)
        OV = sb(nc, es, "OV", [128, 2, 72], BF16)
        Gs = sb(nc, es, "Gs", [128, NT, 24], F32)
        ET = [[sb(nc, es, "ET%d_%d" % (i, j), [128, 512], BF16) for j in range(2)] for i in range(2)]
        PT = [sb(nc, es, "PT%d" % i, [128, 512], BF16) for i in range(4)]
        OCs = sb(nc, es, "OCs", [65, 4, 512], F32)
        OWs = sb(nc, es, "OWs", [65, 4, 512], F32)
        OSs = [sb(nc, es, "OSs%d" % i, [65, 512], F32) for i in range(2)]
        imp = sb(nc, es, "imp", [128, 4, 64], F32)
        impt = sb(nc, es, "impt", [128, 4, 64], F32)
        impm = [sb(nc, es, "impm%d" % i, [128, 64], F32) for i in range(2)]
        rd4 = sb(nc, es, "rd4", [128, 4], F32)
        m1 = sb(nc, es, "m1", [128, 8], F32)
        m2 = sb(nc, es, "m2", [128, 8], F32)
        tmp = sb(nc, es, "tmp", [128, 64], F32)
        thr = sb(nc, es, "thr", [128, 1], F32)
        BN = [sb(nc, es, "BN%d" % i, [128, 128], BF16) for i in range(4)]
        dn = [sb(nc, es, "dn%d" % i, [128, 3], F32) for i in range(2)]
        ost = [sb(nc, es, "ost%d" % i, [128, 4, 256], F32) for i in range(2)]
        psS = [ps(nc, es, "psS%d" % i, [128, 512], F32) for i in range(3)]
        psOC = ps(nc, es, "psOC", [128, 512], F32)
        psOS = ps(nc, es, "psOS", [128, 512], F32)
        psOW = ps(nc, es, "psOW", [128, 512], F32)
        psIB = ps(nc, es, "psIB", [128, 512], F32)
        psI = psIB[:, 0:260].rearrange("p (a b) -> p a b", a=4)
        psBT = psIB[:, 320:384].bitcast(BF16)
        psFb = ps(nc, es, "psFb", [128, 512], F32)
        psF = psFb[:, 0:195].rearrange("p (a b) -> p a b", a=3)
        c0 = P.chan()
        cks = [P.chan() for _ in range(2)]
        cst = [P.chan() for _ in range(2)]

        P.add("sp", lambda e: e.dma_start(out=Gs[:, :, :], in_=T["G"].rearrange("(t p) c -> p t c", p=128)), writes=["Gs"], chan=c0)
        P.add("pool", lambda e: e.memset(KE[64:128, :], BIG), writes=["KEm"])
        P.add("pool", lambda e: asel(e, out=KE[64:128, :], in_=KE[64:128, :], pattern=[[1, S]], compare_op=ALU.is_ge,
                                                fill=0.0, base=0, channel_multiplier=-64), reads=["KEm"], writes=["KEm"])
        P.add("pool", lambda e: asel(e, out=KE[64:128, :], in_=KE[64:128, :], pattern=[[-1, S]], compare_op=ALU.is_ge,
                                                fill=0.0, base=63, channel_multiplier=64), reads=["KEm"], writes=["KEm"])
        P.add("pool", lambda e: e.memset(OVf[:, :, :], 1.0), writes=["OVf"])
        for nt in range(2):
            P.add("pool", lambda e, nt=nt: asel(e,
                out=OVf[:, nt, 0:64], in_=OVf[:, nt, 0:64], pattern=[[64, 64]], compare_op=ALU.is_ge, fill=0.0,
                base=63 - 2048 * nt, channel_multiplier=-16), reads=["OVf"], writes=["OVf"])
            P.add("pool", lambda e, nt=nt: asel(e,
                out=OVf[:, nt, 0:64], in_=OVf[:, nt, 0:64], pattern=[[-64, 64]], compare_op=ALU.is_ge, fill=0.0,
                base=2048 * nt + 31, channel_multiplier=16), reads=["OVf"], writes=["OVf"])
        P.add("pool", lambda e: e.tensor_copy(out=OV[:, :, :], in_=OVf[:, :, :]), reads=["OVf"], writes=["OV"])
        for i in range(4):
            P.add("pool", lambda e, i=i: e.memset(BN[i][:, 0:64], 0.0), writes=[("BN0", i)])
        P.add("pool", lambda e: e.memset(Vs[:, :, 64:65], 1.0), writes=["Vs1"])
        P.add("pool", lambda e: e.memset(Vw[:, :, 64:65], 1.0), writes=["Vw1"])
        Vv = T["V"].rearrange("(t p) c -> p t c", p=128)
        OAv = T["OA"].rearrange("(t p) c -> p t c", p=128)

        def mask_ge(tile, base, cm, step):
            return lambda e: asel(e, out=tile[:, :], in_=tile[:, :], pattern=[[step, 512]], compare_op=ALU.is_ge,
                                             fill=0.0, base=base, channel_multiplier=cm)

        pipe = UnitPipe(P, psS, PT, depth=2)

        for kh in range(DBG['kh']):
            ck = cks[kh]
            for g in range(4):
                P.add("sp", lambda e, g=g, kh=kh: e.dma_start(out=QB[0:64, g, :], in_=T["QKT"][2 + 4 * kh + g, :, :]),
                      writes=[("QBq", g)], chan=ck)
            P.add("sp", lambda e, kh=kh: e.dma_start(out=KE[0:64, :], in_=T["QKT"][10 + kh, :, :]), writes=["KEk"], chan=ck)
            P.add("sp", lambda e, kh=kh: e.dma_start(out=KW[:, :], in_=T["QKT"][12 + kh, :, :]), writes=["KW"], chan=ck)
            P.add("sp", lambda e, kh=kh: e.dma_start(out=KC[:, :], in_=T["KCT"][kh, :, :]), writes=["KC"], chan=ck)
            P.add("sp", lambda e, kh=kh: e.dma_start(out=Vs[:, :, 0:64], in_=Vv[:, :, kh * 64:(kh + 1) * 64]),
                  reads=["Vs1"], writes=["Vs"], chan=ck)
            P.add("sp", lambda e, kh=kh: e.dma_start(out=Vw[:, :, 0:64], in_=Vv[:, :, 128 + kh * 64:128 + (kh + 1) * 64]),
                  reads=["Vw1"], writes=["Vw"], chan=ck)
            P.add("sp", lambda e, kh=kh: e.dma_start(out=VCs[:, :, 0:65], in_=T["VC"][kh].rearrange("(c p) e -> p c e", p=128)),
                  writes=["VCs"], chan=ck)
            ck.seal()
            for i in range(DBG['ng']):
                qsl = slice(i * 512, (i + 1) * 512)
                nts = [0] if i < 4 else [0, 1]

                def s1(g):
                    for nt in nts:
                        u = pipe.u
                        pipe.u += 1
                        sb_ = u % 3
                        et = ET[g % 2][nt]
                        P.add("pe", lambda e, nt=nt, g=g, sb_=sb_, qsl=qsl: e.matmul(
                            out=psS[sb_][:, :], lhsT=KC[:, nt * 128:(nt + 1) * 128], rhs=QB[0:64, g, qsl], start=True, stop=True),
                            reads=["KC", ("QBq", g)], writes=[("psS", sb_)])
                        P.add("act", lambda e, et=et, sb_=sb_: e.activation(out=et[:, :], in_=psS[sb_][:, :], func=AF.Exp, scale=0.125),
                              reads=[("psS", sb_)], writes=[("ET", g % 2, nt)])
                        P.add("pool", mask_ge(et, 512 * i - 2048 * nt - 31, -16, 1), reads=[("ET", g % 2, nt)], writes=[("ET", g % 2, nt)])

                def s2(g):
                    for j, nt in enumerate(nts):
                        P.add("pe", lambda e, nt=nt, j=j, g=g: e.matmul(
                            out=psOC[0:65, :], lhsT=VCs[:, nt, 0:65], rhs=ET[g % 2][nt][:, :], start=(j == 0), stop=(j == len(nts) - 1)),
                            reads=[("ET", g % 2, nt), "VCs"], writes=["psOC"] if j == 0 else [])
                    P.last_w["psOC"] = P.ops["pe"][-1]
                    P.add("act", lambda e, g=g: e.copy(out=OCs[:, g, :], in_=psOC[0:65, :]), reads=["psOC"], writes=[("OCs", g)])
                    for tt in range(4):
                        for j, nt in enumerate(nts):
                            P.add("pe", lambda e, nt=nt, j=j, tt=tt, g=g: e.matmul(
                                out=psI[:, tt, :], lhsT=ET[g % 2][nt][:, tt * 128:(tt + 1) * 128], rhs=OV[:, nt, 0:65],
                                start=(j == 0), stop=(j == len(nts) - 1)),
                                reads=[("ET", g % 2, nt), "OV"], writes=["psI"] if (j == 0 and tt == 0) else [])
                    P.last_w["psI"] = P.ops["pe"][-1]
                    P.add("dve", lambda e: e.tensor_scalar(out=rd4[:, :], in0=psI[:, :, 64], scalar1=TINY, scalar2=None, op0=ALU.max),
                          reads=["psI"], writes=["rd4"])
                    P.add("dve", lambda e: e.reciprocal(out=rd4[:, :], in_=rd4[:, :]), reads=["rd4"], writes=["rd4"])
                    if g == 0:
                        P.add("dve", lambda e: e.tensor_tensor(
                            out=imp[:, :, :], in0=psI[:, :, 0:64], in1=rd4[:, :].unsqueeze(2).to_broadcast([128, 4, 64]), op=ALU.mult),
                            reads=["psI", "rd4"], writes=["imp"])
                    else:
                        P.add("dve", lambda e: e.tensor_tensor(
                            out=impt[:, :, :], in0=psI[:, :, 0:64], in1=rd4[:, :].unsqueeze(2).to_broadcast([128, 4, 64]), op=ALU.mult),
                            reads=["psI", "rd4"], writes=["impt"])
                        P.add("dve", lambda e: e.tensor_tensor(out=imp[:, :, :], in0=imp[:, :, :], in1=impt[:, :, :], op=ALU.add),
                              reads=["imp", "impt"], writes=["imp"])

                s1(0)
                for g in range(4):
                    if g < 3:
                        s1(g + 1)
                    s2(g)
                for tt in range(4):
                    bs = tt % 2
                    t0 = 512 * i + 128 * tt
                    P.add("pool", lambda e, tt=tt, bs=bs, t0=t0: asel(e,
                        out=impm[bs][:, :], in_=imp[:, tt, :], pattern=[[-64, 64]], compare_op=ALU.is_ge, fill=1.0e6,
                        base=t0 - 128, channel_multiplier=1), reads=["imp"], writes=[("impm", bs)])
                    P.add("pool", lambda e, bs=bs, t0=t0: asel(e,
                        out=impm[bs][:, :], in_=impm[bs][:, :], pattern=[[-64, 64]], compare_op=ALU.is_ge, fill=-1.0,
                        base=t0, channel_multiplier=1), reads=[("impm", bs)], writes=[("impm", bs)])
                    P.add("pool", lambda e, bs=bs: e.memset(impm[bs][:, 0:1], 1.0e6), reads=[("impm", bs)], writes=[("impm", bs)])
                    P.add("dve", lambda e, bs=bs: e.max(out=m1[:, :], in_=impm[bs][:, :]), reads=[("impm", bs)], writes=["m1"])
                    P.add("dve", lambda e, bs=bs: e.match_replace(out=tmp[:, :], in_to_replace=m1[:, :], in_values=impm[bs][:, :],
                                                                  imm_value=-2.0), reads=[("impm", bs), "m1"], writes=["tmp"])
                    P.add("dve", lambda e: e.max(out=m2[:, :], in_=tmp[:, :]), reads=["tmp"], writes=["m2"])
                    P.add("dve", lambda e: e.tensor_scalar(out=thr[:, :], in0=m2[:, 7:8], scalar1=0.0, scalar2=None, op0=ALU.max),
                          reads=["m2"], writes=["thr"])
                    P.add("dve", lambda e, bs=bs, tt=tt: e.tensor_scalar(
                        out=BN[tt][:, 64:128], in0=impm[bs][:, :], scalar1=thr[:, 0:1], scalar2=1.0, op0=ALU.is_ge, op1=ALU.subtract),
                        reads=[("impm", bs), "thr", ("BN0", tt)], writes=[("BN", tt)])
                for g in range(4):
                    kts = list(range(max(0, 4 * i - 4), 4 * i + 4))
                    for j, kt in enumerate(kts):
                        if kt >= 4 * i:
                            ms = (-128 * (kt - 4 * i), -1, 1)
                        else:
                            ms = (128 * (kt - 4 * i + 4) - 1, 1, -1)
                        post = (lambda g=g: P.add("dve", lambda e: e.tensor_copy(out=OWs[:, g, :], in_=psOW[0:65, :]),
                                                  reads=["psOW"], writes=[("OWs", g)]))
                        pipe.push(KW[:, kt * 128:(kt + 1) * 128], QB[0:64, g, qsl], Vw[:, kt, 0:65], psOW, "psOW",
                                  j == 0, j == len(kts) - 1, ms, ["KW", ("QBq", g), "Vw"], post=post)
                for tt in range(4):
                    t0 = 512 * i + 128 * tt
                    P.add("pe", lambda e, tt=tt: e.transpose(out=psBT, in_=BN[tt][:, :], identity=ident[:, :]),
                          reads=[("BN", tt), ("BN0", tt), "ident"], writes=["psBT"])
                    P.add("act", lambda e, t0=t0: e.copy(out=QB[64:128, :, t0:t0 + 128],
                                                         in_=psBT[64:128].unsqueeze(1).to_broadcast([64, 4, 128])),
                          reads=["psBT"], writes=[("QBm", tt)])

                def finalize(g):
                    osl = g % 2
                    hd = kh * 4 + g
                    for tt in range(4):
                        tile_i = 4 * i + tt
                        ds = tt % 2
                        tsl = slice(tt * 128, (tt + 1) * 128)
                        for b, (src, key) in enumerate(((OCs[:, g, tsl], ("OCs", g)), (OSs[osl][:, tsl], ("OSs", osl)),
                                                         (OWs[:, g, tsl], ("OWs", g)))):
                            P.add("pe", lambda e, b=b, src=src: e.transpose(out=psF[:, b, :], in_=src, identity=identf[0:65, 0:65]),
                                  reads=[key, "identf"], writes=["psF"] if b == 0 else [])
                        P.last_w["psF"] = P.ops["pe"][-1]
                        P.add("dve", lambda e, ds=ds: e.tensor_scalar(out=dn[ds][:, :], in0=psF[:, :, 64], scalar1=TINY, scalar2=None,
                                                                      op0=ALU.max), reads=["psF"], writes=[("dn", ds)])
                        P.add("dve", lambda e, ds=ds: e.reciprocal(out=dn[ds][:, :], in_=dn[ds][:, :]), reads=[("dn", ds)], writes=[("dn", ds)])
                        P.add("dve", lambda e, ds=ds, tile_i=tile_i, hd=hd: e.tensor_tensor(
                            out=dn[ds][:, :], in0=dn[ds][:, :], in1=Gs[:, tile_i, hd * 3:hd * 3 + 3], op=ALU.mult),
                            reads=[("dn", ds), "Gs"], writes=[("dn", ds)])
                        oo = ost[i % 2][:, tt, g * 64:(g + 1) * 64]
                        P.add("dve", lambda e, ds=ds, oo=oo: e.tensor_scalar(out=oo, in0=psF[:, 0, 0:64], scalar1=dn[ds][:, 0:1],
                                                                             scalar2=None, op0=ALU.mult),
                              reads=["psF", ("dn", ds)], writes=[("ost", i % 2, tt, g)])
                        for b in (1, 2):
                            P.add("dve", lambda e, ds=ds, oo=oo, b=b: e.scalar_tensor_tensor(
                                out=oo, in0=psF[:, b, 0:64], scalar=dn[ds][:, b:b + 1], in1=oo, op0=ALU.mult, op1=ALU.add),
                                reads=["psF", ("dn", ds), ("ost", i % 2, tt, g)], writes=[("ost", i % 2, tt, g)])

                pending = []
                for g in range(4):
                    kts = list(range(0, 4 * i + 4))
                    osl = g % 2
                    for j, kt in enumerate(kts):
                        ms = (-128 * (kt - 4 * i), -1, 1) if kt >= 4 * i else None

                        def post(g=g, osl=osl):
                            P.add("dve", lambda e: e.tensor_copy(out=OSs[osl][:, :], in_=psOS[0:65, :]), reads=["psOS"], writes=[("OSs", osl)])
                            pending.append(g)
                        pipe.push(KE[:, kt * 128:(kt + 1) * 128], QB[:, g, qsl], Vs[:, kt, 0:65], psOS, "psOS",
                                  j == 0, j == len(kts) - 1, ms,
                                  ["KEk", "KEm", ("QBq", g), "Vs"] + [("QBm", tt) for tt in range(4)], post=post)
                        if j == 3 and pending:
                            finalize(pending.pop(0))
                pipe.flush()
                while pending:
                    finalize(pending.pop(0))
                if "DBGB" in T and kh == 0:
                    P.add("sp", lambda e, qsl=qsl: e.dma_start(out=T["DBGB"][:, qsl], in_=QB[64:128, 0, qsl]),
                          reads=[("QBm", tt) for tt in range(4)], chan=c0)
                P.add("sp", lambda e, i=i, kh=kh: e.dma_start(out=OAv[:, 4 * i:4 * i + 4, kh * 256:(kh + 1) * 256], in_=ost[i % 2][:, :, :]),
                      reads=[("ost", i % 2, tt, g) for tt in range(4) for g in range(4)], chan=cst[i % 2])

    run_phase(nc, build)


def fox_phase(nc, T):
    TINY = 1e-30

    def build(P, es):
        ident, identf = make_ident(P, nc, es)
        QT = [sb(nc, es, "QT%d" % i, [64, S], BF16) for i in range(2)]
        KT = [sb(nc, es, "KT%d" % i, [64, S], BF16) for i in range(2)]
        Vf = [sb(nc, es, "Vf%d" % i, [128, NT, 72], BF16) for i in range(2)]
        lf = sb(nc, es, "lf", [128, NT, 8], F32)
        U = sb(nc, es, "U", [128, 128], F32)
        ONES = sb(nc, es, "ONES", [128, 128], F32)
        ones32 = sb(nc, es, "ones32", [128, NT], F32)
        cin = sb(nc, es, "cin", [128, NT, 8], F32)
        tot = sb(nc, es, "tot", [128, NT, 8], F32)
        incl = sb(nc, es, "incl", [128, NT, 8], F32)
        call = sb(nc, es, "call", [128, NT, 8], F32)
        biasT = sb(nc, es, "biasT", [128, NG, 8, NT], F32)
        PT = [sb(nc, es, "PT%d" % i, [128, 512], BF16) for i in range(4)]
        OFs = [sb(nc, es, "OFs%d" % i, [65, 512], F32) for i in range(2)]
        dn = [sb(nc, es, "dn%d" % i, [128, 1], F32) for i in range(2)]
        ostf = [sb(nc, es, "ostf%d" % i, [128, 4, 64], F32) for i in range(2)]
        psS = [ps(nc, es, "psS%d" % i, [128, 512], F32) for i in range(3)]
        psO = [ps(nc, es, "psO%d" % i, [128, 512], F32) for i in range(2)]
        psFF = [ps(nc, es, "psFF%d" % i, [128, 512], F32) for i in range(2)]
        psF = [psFF[0][:, 0:65], psFF[1][:, 0:65]]
        psC = psS[0][:, 0:NT * 8]
        psTt = psS[1][:, 0:NT * 8]
        c0 = P.chan()
        ckh = [P.chan() for _ in range(2)]
        cst = [P.chan() for _ in range(2)]
        Vv = T["V"].rearrange("(t p) c -> p t c", p=128)
        OAv = T["OA"].rearrange("(t p) c -> p t c", p=128)

        P.add("sp", lambda e: e.dma_start(out=lf[:, :, :], in_=T["LF"].rearrange("(t p) c -> p t c", p=128)), writes=["lf"], chan=c0)
        P.add("pool", lambda e: e.memset(U[:, :], 1.0), writes=["U"])
        P.add("pool", lambda e: asel(e, out=U[:, :], in_=U[:, :], pattern=[[1, 128]], compare_op=ALU.is_ge, fill=0.0,
                                     base=0, channel_multiplier=-1), reads=["U"], writes=["U"])
        P.add("pool", lambda e: e.memset(ONES[:, :], 1.0), writes=["ONES"])
        P.add("pool", lambda e: e.memset(ones32[:, :], 1.0), writes=["ones32"])
        for i in range(2):
            P.add("pool", lambda e, i=i: e.memset(Vf[i][:, :, 64:65], 1.0), writes=[("Vf1", i)])
        lff = lf[:, :, :].rearrange("p t c -> p (t c)")
        P.add("pe", lambda e: e.matmul(out=psC, lhsT=U[:, :], rhs=lff, start=True, stop=True), reads=["U", "lf"], writes=[("psS", 0)])
        P.add("pe", lambda e: e.matmul(out=psTt, lhsT=ONES[:, :], rhs=lff, start=True, stop=True), reads=["ONES", "lf"], writes=[("psS", 1)])
        P.add("dve", lambda e: e.tensor_copy(out=cin[:, :, :].rearrange("p t c -> p (t c)"), in_=psC), reads=[("psS", 0)], writes=["cin"])
        P.add("dve", lambda e: e.tensor_copy(out=tot[:, :, :].rearrange("p t c -> p (t c)"), in_=psTt), reads=[("psS", 1)], writes=["tot"])
        for h in range(8):
            P.add("dve", lambda e, h=h: e.tensor_tensor_scan(out=incl[:, :, h], data0=ones32[:, :], data1=tot[:, :, h], initial=0.0,
                                                             op0=ALU.mult, op1=ALU.add), reads=["tot", "ones32"], writes=[("incl", h)])
        inck = [("incl", h) for h in range(8)]
        P.add("dve", lambda e: e.tensor_tensor(out=call[:, :, :], in0=incl[:, :, :], in1=tot[:, :, :], op=ALU.subtract),
              reads=inck + ["tot"], writes=["call"])
        P.add("dve", lambda e: e.tensor_tensor(out=call[:, :, :], in0=call[:, :, :], in1=cin[:, :, :], op=ALU.add),
              reads=["call", "cin"], writes=["call"])
        for i in range(NG):
            for h in range(8):
                P.add("dve", lambda e, i=i, h=h: e.tensor_scalar(
                    out=biasT[:, i, h, :], in0=call[:, :, h], scalar1=-1.0, scalar2=incl[:, 4 * i + 1, h:h + 1],
                    op0=ALU.mult, op1=ALU.add), reads=["call"] + inck, writes=[("biasT", i, h)])
        pipe = UnitPipe(P, psS, PT, depth=2)
        fi = 0
        pending = []

        def finalize(h, i, ob):
            for tt in range(4):
                fb = tt % 2
                P.add("pe", lambda e, fb=fb, ob=ob, tt=tt: e.transpose(
                    out=psF[fb], in_=OFs[ob][:, tt * 128:(tt + 1) * 128], identity=identf[0:65, 0:65]),
                    reads=[("OFs", ob), "identf"], writes=[("psF", fb)])
                P.add("dve", lambda e, fb=fb: e.tensor_scalar(out=dn[fb][:, :], in0=psF[fb][:, 64:65], scalar1=TINY, scalar2=None,
                                                              op0=ALU.max), reads=[("psF", fb)], writes=[("dn", fb)])
                P.add("dve", lambda e, fb=fb: e.reciprocal(out=dn[fb][:, :], in_=dn[fb][:, :]), reads=[("dn", fb)], writes=[("dn", fb)])
                P.add("dve", lambda e, fb=fb, ob=ob, tt=tt: e.tensor_scalar(
                    out=ostf[ob][:, tt, :], in0=psF[fb][:, 0:64], scalar1=dn[fb][:, 0:1], scalar2=None, op0=ALU.mult),
                    reads=[("psF", fb), ("dn", fb)], writes=[("ostf", ob, tt)])
            P.add("sp", lambda e, i=i, h=h, ob=ob: e.dma_start(
                out=OAv[:, 4 * i:4 * i + 4, 512 + 64 * h:512 + 64 * (h + 1)], in_=ostf[ob][:, :, :]),
                reads=[("ostf", ob, tt) for tt in range(4)], chan=cst[ob])

        for h in range(DBG.get('fh', 8)):
            hs_ = h % 2
            ck = ckh[hs_]
            P.add("sp", lambda e, h=h, hs_=hs_: e.dma_start(out=QT[hs_][:, :], in_=T["QKT"][14 + h, :, :]), writes=[("QT", hs_)], chan=ck)
            P.add("sp", lambda e, h=h, hs_=hs_: e.dma_start(out=KT[hs_][:, :], in_=T["QKT"][22 + h, :, :]), writes=[("KT", hs_)], chan=ck)
            P.add("sp", lambda e, h=h, hs_=hs_: e.dma_start(out=Vf[hs_][:, :, 0:64], in_=Vv[:, :, 256 + 64 * h:256 + 64 * (h + 1)]),
                  reads=[("Vf1", hs_)], writes=[("Vf", hs_)], chan=ck)
            for op in ck.ops[-3:]:
                op.chanval = ck.count
            for i in range(NG):
                qsl = slice(i * 512, (i + 1) * 512)
                ob = fi % 2
                fi += 1
                nk = 4 * i + 4
                for kt in range(nk):
                    ms = (-128 * (kt - 4 * i), -1, 1) if kt >= 4 * i else None

                    def post(h=h, i=i, ob=ob):
                        P.add("dve", lambda e: e.tensor_copy(out=OFs[ob][:, :], in_=psO[ob][0:65, :]), reads=[("psO", ob)], writes=[("OFs", ob)])
                        pending.append((h, i, ob))
                    pipe.push(KT[hs_][:, kt * 128:(kt + 1) * 128], QT[hs_][:, qsl], Vf[hs_][:, kt, 0:65], psO[ob], ("psO", ob),
                              kt == 0, kt == nk - 1, ms, [("KT", hs_), ("QT", hs_), ("Vf", hs_)],
                              bias=biasT[:, i, h, kt:kt + 1], bkeys=[("biasT", i, h)], post=post)
                    if pending and (kt == 3 or DBG.get('fox_now', 0)):
                        finalize(*pending.pop(0))
        pipe.flush()
        while pending:
            finalize(*pending.pop(0))

    run_phase(nc, build)


def norm_rows(P, src, junk, ssq, rs, epst, nb, gB, nparts, width, key):
    for j in range(nparts):
        cs = slice(j * width, (j + 1) * width)
        P.add("act", lambda e, cs=cs, j=j: e.activation(out=junk[:, cs], in_=src[:, cs], func=AF.Square, accum_out=ssq[:, j:j + 1]),
              reads=[key], writes=["junk", ("ssq", key, j)])
    P.add("act", lambda e: e.activation(out=rs[:, 0:nparts], in_=ssq[:, 0:nparts], func=AF.Sqrt, scale=1.0 / width, bias=epst[:, 0:1]),
          reads=[("ssq", key, j) for j in range(nparts)] + ["eps"], writes=[("rs", key)])
    P.add("dve", lambda e: e.reciprocal(out=rs[:, 0:nparts], in_=rs[:, 0:nparts]), reads=[("rs", key)], writes=[("rs", key)])
    for j in range(nparts):
        cs = slice(j * width, (j + 1) * width)
        P.add("dve", lambda e, cs=cs, j=j: e.scalar_tensor_tensor(out=nb[:, cs], in0=src[:, cs], scalar=rs[:, j:j + 1], in1=gB[:, cs],
                                                                  op0=ALU.mult, op1=ALU.mult),
              reads=[key, ("rs", key), "gB", "gB2"], writes=[("nb", key)])


def out_phase(nc, T):
    def build(P, es):
        ident, identf = make_ident(P, nc, es)
        epst = sb(nc, es, "epst", [128, 1], F32)
        P.add("pool", lambda e: e.memset(epst[:, :], EPS), writes=["eps"])
        wo = sb(nc, es, "wo", [128, 8, D], BF16)
        stage = [sb(nc, es, "ostg%d" % i, [128, D], F32) for i in range(2)]
        stch = [P.chan() for _ in range(2)]
        gB = sb(nc, es, "gB", [128, D], F32)
        xn = [sb(nc, es, "xn%d" % i, [128, D], F32) for i in range(2)]
        xr = [sb(nc, es, "xr%d" % i, [128, D], F32) for i in range(2)]
        nb = [sb(nc, es, "nb%d" % i, [128, D], BF16) for i in range(2)]
        junk = sb(nc, es, "junk", [128, D], BF16)
        ssq = [sb(nc, es, "ssq%d" % i, [128, 2], F32) for i in range(2)]
        rs = [sb(nc, es, "rs%d" % i, [128, 2], F32) for i in range(2)]
        mT = [sb(nc, es, "mT%d" % i, [128, 8, 128], BF16) for i in range(2)]
        psT = [ps(nc, es, "psT%d" % i, [128, D], BF16) for i in range(2)]
        pso = [ps(nc, es, "pso%d" % i, [128, 512], F32) for i in range(4)]
        cg = P.chan()
        cx = [P.chan() for _ in range(2)]
        cr = [P.chan() for _ in range(2)]
        co = [P.chan() for _ in range(2)]
        P.add("sp", lambda e: e.dma_start(out=gB[:, 0:512], in_=T["out_norm_nsa"].partition_broadcast(128)), writes=["gB"], chan=cg)
        P.add("sp", lambda e: e.dma_start(out=gB[:, 512:1024], in_=T["out_norm_fox"].partition_broadcast(128)), writes=["gB2"], chan=cg)
        cg.seal()
        wv = T["w_out"].rearrange("(c p) f -> p c f", p=128)
        for c in range(8):
            s_ = c % 2
            P.add("sp", lambda e, s_=s_, c=c: e.dma_start(out=stage[s_][:, :], in_=wv[:, c, :]), writes=[("stage", s_)], chan=stch[s_])
            P.add("dve" if c % 2 == 0 else "pool", lambda e, s_=s_, c=c: e.tensor_copy(out=wo[:, c, :], in_=stage[s_][:, :]),
                  reads=[("stage", s_)], writes=[("wo", c)])
        wkeys = [("wo", c) for c in range(8)]
        for t in range(NT):
            xs = t % 2
            P.add("sp", lambda e, xs=xs, t=t: e.dma_start(out=xn[xs][:, :], in_=T["OA"][t * 128:(t + 1) * 128, :]), writes=[("xn", xs)], chan=cx[xs])
            P.add("sp", lambda e, xs=xs, t=t: e.dma_start(out=xr[xs][:, :], in_=T["h1"][t * 128:(t + 1) * 128, :]), writes=[("xr", xs)], chan=cr[xs])
            norm_rows(P, xn[xs], junk, ssq[xs], rs[xs], epst, nb[xs], gB, 2, 512, ("xn", xs))
            for c in range(8):
                P.add("pe", lambda e, xs=xs, c=c: e.transpose(out=psT[xs][:, c * 128:(c + 1) * 128], in_=nb[xs][:, c * 128:(c + 1) * 128],
                                                              identity=ident[:, :]),
                      reads=[("nb", ("xn", xs)), "ident"], writes=[("psT", xs)] if c == 0 else [])
            P.last_w[("psT", xs)] = P.ops["pe"][-1]
            P.add("act", lambda e, xs=xs: e.copy(out=mT[xs][:, :, :], in_=psT[xs][:, :].rearrange("p (c t) -> p c t", c=8)),
                  reads=[("psT", xs)], writes=[("mT", xs)])
            for half in range(2):
                pb = (t * 2 + half) % 4
                for c in range(8):
                    P.add("pe", lambda e, pb=pb, c=c, xs=xs, half=half: e.matmul(
                        out=pso[pb][:, :], lhsT=mT[xs][:, c, :], rhs=wo[:, c, half * 512:(half + 1) * 512], start=(c == 0), stop=(c == 7)),
                        reads=[("mT", xs)] + (wkeys if t == 0 else []), writes=[("pso", pb)] if c == 0 else [])
                P.last_w[("pso", pb)] = P.ops["pe"][-1]
                P.add("dve", lambda e, pb=pb, xs=xs, half=half: e.tensor_tensor(
                    out=xr[xs][:, half * 512:(half + 1) * 512], in0=pso[pb][:, :], in1=xr[xs][:, half * 512:(half + 1) * 512], op=ALU.add),
                    reads=[("pso", pb), ("xr", xs)], writes=[("xr", xs)])
            P.add("sp", lambda e, xs=xs, t=t: e.dma_start(out=T["h1"][t * 128:(t + 1) * 128, :], in_=xr[xs][:, :]),
                  reads=[("xr", xs)], chan=co[xs])

    run_phase(nc, build)


def ple_phase(nc, T):
    def build(P, es):
        ident, identf = make_ident(P, nc, es)
        epst = sb(nc, es, "epst", [128, 1], F32)
        P.add("pool", lambda e: e.memset(epst[:, :], EPS), writes=["eps"])
        wg = sb(nc, es, "wg", [128, 8, D], BF16)
        wp = sb(nc, es, "wp", [128, 2, D], BF16)
        stage = [sb(nc, es, "lstg%d" % i, [128, D], F32) for i in range(2)]
        stch = [P.chan() for _ in range(2)]
        gB = sb(nc, es, "gB", [128, D], F32)
        gE = sb(nc, es, "gE", [128, D], F32)
        xn = [sb(nc, es, "xn%d" % i, [128, D], F32) for i in range(2)]
        pt = [sb(nc, es, "pt%d" % i, [128, 256], F32) for i in range(2)]
        pb16 = [sb(nc, es, "pb%d" % i, [128, 256], BF16) for i in range(2)]
        nb = [sb(nc, es, "nb%d" % i, [128, D], BF16) for i in range(2)]
        junk = sb(nc, es, "junk", [128, D], BF16)
        ssq = [sb(nc, es, "ssq%d" % i, [128, 4], F32) for i in range(2)]
        rs = [sb(nc, es, "rs%d" % i, [128, 4], F32) for i in range(2)]
        mT = [sb(nc, es, "mT%d" % i, [128, 10, 128], BF16) for i in range(2)]
        gate = [sb(nc, es, "gate%d" % i, [128, D], F32) for i in range(2)]
        ev = [sb(nc, es, "ev%d" % i, [128, D], F32) for i in range(2)]
        psT = [ps(nc, es, "psT%d" % i, [128, 10, 128], BF16) for i in range(1)]
        psg = [ps(nc, es, "psg%d" % i, [128, 512], F32) for i in range(2)]
        pse = [ps(nc, es, "pse%d" % i, [128, 512], F32) for i in range(2)]
        cg = P.chan()
        cx = [P.chan() for _ in range(2)]
        cp = [P.chan() for _ in range(2)]
        co = [P.chan() for _ in range(2)]
        P.add("sp", lambda e: e.dma_start(out=gB[:, :], in_=T["ple_gate_norm"].partition_broadcast(128)), writes=["gB"], chan=cg)
        P.add("sp", lambda e: e.dma_start(out=gE[:, :], in_=T["ple_norm"].partition_broadcast(128)), writes=["gE"], chan=cg)
        cg.seal()
        wv = T["ple_w_gate"].rearrange("(c p) f -> p c f", p=128)
        wpv = T["ple_w_proj"].rearrange("(c p) f -> p c f", p=128)
        for c in range(10):
            s_ = c % 2
            src = wv[:, c, :] if c < 8 else wpv[:, c - 8, :]
            dstw = wg[:, c, :] if c < 8 else wp[:, c - 8, :]
            P.add("sp", lambda e, s_=s_, src=src: e.dma_start(out=stage[s_][:, :], in_=src), writes=[("stage", s_)], chan=stch[s_])
            P.add("dve" if c % 2 == 0 else "pool", lambda e, s_=s_, dstw=dstw: e.tensor_copy(out=dstw, in_=stage[s_][:, :]),
                  reads=[("stage", s_)], writes=[("w", c)])
        wkeys = [("w", c) for c in range(10)]
        for t in range(NT):
            xs = t % 2
            P.add("sp", lambda e, xs=xs, t=t: e.dma_start(out=xn[xs][:, :], in_=T["h3"][t * 128:(t + 1) * 128, :]), writes=[("xn", xs)], chan=cx[xs])
            P.add("sp", lambda e, xs=xs, t=t: e.dma_start(out=pt[xs][:, :], in_=T["p"][t * 128:(t + 1) * 128, :]), writes=[("pt", xs)], chan=cp[xs])
            norm_rows(P, xn[xs], junk, ssq[xs], rs[xs], epst, nb[xs], gB, 1, D, ("xn", xs))
            P.add("pool", lambda e, xs=xs: e.tensor_copy(out=pb16[xs][:, :], in_=pt[xs][:, :]), reads=[("pt", xs)], writes=[("pb16", xs)])
            for c in range(10):
                src = nb[xs][:, c * 128:(c + 1) * 128] if c < 8 else pb16[xs][:, (c - 8) * 128:(c - 7) * 128]
                P.add("pe", lambda e, c=c, src=src: e.transpose(out=psT[0][:, c, :], in_=src, identity=ident[:, :]),
                      reads=[("nb", ("xn", xs)), ("pb16", xs), "ident"], writes=["psT"] if c == 0 else [])
            P.last_w["psT"] = P.ops["pe"][-1]
            P.add("act", lambda e, xs=xs: e.copy(out=mT[xs][:, :, :], in_=psT[0][:, :, :]), reads=["psT"], writes=[("mT", xs)])
            for half in range(2):
                hsl = slice(half * 512, (half + 1) * 512)
                for c in range(8):
                    P.add("pe", lambda e, c=c, xs=xs, half=half, hsl=hsl: e.matmul(
                        out=psg[half][:, :], lhsT=mT[xs][:, c, :], rhs=wg[:, c, hsl], start=(c == 0), stop=(c == 7)),
                        reads=[("mT", xs)] + (wkeys if t == 0 else []), writes=[("psg", half)] if c == 0 else [])
                P.last_w[("psg", half)] = P.ops["pe"][-1]
                P.add("act", lambda e, xs=xs, half=half, hsl=hsl: e.activation(out=gate[xs][:, hsl], in_=psg[half][:, :], func=AF.Sigmoid),
                      reads=[("psg", half)], writes=[("gate", xs, half)])
                for c in range(2):
                    P.add("pe", lambda e, c=c, xs=xs, half=half, hsl=hsl: e.matmul(
                        out=pse[half][:, :], lhsT=mT[xs][:, 8 + c, :], rhs=wp[:, c, hsl], start=(c == 0), stop=(c == 1)),
                        reads=[("mT", xs)] + (wkeys if t == 0 else []), writes=[("pse", half)] if c == 0 else [])
                P.last_w[("pse", half)] = P.ops["pe"][-1]
                P.add("act", lambda e, xs=xs, half=half, hsl=hsl: e.activation(
                    out=junk[:, hsl], in_=pse[half][:, :], func=AF.Square, accum_out=ssq[xs][:, 2 + half:3 + half]),
                    reads=[("pse", half)], writes=[("junk2", half), ("ssqe", xs, half)])
            P.add("dve", lambda e, xs=xs: e.tensor_tensor(out=rs[xs][:, 2:3], in0=ssq[xs][:, 2:3], in1=ssq[xs][:, 3:4], op=ALU.add),
                  reads=[("ssqe", xs, 0), ("ssqe", xs, 1)], writes=[("rse", xs)])
            P.add("act", lambda e, xs=xs: e.activation(out=rs[xs][:, 2:3], in_=rs[xs][:, 2:3], func=AF.Sqrt, scale=1.0 / D, bias=epst[:, 0:1]),
                  reads=[("rse", xs), "eps"], writes=[("rse", xs)])
            P.add("dve", lambda e, xs=xs: e.reciprocal(out=rs[xs][:, 2:3], in_=rs[xs][:, 2:3]), reads=[("rse", xs)], writes=[("rse", xs)])
            for half in range(2):
                hsl = slice(half * 512, (half + 1) * 512)
                P.add("dve", lambda e, xs=xs, half=half, hsl=hsl: e.scalar_tensor_tensor(
                    out=ev[xs][:, hsl], in0=pse[half][:, :], scalar=rs[xs][:, 2:3], in1=gE[:, hsl], op0=ALU.mult, op1=ALU.mult),
                    reads=[("pse", half), ("rse", xs), "gE"], writes=[("ev", xs, half)])
                P.add("pool", lambda e, xs=xs, hsl=hsl: e.tensor_tensor(out=ev[xs][:, hsl], in0=ev[xs][:, hsl], in1=gate[xs][:, hsl], op=ALU.mult),
                      reads=[("ev", xs, half), ("gate", xs, half)], writes=[("ev", xs, half)])
                P.add("pool", lambda e, xs=xs, hsl=hsl: e.tensor_tensor(out=ev[xs][:, hsl], in0=ev[xs][:, hsl], in1=xn[xs][:, hsl], op=ALU.add),
                      reads=[("ev", xs, half), ("xn", xs)], writes=[("ev", xs, half)])
            P.add("sp", lambda e, xs=xs, t=t: e.dma_start(out=T["out"][t * 128:(t + 1) * 128, :], in_=ev[xs][:, :]),
                  reads=[("ev", xs, 0), ("ev", xs, 1)], chan=co[xs])

    run_phase(nc, build)


def rope_tables_np():
    pos = np.arange(S, dtype=np.float32)
    inv = (np.float32(500000.0) ** (-np.arange(0, 16, 2, dtype=np.float32) / np.float32(16))).astype(np.float32)
    ang = (pos[:, None] * inv[None, :]).astype(np.float32)
    return np.cos(ang).astype(np.float32), np.sin(ang).astype(np.float32)


IN_SHAPES = dict(
    x=[S, D], p=[S, 256], ffn1_norm=[D], ffn1_wg=[D, DFF], ffn1_wu=[D, DFF], ffn1_wd=[DFF, D],
    mix_norm=[D], w_in=[D, 2848], b_forget=[8], q_norm_nsa=[64], k_norm_cmp=[64], k_norm_slc=[64], k_norm_win=[64],
    cmp_pos_k=[32, 64], cmp_pos_v=[32, 64], cmp_k_w1=[2048, 256], cmp_k_w2=[256, 64], cmp_v_w1=[2048, 256],
    cmp_v_w2=[256, 64], q_norm_fox=[64], k_norm_fox=[64], out_norm_nsa=[512], out_norm_fox=[512], w_out=[D, D],
    ffn2_norm=[D], ffn2_wg=[D, DFF], ffn2_wu=[D, DFF], ffn2_wd=[DFF, D], ple_gate_norm=[D], ple_w_gate=[D, D],
    ple_w_proj=[256, D], ple_norm=[D], rope_cos=[S, 8], rope_sin=[S, 8])


def build_nc(nph=99, debug=(), skip=()):
    nc = bass.Bass("TRN2", target_bir_lowering=False)
    T = {}
    for name, shape in IN_SHAPES.items():
        T[name] = nc.dram_tensor(name, shape, F32, kind="ExternalInput").ap()

    def scratch(name, shape, dt):
        kind = "ExternalOutput" if name in debug else "Internal"
        T[name] = nc.dram_tensor(name, shape, dt, kind=kind).ap()

    T["out"] = nc.dram_tensor("out", [S, D], F32, kind="ExternalOutput").ap()
    scratch("h1", [S, D], F32)
    scratch("QKT", [32, 64, S], BF16)
    scratch("V", [S, 768], BF16)
    scratch("G", [S, 24], F32)
    scratch("LF", [S, 8], F32)
    scratch("KCT", [2, 64, 256], BF16)
    scratch("VC", [2, 256, 65], BF16)
    scratch("OA", [S, D], F32)
    scratch("h3", [S, D], F32)
    if "DBGB" in debug:
        scratch("DBGB", [64, S], BF16)
    if "DBGT" in debug:
        scratch("DBGT", [3, 65, 512], F32)
    phases = [
        lambda: ffn_half_phase(nc, T, T["x"], T["x"], T["h1"], T["ffn1_norm"], T["ffn1_wg"], T["ffn1_wu"], T["ffn1_wd"], 0, 11, "f1a"),
        lambda: ffn_half_phase(nc, T, T["x"], T["h1"], T["h1"], T["ffn1_norm"], T["ffn1_wg"], T["ffn1_wu"], T["ffn1_wd"], 11, 11, "f1b"),
        lambda: proj_phase(nc, T),
        lambda: cmp_phase(nc, T),
        lambda: nsa_phase(nc, T),
        lambda: fox_phase(nc, T),
        lambda: out_phase(nc, T),
        lambda: ffn_half_phase(nc, T, T["h1"], T["h1"], T["h3"], T["ffn2_norm"], T["ffn2_wg"], T["ffn2_wu"], T["ffn2_wd"], 0, 11, "f2a"),
        lambda: ffn_half_phase(nc, T, T["h1"], T["h3"], T["h3"], T["ffn2_norm"], T["ffn2_wg"], T["ffn2_wu"], T["ffn2_wd"], 11, 11, "f2b"),
        lambda: ple_phase(nc, T),
    ]
    for k, ph in enumerate(phases[:nph]):
        if k not in skip:
            ph()
    return nc


def make_in_maps(inputs, cores=range(8)):
    cos, sin = rope_tables_np()
    in_maps = []
    for b in cores:
        m = {}
        for name in IN_SHAPES:
            if name == "x":
                a = inputs["x"][b]
            elif name == "p":
                a = inputs["p"][0, b]
            elif name == "rope_cos":
                a = cos
            elif name == "rope_sin":
                a = sin
            elif name == "w_in":
                a = inputs["w_in"][0][:, W_IN_PERM]
            else:
                a = inputs[name][0]
            m[name] = np.ascontiguousarray(a, dtype=np.float32)
        in_maps.append(m)
    return in_maps


def kernel(**inputs):
    nc = build_nc()
    res = run_bass_kernel_spmd(nc, make_in_maps(inputs), core_ids=list(range(8)))
    return np.stack([np.asarray(r["out"]) for r in res.results], axis=0)
```

```python
import numpy as np
from contextlib import ExitStack
import concourse.bass as bass
import concourse.mybir as mybir
from concourse.bass_utils import run_bass_kernel_spmd

F32 = mybir.dt.float32
BF16 = mybir.dt.bfloat16
AF = mybir.ActivationFunctionType
ALU = mybir.AluOpType
AX = mybir.AxisListType

S = 4096
D = 1024
DFF = 2816
NT = S // 128
NG = S // 512
EPS = 1e-6
SAME_ENGINE_SYNC = True
DBG = dict(kh=2, ng=NG, stage=5)
_UID = [0]


def _u(name):
    _UID[0] += 1
    return "%s_%d" % (name, _UID[0])

_OFF = dict(qa=0, kc=512, vc=640, ksl=768, vsl=896, kwn=1024, vwn=1152, ga=1280, qf=1304, kf=1816, vf=2328, fl=2840)
_SZ = dict(qa=512, kc=128, vc=128, ksl=128, vsl=128, kwn=128, vwn=128, ga=24, qf=512, kf=512, vf=512, fl=8)
_ORDER = ['kc', 'qa', 'ksl', 'kwn', 'qf', 'kf', 'vc', 'vsl', 'vwn', 'vf', 'ga', 'fl']
W_IN_PERM = np.concatenate([np.arange(_OFF[k], _OFF[k] + _SZ[k]) for k in _ORDER])


class Chan:
    def __init__(self, sem):
        self.sem = sem
        self.count = 0
        self.ops = []

    def seal(self):
        for op in self.ops:
            op.chanval = self.count


class Op:
    __slots__ = ("eng", "fn", "deps", "sig", "sigval", "chan", "chanval", "idx")


class Prog:
    ENGS = ("pe", "act", "dve", "pool", "sp")

    def __init__(self, nc, es):
        self.nc = nc
        self.es = es
        self.ops = {e: [] for e in self.ENGS}
        self.last_w = {}
        self.readers = {}
        self.engsem = {e: es.enter_context(nc.semaphore(_u("s_" + e))) for e in self.ENGS}
        self.chans = []

    def chan(self):
        c = Chan(self.es.enter_context(self.nc.semaphore(_u("c"))))
        self.chans.append(c)
        return c

    def add(self, eng, fn, reads=(), writes=(), chan=None):
        op = Op()
        op.eng = eng
        op.fn = fn
        op.sig = False
        op.sigval = 0
        op.chan = chan
        deps = []
        for r in reads:
            w = self.last_w.get(r)
            if w is not None:
                deps.append(w)
        for w in writes:
            lw = self.last_w.get(w)
            if lw is not None:
                deps.append(lw)
            deps.extend(self.readers.get(w, ()))
        best = {}
        for d in deps:
            k = ("c", id(d.chan)) if d.chan is not None else ("e", d.eng)
            if k not in best or best[k].idx < d.idx:
                best[k] = d
        op.deps = list(best.values())
        op.idx = len(self.ops[eng])
        for r in reads:
            self.readers.setdefault(r, []).append(op)
        for w in writes:
            self.last_w[w] = op
            self.readers[w] = []
        if chan is not None:
            chan.count += 16
            op.chanval = chan.count
            chan.ops.append(op)
        self.ops[eng].append(op)
        return op

    def emit(self, block):
        for e in self.ENGS:
            for op in self.ops[e]:
                for d in op.deps:
                    if d.chan is None and (d.eng != op.eng or SAME_ENGINE_SYNC):
                        d.sig = True
        for e in self.ENGS:
            c = 0
            for op in self.ops[e]:
                if op.sig and op.chan is None:
                    c += 1
                    op.sigval = c
        final = [(c.sem, c.count) for c in self.chans if c.count > 0]

        def mk(ename):
            ops = self.ops[ename]

            def body(eng):
                waited = {}
                if ename == "pool":
                    _FILL.clear()
                for op in ops:
                    need = []
                    for d in op.deps:
                        if d.chan is not None:
                            sem, val = d.chan.sem, d.chanval
                        elif d.eng != ename or SAME_ENGINE_SYNC:
                            sem, val = self.engsem[d.eng], d.sigval
                        else:
                            continue
                        k = id(sem)
                        if waited.get(k, 0) >= val:
                            continue
                        need.append((sem, val))
                        waited[k] = val
                    for sem, val in need[:-1]:
                        eng.wait_ge(sem, val)
                    ins = op.fn(eng)
                    if need:
                        ins._wait_ge(need[-1][0], need[-1][1])
                    if op.chan is not None:
                        ins.then_inc(op.chan.sem, 16)
                    elif op.sig:
                        ins.then_inc(self.engsem[ename], 1)
                if ename == "sp":
                    for sem, val in final:
                        eng.wait_ge(sem, val)
                if ename == "pool":
                    for r in _FILL.values():
                        eng.free_register(r)
                    _FILL.clear()
            return body

        block.tensor(mk("pe"))
        block.scalar(mk("act"))
        block.vector(mk("dve"))
        block.gpsimd(mk("pool"))
        block.sync(mk("sp"))


def run_phase(nc, build):
    with ExitStack() as es:
        P = Prog(nc, es)
        build(P, es)
        sems = list(P.engsem.values()) + [c.sem for c in P.chans]
        with nc.Block() as b0:
            def clr(e):
                for sm in sems:
                    e.sem_clear(sm)
            b0.sync(clr)
        with nc.Block() as block:
            P.emit(block)


def sb(nc, es, name, shape, dt):
    return es.enter_context(nc.sbuf_tensor(_u(name), shape, dt))


def ps(nc, es, name, shape, dt):
    return es.enter_context(nc.psum_tensor(_u(name), shape, dt))


def load_cast_rows(P, nc, es, dst, src_rows, ncols, chans, stage, key):
    n = len(src_rows)
    for k in range(n):
        s = k % len(stage)
        st = stage[s]
        ch = chans[s]
        src = src_rows[k]
        P.add("sp", lambda e, st=st, src=src: e.dma_start(out=st[:, 0:ncols], in_=src),
              writes=[("stage", s)], chan=ch)
        eng = "dve" if k % 2 == 0 else "pool"
        P.add(eng, lambda e, st=st, k=k: e.tensor_copy(out=dst[:, k, 0:ncols], in_=st[:, 0:ncols]),
              reads=[("stage", s)], writes=[(key, k)])


def ffn_half_phase(nc, T, src_norm, src_res, dst, gain, wg, wu, wd, f0, nfc, tagp):
    def build(P, es):
        FW = nfc * 128
        wg_s = sb(nc, es, "wg_s", [128, 8, FW], BF16)
        wu_s = sb(nc, es, "wu_s", [128, 8, FW], BF16)
        wd_s = sb(nc, es, "wd_s", [128, nfc, D], BF16)
        stage = [sb(nc, es, "stg%d" % i, [128, FW], F32) for i in range(3)]
        stch = [P.chan() for _ in range(3)]
        gB = sb(nc, es, "gB", [128, D], F32)
        ident = sb(nc, es, "ident", [128, 128], BF16)
        identf = sb(nc, es, "identf", [128, 128], F32)
        epst = sb(nc, es, "epst", [128, 1], F32)
        xn = [sb(nc, es, "xn%d" % i, [128, D], F32) for i in range(2)]
        xr = [sb(nc, es, "xr%d" % i, [128, D], F32) for i in range(2)]
        nb = [sb(nc, es, "nb%d" % i, [128, D], BF16) for i in range(4)]
        junk = sb(nc, es, "junk", [128, D], BF16)
        ssq = [sb(nc, es, "ssq%d" % i, [128, 4], F32) for i in range(2)]
        rs = [sb(nc, es, "rs%d" % i, [128, 4], F32) for i in range(2)]
        nT = [sb(nc, es, "nT%d" % i, [128, 8, 512], BF16) for i in range(2)]
        hT = sb(nc, es, "hT", [128, nfc, 512], BF16)
        sg = [sb(nc, es, "sg%d" % i, [128, 512], F32) for i in range(2)]
        psT = [ps(nc, es, "psT%d" % i, [128, D], BF16) for i in range(2)]
        psg = [ps(nc, es, "psg%d" % i, [128, 512], F32) for i in range(2)]
        psu = [ps(nc, es, "psu%d" % i, [128, 512], F32) for i in range(2)]
        pso = [ps(nc, es, "pso%d" % i, [128, 512], F32) for i in range(2)]
        cx = [P.chan() for _ in range(2)]
        cr = [P.chan() for _ in range(2)]
        co = [P.chan() for _ in range(2)]
        cg = P.chan()

        P.add("sp", lambda e: e.dma_start(out=gB[:, :], in_=gain.partition_broadcast(128)),
              writes=["gB"], chan=cg)
        P.add("pool", lambda e: e.memset(identf[:, :], 0.0), writes=["identf"])
        P.add("pool", lambda e: asel(e, out=identf[:, :], in_=identf[:, :], pattern=[[-1, 128]],
                                                compare_op=ALU.not_equal, fill=1.0, base=0,
                                                channel_multiplier=1),
              reads=["identf"], writes=["identf"])
        P.add("pool", lambda e: e.tensor_copy(out=ident[:, :], in_=identf[:, :]), reads=["identf"], writes=["ident"])
        P.add("pool", lambda e: e.memset(epst[:, :], EPS), writes=["eps"])

        wgv = wg.rearrange("(c p) f -> p c f", p=128)
        wuv = wu.rearrange("(c p) f -> p c f", p=128)
        load_cast_rows(P, nc, es, wg_s, [wgv[:, c, f0 * 128:f0 * 128 + FW] for c in range(8)], FW, stch, stage, "wg")
        load_cast_rows(P, nc, es, wu_s, [wuv[:, c, f0 * 128:f0 * 128 + FW] for c in range(8)], FW, stch, stage, "wu")
        load_cast_rows(P, nc, es, wd_s, [wd[(f0 + c) * 128:(f0 + c + 1) * 128, :] for c in range(nfc)], D, stch, stage, "wd")
        wkeys = [("wg", c) for c in range(8)] + [("wu", c) for c in range(8)]
        wdkeys = [("wd", c) for c in range(nfc)]

        def prep_group(g):
            sl = g % 2
            for k in range(4):
                t = 4 * g + k
                xs = t % 2
                P.add("sp", lambda e, xs=xs, t=t: e.dma_start(out=xn[xs][:, :], in_=src_norm[t * 128:(t + 1) * 128, :]),
                      writes=[("xn", xs)], chan=cx[xs])
                P.add("act", lambda e, xs=xs, sl=sl, k=k: e.activation(
                    out=junk[:, :], in_=xn[xs][:, :], func=AF.Square, accum_out=ssq[sl][:, k:k + 1]),
                    reads=[("xn", xs)], writes=["junk", ("ssq", sl, k)])
                P.add("act", lambda e, sl=sl, k=k: e.activation(out=rs[sl][:, k:k + 1], in_=ssq[sl][:, k:k + 1],
                                                                func=AF.Sqrt, scale=1.0 / D, bias=epst[:, 0:1]),
                      reads=[("ssq", sl, k), "eps"], writes=[("rs", sl, k)])
                P.add("dve", lambda e, sl=sl, k=k: e.reciprocal(out=rs[sl][:, k:k + 1], in_=rs[sl][:, k:k + 1]),
                      reads=[("rs", sl, k)], writes=[("rs", sl, k)])
                P.add("dve", lambda e, xs=xs, sl=sl, k=k: e.scalar_tensor_tensor(
                    out=nb[k][:, :], in0=xn[xs][:, :], scalar=rs[sl][:, k:k + 1], in1=gB[:, :],
                    op0=ALU.mult, op1=ALU.mult),
                    reads=[("xn", xs), ("rs", sl, k), "gB"], writes=[("nb", k)])

        def transposes(g):
            sl = g % 2
            for k in range(4):
                pb = k % 2
                for c in range(8):
                    P.add("pe", lambda e, pb=pb, k=k, c=c: e.transpose(
                        out=psT[pb][:, c * 128:(c + 1) * 128], in_=nb[k][:, c * 128:(c + 1) * 128], identity=ident[:, :]),
                        reads=[("nb", k), "ident"], writes=[("psT", pb)] if c == 0 else [])
                P.last_w[("psT", pb)] = P.ops["pe"][-1]
                P.add("act", lambda e, pb=pb, sl=sl, k=k: e.copy(
                    out=nT[sl][:, :, k * 128:(k + 1) * 128],
                    in_=psT[pb][:, :].rearrange("p (c t) -> p c t", c=8)),
                    reads=[("psT", pb)], writes=[("nT", sl, k)])

        def upgate(g):
            sl = g % 2
            for fc in range(nfc):
                b = fc % 2
                for (wt, pst, nm) in ((wg_s, psg, "psg"), (wu_s, psu, "psu")):
                    for c in range(8):
                        P.add("pe", lambda e, wt=wt, pst=pst, b=b, c=c, fc=fc, sl=sl: e.matmul(
                            out=pst[b][:, :], lhsT=wt[:, c, fc * 128:(fc + 1) * 128], rhs=nT[sl][:, c, :],
                            start=(c == 0), stop=(c == 7)),
                            reads=[("nT", sl, 0), ("nT", sl, 1), ("nT", sl, 2), ("nT", sl, 3)] + (wkeys if g == 0 else []),
                            writes=[(nm, b)] if c == 0 else [])
                    P.last_w[(nm, b)] = P.ops["pe"][-1]
                P.add("act", lambda e, b=b: e.activation(out=sg[b][:, :], in_=psg[b][:, :], func=AF.Silu),
                      reads=[("psg", b)], writes=[("sg", b)])
                P.add("dve", lambda e, b=b, fc=fc: e.tensor_tensor(out=hT[:, fc, :], in0=psu[b][:, :], in1=sg[b][:, :],
                                                                  op=ALU.mult),
                      reads=[("psu", b), ("sg", b)], writes=[("hT", fc)])

        def down(g):
            for k in range(4):
                t = 4 * g + k
                rsl = t % 2
                P.add("sp", lambda e, rsl=rsl, t=t: e.dma_start(out=xr[rsl][:, :], in_=src_res[t * 128:(t + 1) * 128, :]),
                      reads=[("dram", t)], writes=[("xr", rsl)], chan=cr[rsl])
                for half in range(2):
                    b = half
                    for fc in range(nfc):
                        P.add("pe", lambda e, b=b, fc=fc, k=k, half=half: e.matmul(
                            out=pso[b][:, :], lhsT=hT[:, fc, k * 128:(k + 1) * 128],
                            rhs=wd_s[:, fc, half * 512:(half + 1) * 512], start=(fc == 0), stop=(fc == nfc - 1)),
                            reads=[("hT", fc)] + (wdkeys if g == 0 else []),
                            writes=[("pso", b)] if fc == 0 else [])
                    P.last_w[("pso", b)] = P.ops["pe"][-1]
                    P.add("dve", lambda e, b=b, rsl=rsl, half=half: e.scalar_tensor_tensor(
                        out=xr[rsl][:, half * 512:(half + 1) * 512], in0=pso[b][:, :], scalar=0.5,
                        in1=xr[rsl][:, half * 512:(half + 1) * 512], op0=ALU.mult, op1=ALU.add),
                        reads=[("pso", b), ("xr", rsl)], writes=[("xr", rsl)])
                P.add("sp", lambda e, rsl=rsl, t=t: e.dma_start(out=dst[t * 128:(t + 1) * 128, :], in_=xr[rsl][:, :]),
                      reads=[("xr", rsl)], writes=[("dram", t)], chan=co[rsl])

        prep_group(0)
        transposes(0)
        for g in range(NG):
            if g + 1 < NG:
                prep_group(g + 1)
            upgate(g)
            if g + 1 < NG:
                transposes(g + 1)
            down(g)

    run_phase(nc, build)


def proj_phase(nc, T):
    def build(P, es):
        h1 = T["h1"]
        WIN = 2848
        win_s = sb(nc, es, "win_s", [128, 8, WIN], BF16)
        HW_ = WIN // 2
        stage = [sb(nc, es, "pstg%d" % i, [128, HW_], F32) for i in range(3)]
        stch = [P.chan() for _ in range(3)]
        gB = sb(nc, es, "gB", [128, D], F32)
        ident = sb(nc, es, "ident", [128, 128], BF16)
        identf = sb(nc, es, "identf", [128, 128], F32)
        epst = sb(nc, es, "epst", [128, 1], F32)
        g5 = sb(nc, es, "g5", [128, 5, 64], F32)
        GQ = sb(nc, es, "GQ", [128, 28, 64], F32)
        bfg = sb(nc, es, "bfg", [128, 8], F32)
        cosT = sb(nc, es, "cosT", [128, NT, 8], F32)
        sinT = sb(nc, es, "sinT", [128, NT, 8], F32)
        Gall = sb(nc, es, "Gall", [128, NT, 24], F32)
        LFall = sb(nc, es, "LFall", [128, NT, 8], F32)
        xn = [sb(nc, es, "xn%d" % i, [128, D], F32) for i in range(2)]
        nb = [sb(nc, es, "nb%d" % i, [128, D], BF16) for i in range(2)]
        junk = sb(nc, es, "junk", [128, D], BF16)
        ssq = sb(nc, es, "ssq", [128, 2], F32)
        rs = sb(nc, es, "rs", [128, 2], F32)
        aT = [sb(nc, es, "aT%d" % i, [128, 8, 128], BF16) for i in range(2)]
        qk = [sb(nc, es, "qk%d" % i, [128, 32, 64], F32) for i in range(2)]
        sq = sb(nc, es, "sq", [128, 32, 64], F32)
        hs = [sb(nc, es, "hs%d" % i, [128, 32], F32) for i in range(2)]
        rt = [sb(nc, es, "rt%d" % i, [128, 14, 8], F32) for i in range(4)]
        qkb = [sb(nc, es, "qkb%d" % i, [128, 2048], BF16) for i in range(2)]
        qkT = sb(nc, es, "qkT", [128, 16, 512], BF16)
        vst = [sb(nc, es, "vst%d" % i, [128, 12, 4, 72], BF16) for i in range(2)]
        psT = [ps(nc, es, "psT%d" % i, [128, D], BF16) for i in range(2)]
        pq = [ps(nc, es, "pq%d" % i, [128, 512], F32) for i in range(4)]
        psQ = [ps(nc, es, "psQ%d" % i, [128, 8, 128], BF16) for i in range(2)]
        cx = [P.chan() for _ in range(2)]
        cg = P.chan()
        cq = P.chan()
        cv = [P.chan() for _ in range(2)]
        cf = P.chan()

        P.add("sp", lambda e: e.dma_start(out=gB[:, :], in_=T["mix_norm"].partition_broadcast(128)), writes=["gB"], chan=cg)
        for i, nm in enumerate(("q_norm_nsa", "k_norm_slc", "k_norm_win", "q_norm_fox", "k_norm_fox")):
            P.add("sp", lambda e, i=i, nm=nm: e.dma_start(out=g5[:, i, :], in_=T[nm].partition_broadcast(128)),
                  writes=[("g5", i)], chan=cg)
        P.add("sp", lambda e: e.dma_start(out=bfg[:, :], in_=T["b_forget"].partition_broadcast(128)), writes=["bfg"], chan=cg)
        P.add("sp", lambda e: e.dma_start(out=cosT[:, :, :], in_=T["rope_cos"].rearrange("(t p) c -> p t c", p=128)),
              writes=["cosT"], chan=cg)
        P.add("sp", lambda e: e.dma_start(out=sinT[:, :, :], in_=T["rope_sin"].rearrange("(t p) c -> p t c", p=128)),
              writes=["sinT"], chan=cg)
        cg.seal()
        for (i, h0, nh) in ((0, 0, 8), (1, 8, 2), (2, 10, 2), (3, 12, 8), (4, 20, 8)):
            P.add("dve", lambda e, i=i, h0=h0, nh=nh: e.tensor_copy(
                out=GQ[:, h0:h0 + nh, :], in_=g5[:, i, :].unsqueeze(1).to_broadcast([128, nh, 64])),
                reads=[("g5", i)], writes=[("GQ", i)])
        gqk = [("GQ", i) for i in range(5)]
        P.add("pool", lambda e: e.memset(identf[:, :], 0.0), writes=["identf"])
        P.add("pool", lambda e: asel(e, out=identf[:, :], in_=identf[:, :], pattern=[[-1, 128]],
                                                compare_op=ALU.not_equal, fill=1.0, base=0, channel_multiplier=1),
              reads=["identf"], writes=["identf"])
        P.add("pool", lambda e: e.tensor_copy(out=ident[:, :], in_=identf[:, :]), reads=["identf"], writes=["ident"])
        P.add("pool", lambda e: e.memset(epst[:, :], EPS), writes=["eps"])
        wv = T["w_in"].rearrange("(c p) f -> p c f", p=128)
        for hf in range(2):
            for c in range(8):
                k = hf * 8 + c
                s = k % 3
                P.add("sp", lambda e, s=s, c=c, hf=hf: e.dma_start(out=stage[s][:, :], in_=wv[:, c, hf * HW_:(hf + 1) * HW_]),
                      writes=[("stage", s)], chan=stch[s])
                P.add("dve" if k % 2 == 0 else "pool", lambda e, s=s, c=c, hf=hf: e.tensor_copy(
                    out=win_s[:, c, hf * HW_:(hf + 1) * HW_], in_=stage[s][:, :]),
                    reads=[("stage", s)], writes=[("win", c, hf)])
        wkeys = [("win", c, hf) for c in range(8) for hf in range(2)]
        QKTv = T["QKT"].rearrange("(pr two) d s -> (two d) pr s", two=2)
        for i in range(2):
            P.add("pool", lambda e, i=i: e.memset(vst[i][:, :, :, 64:72], 1.0), writes=[("vst1", i)])
        CH = [(0, 512), (512, 512), (1024, 512), (1536, 512), (2048, 512), (2560, 288)]

        for t in range(NT):
            xs = t % 2
            g, k = t // 4, t % 4
            P.add("sp", lambda e, xs=xs, t=t: e.dma_start(out=xn[xs][:, :], in_=h1[t * 128:(t + 1) * 128, :]),
                  writes=[("xn", xs)], chan=cx[xs])
            P.add("act", lambda e, xs=xs: e.activation(out=junk[:, :], in_=xn[xs][:, :], func=AF.Square,
                                                       accum_out=ssq[:, xs:xs + 1]),
                  reads=[("xn", xs)], writes=["junk", ("ssq", xs)])
            P.add("act", lambda e, xs=xs: e.activation(out=rs[:, xs:xs + 1], in_=ssq[:, xs:xs + 1], func=AF.Sqrt,
                                                       scale=1.0 / D, bias=epst[:, 0:1]),
                  reads=[("ssq", xs), "eps"], writes=[("rs", xs)])
            P.add("dve", lambda e, xs=xs: e.reciprocal(out=rs[:, xs:xs + 1], in_=rs[:, xs:xs + 1]),
                  reads=[("rs", xs)], writes=[("rs", xs)])
            P.add("dve", lambda e, xs=xs: e.scalar_tensor_tensor(
                out=nb[xs][:, :], in0=xn[xs][:, :], scalar=rs[:, xs:xs + 1], in1=gB[:, :], op0=ALU.mult, op1=ALU.mult),
                reads=[("xn", xs), ("rs", xs), "gB"], writes=[("nb", xs)])
            for c in range(8):
                P.add("pe", lambda e, xs=xs, c=c: e.transpose(
                    out=psT[xs][:, c * 128:(c + 1) * 128], in_=nb[xs][:, c * 128:(c + 1) * 128], identity=ident[:, :]),
                    reads=[("nb", xs), "ident"], writes=[("psT", xs)] if c == 0 else [])
            P.last_w[("psT", xs)] = P.ops["pe"][-1]
            P.add("act", lambda e, xs=xs: e.copy(out=aT[xs][:, :, :], in_=psT[xs][:, :].rearrange("p (c t) -> p c t", c=8)),
                  reads=[("psT", xs)], writes=[("aT", xs)])
            for ci, (c0, cw) in enumerate(CH):
                pb = (t * 6 + ci) % 4
                for c in range(8):
                    P.add("pe", lambda e, pb=pb, c=c, c0=c0, cw=cw, xs=xs: e.matmul(
                        out=pq[pb][:, 0:cw], lhsT=aT[xs][:, c, :], rhs=win_s[:, c, c0:c0 + cw],
                        start=(c == 0), stop=(c == 7)),
                        reads=[("aT", xs)] + (wkeys if t == 0 else []), writes=[("pq", pb)] if c == 0 else [])
                P.last_w[("pq", pb)] = P.ops["pe"][-1]
                if ci < 4:
                    P.add("act", lambda e, pb=pb, ci=ci, xs=xs: e.copy(
                        out=qk[xs][:, ci * 8:(ci + 1) * 8, :], in_=pq[pb][:, :].rearrange("p (h d) -> p h d", h=8)),
                        reads=[("pq", pb)], writes=[("qk", xs, ci)])
                    P.add("act", lambda e, pb=pb, ci=ci: e.activation(
                        out=sq[:, ci * 8:(ci + 1) * 8, :], in_=pq[pb][:, :].rearrange("p (h d) -> p h d", h=8), func=AF.Square),
                        reads=[("pq", pb)], writes=[("sq", ci)])
                elif ci == 4:
                    P.add("dve", lambda e, pb=pb, g=g, k=k: e.tensor_copy(
                        out=vst[g % 2][:, 0:8, k, 0:64], in_=pq[pb][:, 0:512].rearrange("p (h d) -> p h d", h=8)),
                        reads=[("pq", pb), ("vst1", g % 2)], writes=[("vst", g % 2, k, 0)])
                else:
                    P.add("dve", lambda e, pb=pb, g=g, k=k: e.tensor_copy(
                        out=vst[g % 2][:, 8:12, k, 0:64], in_=pq[pb][:, 0:256].rearrange("p (h d) -> p h d", h=4)),
                        reads=[("pq", pb), ("vst1", g % 2)], writes=[("vst", g % 2, k, 1)])
                    P.add("dve", lambda e, pb=pb, t=t: e.tensor_copy(out=Gall[:, t, :], in_=pq[pb][:, 256:280]),
                          reads=[("pq", pb)], writes=[("Gall", t)])
                    P.add("dve", lambda e, pb=pb, t=t: e.tensor_tensor(out=LFall[:, t, :], in0=pq[pb][:, 280:288], in1=bfg[:, :],
                                                                      op=ALU.add),
                          reads=[("pq", pb), "bfg"], writes=[("LFall", t)])
            if k == 3:
                P.add("sp", lambda e, g=g: e.dma_start(
                    out=T["V"].rearrange("h p t c -> p h (t c)")[:, :, 4 * g * 72:(4 * g + 4) * 72],
                    in_=vst[g % 2][:, :, :, :].rearrange("p h t c -> p h (t c)")),
                    reads=[("vst", g % 2, kk, j) for kk in range(4) for j in range(2)], chan=cv[g % 2])
            P.add("dve", lambda e, xs=xs: e.tensor_reduce(out=hs[xs][:, :], in_=sq[:, :, :], axis=AX.X, op=ALU.add),
                  reads=[("sq", i) for i in range(4)], writes=[("hs", xs)])
            P.add("act", lambda e, xs=xs: e.activation(out=hs[xs][:, :], in_=hs[xs][:, :], func=AF.Sqrt,
                                                       scale=1.0 / 64, bias=epst[:, 0:1]),
                  reads=[("hs", xs), "eps"], writes=[("hs", xs)])
            P.add("dve", lambda e, xs=xs: e.reciprocal(out=hs[xs][:, :], in_=hs[xs][:, :]),
                  reads=[("hs", xs)], writes=[("hs", xs)])
            qkk = [("qk", xs, i) for i in range(4)]
            P.add("dve", lambda e, xs=xs: e.tensor_tensor(
                out=qk[xs][:, 2:30, :], in0=qk[xs][:, 2:30, :], in1=hs[xs][:, 2:30].unsqueeze(2).to_broadcast([128, 28, 64]),
                op=ALU.mult), reads=qkk + [("hs", xs)], writes=qkk)
            P.add("dve", lambda e, xs=xs: e.tensor_tensor(
                out=qk[xs][:, 2:30, :], in0=qk[xs][:, 2:30, :], in1=GQ[:, :, :], op=ALU.mult),
                reads=qkk + gqk, writes=qkk)
            cb = lambda tab, t=t: tab[:, t, :].unsqueeze(1).to_broadcast([128, 14, 8])
            x1 = lambda xs=xs: qk[xs][:, 0:14, 0:8]
            x2 = lambda xs=xs: qk[xs][:, 0:14, 8:16]
            for j, (src, tab) in enumerate(((x1, cosT), (x2, sinT), (x2, cosT), (x1, sinT))):
                P.add("pool", lambda e, j=j, src=src, tab=tab, cb=cb: e.tensor_tensor(
                    out=rt[j][:, :, :], in0=src(), in1=cb(tab), op=ALU.mult),
                    reads=qkk + ["cosT", "sinT"], writes=[("rt", j)])
            P.add("pool", lambda e, x1=x1: e.tensor_tensor(out=x1(), in0=rt[0][:, :, :], in1=rt[1][:, :, :], op=ALU.subtract),
                  reads=[("rt", 0), ("rt", 1)], writes=qkk)
            P.add("pool", lambda e, x2=x2: e.tensor_tensor(out=x2(), in0=rt[2][:, :, :], in1=rt[3][:, :, :], op=ALU.add),
                  reads=[("rt", 2), ("rt", 3)], writes=qkk)
            P.add("pool", lambda e, xs=xs: e.tensor_copy(out=qkb[xs][:, :], in_=qk[xs][:, :, :].rearrange("p h d -> p (h d)")),
                  reads=qkk, writes=[("qkb", xs)])
            for pr in range(16):
                hb = pr // 8
                P.add("pe", lambda e, pr=pr, hb=hb, xs=xs: e.transpose(
                    out=psQ[hb][:, pr % 8, :], in_=qkb[xs][:, pr * 128:(pr + 1) * 128], identity=ident[:, :]),
                    reads=[("qkb", xs), "ident"], writes=[("psQ", hb)] if pr % 8 == 0 else [])
                if pr % 8 == 7:
                    P.last_w[("psQ", hb)] = P.ops["pe"][-1]
                    P.add("act" if hb == 0 else "dve", (lambda e, hb=hb, k=k: e.copy(
                        out=qkT[:, hb * 8:(hb + 1) * 8, k * 128:(k + 1) * 128], in_=psQ[hb][:, :, :])) if hb == 0 else
                        (lambda e, hb=hb, k=k: e.tensor_copy(
                            out=qkT[:, hb * 8:(hb + 1) * 8, k * 128:(k + 1) * 128], in_=psQ[hb][:, :, :])),
                        reads=[("psQ", hb)], writes=[("qkT", k, hb)])
            if k == 3:
                P.add("sp", lambda e, g=g: e.dma_start(out=QKTv[:, :, g * 512:(g + 1) * 512], in_=qkT[:, :, :]),
                      reads=[("qkT", kk, hb) for kk in range(4) for hb in range(2)], chan=cq)
        P.add("act", lambda e: e.activation(out=Gall[:, :, :], in_=Gall[:, :, :], func=AF.Sigmoid),
              reads=[("Gall", t) for t in range(NT)], writes=["GallF"])
        P.add("sp", lambda e: e.dma_start(out=T["G"], in_=Gall[:, :, :].rearrange("p t c -> p (t c)")),
              reads=["GallF"], chan=cf)
        P.add("act", lambda e: e.activation(out=LFall[:, :, :], in_=LFall[:, :, :], func=AF.Exp, scale=-1.0),
              reads=[("LFall", t) for t in range(NT)], writes=["LF1"])
        P.add("act", lambda e: e.activation(out=LFall[:, :, :], in_=LFall[:, :, :], func=AF.Ln, bias=1.0),
              reads=["LF1"], writes=["LF2"])
        P.add("dve", lambda e: e.tensor_scalar(out=LFall[:, :, :], in0=LFall[:, :, :], scalar1=-1.0, scalar2=None, op0=ALU.mult),
              reads=["LF2"], writes=["LF3"])
        P.add("sp", lambda e: e.dma_start(out=T["LF"], in_=LFall[:, :, :].rearrange("p t c -> p (t c)")),
              reads=["LF3"], chan=cf)

    run_phase(nc, build)


_FILL = {}


def asel(e, **kw):
    v = float(kw.pop("fill"))
    r = _FILL.get(v)
    if r is None:
        r = e.alloc_register()
        e.reg_mov(r, v)
        _FILL[v] = r
    return e.affine_select(fill=r, **kw)


def make_ident(P, nc, es):
    ident = sb(nc, es, "ident", [128, 128], BF16)
    identf = sb(nc, es, "identf", [128, 128], F32)
    P.add("pool", lambda e: e.memset(identf[:, :], 0.0), writes=["identf"])
    P.add("pool", lambda e: asel(e, out=identf[:, :], in_=identf[:, :], pattern=[[-1, 128]],
                                            compare_op=ALU.not_equal, fill=1.0, base=0, channel_multiplier=1),
          reads=["identf"], writes=["identf"])
    P.add("pool", lambda e: e.tensor_copy(out=ident[:, :], in_=identf[:, :]), reads=["identf"], writes=["ident"])
    return ident, identf


def cmp_phase(nc, T):
    def build(P, es):
        ident, identf = make_ident(P, nc, es)
        epst = sb(nc, es, "epst", [128, 1], F32)
        P.add("pool", lambda e: e.memset(epst[:, :], EPS), writes=["eps"])
        tok = sb(nc, es, "tok", [64, 4, S], BF16)
        w1s = [sb(nc, es, "w1s%d" % i, [64, 32, 256], BF16) for i in range(2)]
        stg = [sb(nc, es, "cstg%d" % i, [64, 32, 256], F32) for i in range(2)]
        w1f = [sb(nc, es, "w1f%d" % i, [128, 16, 256], F32) for i in range(2)]
        posr = sb(nc, es, "posr", [16, 2, 128], F32)
        posc = sb(nc, es, "posc", [128, 2, 16], F32)
        w2f = sb(nc, es, "w2f", [128, 2, 2, 64], F32)
        w2s = sb(nc, es, "w2s", [128, 2, 2, 64], BF16)
        biasT = sb(nc, es, "biasT", [128, 4], F32)
        gk = sb(nc, es, "gk", [128, 64], F32)
        hidT = [sb(nc, es, "hidT%d" % i, [128, 2, 256], BF16) for i in range(2)]
        ssq = sb(nc, es, "ssq", [128, 4], F32)
        junk = sb(nc, es, "junk", [128, 64], F32)
        kcb = [sb(nc, es, "kcb%d" % i, [128, 64], BF16) for i in range(2)]
        kcT = [sb(nc, es, "kcT%d" % i, [64, 256], BF16) for i in range(2)]
        vce = [sb(nc, es, "vce%d" % i, [128, 2, 65], BF16) for i in range(2)]
        psHf = [ps(nc, es, "psH%d" % i, [128, 512], F32) for i in range(2)]
        psH = [t[:, 0:256] for t in psHf]
        psOf = [ps(nc, es, "psO%d" % i, [128, 512], F32) for i in range(2)]
        psO = [t[:, 0:64] for t in psOf]
        psBf = ps(nc, es, "psB", [128, 512], F32)
        psB = psBf[:, 0:4]
        psPf = ps(nc, es, "psP", [128, 512], F32)
        psP = psPf[:, 0:32].rearrange("p (a b) -> p a b", a=2)
        psKf = ps(nc, es, "psK", [128, 1024], BF16)
        psK = psKf[0:64, 0:128]
        c0 = P.chan()
        c1 = [P.chan() for _ in range(2)]
        co = P.chan()

        for j, h in enumerate((0, 1, 30, 31)):
            P.add("sp", lambda e, j=j, h=h: e.dma_start(out=tok[:, j, :], in_=T["QKT"][h, :, :]), writes=[("tok", j)], chan=c0)
        P.add("sp", lambda e: e.dma_start(out=gk[:, :], in_=T["k_norm_cmp"].partition_broadcast(128)), writes=["gk"], chan=c0)
        for kv, nm in enumerate(("cmp_pos_k", "cmp_pos_v")):
            P.add("sp", lambda e, kv=kv, nm=nm: e.dma_start(
                out=posr[:, kv, :], in_=T[nm].rearrange("(c a) d -> c (a d)", a=2)), writes=[("posr", kv)], chan=c0)
        for kv, nm in enumerate(("cmp_k_w2", "cmp_v_w2")):
            P.add("sp", lambda e, kv=kv, nm=nm: e.dma_start(
                out=w2f[:, kv, :, :], in_=T[nm].rearrange("(c p) d -> p c d", p=128)), writes=[("w2f", kv)], chan=c0)
        for kv, nm in enumerate(("cmp_k_w1", "cmp_v_w1")):
            P.add("sp", lambda e, kv=kv, nm=nm: e.dma_start(
                out=w1f[kv][:, :, :], in_=T[nm].rearrange("(c p) h -> p c h", p=128)), writes=[("w1f", kv)], chan=c0)
        c0.seal()
        for kv, nm in enumerate(("cmp_k_w1", "cmp_v_w1")):
            P.add("sp", lambda e, kv=kv, nm=nm: e.dma_start(
                out=stg[kv][:, :, :], in_=T[nm].rearrange("(l d) h -> d l h", d=64)), writes=[("stg", kv)], chan=c1[kv])
            P.add("dve" if kv == 0 else "pool", lambda e, kv=kv: e.tensor_copy(out=w1s[kv][:, :, :], in_=stg[kv][:, :, :]),
                  reads=[("stg", kv)], writes=[("w1s", kv)])
        P.add("dve", lambda e: e.tensor_copy(out=w2s[:, :, :, :], in_=w2f[:, :, :, :]),
              reads=[("w2f", 0), ("w2f", 1)], writes=["w2s"])
        for kv in range(2):
            P.add("pe", lambda e, kv=kv: e.transpose(out=psP[:, kv, :], in_=posr[:, kv, :], identity=identf[0:16, 0:16]),
                  reads=[("posr", kv), "identf"], writes=[("psP", kv)])
        P.add("dve", lambda e: e.tensor_copy(out=posc[:, :, :], in_=psP),
              reads=[("psP", 0), ("psP", 1)], writes=["posc"])
        for kv in range(2):
            for hc in range(2):
                for c in range(16):
                    P.add("pe", lambda e, kv=kv, hc=hc, c=c: e.matmul(
                        out=psB[:, kv * 2 + hc:kv * 2 + hc + 1], lhsT=w1f[kv][:, c, hc * 128:(hc + 1) * 128],
                        rhs=posc[:, kv, c:c + 1], start=(c == 0), stop=(c == 15)),
                        reads=[("w1f", kv), "posc"], writes=["psB"] if (c == 0 and kv == 0 and hc == 0) else [])
        P.last_w["psB"] = P.ops["pe"][-1]
        P.add("dve", lambda e: e.tensor_copy(out=biasT[:, :], in_=psB), reads=["psB"], writes=["biasT"])
        for i in range(2):
            P.add("pool", lambda e, i=i: e.memset(kcb[i][:, :], 0.0), writes=[("kcb", i)])
            P.add("pool", lambda e, i=i: e.memset(vce[i][:, :, :], 0.0), writes=[("vce", i)])
            P.add("pool", lambda e, i=i: e.memset(vce[i][:, :, 64:65], 1.0), reads=[("vce", i)], writes=[("vce", i)])
            P.add("pool", lambda e, i=i: e.memset(hidT[i][:, :, :], 0.0), writes=[("hidT", i, 0), ("hidT", i, 1)])
        VCv = T["VC"].rearrange("h (c p) e -> h p c e", p=128)
        it = 0
        for kv in range(2):
            for head in range(2):
                sl = it % 2
                it += 1
                tv = tok[:, kv * 2 + head, :].rearrange("p (n r) -> p n r", r=16)
                for hc in range(2):
                    for l in range(32):
                        q, r = l // 16, l % 16
                        P.add("pe", lambda e, kv=kv, hc=hc, l=l, q=q, r=r, tv=tv: e.matmul(
                            out=psH[hc][:, 0:255], lhsT=w1s[kv][:, l, hc * 128:(hc + 1) * 128], rhs=tv[:, q:q + 255, r],
                            start=(l == 0), stop=(l == 31)),
                            reads=[("tok", kv * 2 + head), ("w1s", kv)], writes=[("psH", hc)] if l == 0 else [])
                    P.last_w[("psH", hc)] = P.ops["pe"][-1]
                    P.add("act", lambda e, kv=kv, hc=hc, sl=sl: e.activation(
                        out=hidT[sl][:, hc, 0:255], in_=psH[hc][:, 0:255], func=AF.Silu,
                        bias=biasT[:, kv * 2 + hc:kv * 2 + hc + 1]),
                        reads=[("psH", hc), "biasT"], writes=[("hidT", sl, hc)])
                for ci, (n0, nn) in enumerate(((0, 128), (128, 127))):
                    for hc in range(2):
                        P.add("pe", lambda e, kv=kv, hc=hc, sl=sl, ci=ci, n0=n0, nn=nn: e.matmul(
                            out=psO[ci][0:nn, :], lhsT=hidT[sl][:, hc, n0:n0 + nn], rhs=w2s[:, kv, hc, :],
                            start=(hc == 0), stop=(hc == 1)),
                            reads=[("hidT", sl, 0), ("hidT", sl, 1), "w2s"], writes=[("psO", ci)] if hc == 0 else [])
                    P.last_w[("psO", ci)] = P.ops["pe"][-1]
                    if kv == 0:
                        col = head * 2 + ci
                        P.add("act", lambda e, ci=ci, nn=nn, col=col: e.activation(
                            out=junk[0:nn, :], in_=psO[ci][0:nn, :], func=AF.Square, accum_out=ssq[0:nn, col:col + 1]),
                            reads=[("psO", ci)], writes=["junk", ("ssq", col)])
                        P.add("act", lambda e, nn=nn, col=col: e.activation(
                            out=ssq[0:nn, col:col + 1], in_=ssq[0:nn, col:col + 1], func=AF.Sqrt, scale=1.0 / 64,
                            bias=epst[0:nn, 0:1]), reads=[("ssq", col), "eps"], writes=[("ssq", col)])
                        P.add("dve", lambda e, nn=nn, col=col: e.reciprocal(out=ssq[0:nn, col:col + 1], in_=ssq[0:nn, col:col + 1]),
                              reads=[("ssq", col)], writes=[("ssq", col)])
                        P.add("dve", lambda e, ci=ci, nn=nn, col=col: e.scalar_tensor_tensor(
                            out=kcb[ci][0:nn, :], in0=psO[ci][0:nn, :], scalar=ssq[0:nn, col:col + 1], in1=gk[0:nn, :],
                            op0=ALU.mult, op1=ALU.mult), reads=[("psO", ci), ("ssq", col), "gk"], writes=[("kcb", ci)])
                        P.add("pe", lambda e, ci=ci: e.transpose(out=psK, in_=kcb[ci][:, :], identity=ident[:, :]),
                              reads=[("kcb", ci), "ident"], writes=["psK"])
                        P.add("act", lambda e, head=head, n0=n0: e.copy(out=kcT[head][:, n0:n0 + 128], in_=psK),
                              reads=["psK"], writes=[("kcT", head, n0)])
                    else:
                        P.add("dve", lambda e, ci=ci, nn=nn, head=head: e.tensor_copy(
                            out=vce[head][0:nn, ci, 0:64], in_=psO[ci][0:nn, :]), reads=[("psO", ci)], writes=[("vce", head)])
                if kv == 0:
                    P.add("sp", lambda e, head=head: e.dma_start(out=T["KCT"][head, :, :], in_=kcT[head][:, :]),
                          reads=[("kcT", head, 0), ("kcT", head, 128)], chan=co)
                else:
                    P.add("sp", lambda e, head=head: e.dma_start(out=VCv[head], in_=vce[head][:, :, :]),
                          reads=[("vce", head)], chan=co)

    run_phase(nc, build)


class UnitPipe:
    def __init__(self, P, psS, PT, depth=2):
        self.P, self.psS, self.PT, self.depth = P, psS, PT, depth
        self.q = []
        self.u = 0

    def push(self, lhsT, rhs, vlhsT, pacc, acc_key, first, last, mask, kdeps, bias=None, bkeys=(), post=None):
        P = self.P
        u = self.u
        self.u += 1
        sb_, pb = u % len(self.psS), u % len(self.PT)
        psS, PT = self.psS[sb_], self.PT[pb]
        P.add("pe", lambda e: e.matmul(out=psS[:, :], lhsT=lhsT, rhs=rhs, start=True, stop=True),
              reads=kdeps, writes=[("psS", sb_)])
        if bias is None:
            P.add("act", lambda e: e.activation(out=PT[:, :], in_=psS[:, :], func=AF.Exp, scale=0.125),
                  reads=[("psS", sb_)], writes=[("PT", pb)])
        else:
            P.add("act", lambda e: e.activation(out=PT[:, :], in_=psS[:, :], func=AF.Exp, scale=0.125, bias=bias),
                  reads=[("psS", sb_)] + list(bkeys), writes=[("PT", pb)])
        if mask is not None:
            base, cm, step = mask
            P.add("pool", lambda e: asel(e, out=PT[:, :], in_=PT[:, :], pattern=[[step, 512]], compare_op=ALU.is_ge,
                                         fill=0.0, base=base, channel_multiplier=cm), reads=[("PT", pb)], writes=[("PT", pb)])
        self.q.append((PT, pb, vlhsT, pacc, acc_key, first, last, kdeps, post))
        if len(self.q) > self.depth:
            self._pv()

    def _pv(self):
        P = self.P
        PT, pb, vlhsT, pacc, acc_key, first, last, kdeps, post = self.q.pop(0)
        P.add("pe", lambda e: e.matmul(out=pacc[0:65, :], lhsT=vlhsT, rhs=PT[:, :], start=first, stop=last),
              reads=[("PT", pb)] + list(kdeps), writes=[acc_key] if first else [])
        if last:
            P.last_w[acc_key] = P.ops["pe"][-1]
            if post is not None:
                post()

    def flush(self):
        while self.q:
            self._pv()


def nsa_phase(nc, T):
    BIG = 2048.0
    TINY = 1e-30

    def build(P, es):
        ident, identf = make_ident(P, nc, es)
        QB = sb(nc, es, "QB", [128, 4, S], BF16)
        KE = sb(nc, es, "KE", [128, S], BF16)
        KW = sb(nc, es, "KW", [128, S], BF16)
        KC = sb(nc, es, "KC", [128, 256], BF16)
        Vs = sb(nc, es, "Vs", [128, NT, 72], BF16)
        Vw = sb(nc, es, "Vw", [128, NT, 72], BF16)
        VCs = sb(nc, es, "VCs", [128, 2, 72], BF16)
        OVf = sb(nc, es, "OVf", [128, 2, 72], F32)
        OV = sb(nc, es, "OV", [128, 2, 72], BF16)
        Gs = sb(nc, es, "Gs", [128, NT, 24], F32)
        ET = [[sb(nc, es, "ET%d_%d" % (i, j), [128, 512], BF16) for j in range(2)] for i in range(2)]
        PT = [sb(nc, es, "PT%d" % i, [128, 512], BF16) for i in range(4)]
        OCs = sb(nc, es, "OCs", [65, 4, 512], F32)
        OWs = sb(nc, es, "OWs", [65, 4, 512], F32)
        OSs = [sb(nc, es, "OSs%d" % i, [65, 512], F32) for i in range(2)]
        imp = sb(nc, es, "imp", [128, 4, 64], F32)
        impt = sb(nc, es, "impt", [128, 4, 64], F32)
        impm = [sb(nc, es, "impm%d" % i, [128, 64], F32) for i in range(2)]
        rd4 = sb(nc, es, "rd4", [128, 4], F32)
        m1 = sb(nc, es, "m1", [128, 8], F32)
        m2 = sb(nc, es, "m2", [128, 8], F32)
        tmp = sb(nc, es, "tmp", [128, 64], F32)
        thr = sb(nc, es, "thr", [128, 1], F32)
        BN = [sb(nc, es, "BN%d" % i, [128, 128], BF16) for i in range(4)]
        dn = [sb(nc, es, "dn%d" % i, [128, 3], F32) for i in range(2)]
        ost = [sb(nc, es, "ost%d" % i, [128, 4, 256], F32) for i in range(2)]
        psS = [ps(nc, es, "psS%d" % i, [128, 512], F32) for i in range(3)]
        psOC = ps(nc, es, "psOC", [128, 512], F32)
        psOS = ps(nc, es, "psOS", [128, 512], F32)
        psOW = ps(nc, es, "psOW", [128, 512], F32)
        psIB = ps(nc, es, "psIB", [128, 512], F32)
        psI = psIB[:, 0:260].rearrange("p (a b) -> p a b", a=4)
        psBT = psIB[:, 320:384].bitcast(BF16)
        psFb = ps(nc, es, "psFb", [128, 512], F32)
        psF = psFb[:, 0:195].rearrange("p (a b) -> p a b", a=3)
        c0 = P.chan()
        cks = [P.chan() for _ in range(2)]
        cst = [P.chan() for _ in range(2)]

        P.add("sp", lambda e: e.dma_start(out=Gs[:, :, :].rearrange("p t c -> p (t c)"), in_=T["G"]), writes=["Gs"], chan=c0)
        P.add("pool", lambda e: e.memset(KE[64:128, :], BIG), writes=["KEm"])
        P.add("pool", lambda e: asel(e, out=KE[64:128, :], in_=KE[64:128, :], pattern=[[1, S]], compare_op=ALU.is_ge,
                                                fill=0.0, base=0, channel_multiplier=-64), reads=["KEm"], writes=["KEm"])
        P.add("pool", lambda e: asel(e, out=KE[64:128, :], in_=KE[64:128, :], pattern=[[-1, S]], compare_op=ALU.is_ge,
                                                fill=0.0, base=63, channel_multiplier=64), reads=["KEm"], writes=["KEm"])
        P.add("pool", lambda e: e.memset(KW[64:128, :], 0.0), writes=["KW0"])
        P.add("pool", lambda e: e.memset(KC[64:128, :], 0.0), writes=["KC0"])
        for g in range(4):
            P.add("pool", lambda e, g=g: e.memset(QB[64:128, g, :], 0.0), writes=[("QB0", g)])
        P.add("pool", lambda e: e.memset(OVf[:, :, :], 1.0), writes=["OVf"])
        for nt in range(2):
            P.add("pool", lambda e, nt=nt: asel(e,
                out=OVf[:, nt, 0:64], in_=OVf[:, nt, 0:64], pattern=[[64, 64]], compare_op=ALU.is_ge, fill=0.0,
                base=63 - 2048 * nt, channel_multiplier=-16), reads=["OVf"], writes=["OVf"])
            P.add("pool", lambda e, nt=nt: asel(e,
                out=OVf[:, nt, 0:64], in_=OVf[:, nt, 0:64], pattern=[[-64, 64]], compare_op=ALU.is_ge, fill=0.0,
                base=2048 * nt + 31, channel_multiplier=16), reads=["OVf"], writes=["OVf"])
        P.add("pool", lambda e: e.tensor_copy(out=OV[:, :, :], in_=OVf[:, :, :]), reads=["OVf"], writes=["OV"])
        for i in range(4):
            P.add("pool", lambda e, i=i: e.memset(BN[i][:, 0:64], 0.0), writes=[("BN0", i)])
        OAv = T["OA"].rearrange("(t p) c -> p t c", p=128)

        def mask_ge(tile, base, cm, step):
            return lambda e: asel(e, out=tile[:, :], in_=tile[:, :], pattern=[[step, 512]], compare_op=ALU.is_ge,
                                             fill=0.0, base=base, channel_multiplier=cm)

        pipe = UnitPipe(P, psS, PT, depth=2)

        for kh in range(DBG['kh']):
            ck = cks[kh]
            for g in range(4):
                P.add("sp", lambda e, g=g, kh=kh: e.dma_start(out=QB[0:64, g, :], in_=T["QKT"][2 + 4 * kh + g, :, :]),
                      writes=[("QBq", g)], chan=ck)
            P.add("sp", lambda e, kh=kh: e.dma_start(out=KE[0:64, :], in_=T["QKT"][10 + kh, :, :]), writes=["KEk"], chan=ck)
            P.add("sp", lambda e, kh=kh: e.dma_start(out=KW[0:64, :], in_=T["QKT"][12 + kh, :, :]), writes=["KW"], chan=ck)
            P.add("sp", lambda e, kh=kh: e.dma_start(out=KC[0:64, :], in_=T["KCT"][kh, :, :]), writes=["KC"], chan=ck)
            P.add("sp", lambda e, kh=kh: e.dma_start(out=Vs[:, :, :], in_=T["V"][kh]), writes=["Vs"], chan=ck)
            P.add("sp", lambda e, kh=kh: e.dma_start(out=Vw[:, :, :], in_=T["V"][2 + kh]), writes=["Vw"], chan=ck)
            P.add("sp", lambda e, kh=kh: e.dma_start(out=VCs[:, :, 0:65], in_=T["VC"][kh].rearrange("(c p) e -> p c e", p=128)),
                  writes=["VCs"], chan=ck)
            ck.seal()
            for i in range(DBG['ng']):
                qsl = slice(i * 512, (i + 1) * 512)
                nts = [0] if i < 4 else [0, 1]

                def s1(g):
                    for nt in nts:
                        u = pipe.u
                        pipe.u += 1
                        sb_ = u % 3
                        et = ET[g % 2][nt]
                        P.add("pe", lambda e, nt=nt, g=g, sb_=sb_, qsl=qsl: e.matmul(
                            out=psS[sb_][:, :], lhsT=KC[:, nt * 128:(nt + 1) * 128], rhs=QB[:, g, qsl], start=True, stop=True),
                            reads=["KC", "KC0", ("QBq", g), ("QB0", g)] + ([("QBm", tt) for tt in range(4)] if i > 0 or kh > 0 else []), writes=[("psS", sb_)])
                        P.add("act", lambda e, et=et, sb_=sb_: e.activation(out=et[:, :], in_=psS[sb_][:, :], func=AF.Exp, scale=0.125),
                              reads=[("psS", sb_)], writes=[("ET", g % 2, nt)])
                        P.add("pool", mask_ge(et, 512 * i - 2048 * nt - 31, -16, 1), reads=[("ET", g % 2, nt)], writes=[("ET", g % 2, nt)])

                def s2(g):
                    for j, nt in enumerate(nts):
                        P.add("pe", lambda e, nt=nt, j=j, g=g: e.matmul(
                            out=psOC[0:65, :], lhsT=VCs[:, nt, 0:65], rhs=ET[g % 2][nt][:, :], start=(j == 0), stop=(j == len(nts) - 1)),
                            reads=[("ET", g % 2, nt), "VCs"], writes=["psOC"] if j == 0 else [])
                    P.last_w["psOC"] = P.ops["pe"][-1]
                    P.add("act", lambda e, g=g: e.copy(out=OCs[:, g, :], in_=psOC[0:65, :]), reads=["psOC"], writes=[("OCs", g)])
                    for tt in range(4):
                        for j, nt in enumerate(nts):
                            P.add("pe", lambda e, nt=nt, j=j, tt=tt, g=g: e.matmul(
                                out=psI[:, tt, :], lhsT=ET[g % 2][nt][:, tt * 128:(tt + 1) * 128], rhs=OV[:, nt, 0:65],
                                start=(j == 0), stop=(j == len(nts) - 1)),
                                reads=[("ET", g % 2, nt), "OV"], writes=["psI"] if (j == 0 and tt == 0) else [])
                    P.last_w["psI"] = P.ops["pe"][-1]
                    P.add("dve", lambda e: e.tensor_scalar(out=rd4[:, :], in0=psI[:, :, 64], scalar1=TINY, scalar2=None, op0=ALU.max),
                          reads=["psI"], writes=["rd4"])
                    P.add("dve", lambda e: e.reciprocal(out=rd4[:, :], in_=rd4[:, :]), reads=["rd4"], writes=["rd4"])
                    if g == 0:
                        P.add("dve", lambda e: e.tensor_tensor(
                            out=imp[:, :, :], in0=psI[:, :, 0:64], in1=rd4[:, :].unsqueeze(2).to_broadcast([128, 4, 64]), op=ALU.mult),
                            reads=["psI", "rd4"], writes=["imp"])
                    else:
                        P.add("dve", lambda e: e.tensor_tensor(
                            out=impt[:, :, :], in0=psI[:, :, 0:64], in1=rd4[:, :].unsqueeze(2).to_broadcast([128, 4, 64]), op=ALU.mult),
                            reads=["psI", "rd4"], writes=["impt"])
                        P.add("dve", lambda e: e.tensor_tensor(out=imp[:, :, :], in0=imp[:, :, :], in1=impt[:, :, :], op=ALU.add),
                              reads=["imp", "impt"], writes=["imp"])

                s1(0)
                for g in range(4):
                    if g < 3:
                        s1(g + 1)
                    s2(g)
                for tt in range(4):
                    bs = tt % 2
                    t0 = 512 * i + 128 * tt
                    P.add("pool", lambda e, tt=tt, bs=bs, t0=t0: asel(e,
                        out=impm[bs][:, :], in_=imp[:, tt, :], pattern=[[-64, 64]], compare_op=ALU.is_ge, fill=1.0e6,
                        base=t0 - 128, channel_multiplier=1), reads=["imp"], writes=[("impm", bs)])
                    P.add("pool", lambda e, bs=bs, t0=t0: asel(e,
                        out=impm[bs][:, :], in_=impm[bs][:, :], pattern=[[-64, 64]], compare_op=ALU.is_ge, fill=-1.0,
                        base=t0, channel_multiplier=1), reads=[("impm", bs)], writes=[("impm", bs)])
                    P.add("pool", lambda e, bs=bs: e.memset(impm[bs][:, 0:1], 1.0e6), reads=[("impm", bs)], writes=[("impm", bs)])
                    P.add("dve", lambda e, bs=bs: e.max(out=m1[:, :], in_=impm[bs][:, :]), reads=[("impm", bs)], writes=["m1"])
                    P.add("dve", lambda e, bs=bs: e.match_replace(out=tmp[:, :], in_to_replace=m1[:, :], in_values=impm[bs][:, :],
                                                                  imm_value=-2.0), reads=[("impm", bs), "m1"], writes=["tmp"])
                    P.add("dve", lambda e: e.max(out=m2[:, :], in_=tmp[:, :]), reads=["tmp"], writes=["m2"])
                    P.add("dve", lambda e: e.tensor_scalar(out=thr[:, :], in0=m2[:, 7:8], scalar1=0.0, scalar2=None, op0=ALU.max),
                          reads=["m2"], writes=["thr"])
                    P.add("dve", lambda e, bs=bs, tt=tt: e.tensor_scalar(
                        out=BN[tt][:, 64:128], in0=impm[bs][:, :], scalar1=thr[:, 0:1], scalar2=1.0, op0=ALU.is_ge, op1=ALU.subtract),
                        reads=[("impm", bs), "thr", ("BN0", tt)], writes=[("BN", tt)])
                for g in range(4):
                    kts = list(range(max(0, 4 * i - 4), 4 * i + 4))
                    for j, kt in enumerate(kts):
                        if kt >= 4 * i:
                            ms = (-128 * (kt - 4 * i), -1, 1)
                        else:
                            ms = (128 * (kt - 4 * i + 4) - 1, 1, -1)
                        post = (lambda g=g: P.add("dve", lambda e: e.tensor_copy(out=OWs[:, g, :], in_=psOW[0:65, :]),
                                                  reads=["psOW"], writes=[("OWs", g)]))
                        pipe.push(KW[:, kt * 128:(kt + 1) * 128], QB[:, g, qsl], Vw[:, kt, 0:65], psOW, "psOW",
                                  j == 0, j == len(kts) - 1, ms, ["KW", "KW0", ("QBq", g), ("QB0", g), "Vw"], post=post)
                for tt in range(4):
                    t0 = 512 * i + 128 * tt
                    P.add("pe", lambda e, tt=tt: e.transpose(out=psBT, in_=BN[tt][:, :], identity=ident[:, :]),
                          reads=[("BN", tt), ("BN0", tt), "ident"], writes=["psBT"])
                    P.add("act", lambda e, t0=t0: e.copy(out=QB[64:128, :, t0:t0 + 128],
                                                         in_=psBT[64:128].unsqueeze(1).to_broadcast([64, 4, 128])),
                          reads=["psBT"] + [("QB0", g_) for g_ in range(4)], writes=[("QBm", tt)])

                def finalize(g):
                    osl = g % 2
                    hd = kh * 4 + g
                    for tt in range(4):
                        tile_i = 4 * i + tt
                        ds = tt % 2
                        tsl = slice(tt * 128, (tt + 1) * 128)
                        for b, (src, key) in enumerate(((OCs[:, g, tsl], ("OCs", g)), (OSs[osl][:, tsl], ("OSs", osl)),
                                                         (OWs[:, g, tsl], ("OWs", g)))):
                            P.add("pe", lambda e, b=b, src=src: e.transpose(out=psF[:, b, :], in_=src, identity=identf[0:65, 0:65]),
                                  reads=[key, "identf"], writes=["psF"] if b == 0 else [])
                        P.last_w["psF"] = P.ops["pe"][-1]
                        P.add("dve", lambda e, ds=ds: e.tensor_scalar(out=dn[ds][:, :], in0=psF[:, :, 64], scalar1=TINY, scalar2=None,
                                                                      op0=ALU.max), reads=["psF"], writes=[("dn", ds)])
                        P.add("dve", lambda e, ds=ds: e.reciprocal(out=dn[ds][:, :], in_=dn[ds][:, :]), reads=[("dn", ds)], writes=[("dn", ds)])
                        P.add("dve", lambda e, ds=ds, tile_i=tile_i, hd=hd: e.tensor_tensor(
                            out=dn[ds][:, :], in0=dn[ds][:, :], in1=Gs[:, tile_i, hd * 3:hd * 3 + 3], op=ALU.mult),
                            reads=[("dn", ds), "Gs"], writes=[("dn", ds)])
                        oo = ost[i % 2][:, tt, g * 64:(g + 1) * 64]
                        P.add("dve", lambda e, ds=ds, oo=oo: e.tensor_scalar(out=oo, in0=psF[:, 0, 0:64], scalar1=dn[ds][:, 0:1],
                                                                             scalar2=None, op0=ALU.mult),
                              reads=["psF", ("dn", ds)], writes=[("ost", i % 2, tt, g)])
                        for b in (1, 2):
                            P.add("dve", lambda e, ds=ds, oo=oo, b=b: e.scalar_tensor_tensor(
                                out=oo, in0=psF[:, b, 0:64], scalar=dn[ds][:, b:b + 1], in1=oo, op0=ALU.mult, op1=ALU.add),
                                reads=["psF", ("dn", ds), ("ost", i % 2, tt, g)], writes=[("ost", i % 2, tt, g)])

                pending = []
                for g in range(4):
                    kts = list(range(0, 4 * i + 4))
                    osl = g % 2
                    for j, kt in enumerate(kts):
                        ms = (-128 * (kt - 4 * i), -1, 1) if kt >= 4 * i else None

                        def post(g=g, osl=osl):
                            P.add("dve", lambda e: e.tensor_copy(out=OSs[osl][:, :], in_=psOS[0:65, :]), reads=["psOS"], writes=[("OSs", osl)])
                            pending.append(g)
                        pipe.push(KE[:, kt * 128:(kt + 1) * 128], QB[:, g, qsl], Vs[:, kt, 0:65], psOS, "psOS",
                                  j == 0, j == len(kts) - 1, ms,
                                  ["KEk", "KEm", ("QBq", g), "Vs"] + [("QBm", tt) for tt in range(4)], post=post)
                        if j == 3 and pending:
                            finalize(pending.pop(0))
                pipe.flush()
                while pending:
                    finalize(pending.pop(0))
                if "DBGB" in T and kh == 0:
                    P.add("sp", lambda e, qsl=qsl: e.dma_start(out=T["DBGB"][:, qsl], in_=QB[64:128, 0, qsl]),
                          reads=[("QBm", tt) for tt in range(4)], chan=c0)
                P.add("sp", lambda e, i=i, kh=kh: e.dma_start(out=OAv[:, 4 * i:4 * i + 4, kh * 256:(kh + 1) * 256], in_=ost[i % 2][:, :, :]),
                      reads=[("ost", i % 2, tt, g) for tt in range(4) for g in range(4)], chan=cst[i % 2])

    run_phase(nc, build)


def fox_phase(nc, T):
    TINY = 1e-30

    def build(P, es):
        ident, identf = make_ident(P, nc, es)
        QT = [sb(nc, es, "QT%d" % i, [128, S], BF16) for i in range(2)]
        KT = [sb(nc, es, "KT%d" % i, [128, S], BF16) for i in range(2)]
        Vf = [sb(nc, es, "Vf%d" % i, [128, NT, 72], BF16) for i in range(2)]
        lf = sb(nc, es, "lf", [128, NT, 8], F32)
        U = sb(nc, es, "U", [128, 128], F32)
        ONES = sb(nc, es, "ONES", [128, 128], F32)
        ones32 = sb(nc, es, "ones32", [128, NT], F32)
        cin = sb(nc, es, "cin", [128, NT, 8], F32)
        tot = sb(nc, es, "tot", [128, NT, 8], F32)
        incl = sb(nc, es, "incl", [128, NT, 8], F32)
        call = sb(nc, es, "call", [128, NT, 8], F32)
        biasT = sb(nc, es, "biasT", [128, NG, 8, NT], F32)
        PT = [sb(nc, es, "PT%d" % i, [128, 512], BF16) for i in range(4)]
        OFs = [sb(nc, es, "OFs%d" % i, [65, 512], F32) for i in range(2)]
        dn = [sb(nc, es, "dn%d" % i, [128, 1], F32) for i in range(2)]
        ostf = [sb(nc, es, "ostf%d" % i, [128, 4, 64], F32) for i in range(2)]
        psS = [ps(nc, es, "psS%d" % i, [128, 512], F32) for i in range(3)]
        psO = [ps(nc, es, "psO%d" % i, [128, 512], F32) for i in range(2)]
        psFF = [ps(nc, es, "psFF%d" % i, [128, 512], F32) for i in range(2)]
        psF = [psFF[0][:, 0:65], psFF[1][:, 0:65]]
        psC = psS[0][:, 0:NT * 8]
        psTt = psS[1][:, 0:NT * 8]
        c0 = P.chan()
        ckh = [P.chan() for _ in range(2)]
        cst = [P.chan() for _ in range(2)]
        OAv = T["OA"].rearrange("(t p) c -> p t c", p=128)

        P.add("sp", lambda e: e.dma_start(out=lf[:, :, :].rearrange("p t c -> p (t c)"), in_=T["LF"]), writes=["lf"], chan=c0)
        P.add("pool", lambda e: e.memset(U[:, :], 1.0), writes=["U"])
        P.add("pool", lambda e: asel(e, out=U[:, :], in_=U[:, :], pattern=[[1, 128]], compare_op=ALU.is_ge, fill=0.0,
                                     base=0, channel_multiplier=-1), reads=["U"], writes=["U"])
        P.add("pool", lambda e: e.memset(ONES[:, :], 1.0), writes=["ONES"])
        for i in range(2):
            P.add("pool", lambda e, i=i: e.memset(QT[i][64:128, :], 0.0), writes=[("QT0", i)])
            P.add("pool", lambda e, i=i: e.memset(KT[i][64:128, :], 0.0), writes=[("KT0", i)])
        P.add("pool", lambda e: e.memset(ones32[:, :], 1.0), writes=["ones32"])
        lff = lf[:, :, :].rearrange("p t c -> p (t c)")
        P.add("pe", lambda e: e.matmul(out=psC, lhsT=U[:, :], rhs=lff, start=True, stop=True), reads=["U", "lf"], writes=[("psS", 0)])
        P.add("pe", lambda e: e.matmul(out=psTt, lhsT=ONES[:, :], rhs=lff, start=True, stop=True), reads=["ONES", "lf"], writes=[("psS", 1)])
        P.add("dve", lambda e: e.tensor_copy(out=cin[:, :, :].rearrange("p t c -> p (t c)"), in_=psC), reads=[("psS", 0)], writes=["cin"])
        P.add("dve", lambda e: e.tensor_copy(out=tot[:, :, :].rearrange("p t c -> p (t c)"), in_=psTt), reads=[("psS", 1)], writes=["tot"])
        for h in range(8):
            P.add("dve", lambda e, h=h: e.tensor_tensor_scan(out=incl[:, :, h], data0=ones32[:, :], data1=tot[:, :, h], initial=0.0,
                                                             op0=ALU.mult, op1=ALU.add), reads=["tot", "ones32"], writes=[("incl", h)])
        inck = [("incl", h) for h in range(8)]
        P.add("dve", lambda e: e.tensor_tensor(out=call[:, :, :], in0=incl[:, :, :], in1=tot[:, :, :], op=ALU.subtract),
              reads=inck + ["tot"], writes=["call"])
        P.add("dve", lambda e: e.tensor_tensor(out=call[:, :, :], in0=call[:, :, :], in1=cin[:, :, :], op=ALU.add),
              reads=["call", "cin"], writes=["call"])
        for i in range(NG):
            for h in range(8):
                P.add("dve", lambda e, i=i, h=h: e.tensor_scalar(
                    out=biasT[:, i, h, :], in0=call[:, :, h], scalar1=-1.0, scalar2=incl[:, 4 * i + 1, h:h + 1],
                    op0=ALU.mult, op1=ALU.add), reads=["call"] + inck, writes=[("biasT", i, h)])
        pipe = UnitPipe(P, psS, PT, depth=2)
        fi = 0
        pending = []

        def finalize(h, i, ob):
            for tt in range(4):
                fb = tt % 2
                P.add("pe", lambda e, fb=fb, ob=ob, tt=tt: e.transpose(
                    out=psF[fb], in_=OFs[ob][:, tt * 128:(tt + 1) * 128], identity=identf[0:65, 0:65]),
                    reads=[("OFs", ob), "identf"], writes=[("psF", fb)])
                P.add("dve", lambda e, fb=fb: e.tensor_scalar(out=dn[fb][:, :], in0=psF[fb][:, 64:65], scalar1=TINY, scalar2=None,
                                                              op0=ALU.max), reads=[("psF", fb)], writes=[("dn", fb)])
                P.add("dve", lambda e, fb=fb: e.reciprocal(out=dn[fb][:, :], in_=dn[fb][:, :]), reads=[("dn", fb)], writes=[("dn", fb)])
                P.add("dve", lambda e, fb=fb, ob=ob, tt=tt: e.tensor_scalar(
                    out=ostf[ob][:, tt, :], in0=psF[fb][:, 0:64], scalar1=dn[fb][:, 0:1], scalar2=None, op0=ALU.mult),
                    reads=[("psF", fb), ("dn", fb)], writes=[("ostf", ob, tt)])
            P.add("sp", lambda e, i=i, h=h, ob=ob: e.dma_start(
                out=OAv[:, 4 * i:4 * i + 4, 512 + 64 * h:512 + 64 * (h + 1)], in_=ostf[ob][:, :, :]),
                reads=[("ostf", ob, tt) for tt in range(4)], chan=cst[ob])

        for h in range(DBG.get('fh', 8)):
            hs_ = h % 2
            ck = ckh[hs_]
            P.add("sp", lambda e, h=h, hs_=hs_: e.dma_start(out=QT[hs_][0:64, :], in_=T["QKT"][14 + h, :, :]), writes=[("QT", hs_)], chan=ck)
            P.add("sp", lambda e, h=h, hs_=hs_: e.dma_start(out=KT[hs_][0:64, :], in_=T["QKT"][22 + h, :, :]), writes=[("KT", hs_)], chan=ck)
            P.add("sp", lambda e, h=h, hs_=hs_: e.dma_start(out=Vf[hs_][:, :, :], in_=T["V"][4 + h]), writes=[("Vf", hs_)], chan=ck)
            for op in ck.ops[-3:]:
                op.chanval = ck.count
            for i in range(NG):
                qsl = slice(i * 512, (i + 1) * 512)
                ob = fi % 2
                fi += 1
                nk = 4 * i + 4
                for kt in range(nk):
                    ms = (-128 * (kt - 4 * i), -1, 1) if kt >= 4 * i else None

                    def post(h=h, i=i, ob=ob):
                        P.add("dve", lambda e: e.tensor_copy(out=OFs[ob][:, :], in_=psO[ob][0:65, :]), reads=[("psO", ob)], writes=[("OFs", ob)])
                        pending.append((h, i, ob))
                    pipe.push(KT[hs_][:, kt * 128:(kt + 1) * 128], QT[hs_][:, qsl], Vf[hs_][:, kt, 0:65], psO[ob], ("psO", ob),
                              kt == 0, kt == nk - 1, ms, [("KT", hs_), ("QT", hs_), ("Vf", hs_), ("QT0", hs_), ("KT0", hs_)],
                              bias=biasT[:, i, h, kt:kt + 1], bkeys=[("biasT", i, h)], post=post)
                    if pending and (kt == 3 or DBG.get('fox_now', 0)):
                        finalize(*pending.pop(0))
        pipe.flush()
        while pending:
            finalize(*pending.pop(0))

    run_phase(nc, build)


def norm_rows(P, src, junk, ssq, rs, epst, nb, gB, nparts, width, key):
    for j in range(nparts):
        cs = slice(j * width, (j + 1) * width)
        P.add("act", lambda e, cs=cs, j=j: e.activation(out=junk[:, cs], in_=src[:, cs], func=AF.Square, accum_out=ssq[:, j:j + 1]),
              reads=[key], writes=["junk", ("ssq", key, j)])
    P.add("act", lambda e: e.activation(out=rs[:, 0:nparts], in_=ssq[:, 0:nparts], func=AF.Sqrt, scale=1.0 / width, bias=epst[:, 0:1]),
          reads=[("ssq", key, j) for j in range(nparts)] + ["eps"], writes=[("rs", key)])
    P.add("dve", lambda e: e.reciprocal(out=rs[:, 0:nparts], in_=rs[:, 0:nparts]), reads=[("rs", key)], writes=[("rs", key)])
    for j in range(nparts):
        cs = slice(j * width, (j + 1) * width)
        P.add("dve", lambda e, cs=cs, j=j: e.scalar_tensor_tensor(out=nb[:, cs], in0=src[:, cs], scalar=rs[:, j:j + 1], in1=gB[:, cs],
                                                                  op0=ALU.mult, op1=ALU.mult),
              reads=[key, ("rs", key), "gB", "gB2"], writes=[("nb", key)])


def out_phase(nc, T):
    def build(P, es):
        ident, identf = make_ident(P, nc, es)
        epst = sb(nc, es, "epst", [128, 1], F32)
        P.add("pool", lambda e: e.memset(epst[:, :], EPS), writes=["eps"])
        wo = sb(nc, es, "wo", [128, 8, D], BF16)
        stage = [sb(nc, es, "ostg%d" % i, [128, D], F32) for i in range(2)]
        stch = [P.chan() for _ in range(2)]
        gB = sb(nc, es, "gB", [128, D], F32)
        xn = [sb(nc, es, "xn%d" % i, [128, D], F32) for i in range(2)]
        xr = [sb(nc, es, "xr%d" % i, [128, D], F32) for i in range(2)]
        nb = [sb(nc, es, "nb%d" % i, [128, D], BF16) for i in range(2)]
        junk = sb(nc, es, "junk", [128, D], BF16)
        ssq = [sb(nc, es, "ssq%d" % i, [128, 2], F32) for i in range(2)]
        rs = [sb(nc, es, "rs%d" % i, [128, 2], F32) for i in range(2)]
        mT = [sb(nc, es, "mT%d" % i, [128, 8, 128], BF16) for i in range(2)]
        psT = [ps(nc, es, "psT%d" % i, [128, D], BF16) for i in range(2)]
        pso = [ps(nc, es, "pso%d" % i, [128, 512], F32) for i in range(4)]
        cg = P.chan()
        cx = [P.chan() for _ in range(2)]
        cr = [P.chan() for _ in range(2)]
        co = [P.chan() for _ in range(2)]
        P.add("sp", lambda e: e.dma_start(out=gB[:, 0:512], in_=T["out_norm_nsa"].partition_broadcast(128)), writes=["gB"], chan=cg)
        P.add("sp", lambda e: e.dma_start(out=gB[:, 512:1024], in_=T["out_norm_fox"].partition_broadcast(128)), writes=["gB2"], chan=cg)
        cg.seal()
        wv = T["w_out"].rearrange("(c p) f -> p c f", p=128)
        for c in range(8):
            s_ = c % 2
            P.add("sp", lambda e, s_=s_, c=c: e.dma_start(out=stage[s_][:, :], in_=wv[:, c, :]), writes=[("stage", s_)], chan=stch[s_])
            P.add("dve" if c % 2 == 0 else "pool", lambda e, s_=s_, c=c: e.tensor_copy(out=wo[:, c, :], in_=stage[s_][:, :]),
                  reads=[("stage", s_)], writes=[("wo", c)])
        wkeys = [("wo", c) for c in range(8)]
        for t in range(NT):
            xs = t % 2
            P.add("sp", lambda e, xs=xs, t=t: e.dma_start(out=xn[xs][:, :], in_=T["OA"][t * 128:(t + 1) * 128, :]), writes=[("xn", xs)], chan=cx[xs])
            P.add("sp", lambda e, xs=xs, t=t: e.dma_start(out=xr[xs][:, :], in_=T["h1"][t * 128:(t + 1) * 128, :]), writes=[("xr", xs)], chan=cr[xs])
            norm_rows(P, xn[xs], junk, ssq[xs], rs[xs], epst, nb[xs], gB, 2, 512, ("xn", xs))
            for c in range(8):
                P.add("pe", lambda e, xs=xs, c=c: e.transpose(out=psT[xs][:, c * 128:(c + 1) * 128], in_=nb[xs][:, c * 128:(c + 1) * 128],
                                                              identity=ident[:, :]),
                      reads=[("nb", ("xn", xs)), "ident"], writes=[("psT", xs)] if c == 0 else [])
            P.last_w[("psT", xs)] = P.ops["pe"][-1]
            P.add("act", lambda e, xs=xs: e.copy(out=mT[xs][:, :, :], in_=psT[xs][:, :].rearrange("p (c t) -> p c t", c=8)),
                  reads=[("psT", xs)], writes=[("mT", xs)])
            for half in range(2):
                pb = (t * 2 + half) % 4
                for c in range(8):
                    P.add("pe", lambda e, pb=pb, c=c, xs=xs, half=half: e.matmul(
                        out=pso[pb][:, :], lhsT=mT[xs][:, c, :], rhs=wo[:, c, half * 512:(half + 1) * 512], start=(c == 0), stop=(c == 7)),
                        reads=[("mT", xs)] + (wkeys if t == 0 else []), writes=[("pso", pb)] if c == 0 else [])
                P.last_w[("pso", pb)] = P.ops["pe"][-1]
                P.add("dve", lambda e, pb=pb, xs=xs, half=half: e.tensor_tensor(
                    out=xr[xs][:, half * 512:(half + 1) * 512], in0=pso[pb][:, :], in1=xr[xs][:, half * 512:(half + 1) * 512], op=ALU.add),
                    reads=[("pso", pb), ("xr", xs)], writes=[("xr", xs)])
            P.add("sp", lambda e, xs=xs, t=t: e.dma_start(out=T["h1"][t * 128:(t + 1) * 128, :], in_=xr[xs][:, :]),
                  reads=[("xr", xs)], chan=co[xs])

    run_phase(nc, build)


def ple_phase(nc, T):
    def build(P, es):
        ident, identf = make_ident(P, nc, es)
        epst = sb(nc, es, "epst", [128, 1], F32)
        P.add("pool", lambda e: e.memset(epst[:, :], EPS), writes=["eps"])
        wg = sb(nc, es, "wg", [128, 8, D], BF16)
        wp = sb(nc, es, "wp", [128, 2, D], BF16)
        stage = [sb(nc, es, "lstg%d" % i, [128, D], F32) for i in range(2)]
        stch = [P.chan() for _ in range(2)]
        gB = sb(nc, es, "gB", [128, D], F32)
        gE = sb(nc, es, "gE", [128, D], F32)
        xn = [sb(nc, es, "xn%d" % i, [128, D], F32) for i in range(2)]
        pt = [sb(nc, es, "pt%d" % i, [128, 256], F32) for i in range(2)]
        pb16 = [sb(nc, es, "pb%d" % i, [128, 256], BF16) for i in range(2)]
        nb = [sb(nc, es, "nb%d" % i, [128, D], BF16) for i in range(2)]
        junk = sb(nc, es, "junk", [128, D], BF16)
        ssq = [sb(nc, es, "ssq%d" % i, [128, 4], F32) for i in range(2)]
        rs = [sb(nc, es, "rs%d" % i, [128, 4], F32) for i in range(2)]
        mT = [sb(nc, es, "mT%d" % i, [128, 10, 128], BF16) for i in range(2)]
        gate = [sb(nc, es, "gate%d" % i, [128, D], F32) for i in range(2)]
        ev = [sb(nc, es, "ev%d" % i, [128, D], F32) for i in range(2)]
        psTa = ps(nc, es, "psTa", [128, 8, 128], BF16)
        psTb = ps(nc, es, "psTb", [128, 8, 128], BF16)
        psg = [ps(nc, es, "psg%d" % i, [128, 512], F32) for i in range(2)]
        pse = [ps(nc, es, "pse%d" % i, [128, 512], F32) for i in range(2)]
        cg = P.chan()
        cx = [P.chan() for _ in range(2)]
        cp = [P.chan() for _ in range(2)]
        co = [P.chan() for _ in range(2)]
        P.add("sp", lambda e: e.dma_start(out=gB[:, :], in_=T["ple_gate_norm"].partition_broadcast(128)), writes=["gB"], chan=cg)
        P.add("sp", lambda e: e.dma_start(out=gE[:, :], in_=T["ple_norm"].partition_broadcast(128)), writes=["gE"], chan=cg)
        cg.seal()
        wv = T["ple_w_gate"].rearrange("(c p) f -> p c f", p=128)
        wpv = T["ple_w_proj"].rearrange("(c p) f -> p c f", p=128)
        for c in range(10):
            s_ = c % 2
            src = wv[:, c, :] if c < 8 else wpv[:, c - 8, :]
            dstw = wg[:, c, :] if c < 8 else wp[:, c - 8, :]
            P.add("sp", lambda e, s_=s_, src=src: e.dma_start(out=stage[s_][:, :], in_=src), writes=[("stage", s_)], chan=stch[s_])
            P.add("dve" if c % 2 == 0 else "pool", lambda e, s_=s_, dstw=dstw: e.tensor_copy(out=dstw, in_=stage[s_][:, :]),
                  reads=[("stage", s_)], writes=[("w", c)])
        wkeys = [("w", c) for c in range(10)]
        for t in range(NT):
            xs = t % 2
            P.add("sp", lambda e, xs=xs, t=t: e.dma_start(out=xn[xs][:, :], in_=T["h3"][t * 128:(t + 1) * 128, :]), writes=[("xn", xs)], chan=cx[xs])
            P.add("sp", lambda e, xs=xs, t=t: e.dma_start(out=pt[xs][:, :], in_=T["p"][t * 128:(t + 1) * 128, :]), writes=[("pt", xs)], chan=cp[xs])
            norm_rows(P, xn[xs], junk, ssq[xs], rs[xs], epst, nb[xs], gB, 1, D, ("xn", xs))
            P.add("pool", lambda e, xs=xs: e.tensor_copy(out=pb16[xs][:, :], in_=pt[xs][:, :]), reads=[("pt", xs)], writes=[("pb16", xs)])
            for c in range(10):
                src = nb[xs][:, c * 128:(c + 1) * 128] if c < 8 else pb16[xs][:, (c - 8) * 128:(c - 7) * 128]
                dstp = psTa[:, c, :] if c < 8 else psTb[:, c - 8, :]
                P.add("pe", lambda e, dstp=dstp, src=src: e.transpose(out=dstp, in_=src, identity=ident[:, :]),
                      reads=[("nb", ("xn", xs)), ("pb16", xs), "ident"], writes=["psTa" if c < 8 else "psTb"] if c in (0, 8) else [])
                if c == 7:
                    P.last_w["psTa"] = P.ops["pe"][-1]
            P.last_w["psTb"] = P.ops["pe"][-1]
            P.add("act", lambda e, xs=xs: e.copy(out=mT[xs][:, 0:8, :], in_=psTa[:, :, :]), reads=["psTa"], writes=[("mT", xs)])
            P.add("act", lambda e, xs=xs: e.copy(out=mT[xs][:, 8:10, :], in_=psTb[:, 0:2, :]), reads=["psTb"], writes=[("mT2", xs)])
            for half in range(2):
                hsl = slice(half * 512, (half + 1) * 512)
                for c in range(8):
                    P.add("pe", lambda e, c=c, xs=xs, half=half, hsl=hsl: e.matmul(
                        out=psg[half][:, :], lhsT=mT[xs][:, c, :], rhs=wg[:, c, hsl], start=(c == 0), stop=(c == 7)),
                        reads=[("mT", xs), ("mT2", xs)] + (wkeys if t == 0 else []), writes=[("psg", half)] if c == 0 else [])
                P.last_w[("psg", half)] = P.ops["pe"][-1]
                P.add("act", lambda e, xs=xs, half=half, hsl=hsl: e.activation(out=gate[xs][:, hsl], in_=psg[half][:, :], func=AF.Sigmoid),
                      reads=[("psg", half)], writes=[("gate", xs, half)])
                for c in range(2):
                    P.add("pe", lambda e, c=c, xs=xs, half=half, hsl=hsl: e.matmul(
                        out=pse[half][:, :], lhsT=mT[xs][:, 8 + c, :], rhs=wp[:, c, hsl], start=(c == 0), stop=(c == 1)),
                        reads=[("mT", xs), ("mT2", xs)] + (wkeys if t == 0 else []), writes=[("pse", half)] if c == 0 else [])
                P.last_w[("pse", half)] = P.ops["pe"][-1]
                P.add("act", lambda e, xs=xs, half=half, hsl=hsl: e.activation(
                    out=junk[:, hsl], in_=pse[half][:, :], func=AF.Square, accum_out=ssq[xs][:, 2 + half:3 + half]),
                    reads=[("pse", half)], writes=[("junk2", half), ("ssqe", xs, half)])
            P.add("dve", lambda e, xs=xs: e.tensor_tensor(out=rs[xs][:, 2:3], in0=ssq[xs][:, 2:3], in1=ssq[xs][:, 3:4], op=ALU.add),
                  reads=[("ssqe", xs, 0), ("ssqe", xs, 1)], writes=[("rse", xs)])
            P.add("act", lambda e, xs=xs: e.activation(out=rs[xs][:, 2:3], in_=rs[xs][:, 2:3], func=AF.Sqrt, scale=1.0 / D, bias=epst[:, 0:1]),
                  reads=[("rse", xs), "eps"], writes=[("rse", xs)])
            P.add("dve", lambda e, xs=xs: e.reciprocal(out=rs[xs][:, 2:3], in_=rs[xs][:, 2:3]), reads=[("rse", xs)], writes=[("rse", xs)])
            for half in range(2):
                hsl = slice(half * 512, (half + 1) * 512)
                P.add("dve", lambda e, xs=xs, half=half, hsl=hsl: e.scalar_tensor_tensor(
                    out=ev[xs][:, hsl], in0=pse[half][:, :], scalar=rs[xs][:, 2:3], in1=gE[:, hsl], op0=ALU.mult, op1=ALU.mult),
                    reads=[("pse", half), ("rse", xs), "gE"], writes=[("ev", xs, half)])
                P.add("pool", lambda e, xs=xs, hsl=hsl: e.tensor_tensor(out=ev[xs][:, hsl], in0=ev[xs][:, hsl], in1=gate[xs][:, hsl], op=ALU.mult),
                      reads=[("ev", xs, half), ("gate", xs, half)], writes=[("ev", xs, half)])
                P.add("pool", lambda e, xs=xs, hsl=hsl: e.tensor_tensor(out=ev[xs][:, hsl], in0=ev[xs][:, hsl], in1=xn[xs][:, hsl], op=ALU.add),
                      reads=[("ev", xs, half), ("xn", xs)], writes=[("ev", xs, half)])
            P.add("sp", lambda e, xs=xs, t=t: e.dma_start(out=T["out"][t * 128:(t + 1) * 128, :], in_=ev[xs][:, :]),
                  reads=[("ev", xs, 0), ("ev", xs, 1)], chan=co[xs])

    run_phase(nc, build)


def rope_tables_np():
    pos = np.arange(S, dtype=np.float32)
    inv = (np.float32(500000.0) ** (-np.arange(0, 16, 2, dtype=np.float32) / np.float32(16))).astype(np.float32)
    ang = (pos[:, None] * inv[None, :]).astype(np.float32)
    return np.cos(ang).astype(np.float32), np.sin(ang).astype(np.float32)


IN_SHAPES = dict(
    x=[S, D], p=[S, 256], ffn1_norm=[D], ffn1_wg=[D, DFF], ffn1_wu=[D, DFF], ffn1_wd=[DFF, D],
    mix_norm=[D], w_in=[D, 2848], b_forget=[8], q_norm_nsa=[64], k_norm_cmp=[64], k_norm_slc=[64], k_norm_win=[64],
    cmp_pos_k=[32, 64], cmp_pos_v=[32, 64], cmp_k_w1=[2048, 256], cmp_k_w2=[256, 64], cmp_v_w1=[2048, 256],
    cmp_v_w2=[256, 64], q_norm_fox=[64], k_norm_fox=[64], out_norm_nsa=[512], out_norm_fox=[512], w_out=[D, D],
    ffn2_norm=[D], ffn2_wg=[D, DFF], ffn2_wu=[D, DFF], ffn2_wd=[DFF, D], ple_gate_norm=[D], ple_w_gate=[D, D],
    ple_w_proj=[256, D], ple_norm=[D], rope_cos=[S, 8], rope_sin=[S, 8])


def build_nc(nph=99, debug=(), skip=()):
    nc = bass.Bass("TRN2", target_bir_lowering=False)
    T = {}
    for name, shape in IN_SHAPES.items():
        T[name] = nc.dram_tensor(name, shape, F32, kind="ExternalInput").ap()

    def scratch(name, shape, dt):
        kind = "ExternalOutput" if name in debug else "Internal"
        T[name] = nc.dram_tensor(name, shape, dt, kind=kind).ap()

    T["out"] = nc.dram_tensor("out", [S, D], F32, kind="ExternalOutput").ap()
    scratch("h1", [S, D], F32)
    scratch("QKT", [32, 64, S], BF16)
    scratch("V", [12, 128, NT, 72], BF16)
    scratch("G", [128, NT * 24], F32)
    scratch("LF", [128, NT * 8], F32)
    scratch("KCT", [2, 64, 256], BF16)
    scratch("VC", [2, 256, 65], BF16)
    scratch("OA", [S, D], F32)
    scratch("h3", [S, D], F32)
    if "DBGB" in debug:
        scratch("DBGB", [64, S], BF16)
    if "DBGT" in debug:
        scratch("DBGT", [3, 65, 512], F32)
    phases = [
        lambda: ffn_half_phase(nc, T, T["x"], T["x"], T["h1"], T["ffn1_norm"], T["ffn1_wg"], T["ffn1_wu"], T["ffn1_wd"], 0, 11, "f1a"),
        lambda: ffn_half_phase(nc, T, T["x"], T["h1"], T["h1"], T["ffn1_norm"], T["ffn1_wg"], T["ffn1_wu"], T["ffn1_wd"], 11, 11, "f1b"),
        lambda: proj_phase(nc, T),
        lambda: cmp_phase(nc, T),
        lambda: nsa_phase(nc, T),
        lambda: fox_phase(nc, T),
        lambda: out_phase(nc, T),
        lambda: ffn_half_phase(nc, T, T["h1"], T["h1"], T["h3"], T["ffn2_norm"], T["ffn2_wg"], T["ffn2_wu"], T["ffn2_wd"], 0, 11, "f2a"),
        lambda: ffn_half_phase(nc, T, T["h1"], T["h3"], T["h3"], T["ffn2_norm"], T["ffn2_wg"], T["ffn2_wu"], T["ffn2_wd"], 11, 11, "f2b"),
        lambda: ple_phase(nc, T),
    ]
    for k, ph in enumerate(phases[:nph]):
        if k not in skip:
            ph()
    return nc


def make_in_maps(inputs, cores=range(8)):
    cos, sin = rope_tables_np()
    in_maps = []
    for b in cores:
        m = {}
        for name in IN_SHAPES:
            if name == "x":
                a = inputs["x"][b]
            elif name == "p":
                a = inputs["p"][0, b]
            elif name == "rope_cos":
                a = cos
            elif name == "rope_sin":
                a = sin
            elif name == "w_in":
                a = inputs["w_in"][0][:, W_IN_PERM]
            else:
                a = inputs[name][0]
            m[name] = np.ascontiguousarray(a, dtype=np.float32)
        in_maps.append(m)
    return in_maps


def kernel(**inputs):
    nc = build_nc()
    res = run_bass_kernel_spmd(nc, make_in_maps(inputs), core_ids=list(range(8)))
    return np.stack([np.asarray(r["out"]) for r in res.results], axis=0)
```

```python
import numpy as np
from contextlib import ExitStack
import concourse.bass as bass
import concourse.mybir as mybir
from concourse.bass_utils import run_bass_kernel_spmd

F32 = mybir.dt.float32
BF16 = mybir.dt.bfloat16
AF = mybir.ActivationFunctionType
ALU = mybir.AluOpType
AX = mybir.AxisListType

S = 4096
D = 1024
DFF = 2816
NT = S // 128
NG = S // 512
EPS = 1e-6
SAME_ENGINE_SYNC = True
DBG = dict(kh=2, ng=NG, stage=5)
_UID = [0]


def _u(name):
    _UID[0] += 1
    return "%s_%d" % (name, _UID[0])

_OFF = dict(qa=0, kc=512, vc=640, ksl=768, vsl=896, kwn=1024, vwn=1152, ga=1280, qf=1304, kf=1816, vf=2328, fl=2840)
_SZ = dict(qa=512, kc=128, vc=128, ksl=128, vsl=128, kwn=128, vwn=128, ga=24, qf=512, kf=512, vf=512, fl=8)
_ORDER = ['kc', 'qa', 'ksl', 'kwn', 'qf', 'kf', 'vc', 'vsl', 'vwn', 'vf', 'ga', 'fl']
W_IN_PERM = np.concatenate([np.arange(_OFF[k], _OFF[k] + _SZ[k]) for k in _ORDER])


class Chan:
    def __init__(self, sem):
        self.sem = sem
        self.count = 0
        self.ops = []

    def seal(self):
        for op in self.ops:
            op.chanval = self.count


class Op:
    __slots__ = ("eng", "fn", "deps", "sig", "sigval", "chan", "chanval", "idx")


class Prog:
    ENGS = ("pe", "act", "dve", "pool", "sp")

    def __init__(self, nc, es):
        self.nc = nc
        self.es = es
        self.ops = {e: [] for e in self.ENGS}
        self.last_w = {}
        self.readers = {}
        self.engsem = {e: es.enter_context(nc.semaphore(_u("s_" + e))) for e in self.ENGS}
        self.chans = []

    def chan(self):
        c = Chan(self.es.enter_context(self.nc.semaphore(_u("c"))))
        self.chans.append(c)
        return c

    def add(self, eng, fn, reads=(), writes=(), chan=None):
        op = Op()
        op.eng = eng
        op.fn = fn
        op.sig = False
        op.sigval = 0
        op.chan = chan
        deps = []
        for r in reads:
            w = self.last_w.get(r)
            if w is not None:
                deps.append(w)
        for w in writes:
            lw = self.last_w.get(w)
            if lw is not None:
                deps.append(lw)
            deps.extend(self.readers.get(w, ()))
        best = {}
        for d in deps:
            k = ("c", id(d.chan)) if d.chan is not None else ("e", d.eng)
            if k not in best or best[k].idx < d.idx:
                best[k] = d
        op.deps = list(best.values())
        op.idx = len(self.ops[eng])
        for r in reads:
            self.readers.setdefault(r, []).append(op)
        for w in writes:
            self.last_w[w] = op
            self.readers[w] = []
        if chan is not None:
            chan.count += 16
            op.chanval = chan.count
            chan.ops.append(op)
        self.ops[eng].append(op)
        return op

    def emit(self, block):
        for e in self.ENGS:
            for op in self.ops[e]:
                for d in op.deps:
                    if d.chan is None and (d.eng != op.eng or SAME_ENGINE_SYNC):
                        d.sig = True
        for e in self.ENGS:
            c = 0
            for op in self.ops[e]:
                if op.sig and op.chan is None:
                    c += 1
                    op.sigval = c
        final = [(c.sem, c.count) for c in self.chans if c.count > 0]

        def mk(ename):
            ops = self.ops[ename]

            def body(eng):
                waited = {}
                if ename == "pool":
                    _FILL.clear()
                for op in ops:
                    need = []
                    for d in op.deps:
                        if d.chan is not None:
                            sem, val = d.chan.sem, d.chanval
                        elif d.eng != ename or SAME_ENGINE_SYNC:
                            sem, val = self.engsem[d.eng], d.sigval
                        else:
                            continue
                        k = id(sem)
                        if waited.get(k, 0) >= val:
                            continue
                        need.append((sem, val))
                        waited[k] = val
                    for sem, val in need[:-1]:
                        eng.wait_ge(sem, val)
                    ins = op.fn(eng)
                    if need:
                        ins._wait_ge(need[-1][0], need[-1][1])
                    if op.chan is not None:
                        ins.then_inc(op.chan.sem, 16)
                    elif op.sig:
                        ins.then_inc(self.engsem[ename], 1)
                if ename == "sp":
                    for sem, val in final:
                        eng.wait_ge(sem, val)
                if ename == "pool":
                    for r in _FILL.values():
                        eng.free_register(r)
                    _FILL.clear()
            return body

        block.tensor(mk("pe"))
        block.scalar(mk("act"))
        block.vector(mk("dve"))
        block.gpsimd(mk("pool"))
        block.sync(mk("sp"))


def run_phase(nc, build):
    with ExitStack() as es:
        P = Prog(nc, es)
        build(P, es)
        sems = list(P.engsem.values()) + [c.sem for c in P.chans]
        with nc.Block() as b0:
            def clr(e):
                for sm in sems:
                    e.sem_clear(sm)
            b0.sync(clr)
        with nc.Block() as block:
            P.emit(block)


def sb(nc, es, name, shape, dt):
    return es.enter_context(nc.sbuf_tensor(_u(name), shape, dt))


def ps(nc, es, name, shape, dt):
    return es.enter_context(nc.psum_tensor(_u(name), shape, dt))


def load_cast_rows(P, nc, es, dst, src_rows, ncols, chans, stage, key):
    n = len(src_rows)
    for k in range(n):
        s = k % len(stage)
        st = stage[s]
        ch = chans[s]
        src = src_rows[k]
        P.add("sp", lambda e, st=st, src=src: e.dma_start(out=st[:, 0:ncols], in_=src),
              writes=[("stage", s)], chan=ch)
        eng = "dve" if k % 2 == 0 else "pool"
        P.add(eng, lambda e, st=st, k=k: e.tensor_copy(out=dst[:, k, 0:ncols], in_=st[:, 0:ncols]),
              reads=[("stage", s)], writes=[(key, k)])


def ffn_half_phase(nc, T, src_norm, src_res, dst, gain, wg, wu, wd, f0, nfc, tagp):
    def build(P, es):
        FW = nfc * 128
        wg_s = sb(nc, es, "wg_s", [128, 8, FW], BF16)
        wu_s = sb(nc, es, "wu_s", [128, 8, FW], BF16)
        wd_s = sb(nc, es, "wd_s", [128, nfc, D], BF16)
        stage = [sb(nc, es, "stg%d" % i, [128, FW], F32) for i in range(3)]
        stch = [P.chan() for _ in range(3)]
        gB = sb(nc, es, "gB", [128, D], F32)
        ident = sb(nc, es, "ident", [128, 128], BF16)
        identf = sb(nc, es, "identf", [128, 128], F32)
        epst = sb(nc, es, "epst", [128, 1], F32)
        xn = [sb(nc, es, "xn%d" % i, [128, D], F32) for i in range(2)]
        xr = [sb(nc, es, "xr%d" % i, [128, D], F32) for i in range(2)]
        nb = [sb(nc, es, "nb%d" % i, [128, D], BF16) for i in range(4)]
        junk = sb(nc, es, "junk", [128, D], BF16)
        ssq = [sb(nc, es, "ssq%d" % i, [128, 4], F32) for i in range(2)]
        rs = [sb(nc, es, "rs%d" % i, [128, 4], F32) for i in range(2)]
        nT = [sb(nc, es, "nT%d" % i, [128, 8, 512], BF16) for i in range(2)]
        hT = sb(nc, es, "hT", [128, nfc, 512], BF16)
        sg = [sb(nc, es, "sg%d" % i, [128, 512], F32) for i in range(2)]
        psT = [ps(nc, es, "psT%d" % i, [128, D], BF16) for i in range(2)]
        psg = [ps(nc, es, "psg%d" % i, [128, 512], F32) for i in range(2)]
        psu = [ps(nc, es, "psu%d" % i, [128, 512], F32) for i in range(2)]
        pso = [ps(nc, es, "pso%d" % i, [128, 512], F32) for i in range(2)]
        cx = [P.chan() for _ in range(2)]
        cr = [P.chan() for _ in range(2)]
        co = [P.chan() for _ in range(2)]
        cg = P.chan()

        P.add("sp", lambda e: e.dma_start(out=gB[:, :], in_=gain.partition_broadcast(128)),
              writes=["gB"], chan=cg)
        P.add("pool", lambda e: e.memset(identf[:, :], 0.0), writes=["identf"])
        P.add("pool", lambda e: asel(e, out=identf[:, :], in_=identf[:, :], pattern=[[-1, 128]],
                                                compare_op=ALU.not_equal, fill=1.0, base=0,
                                                channel_multiplier=1),
              reads=["identf"], writes=["identf"])
        P.add("pool", lambda e: e.tensor_copy(out=ident[:, :], in_=identf[:, :]), reads=["identf"], writes=["ident"])
        P.add("pool", lambda e: e.memset(epst[:, :], EPS), writes=["eps"])

        wgv = wg.rearrange("(c p) f -> p c f", p=128)
        wuv = wu.rearrange("(c p) f -> p c f", p=128)
        load_cast_rows(P, nc, es, wg_s, [wgv[:, c, f0 * 128:f0 * 128 + FW] for c in range(8)], FW, stch, stage, "wg")
        load_cast_rows(P, nc, es, wu_s, [wuv[:, c, f0 * 128:f0 * 128 + FW] for c in range(8)], FW, stch, stage, "wu")
        load_cast_rows(P, nc, es, wd_s, [wd[(f0 + c) * 128:(f0 + c + 1) * 128, :] for c in range(nfc)], D, stch, stage, "wd")
        wkeys = [("wg", c) for c in range(8)] + [("wu", c) for c in range(8)]
        wdkeys = [("wd", c) for c in range(nfc)]

        def prep_group(g):
            sl = g % 2
            for k in range(4):
                t = 4 * g + k
                xs = t % 2
                P.add("sp", lambda e, xs=xs, t=t: e.dma_start(out=xn[xs][:, :], in_=src_norm[t * 128:(t + 1) * 128, :]),
                      writes=[("xn", xs)], chan=cx[xs])
                P.add("act", lambda e, xs=xs, sl=sl, k=k: e.activation(
                    out=junk[:, :], in_=xn[xs][:, :], func=AF.Square, accum_out=ssq[sl][:, k:k + 1]),
                    reads=[("xn", xs)], writes=["junk", ("ssq", sl, k)])
                P.add("act", lambda e, sl=sl, k=k: e.activation(out=rs[sl][:, k:k + 1], in_=ssq[sl][:, k:k + 1],
                                                                func=AF.Sqrt, scale=1.0 / D, bias=epst[:, 0:1]),
                      reads=[("ssq", sl, k), "eps"], writes=[("rs", sl, k)])
                P.add("dve", lambda e, sl=sl, k=k: e.reciprocal(out=rs[sl][:, k:k + 1], in_=rs[sl][:, k:k + 1]),
                      reads=[("rs", sl, k)], writes=[("rs", sl, k)])
                P.add("dve", lambda e, xs=xs, sl=sl, k=k: e.scalar_tensor_tensor(
                    out=nb[k][:, :], in0=xn[xs][:, :], scalar=rs[sl][:, k:k + 1], in1=gB[:, :],
                    op0=ALU.mult, op1=ALU.mult),
                    reads=[("xn", xs), ("rs", sl, k), "gB"], writes=[("nb", k)])

        def transposes(g):
            sl = g % 2
            for k in range(4):
                pb = k % 2
                for c in range(8):
                    P.add("pe", lambda e, pb=pb, k=k, c=c: e.transpose(
                        out=psT[pb][:, c * 128:(c + 1) * 128], in_=nb[k][:, c * 128:(c + 1) * 128], identity=ident[:, :]),
                        reads=[("nb", k), "ident"], writes=[("psT", pb)] if c == 0 else [])
                P.last_w[("psT", pb)] = P.ops["pe"][-1]
                P.add("act", lambda e, pb=pb, sl=sl, k=k: e.copy(
                    out=nT[sl][:, :, k * 128:(k + 1) * 128],
                    in_=psT[pb][:, :].rearrange("p (c t) -> p c t", c=8)),
                    reads=[("psT", pb)], writes=[("nT", sl, k)])

        def upgate(g):
            sl = g % 2
            for fc in range(nfc):
                b = fc % 2
                for (wt, pst, nm) in ((wg_s, psg, "psg"), (wu_s, psu, "psu")):
                    for c in range(8):
                        P.add("pe", lambda e, wt=wt, pst=pst, b=b, c=c, fc=fc, sl=sl: e.matmul(
                            out=pst[b][:, :], lhsT=wt[:, c, fc * 128:(fc + 1) * 128], rhs=nT[sl][:, c, :],
                            start=(c == 0), stop=(c == 7)),
                            reads=[("nT", sl, 0), ("nT", sl, 1), ("nT", sl, 2), ("nT", sl, 3)] + (wkeys if g == 0 else []),
                            writes=[(nm, b)] if c == 0 else [])
                    P.last_w[(nm, b)] = P.ops["pe"][-1]
                P.add("act", lambda e, b=b: e.activation(out=sg[b][:, :], in_=psg[b][:, :], func=AF.Silu),
                      reads=[("psg", b)], writes=[("sg", b)])
                P.add("dve", lambda e, b=b, fc=fc: e.tensor_tensor(out=hT[:, fc, :], in0=psu[b][:, :], in1=sg[b][:, :],
                                                                  op=ALU.mult),
                      reads=[("psu", b), ("sg", b)], writes=[("hT", fc)])

        def down(g):
            for k in range(4):
                t = 4 * g + k
                rsl = t % 2
                P.add("sp", lambda e, rsl=rsl, t=t: e.dma_start(out=xr[rsl][:, :], in_=src_res[t * 128:(t + 1) * 128, :]),
                      reads=[("dram", t)], writes=[("xr", rsl)], chan=cr[rsl])
                for half in range(2):
                    b = half
                    for fc in range(nfc):
                        P.add("pe", lambda e, b=b, fc=fc, k=k, half=half: e.matmul(
                            out=pso[b][:, :], lhsT=hT[:, fc, k * 128:(k + 1) * 128],
                            rhs=wd_s[:, fc, half * 512:(half + 1) * 512], start=(fc == 0), stop=(fc == nfc - 1)),
                            reads=[("hT", fc)] + (wdkeys if g == 0 else []),
                            writes=[("pso", b)] if fc == 0 else [])
                    P.last_w[("pso", b)] = P.ops["pe"][-1]
                    P.add("dve", lambda e, b=b, rsl=rsl, half=half: e.scalar_tensor_tensor(
                        out=xr[rsl][:, half * 512:(half + 1) * 512], in0=pso[b][:, :], scalar=0.5,
                        in1=xr[rsl][:, half * 512:(half + 1) * 512], op0=ALU.mult, op1=ALU.add),
                        reads=[("pso", b), ("xr", rsl)], writes=[("xr", rsl)])
                P.add("sp", lambda e, rsl=rsl, t=t: e.dma_start(out=dst[t * 128:(t + 1) * 128, :], in_=xr[rsl][:, :]),
                      reads=[("xr", rsl)], writes=[("dram", t)], chan=co[rsl])

        prep_group(0)
        transposes(0)
        for g in range(NG):
            if g + 1 < NG:
                prep_group(g + 1)
            upgate(g)
            if g + 1 < NG:
                transposes(g + 1)
            down(g)

    run_phase(nc, build)


def proj_phase(nc, T):
    def build(P, es):
        h1 = T["h1"]
        WIN = 2848
        win_s = sb(nc, es, "win_s", [128, 8, WIN], BF16)
        HW_ = WIN // 2
        stage = [sb(nc, es, "pstg%d" % i, [128, HW_], F32) for i in range(3)]
        stch = [P.chan() for _ in range(3)]
        gB = sb(nc, es, "gB", [128, D], F32)
        ident = sb(nc, es, "ident", [128, 128], BF16)
        identf = sb(nc, es, "identf", [128, 128], F32)
        epst = sb(nc, es, "epst", [128, 1], F32)
        g5 = sb(nc, es, "g5", [128, 5, 64], F32)
        GQ = sb(nc, es, "GQ", [128, 28, 64], F32)
        bfg = sb(nc, es, "bfg", [128, 8], F32)
        cosT = sb(nc, es, "cosT", [128, NT, 8], F32)
        sinT = sb(nc, es, "sinT", [128, NT, 8], F32)
        Gall = sb(nc, es, "Gall", [128, NT, 24], F32)
        LFall = sb(nc, es, "LFall", [128, NT, 8], F32)
        xn = [sb(nc, es, "xn%d" % i, [128, D], F32) for i in range(2)]
        nb = [sb(nc, es, "nb%d" % i, [128, D], BF16) for i in range(2)]
        junk = sb(nc, es, "junk", [128, D], BF16)
        ssq = sb(nc, es, "ssq", [128, 2], F32)
        rs = sb(nc, es, "rs", [128, 2], F32)
        aT = [sb(nc, es, "aT%d" % i, [128, 8, 128], BF16) for i in range(2)]
        qk = [sb(nc, es, "qk%d" % i, [128, 32, 64], F32) for i in range(2)]
        sq = [sb(nc, es, "sq%d" % i, [128, 32, 64], F32) for i in range(2)]
        hs = [sb(nc, es, "hs%d" % i, [128, 32], F32) for i in range(2)]
        rt = [sb(nc, es, "rt%d" % i, [128, 14, 8], F32) for i in range(4)]
        qkb = [sb(nc, es, "qkb%d" % i, [128, 2048], BF16) for i in range(2)]
        qkT = sb(nc, es, "qkT", [128, 16, 512], BF16)
        vst = [sb(nc, es, "vst%d" % i, [128, 12, 4, 72], BF16) for i in range(2)]
        psT = [ps(nc, es, "psT%d" % i, [128, D], BF16) for i in range(2)]
        pq = [ps(nc, es, "pq%d" % i, [128, 512], F32) for i in range(4)]
        psQ = [ps(nc, es, "psQ%d" % i, [128, 8, 128], BF16) for i in range(2)]
        cx = [P.chan() for _ in range(2)]
        cg = P.chan()
        cq = P.chan()
        cv = [P.chan() for _ in range(2)]
        cf = P.chan()

        P.add("sp", lambda e: e.dma_start(out=gB[:, :], in_=T["mix_norm"].partition_broadcast(128)), writes=["gB"], chan=cg)
        for i, nm in enumerate(("q_norm_nsa", "k_norm_slc", "k_norm_win", "q_norm_fox", "k_norm_fox")):
            P.add("sp", lambda e, i=i, nm=nm: e.dma_start(out=g5[:, i, :], in_=T[nm].partition_broadcast(128)),
                  writes=[("g5", i)], chan=cg)
        P.add("sp", lambda e: e.dma_start(out=bfg[:, :], in_=T["b_forget"].partition_broadcast(128)), writes=["bfg"], chan=cg)
        P.add("sp", lambda e: e.dma_start(out=cosT[:, :, :], in_=T["rope_cos"].rearrange("(t p) c -> p t c", p=128)),
              writes=["cosT"], chan=cg)
        P.add("sp", lambda e: e.dma_start(out=sinT[:, :, :], in_=T["rope_sin"].rearrange("(t p) c -> p t c", p=128)),
              writes=["sinT"], chan=cg)
        cg.seal()
        for (i, h0, nh) in ((0, 0, 8), (1, 8, 2), (2, 10, 2), (3, 12, 8), (4, 20, 8)):
            P.add("dve", lambda e, i=i, h0=h0, nh=nh: e.tensor_copy(
                out=GQ[:, h0:h0 + nh, :], in_=g5[:, i, :].unsqueeze(1).to_broadcast([128, nh, 64])),
                reads=[("g5", i)], writes=[("GQ", i)])
        gqk = [("GQ", i) for i in range(5)]
        P.add("pool", lambda e: e.memset(identf[:, :], 0.0), writes=["identf"])
        P.add("pool", lambda e: asel(e, out=identf[:, :], in_=identf[:, :], pattern=[[-1, 128]],
                                                compare_op=ALU.not_equal, fill=1.0, base=0, channel_multiplier=1),
              reads=["identf"], writes=["identf"])
        P.add("pool", lambda e: e.tensor_copy(out=ident[:, :], in_=identf[:, :]), reads=["identf"], writes=["ident"])
        P.add("pool", lambda e: e.memset(epst[:, :], EPS), writes=["eps"])
        wv = T["w_in"].rearrange("(c p) f -> p c f", p=128)
        for hf in range(2):
            for c in range(8):
                k = hf * 8 + c
                s = k % 3
                P.add("sp", lambda e, s=s, c=c, hf=hf: e.dma_start(out=stage[s][:, :], in_=wv[:, c, hf * HW_:(hf + 1) * HW_]),
                      writes=[("stage", s)], chan=stch[s])
                P.add("dve" if k % 2 == 0 else "pool", lambda e, s=s, c=c, hf=hf: e.tensor_copy(
                    out=win_s[:, c, hf * HW_:(hf + 1) * HW_], in_=stage[s][:, :]),
                    reads=[("stage", s)], writes=[("win", c, hf)])
        wkeys = [("win", c, hf) for c in range(8) for hf in range(2)]
        QKTv = T["QKT"].rearrange("(pr two) d s -> (two d) pr s", two=2)
        for i in range(2):
            P.add("pool", lambda e, i=i: e.memset(vst[i][:, :, :, 64:72], 1.0), writes=[("vst1", i)])
        CH = [(0, 512), (512, 512), (1024, 512), (1536, 512), (2048, 512), (2560, 288)]

        def stageA(t):
            xs = t % 2
            g, k = t // 4, t % 4
            P.add("sp", lambda e, xs=xs, t=t: e.dma_start(out=xn[xs][:, :], in_=h1[t * 128:(t + 1) * 128, :]),
                  writes=[("xn", xs)], chan=cx[xs])
            P.add("act", lambda e, xs=xs: e.activation(out=junk[:, :], in_=xn[xs][:, :], func=AF.Square,
                                                       accum_out=ssq[:, xs:xs + 1]),
                  reads=[("xn", xs)], writes=["junk", ("ssq", xs)])
            P.add("act", lambda e, xs=xs: e.activation(out=rs[:, xs:xs + 1], in_=ssq[:, xs:xs + 1], func=AF.Sqrt,
                                                       scale=1.0 / D, bias=epst[:, 0:1]),
                  reads=[("ssq", xs), "eps"], writes=[("rs", xs)])
            P.add("dve", lambda e, xs=xs: e.reciprocal(out=rs[:, xs:xs + 1], in_=rs[:, xs:xs + 1]),
                  reads=[("rs", xs)], writes=[("rs", xs)])
            P.add("dve", lambda e, xs=xs: e.scalar_tensor_tensor(
                out=nb[xs][:, :], in0=xn[xs][:, :], scalar=rs[:, xs:xs + 1], in1=gB[:, :], op0=ALU.mult, op1=ALU.mult),
                reads=[("xn", xs), ("rs", xs), "gB"], writes=[("nb", xs)])
            for c in range(8):
                P.add("pe", lambda e, xs=xs, c=c: e.transpose(
                    out=psT[xs][:, c * 128:(c + 1) * 128], in_=nb[xs][:, c * 128:(c + 1) * 128], identity=ident[:, :]),
                    reads=[("nb", xs), "ident"], writes=[("psT", xs)] if c == 0 else [])
            P.last_w[("psT", xs)] = P.ops["pe"][-1]
            P.add("act", lambda e, xs=xs: e.copy(out=aT[xs][:, :, :], in_=psT[xs][:, :].rearrange("p (c t) -> p c t", c=8)),
                  reads=[("psT", xs)], writes=[("aT", xs)])
            for ci, (c0, cw) in enumerate(CH):
                pb = (t * 6 + ci) % 4
                for c in range(8):
                    P.add("pe", lambda e, pb=pb, c=c, c0=c0, cw=cw, xs=xs: e.matmul(
                        out=pq[pb][:, 0:cw], lhsT=aT[xs][:, c, :], rhs=win_s[:, c, c0:c0 + cw],
                        start=(c == 0), stop=(c == 7)),
                        reads=[("aT", xs)] + (wkeys if t == 0 else []), writes=[("pq", pb)] if c == 0 else [])
                P.last_w[("pq", pb)] = P.ops["pe"][-1]
                if ci < 4:
                    P.add("act", lambda e, pb=pb, ci=ci, xs=xs: e.copy(
                        out=qk[xs][:, ci * 8:(ci + 1) * 8, :], in_=pq[pb][:, :].rearrange("p (h d) -> p h d", h=8)),
                        reads=[("pq", pb)], writes=[("qk", xs, ci)])
                    P.add("act", lambda e, pb=pb, ci=ci, xs=xs: e.activation(
                        out=sq[xs][:, ci * 8:(ci + 1) * 8, :], in_=pq[pb][:, :].rearrange("p (h d) -> p h d", h=8), func=AF.Square),
                        reads=[("pq", pb)], writes=[("sq", xs, ci)])
                elif ci == 4:
                    P.add("dve", lambda e, pb=pb, g=g, k=k: e.tensor_copy(
                        out=vst[g % 2][:, 0:8, k, 0:64], in_=pq[pb][:, 0:512].rearrange("p (h d) -> p h d", h=8)),
                        reads=[("pq", pb), ("vst1", g % 2)], writes=[("vst", g % 2, k, 0)])
                else:
                    P.add("dve", lambda e, pb=pb, g=g, k=k: e.tensor_copy(
                        out=vst[g % 2][:, 8:12, k, 0:64], in_=pq[pb][:, 0:256].rearrange("p (h d) -> p h d", h=4)),
                        reads=[("pq", pb), ("vst1", g % 2)], writes=[("vst", g % 2, k, 1)])
                    P.add("dve", lambda e, pb=pb, t=t: e.tensor_copy(out=Gall[:, t, :], in_=pq[pb][:, 256:280]),
                          reads=[("pq", pb)], writes=[("Gall", t)])
                    P.add("dve", lambda e, pb=pb, t=t: e.tensor_tensor(out=LFall[:, t, :], in0=pq[pb][:, 280:288], in1=bfg[:, :],
                                                                      op=ALU.add),
                          reads=[("pq", pb), "bfg"], writes=[("LFall", t)])
            if k == 3:
                P.add("sp", lambda e, g=g: e.dma_start(
                    out=T["V"].rearrange("h p t c -> p h (t c)")[:, :, 4 * g * 72:(4 * g + 4) * 72],
                    in_=vst[g % 2][:, :, :, :].rearrange("p h t c -> p h (t c)")),
                    reads=[("vst", g % 2, kk, j) for kk in range(4) for j in range(2)], chan=cv[g % 2])

        def stageB(t):
            xs = t % 2
            g, k = t // 4, t % 4
            P.add("dve", lambda e, xs=xs: e.tensor_reduce(out=hs[xs][:, :], in_=sq[xs][:, :, :], axis=AX.X, op=ALU.add),
                  reads=[("sq", xs, i) for i in range(4)], writes=[("hs", xs)])
            P.add("act", lambda e, xs=xs: e.activation(out=hs[xs][:, :], in_=hs[xs][:, :], func=AF.Sqrt,
                                                       scale=1.0 / 64, bias=epst[:, 0:1]),
                  reads=[("hs", xs), "eps"], writes=[("hs", xs)])
            P.add("dve", lambda e, xs=xs: e.reciprocal(out=hs[xs][:, :], in_=hs[xs][:, :]),
                  reads=[("hs", xs)], writes=[("hs", xs)])
            qkk = [("qk", xs, i) for i in range(4)]
            P.add("dve", lambda e, xs=xs: e.tensor_tensor(
                out=qk[xs][:, 2:30, :], in0=qk[xs][:, 2:30, :], in1=hs[xs][:, 2:30].unsqueeze(2).to_broadcast([128, 28, 64]),
                op=ALU.mult), reads=qkk + [("hs", xs)], writes=qkk)
            P.add("dve", lambda e, xs=xs: e.tensor_tensor(
                out=qk[xs][:, 2:30, :], in0=qk[xs][:, 2:30, :], in1=GQ[:, :, :], op=ALU.mult),
                reads=qkk + gqk, writes=qkk)
            cb = lambda tab, t=t: tab[:, t, :].unsqueeze(1).to_broadcast([128, 14, 8])
            x1 = lambda xs=xs: qk[xs][:, 0:14, 0:8]
            x2 = lambda xs=xs: qk[xs][:, 0:14, 8:16]
            for j, (src, tab) in enumerate(((x1, cosT), (x2, sinT), (x2, cosT), (x1, sinT))):
                P.add("pool", lambda e, j=j, src=src, tab=tab, cb=cb: e.tensor_tensor(
                    out=rt[j][:, :, :], in0=src(), in1=cb(tab), op=ALU.mult),
                    reads=qkk + ["cosT", "sinT"], writes=[("rt", j)])
            P.add("pool", lambda e, x1=x1: e.tensor_tensor(out=x1(), in0=rt[0][:, :, :], in1=rt[1][:, :, :], op=ALU.subtract),
                  reads=[("rt", 0), ("rt", 1)], writes=qkk)
            P.add("pool", lambda e, x2=x2: e.tensor_tensor(out=x2(), in0=rt[2][:, :, :], in1=rt[3][:, :, :], op=ALU.add),
                  reads=[("rt", 2), ("rt", 3)], writes=qkk)
            P.add("pool", lambda e, xs=xs: e.tensor_copy(out=qkb[xs][:, :], in_=qk[xs][:, :, :].rearrange("p h d -> p (h d)")),
                  reads=qkk, writes=[("qkb", xs)])
            for pr in range(16):
                hb = pr // 8
                P.add("pe", lambda e, pr=pr, hb=hb, xs=xs: e.transpose(
                    out=psQ[hb][:, pr % 8, :], in_=qkb[xs][:, pr * 128:(pr + 1) * 128], identity=ident[:, :]),
                    reads=[("qkb", xs), "ident"], writes=[("psQ", hb)] if pr % 8 == 0 else [])
                if pr % 8 == 7:
                    P.last_w[("psQ", hb)] = P.ops["pe"][-1]
                    P.add("act" if hb == 0 else "dve", (lambda e, hb=hb, k=k: e.copy(
                        out=qkT[:, hb * 8:(hb + 1) * 8, k * 128:(k + 1) * 128], in_=psQ[hb][:, :, :])) if hb == 0 else
                        (lambda e, hb=hb, k=k: e.tensor_copy(
                            out=qkT[:, hb * 8:(hb + 1) * 8, k * 128:(k + 1) * 128], in_=psQ[hb][:, :, :])),
                        reads=[("psQ", hb)], writes=[("qkT", k, hb)])
            if k == 3:
                P.add("sp", lambda e, g=g: e.dma_start(out=QKTv[:, :, g * 512:(g + 1) * 512], in_=qkT[:, :, :]),
                      reads=[("qkT", kk, hb) for kk in range(4) for hb in range(2)], chan=cq)

        stageA(0)
        for t in range(NT):
            if t + 1 < NT:
                stageA(t + 1)
            stageB(t)

        P.add("act", lambda e: e.activation(out=Gall[:, :, :], in_=Gall[:, :, :], func=AF.Sigmoid),
              reads=[("Gall", t) for t in range(NT)], writes=["GallF"])
        P.add("sp", lambda e: e.dma_start(out=T["G"], in_=Gall[:, :, :].rearrange("p t c -> p (t c)")),
              reads=["GallF"], chan=cf)
        P.add("act", lambda e: e.activation(out=LFall[:, :, :], in_=LFall[:, :, :], func=AF.Exp, scale=-1.0),
              reads=[("LFall", t) for t in range(NT)], writes=["LF1"])
        P.add("act", lambda e: e.activation(out=LFall[:, :, :], in_=LFall[:, :, :], func=AF.Ln, bias=1.0),
              reads=["LF1"], writes=["LF2"])
        P.add("dve", lambda e: e.tensor_scalar(out=LFall[:, :, :], in0=LFall[:, :, :], scalar1=-1.0, scalar2=None, op0=ALU.mult),
              reads=["LF2"], writes=["LF3"])
        P.add("sp", lambda e: e.dma_start(out=T["LF"], in_=LFall[:, :, :].rearrange("p t c -> p (t c)")),
              reads=["LF3"], chan=cf)

    run_phase(nc, build)


_FILL = {}


def asel(e, **kw):
    v = float(kw.pop("fill"))
    r = _FILL.get(v)
    if r is None:
        r = e.alloc_register()
        e.reg_mov(r, v)
        _FILL[v] = r
    return e.affine_select(fill=r, **kw)


def make_ident(P, nc, es):
    ident = sb(nc, es, "ident", [128, 128], BF16)
    identf = sb(nc, es, "identf", [128, 128], F32)
    P.add("pool", lambda e: e.memset(identf[:, :], 0.0), writes=["identf"])
    P.add("pool", lambda e: asel(e, out=identf[:, :], in_=identf[:, :], pattern=[[-1, 128]],
                                            compare_op=ALU.not_equal, fill=1.0, base=0, channel_multiplier=1),
          reads=["identf"], writes=["identf"])
    P.add("pool", lambda e: e.tensor_copy(out=ident[:, :], in_=identf[:, :]), reads=["identf"], writes=["ident"])
    return ident, identf


def cmp_phase(nc, T):
    def build(P, es):
        ident, identf = make_ident(P, nc, es)
        epst = sb(nc, es, "epst", [128, 1], F32)
        P.add("pool", lambda e: e.memset(epst[:, :], EPS), writes=["eps"])
        tok = sb(nc, es, "tok", [64, 4, S], BF16)
        w1s = [sb(nc, es, "w1s%d" % i, [64, 32, 256], BF16) for i in range(2)]
        stg = [sb(nc, es, "cstg%d" % i, [64, 32, 256], F32) for i in range(2)]
        w1f = [sb(nc, es, "w1f%d" % i, [128, 16, 256], F32) for i in range(2)]
        posr = sb(nc, es, "posr", [16, 2, 128], F32)
        posc = sb(nc, es, "posc", [128, 2, 16], F32)
        w2f = sb(nc, es, "w2f", [128, 2, 2, 64], F32)
        w2s = sb(nc, es, "w2s", [128, 2, 2, 64], BF16)
        biasT = sb(nc, es, "biasT", [128, 4], F32)
        gk = sb(nc, es, "gk", [128, 64], F32)
        hidT = [sb(nc, es, "hidT%d" % i, [128, 2, 256], BF16) for i in range(2)]
        ssq = sb(nc, es, "ssq", [128, 4], F32)
        junk = sb(nc, es, "junk", [128, 64], F32)
        kcb = [sb(nc, es, "kcb%d" % i, [128, 64], BF16) for i in range(2)]
        kcT = [sb(nc, es, "kcT%d" % i, [64, 256], BF16) for i in range(2)]
        vce = [sb(nc, es, "vce%d" % i, [128, 2, 65], BF16) for i in range(2)]
        psHf = [ps(nc, es, "psH%d" % i, [128, 512], F32) for i in range(2)]
        psH = [t[:, 0:256] for t in psHf]
        psOf = [ps(nc, es, "psO%d" % i, [128, 512], F32) for i in range(2)]
        psO = [t[:, 0:64] for t in psOf]
        psBf = ps(nc, es, "psB", [128, 512], F32)
        psB = psBf[:, 0:4]
        psPf = ps(nc, es, "psP", [128, 512], F32)
        psP = psPf[:, 0:32].rearrange("p (a b) -> p a b", a=2)
        psKf = ps(nc, es, "psK", [128, 1024], BF16)
        psK = psKf[0:64, 0:128]
        c0 = P.chan()
        c1 = [P.chan() for _ in range(2)]
        co = P.chan()

        for j, h in enumerate((0, 1, 30, 31)):
            P.add("sp", lambda e, j=j, h=h: e.dma_start(out=tok[:, j, :], in_=T["QKT"][h, :, :]), writes=[("tok", j)], chan=c0)
        P.add("sp", lambda e: e.dma_start(out=gk[:, :], in_=T["k_norm_cmp"].partition_broadcast(128)), writes=["gk"], chan=c0)
        for kv, nm in enumerate(("cmp_pos_k", "cmp_pos_v")):
            P.add("sp", lambda e, kv=kv, nm=nm: e.dma_start(
                out=posr[:, kv, :], in_=T[nm].rearrange("(c a) d -> c (a d)", a=2)), writes=[("posr", kv)], chan=c0)
        for kv, nm in enumerate(("cmp_k_w2", "cmp_v_w2")):
            P.add("sp", lambda e, kv=kv, nm=nm: e.dma_start(
                out=w2f[:, kv, :, :], in_=T[nm].rearrange("(c p) d -> p c d", p=128)), writes=[("w2f", kv)], chan=c0)
        for kv, nm in enumerate(("cmp_k_w1", "cmp_v_w1")):
            P.add("sp", lambda e, kv=kv, nm=nm: e.dma_start(
                out=w1f[kv][:, :, :], in_=T[nm].rearrange("(c p) h -> p c h", p=128)), writes=[("w1f", kv)], chan=c0)
        c0.seal()
        for kv, nm in enumerate(("cmp_k_w1", "cmp_v_w1")):
            P.add("sp", lambda e, kv=kv, nm=nm: e.dma_start(
                out=stg[kv][:, :, :], in_=T[nm].rearrange("(l d) h -> d l h", d=64)), writes=[("stg", kv)], chan=c1[kv])
            P.add("dve" if kv == 0 else "pool", lambda e, kv=kv: e.tensor_copy(out=w1s[kv][:, :, :], in_=stg[kv][:, :, :]),
                  reads=[("stg", kv)], writes=[("w1s", kv)])
        P.add("dve", lambda e: e.tensor_copy(out=w2s[:, :, :, :], in_=w2f[:, :, :, :]),
              reads=[("w2f", 0), ("w2f", 1)], writes=["w2s"])
        for kv in range(2):
            P.add("pe", lambda e, kv=kv: e.transpose(out=psP[:, kv, :], in_=posr[:, kv, :], identity=identf[0:16, 0:16]),
                  reads=[("posr", kv), "identf"], writes=[("psP", kv)])
        P.add("dve", lambda e: e.tensor_copy(out=posc[:, :, :], in_=psP),
              reads=[("psP", 0), ("psP", 1)], writes=["posc"])
        for kv in range(2):
            for hc in range(2):
                for c in range(16):
                    P.add("pe", lambda e, kv=kv, hc=hc, c=c: e.matmul(
                        out=psB[:, kv * 2 + hc:kv * 2 + hc + 1], lhsT=w1f[kv][:, c, hc * 128:(hc + 1) * 128],
                        rhs=posc[:, kv, c:c + 1], start=(c == 0), stop=(c == 15)),
                        reads=[("w1f", kv), "posc"], writes=["psB"] if (c == 0 and kv == 0 and hc == 0) else [])
        P.last_w["psB"] = P.ops["pe"][-1]
        P.add("dve", lambda e: e.tensor_copy(out=biasT[:, :], in_=psB), reads=["psB"], writes=["biasT"])
        for i in range(2):
            P.add("pool", lambda e, i=i: e.memset(kcb[i][:, :], 0.0), writes=[("kcb", i)])
            P.add("pool", lambda e, i=i: e.memset(vce[i][:, :, :], 0.0), writes=[("vce", i)])
            P.add("pool", lambda e, i=i: e.memset(vce[i][:, :, 64:65], 1.0), reads=[("vce", i)], writes=[("vce", i)])
            P.add("pool", lambda e, i=i: e.memset(hidT[i][:, :, :], 0.0), writes=[("hidT", i, 0), ("hidT", i, 1)])
        VCv = T["VC"].rearrange("h (c p) e -> h p c e", p=128)
        it = 0
        for kv in range(2):
            for head in range(2):
                sl = it % 2
                it += 1
                tv = tok[:, kv * 2 + head, :].rearrange("p (n r) -> p n r", r=16)
                for hc in range(2):
                    for l in range(32):
                        q, r = l // 16, l % 16
                        P.add("pe", lambda e, kv=kv, hc=hc, l=l, q=q, r=r, tv=tv: e.matmul(
                            out=psH[hc][:, 0:255], lhsT=w1s[kv][:, l, hc * 128:(hc + 1) * 128], rhs=tv[:, q:q + 255, r],
                            start=(l == 0), stop=(l == 31)),
                            reads=[("tok", kv * 2 + head), ("w1s", kv)], writes=[("psH", hc)] if l == 0 else [])
                    P.last_w[("psH", hc)] = P.ops["pe"][-1]
                    P.add("act", lambda e, kv=kv, hc=hc, sl=sl: e.activation(
                        out=hidT[sl][:, hc, 0:255], in_=psH[hc][:, 0:255], func=AF.Silu,
                        bias=biasT[:, kv * 2 + hc:kv * 2 + hc + 1]),
                        reads=[("psH", hc), "biasT"], writes=[("hidT", sl, hc)])
                for ci, (n0, nn) in enumerate(((0, 128), (128, 127))):
                    for hc in range(2):
                        P.add("pe", lambda e, kv=kv, hc=hc, sl=sl, ci=ci, n0=n0, nn=nn: e.matmul(
                            out=psO[ci][0:nn, :], lhsT=hidT[sl][:, hc, n0:n0 + nn], rhs=w2s[:, kv, hc, :],
                            start=(hc == 0), stop=(hc == 1)),
                            reads=[("hidT", sl, 0), ("hidT", sl, 1), "w2s"], writes=[("psO", ci)] if hc == 0 else [])
                    P.last_w[("psO", ci)] = P.ops["pe"][-1]
                    if kv == 0:
                        col = head * 2 + ci
                        P.add("act", lambda e, ci=ci, nn=nn, col=col: e.activation(
                            out=junk[0:nn, :], in_=psO[ci][0:nn, :], func=AF.Square, accum_out=ssq[0:nn, col:col + 1]),
                            reads=[("psO", ci)], writes=["junk", ("ssq", col)])
                        P.add("act", lambda e, nn=nn, col=col: e.activation(
                            out=ssq[0:nn, col:col + 1], in_=ssq[0:nn, col:col + 1], func=AF.Sqrt, scale=1.0 / 64,
                            bias=epst[0:nn, 0:1]), reads=[("ssq", col), "eps"], writes=[("ssq", col)])
                        P.add("dve", lambda e, nn=nn, col=col: e.reciprocal(out=ssq[0:nn, col:col + 1], in_=ssq[0:nn, col:col + 1]),
                              reads=[("ssq", col)], writes=[("ssq", col)])
                        P.add("dve", lambda e, ci=ci, nn=nn, col=col: e.scalar_tensor_tensor(
                            out=kcb[ci][0:nn, :], in0=psO[ci][0:nn, :], scalar=ssq[0:nn, col:col + 1], in1=gk[0:nn, :],
                            op0=ALU.mult, op1=ALU.mult), reads=[("psO", ci), ("ssq", col), "gk"], writes=[("kcb", ci)])
                        P.add("pe", lambda e, ci=ci: e.transpose(out=psK, in_=kcb[ci][:, :], identity=ident[:, :]),
                              reads=[("kcb", ci), "ident"], writes=["psK"])
                        P.add("act", lambda e, head=head, n0=n0: e.copy(out=kcT[head][:, n0:n0 + 128], in_=psK),
                              reads=["psK"], writes=[("kcT", head, n0)])
                    else:
                        P.add("dve", lambda e, ci=ci, nn=nn, head=head: e.tensor_copy(
                            out=vce[head][0:nn, ci, 0:64], in_=psO[ci][0:nn, :]), reads=[("psO", ci)], writes=[("vce", head)])
                if kv == 0:
                    P.add("sp", lambda e, head=head: e.dma_start(out=T["KCT"][head, :, :], in_=kcT[head][:, :]),
                          reads=[("kcT", head, 0), ("kcT", head, 128)], chan=co)
                else:
                    P.add("sp", lambda e, head=head: e.dma_start(out=VCv[head], in_=vce[head][:, :, :]),
                          reads=[("vce", head)], chan=co)

    run_phase(nc, build)


class UnitPipe:
    def __init__(self, P, psS, PT, depth=2):
        self.P, self.psS, self.PT, self.depth = P, psS, PT, depth
        self.q = []
        self.u = 0

    def push(self, lhsT, rhs, vlhsT, pacc, acc_key, first, last, mask, kdeps, bias=None, bkeys=(), post=None):
        P = self.P
        u = self.u
        self.u += 1
        sb_, pb = u % len(self.psS), u % len(self.PT)
        psS, PT = self.psS[sb_], self.PT[pb]
        P.add("pe", lambda e: e.matmul(out=psS[:, :], lhsT=lhsT, rhs=rhs, start=True, stop=True),
              reads=kdeps, writes=[("psS", sb_)])
        if bias is None:
            P.add("act", lambda e: e.activation(out=PT[:, :], in_=psS[:, :], func=AF.Exp, scale=0.125),
                  reads=[("psS", sb_)], writes=[("PT", pb)])
        else:
            P.add("act", lambda e: e.activation(out=PT[:, :], in_=psS[:, :], func=AF.Exp, scale=0.125, bias=bias),
                  reads=[("psS", sb_)] + list(bkeys), writes=[("PT", pb)])
        if mask is not None:
            base, cm, step = mask
            P.add("pool", lambda e: asel(e, out=PT[:, :], in_=PT[:, :], pattern=[[step, 512]], compare_op=ALU.is_ge,
                                         fill=0.0, base=base, channel_multiplier=cm), reads=[("PT", pb)], writes=[("PT", pb)])
        self.q.append((PT, pb, vlhsT, pacc, acc_key, first, last, kdeps, post))
        if len(self.q) > self.depth:
            self._pv()

    def _pv(self):
        P = self.P
        PT, pb, vlhsT, pacc, acc_key, first, last, kdeps, post = self.q.pop(0)
        P.add("pe", lambda e: e.matmul(out=pacc[0:65, :], lhsT=vlhsT, rhs=PT[:, :], start=first, stop=last),
              reads=[("PT", pb)] + list(kdeps), writes=[acc_key] if first else [])
        if last:
            P.last_w[acc_key] = P.ops["pe"][-1]
            if post is not None:
                post()

    def flush(self):
        while self.q:
            self._pv()


def nsa_phase(nc, T):
    BIG = 2048.0
    TINY = 1e-30

    def build(P, es):
        ident, identf = make_ident(P, nc, es)
        QB = sb(nc, es, "QB", [128, 4, S], BF16)
        KE = sb(nc, es, "KE", [128, S], BF16)
        KW = sb(nc, es, "KW", [128, S], BF16)
        KC = sb(nc, es, "KC", [128, 256], BF16)
        Vs = sb(nc, es, "Vs", [128, NT, 72], BF16)
        Vw = sb(nc, es, "Vw", [128, NT, 72], BF16)
        VCs = sb(nc, es, "VCs", [128, 2, 72], BF16)
        OVf = sb(nc, es, "OVf", [128, 2, 72], F32)
        OV = sb(nc, es, "OV", [128, 2, 72], BF16)
        Gs = sb(nc, es, "Gs", [128, NT, 24], F32)
        ET = [[sb(nc, es, "ET%d_%d" % (i, j), [128, 512], BF16) for j in range(2)] for i in range(2)]
        PT = [sb(nc, es, "PT%d" % i, [128, 512], BF16) for i in range(4)]
        OCs = sb(nc, es, "OCs", [65, 4, 512], F32)
        OWs = sb(nc, es, "OWs", [65, 4, 512], F32)
        OSs = [sb(nc, es, "OSs%d" % i, [65, 512], F32) for i in range(2)]
        imp = sb(nc, es, "imp", [128, 4, 64], F32)
        impt = sb(nc, es, "impt", [128, 4, 64], F32)
        impm = [sb(nc, es, "impm%d" % i, [128, 64], F32) for i in range(2)]
        rd4 = sb(nc, es, "rd4", [128, 4], F32)
        m1 = sb(nc, es, "m1", [128, 8], F32)
        m2 = sb(nc, es, "m2", [128, 8], F32)
        tmp = sb(nc, es, "tmp", [128, 64], F32)
        thr = sb(nc, es, "thr", [128, 1], F32)
        BN = [sb(nc, es, "BN%d" % i, [128, 128], BF16) for i in range(4)]
        dn = [sb(nc, es, "dn%d" % i, [128, 3], F32) for i in range(2)]
        ost = [sb(nc, es, "ost%d" % i, [128, 4, 256], F32) for i in range(2)]
        psS = [ps(nc, es, "psS%d" % i, [128, 512], F32) for i in range(3)]
        psOC = ps(nc, es, "psOC", [128, 512], F32)
        psOS = ps(nc, es, "psOS", [128, 512], F32)
        psOW = ps(nc, es, "psOW", [128, 512], F32)
        psIB = ps(nc, es, "psIB", [128, 512], F32)
        psI = psIB[:, 0:260].rearrange("p (a b) -> p a b", a=4)
        psBT = psIB[:, 320:384].bitcast(BF16)
        psFb = ps(nc, es, "psFb", [128, 512], F32)
        psF = psFb[:, 0:195].rearrange("p (a b) -> p a b", a=3)
        c0 = P.chan()
        cks = [P.chan() for _ in range(2)]
        cst = [P.chan() for _ in range(2)]

        P.add("sp", lambda e: e.dma_start(out=Gs[:, :, :].rearrange("p t c -> p (t c)"), in_=T["G"]), writes=["Gs"], chan=c0)
        P.add("pool", lambda e: e.memset(KE[64:128, :], BIG), writes=["KEm"])
        P.add("pool", lambda e: asel(e, out=KE[64:128, :], in_=KE[64:128, :], pattern=[[1, S]], compare_op=ALU.is_ge,
                                                fill=0.0, base=0, channel_multiplier=-64), reads=["KEm"], writes=["KEm"])
        P.add("pool", lambda e: asel(e, out=KE[64:128, :], in_=KE[64:128, :], pattern=[[-1, S]], compare_op=ALU.is_ge,
                                                fill=0.0, base=63, channel_multiplier=64), reads=["KEm"], writes=["KEm"])
        P.add("pool", lambda e: e.memset(KW[64:128, :], 0.0), writes=["KW0"])
        P.add("pool", lambda e: e.memset(KC[64:128, :], 0.0), writes=["KC0"])
        for g in range(4):
            P.add("pool", lambda e, g=g: e.memset(QB[64:128, g, :], 0.0), writes=[("QB0", g)])
        P.add("pool", lambda e: e.memset(OVf[:, :, :], 1.0), writes=["OVf"])
        for nt in range(2):
            P.add("pool", lambda e, nt=nt: asel(e,
                out=OVf[:, nt, 0:64], in_=OVf[:, nt, 0:64], pattern=[[64, 64]], compare_op=ALU.is_ge, fill=0.0,
                base=63 - 2048 * nt, channel_multiplier=-16), reads=["OVf"], writes=["OVf"])
            P.add("pool", lambda e, nt=nt: asel(e,
                out=OVf[:, nt, 0:64], in_=OVf[:, nt, 0:64], pattern=[[-64, 64]], compare_op=ALU.is_ge, fill=0.0,
                base=2048 * nt + 31, channel_multiplier=16), reads=["OVf"], writes=["OVf"])
        P.add("pool", lambda e: e.tensor_copy(out=OV[:, :, :], in_=OVf[:, :, :]), reads=["OVf"], writes=["OV"])
        for i in range(4):
            P.add("pool", lambda e, i=i: e.memset(BN[i][:, 0:64], 0.0), writes=[("BN0", i)])
        OAv = T["OA"].rearrange("(t p) c -> p t c", p=128)

        def mask_ge(tile, base, cm, step):
            return lambda e: asel(e, out=tile[:, :], in_=tile[:, :], pattern=[[step, 512]], compare_op=ALU.is_ge,
                                             fill=0.0, base=base, channel_multiplier=cm)

        pipe = UnitPipe(P, psS, PT, depth=2)

        for kh in range(DBG['kh']):
            ck = cks[kh]
            for g in range(4):
                P.add("sp", lambda e, g=g, kh=kh: e.dma_start(out=QB[0:64, g, :], in_=T["QKT"][2 + 4 * kh + g, :, :]),
                      writes=[("QBq", g)], chan=ck)
            P.add("sp", lambda e, kh=kh: e.dma_start(out=KE[0:64, :], in_=T["QKT"][10 + kh, :, :]), writes=["KEk"], chan=ck)
            P.add("sp", lambda e, kh=kh: e.dma_start(out=KW[0:64, :], in_=T["QKT"][12 + kh, :, :]), writes=["KW"], chan=ck)
            P.add("sp", lambda e, kh=kh: e.dma_start(out=KC[0:64, :], in_=T["KCT"][kh, :, :]), writes=["KC"], chan=ck)
            P.add("sp", lambda e, kh=kh: e.dma_start(out=Vs[:, :, :], in_=T["V"][kh]), writes=["Vs"], chan=ck)
            P.add("sp", lambda e, kh=kh: e.dma_start(out=Vw[:, :, :], in_=T["V"][2 + kh]), writes=["Vw"], chan=ck)
            P.add("sp", lambda e, kh=kh: e.dma_start(out=VCs[:, :, 0:65], in_=T["VC"][kh].rearrange("(c p) e -> p c e", p=128)),
                  writes=["VCs"], chan=ck)
            ck.seal()
            for i in range(DBG['ng']):
                qsl = slice(i * 512, (i + 1) * 512)
                nts = [0] if i < 4 else [0, 1]

                def s1(g):
                    for nt in nts:
                        u = pipe.u
                        pipe.u += 1
                        sb_ = u % 3
                        et = ET[g % 2][nt]
                        P.add("pe", lambda e, nt=nt, g=g, sb_=sb_, qsl=qsl: e.matmul(
                            out=psS[sb_][:, :], lhsT=KC[:, nt * 128:(nt + 1) * 128], rhs=QB[:, g, qsl], start=True, stop=True),
                            reads=["KC", "KC0", ("QBq", g), ("QB0", g)] + ([("QBm", tt) for tt in range(4)] if i > 0 or kh > 0 else []), writes=[("psS", sb_)])
                        P.add("act", lambda e, et=et, sb_=sb_: e.activation(out=et[:, :], in_=psS[sb_][:, :], func=AF.Exp, scale=0.125),
                              reads=[("psS", sb_)], writes=[("ET", g % 2, nt)])
                        P.add("pool", mask_ge(et, 512 * i - 2048 * nt - 31, -16, 1), reads=[("ET", g % 2, nt)], writes=[("ET", g % 2, nt)])

                def s2(g):
                    for j, nt in enumerate(nts):
                        P.add("pe", lambda e, nt=nt, j=j, g=g: e.matmul(
                            out=psOC[0:65, :], lhsT=VCs[:, nt, 0:65], rhs=ET[g % 2][nt][:, :], start=(j == 0), stop=(j == len(nts) - 1)),
                            reads=[("ET", g % 2, nt), "VCs"], writes=["psOC"] if j == 0 else [])
                    P.last_w["psOC"] = P.ops["pe"][-1]
                    P.add("act", lambda e, g=g: e.copy(out=OCs[:, g, :], in_=psOC[0:65, :]), reads=["psOC"], writes=[("OCs", g)])
                    for tt in range(4):
                        for j, nt in enumerate(nts):
                            P.add("pe", lambda e, nt=nt, j=j, tt=tt, g=g: e.matmul(
                                out=psI[:, tt, :], lhsT=ET[g % 2][nt][:, tt * 128:(tt + 1) * 128], rhs=OV[:, nt, 0:65],
                                start=(j == 0), stop=(j == len(nts) - 1)),
                                reads=[("ET", g % 2, nt), "OV"], writes=["psI"] if (j == 0 and tt == 0) else [])
                    P.last_w["psI"] = P.ops["pe"][-1]
                    P.add("dve", lambda e: e.tensor_scalar(out=rd4[:, :], in0=psI[:, :, 64], scalar1=TINY, scalar2=None, op0=ALU.max),
                          reads=["psI"], writes=["rd4"])
                    P.add("dve", lambda e: e.reciprocal(out=rd4[:, :], in_=rd4[:, :]), reads=["rd4"], writes=["rd4"])
                    if g == 0:
                        P.add("dve", lambda e: e.tensor_tensor(
                            out=imp[:, :, :], in0=psI[:, :, 0:64], in1=rd4[:, :].unsqueeze(2).to_broadcast([128, 4, 64]), op=ALU.mult),
                            reads=["psI", "rd4"], writes=["imp"])
                    else:
                        P.add("dve", lambda e: e.tensor_tensor(
                            out=impt[:, :, :], in0=psI[:, :, 0:64], in1=rd4[:, :].unsqueeze(2).to_broadcast([128, 4, 64]), op=ALU.mult),
                            reads=["psI", "rd4"], writes=["impt"])
                        P.add("dve", lambda e: e.tensor_tensor(out=imp[:, :, :], in0=imp[:, :, :], in1=impt[:, :, :], op=ALU.add),
                              reads=["imp", "impt"], writes=["imp"])

                s1(0)
                for g in range(4):
                    if g < 3:
                        s1(g + 1)
                    s2(g)
                for tt in range(4):
                    bs = tt % 2
                    t0 = 512 * i + 128 * tt
                    P.add("pool", lambda e, tt=tt, bs=bs, t0=t0: asel(e,
                        out=impm[bs][:, :], in_=imp[:, tt, :], pattern=[[-64, 64]], compare_op=ALU.is_ge, fill=1.0e6,
                        base=t0 - 128, channel_multiplier=1), reads=["imp"], writes=[("impm", bs)])
                    P.add("pool", lambda e, bs=bs, t0=t0: asel(e,
                        out=impm[bs][:, :], in_=impm[bs][:, :], pattern=[[-64, 64]], compare_op=ALU.is_ge, fill=-1.0,
                        base=t0, channel_multiplier=1), reads=[("impm", bs)], writes=[("impm", bs)])
                    P.add("pool", lambda e, bs=bs: e.memset(impm[bs][:, 0:1], 1.0e6), reads=[("impm", bs)], writes=[("impm", bs)])
                    P.add("dve", lambda e, bs=bs: e.max(out=m1[:, :], in_=impm[bs][:, :]), reads=[("impm", bs)], writes=["m1"])
                    P.add("dve", lambda e, bs=bs: e.match_replace(out=tmp[:, :], in_to_replace=m1[:, :], in_values=impm[bs][:, :],
                                                                  imm_value=-2.0), reads=[("impm", bs), "m1"], writes=["tmp"])
                    P.add("dve", lambda e: e.max(out=m2[:, :], in_=tmp[:, :]), reads=["tmp"], writes=["m2"])
                    P.add("dve", lambda e: e.tensor_scalar(out=thr[:, :], in0=m2[:, 7:8], scalar1=0.0, scalar2=None, op0=ALU.max),
                          reads=["m2"], writes=["thr"])
                    P.add("dve", lambda e, bs=bs, tt=tt: e.tensor_scalar(
                        out=BN[tt][:, 64:128], in0=impm[bs][:, :], scalar1=thr[:, 0:1], scalar2=1.0, op0=ALU.is_ge, op1=ALU.subtract),
                        reads=[("impm", bs), "thr", ("BN0", tt)], writes=[("BN", tt)])
                for g in range(4):
                    kts = list(range(max(0, 4 * i - 4), 4 * i + 4))
                    for j, kt in enumerate(kts):
                        if kt >= 4 * i:
                            ms = (-128 * (kt - 4 * i), -1, 1)
                        else:
                            ms = (128 * (kt - 4 * i + 4) - 1, 1, -1)
                        post = (lambda g=g: P.add("dve", lambda e: e.tensor_copy(out=OWs[:, g, :], in_=psOW[0:65, :]),
                                                  reads=["psOW"], writes=[("OWs", g)]))
                        pipe.push(KW[:, kt * 128:(kt + 1) * 128], QB[:, g, qsl], Vw[:, kt, 0:65], psOW, "psOW",
                                  j == 0, j == len(kts) - 1, ms, ["KW", "KW0", ("QBq", g), ("QB0", g), "Vw"], post=post)
                for tt in range(4):
                    t0 = 512 * i + 128 * tt
                    P.add("pe", lambda e, tt=tt: e.transpose(out=psBT, in_=BN[tt][:, :], identity=ident[:, :]),
                          reads=[("BN", tt), ("BN0", tt), "ident"], writes=["psBT"])
                    P.add("act", lambda e, t0=t0: e.copy(out=QB[64:128, :, t0:t0 + 128],
                                                         in_=psBT[64:128].unsqueeze(1).to_broadcast([64, 4, 128])),
                          reads=["psBT"] + [("QB0", g_) for g_ in range(4)], writes=[("QBm", tt)])

                def finalize(g):
                    osl = g % 2
                    hd = kh * 4 + g
                    for tt in range(4):
                        tile_i = 4 * i + tt
                        ds = tt % 2
                        tsl = slice(tt * 128, (tt + 1) * 128)
                        for b, (src, key) in enumerate(((OCs[:, g, tsl], ("OCs", g)), (OSs[osl][:, tsl], ("OSs", osl)),
                                                         (OWs[:, g, tsl], ("OWs", g)))):
                            P.add("pe", lambda e, b=b, src=src: e.transpose(out=psF[:, b, :], in_=src, identity=identf[0:65, 0:65]),
                                  reads=[key, "identf"], writes=["psF"] if b == 0 else [])
                        P.last_w["psF"] = P.ops["pe"][-1]
                        P.add("dve", lambda e, ds=ds: e.tensor_scalar(out=dn[ds][:, :], in0=psF[:, :, 64], scalar1=TINY, scalar2=None,
                                                                      op0=ALU.max), reads=["psF"], writes=[("dn", ds)])
                        P.add("dve", lambda e, ds=ds: e.reciprocal(out=dn[ds][:, :], in_=dn[ds][:, :]), reads=[("dn", ds)], writes=[("dn", ds)])
                        P.add("dve", lambda e, ds=ds, tile_i=tile_i, hd=hd: e.tensor_tensor(
                            out=dn[ds][:, :], in0=dn[ds][:, :], in1=Gs[:, tile_i, hd * 3:hd * 3 + 3], op=ALU.mult),
                            reads=[("dn", ds), "Gs"], writes=[("dn", ds)])
                        oo = ost[i % 2][:, tt, g * 64:(g + 1) * 64]
                        P.add("dve", lambda e, ds=ds, oo=oo: e.tensor_scalar(out=oo, in0=psF[:, 0, 0:64], scalar1=dn[ds][:, 0:1],
                                                                             scalar2=None, op0=ALU.mult),
                              reads=["psF", ("dn", ds)], writes=[("ost", i % 2, tt, g)])
                        for b in (1, 2):
                            P.add("dve", lambda e, ds=ds, oo=oo, b=b: e.scalar_tensor_tensor(
                                out=oo, in0=psF[:, b, 0:64], scalar=dn[ds][:, b:b + 1], in1=oo, op0=ALU.mult, op1=ALU.add),
                                reads=["psF", ("dn", ds), ("ost", i % 2, tt, g)], writes=[("ost", i % 2, tt, g)])

                pending = []
                for g in range(4):
                    kts = list(range(0, 4 * i + 4))
                    osl = g % 2
                    for j, kt in enumerate(kts):
                        ms = (-128 * (kt - 4 * i), -1, 1) if kt >= 4 * i else None

                        def post(g=g, osl=osl):
                            P.add("dve", lambda e: e.tensor_copy(out=OSs[osl][:, :], in_=psOS[0:65, :]), reads=["psOS"], writes=[("OSs", osl)])
                            pending.append(g)
                        pipe.push(KE[:, kt * 128:(kt + 1) * 128], QB[:, g, qsl], Vs[:, kt, 0:65], psOS, "psOS",
                                  j == 0, j == len(kts) - 1, ms,
                                  ["KEk", "KEm", ("QBq", g), "Vs"] + [("QBm", tt) for tt in range(4)], post=post)
                        if j == 3 and pending:
                            finalize(pending.pop(0))
                pipe.flush()
                while pending:
                    finalize(pending.pop(0))
                if "DBGB" in T and kh == 0:
                    P.add("sp", lambda e, qsl=qsl: e.dma_start(out=T["DBGB"][:, qsl], in_=QB[64:128, 0, qsl]),
                          reads=[("QBm", tt) for tt in range(4)], chan=c0)
                P.add("sp", lambda e, i=i, kh=kh: e.dma_start(out=OAv[:, 4 * i:4 * i + 4, kh * 256:(kh + 1) * 256], in_=ost[i % 2][:, :, :]),
                      reads=[("ost", i % 2, tt, g) for tt in range(4) for g in range(4)], chan=cst[i % 2])

    run_phase(nc, build)


def fox_phase(nc, T):
    TINY = 1e-30

    def build(P, es):
        ident, identf = make_ident(P, nc, es)
        QT = [sb(nc, es, "QT%d" % i, [128, S], BF16) for i in range(2)]
        KT = [sb(nc, es, "KT%d" % i, [128, S], BF16) for i in range(2)]
        Vf = [sb(nc, es, "Vf%d" % i, [128, NT, 72], BF16) for i in range(2)]
        lf = sb(nc, es, "lf", [128, NT, 8], F32)
        U = sb(nc, es, "U", [128, 128], F32)
        ONES = sb(nc, es, "ONES", [128, 128], F32)
        ones32 = sb(nc, es, "ones32", [128, NT], F32)
        cin = sb(nc, es, "cin", [128, NT, 8], F32)
        tot = sb(nc, es, "tot", [128, NT, 8], F32)
        incl = sb(nc, es, "incl", [128, NT, 8], F32)
        call = sb(nc, es, "call", [128, NT, 8], F32)
        biasT = sb(nc, es, "biasT", [128, NG, 8, NT], F32)
        PT = [sb(nc, es, "PT%d" % i, [128, 512], BF16) for i in range(4)]
        OFs = [sb(nc, es, "OFs%d" % i, [65, 512], F32) for i in range(2)]
        dn = [sb(nc, es, "dn%d" % i, [128, 1], F32) for i in range(2)]
        ostf = [sb(nc, es, "ostf%d" % i, [128, 4, 64], F32) for i in range(2)]
        psS = [ps(nc, es, "psS%d" % i, [128, 512], F32) for i in range(3)]
        psO = [ps(nc, es, "psO%d" % i, [128, 512], F32) for i in range(2)]
        psFF = [ps(nc, es, "psFF%d" % i, [128, 512], F32) for i in range(2)]
        psF = [psFF[0][:, 0:65], psFF[1][:, 0:65]]
        psC = psS[0][:, 0:NT * 8]
        psTt = psS[1][:, 0:NT * 8]
        c0 = P.chan()
        ckh = [P.chan() for _ in range(2)]
        cst = [P.chan() for _ in range(2)]
        OAv = T["OA"].rearrange("(t p) c -> p t c", p=128)

        P.add("sp", lambda e: e.dma_start(out=lf[:, :, :].rearrange("p t c -> p (t c)"), in_=T["LF"]), writes=["lf"], chan=c0)
        P.add("pool", lambda e: e.memset(U[:, :], 1.0), writes=["U"])
        P.add("pool", lambda e: asel(e, out=U[:, :], in_=U[:, :], pattern=[[1, 128]], compare_op=ALU.is_ge, fill=0.0,
                                     base=0, channel_multiplier=-1), reads=["U"], writes=["U"])
        P.add("pool", lambda e: e.memset(ONES[:, :], 1.0), writes=["ONES"])
        for i in range(2):
            P.add("pool", lambda e, i=i: e.memset(QT[i][64:128, :], 0.0), writes=[("QT0", i)])
            P.add("pool", lambda e, i=i: e.memset(KT[i][64:128, :], 0.0), writes=[("KT0", i)])
        P.add("pool", lambda e: e.memset(ones32[:, :], 1.0), writes=["ones32"])
        lff = lf[:, :, :].rearrange("p t c -> p (t c)")
        P.add("pe", lambda e: e.matmul(out=psC, lhsT=U[:, :], rhs=lff, start=True, stop=True), reads=["U", "lf"], writes=[("psS", 0)])
        P.add("pe", lambda e: e.matmul(out=psTt, lhsT=ONES[:, :], rhs=lff, start=True, stop=True), reads=["ONES", "lf"], writes=[("psS", 1)])
        P.add("dve", lambda e: e.tensor_copy(out=cin[:, :, :].rearrange("p t c -> p (t c)"), in_=psC), reads=[("psS", 0)], writes=["cin"])
        P.add("dve", lambda e: e.tensor_copy(out=tot[:, :, :].rearrange("p t c -> p (t c)"), in_=psTt), reads=[("psS", 1)], writes=["tot"])
        for h in range(8):
            P.add("dve", lambda e, h=h: e.tensor_tensor_scan(out=incl[:, :, h], data0=ones32[:, :], data1=tot[:, :, h], initial=0.0,
                                                             op0=ALU.mult, op1=ALU.add), reads=["tot", "ones32"], writes=[("incl", h)])
        inck = [("incl", h) for h in range(8)]
        P.add("dve", lambda e: e.tensor_tensor(out=call[:, :, :], in0=incl[:, :, :], in1=tot[:, :, :], op=ALU.subtract),
              reads=inck + ["tot"], writes=["call"])
        P.add("dve", lambda e: e.tensor_tensor(out=call[:, :, :], in0=call[:, :, :], in1=cin[:, :, :], op=ALU.add),
              reads=["call", "cin"], writes=["call"])
        for i in range(NG):
            for h in range(8):
                P.add("dve", lambda e, i=i, h=h: e.tensor_scalar(
                    out=biasT[:, i, h, :], in0=call[:, :, h], scalar1=-1.0, scalar2=incl[:, 4 * i + 1, h:h + 1],
                    op0=ALU.mult, op1=ALU.add), reads=["call"] + inck, writes=[("biasT", i, h)])
        pipe = UnitPipe(P, psS, PT, depth=2)
        fi = 0
        pending = []

        def finalize(h, i, ob):
            for tt in range(4):
                fb = tt % 2
                P.add("pe", lambda e, fb=fb, ob=ob, tt=tt: e.transpose(
                    out=psF[fb], in_=OFs[ob][:, tt * 128:(tt + 1) * 128], identity=identf[0:65, 0:65]),
                    reads=[("OFs", ob), "identf"], writes=[("psF", fb)])
                P.add("dve", lambda e, fb=fb: e.tensor_scalar(out=dn[fb][:, :], in0=psF[fb][:, 64:65], scalar1=TINY, scalar2=None,
                                                              op0=ALU.max), reads=[("psF", fb)], writes=[("dn", fb)])
                P.add("dve", lambda e, fb=fb: e.reciprocal(out=dn[fb][:, :], in_=dn[fb][:, :]), reads=[("dn", fb)], writes=[("dn", fb)])
                P.add("dve", lambda e, fb=fb, ob=ob, tt=tt: e.tensor_scalar(
                    out=ostf[ob][:, tt, :], in0=psF[fb][:, 0:64], scalar1=dn[fb][:, 0:1], scalar2=None, op0=ALU.mult),
                    reads=[("psF", fb), ("dn", fb)], writes=[("ostf", ob, tt)])
            P.add("sp", lambda e, i=i, h=h, ob=ob: e.dma_start(
                out=OAv[:, 4 * i:4 * i + 4, 512 + 64 * h:512 + 64 * (h + 1)], in_=ostf[ob][:, :, :]),
                reads=[("ostf", ob, tt) for tt in range(4)], chan=cst[ob])

        for h in range(DBG.get('fh', 8)):
            hs_ = h % 2
            ck = ckh[hs_]
            P.add("sp", lambda e, h=h, hs_=hs_: e.dma_start(out=QT[hs_][0:64, :], in_=T["QKT"][14 + h, :, :]), writes=[("QT", hs_)], chan=ck)
            P.add("sp", lambda e, h=h, hs_=hs_: e.dma_start(out=KT[hs_][0:64, :], in_=T["QKT"][22 + h, :, :]), writes=[("KT", hs_)], chan=ck)
            P.add("sp", lambda e, h=h, hs_=hs_: e.dma_start(out=Vf[hs_][:, :, :], in_=T["V"][4 + h]), writes=[("Vf", hs_)], chan=ck)
            for op in ck.ops[-3:]:
                op.chanval = ck.count
            for i in range(NG):
                qsl = slice(i * 512, (i + 1) * 512)
                ob = fi % 2
                fi += 1
                nk = 4 * i + 4
                for kt in range(nk):
                    ms = (-128 * (kt - 4 * i), -1, 1) if kt >= 4 * i else None

                    def post(h=h, i=i, ob=ob):
                        P.add("dve", lambda e: e.tensor_copy(out=OFs[ob][:, :], in_=psO[ob][0:65, :]), reads=[("psO", ob)], writes=[("OFs", ob)])
                        pending.append((h, i, ob))
                    pipe.push(KT[hs_][:, kt * 128:(kt + 1) * 128], QT[hs_][:, qsl], Vf[hs_][:, kt, 0:65], psO[ob], ("psO", ob),
                              kt == 0, kt == nk - 1, ms, [("KT", hs_), ("QT", hs_), ("Vf", hs_), ("QT0", hs_), ("KT0", hs_)],
                              bias=biasT[:, i, h, kt:kt + 1], bkeys=[("biasT", i, h)], post=post)
                    if pending and (kt == 3 or DBG.get('fox_now', 0)):
                        finalize(*pending.pop(0))
        pipe.flush()
        while pending:
            finalize(*pending.pop(0))

    run_phase(nc, build)


def norm_rows(P, src, junk, ssq, rs, epst, nb, gB, nparts, width, key):
    for j in range(nparts):
        cs = slice(j * width, (j + 1) * width)
        P.add("act", lambda e, cs=cs, j=j: e.activation(out=junk[:, cs], in_=src[:, cs], func=AF.Square, accum_out=ssq[:, j:j + 1]),
              reads=[key], writes=["junk", ("ssq", key, j)])
    P.add("act", lambda e: e.activation(out=rs[:, 0:nparts], in_=ssq[:, 0:nparts], func=AF.Sqrt, scale=1.0 / width, bias=epst[:, 0:1]),
          reads=[("ssq", key, j) for j in range(nparts)] + ["eps"], writes=[("rs", key)])
    P.add("dve", lambda e: e.reciprocal(out=rs[:, 0:nparts], in_=rs[:, 0:nparts]), reads=[("rs", key)], writes=[("rs", key)])
    for j in range(nparts):
        cs = slice(j * width, (j + 1) * width)
        P.add("dve", lambda e, cs=cs, j=j: e.scalar_tensor_tensor(out=nb[:, cs], in0=src[:, cs], scalar=rs[:, j:j + 1], in1=gB[:, cs],
                                                                  op0=ALU.mult, op1=ALU.mult),
              reads=[key, ("rs", key), "gB", "gB2"], writes=[("nb", key)])


def out_phase(nc, T):
    def build(P, es):
        ident, identf = make_ident(P, nc, es)
        epst = sb(nc, es, "epst", [128, 1], F32)
        P.add("pool", lambda e: e.memset(epst[:, :], EPS), writes=["eps"])
        wo = sb(nc, es, "wo", [128, 8, D], BF16)
        stage = [sb(nc, es, "ostg%d" % i, [128, D], F32) for i in range(2)]
        stch = [P.chan() for _ in range(2)]
        gB = sb(nc, es, "gB", [128, D], F32)
        xn = [sb(nc, es, "xn%d" % i, [128, D], F32) for i in range(2)]
        xr = [sb(nc, es, "xr%d" % i, [128, D], F32) for i in range(2)]
        nb = [sb(nc, es, "nb%d" % i, [128, D], BF16) for i in range(2)]
        junk = sb(nc, es, "junk", [128, D], BF16)
        ssq = [sb(nc, es, "ssq%d" % i, [128, 2], F32) for i in range(2)]
        rs = [sb(nc, es, "rs%d" % i, [128, 2], F32) for i in range(2)]
        mT = [sb(nc, es, "mT%d" % i, [128, 8, 128], BF16) for i in range(2)]
        psT = [ps(nc, es, "psT%d" % i, [128, D], BF16) for i in range(2)]
        pso = [ps(nc, es, "pso%d" % i, [128, 512], F32) for i in range(4)]
        cg = P.chan()
        cx = [P.chan() for _ in range(2)]
        cr = [P.chan() for _ in range(2)]
        co = [P.chan() for _ in range(2)]
        P.add("sp", lambda e: e.dma_start(out=gB[:, 0:512], in_=T["out_norm_nsa"].partition_broadcast(128)), writes=["gB"], chan=cg)
        P.add("sp", lambda e: e.dma_start(out=gB[:, 512:1024], in_=T["out_norm_fox"].partition_broadcast(128)), writes=["gB2"], chan=cg)
        cg.seal()
        wv = T["w_out"].rearrange("(c p) f -> p c f", p=128)
        for c in range(8):
            s_ = c % 2
            P.add("sp", lambda e, s_=s_, c=c: e.dma_start(out=stage[s_][:, :], in_=wv[:, c, :]), writes=[("stage", s_)], chan=stch[s_])
            P.add("dve" if c % 2 == 0 else "pool", lambda e, s_=s_, c=c: e.tensor_copy(out=wo[:, c, :], in_=stage[s_][:, :]),
                  reads=[("stage", s_)], writes=[("wo", c)])
        wkeys = [("wo", c) for c in range(8)]
        def stage1(t):
            xs = t % 2
            P.add("sp", lambda e, xs=xs, t=t: e.dma_start(out=xn[xs][:, :], in_=T["OA"][t * 128:(t + 1) * 128, :]), writes=[("xn", xs)], chan=cx[xs])
            P.add("sp", lambda e, xs=xs, t=t: e.dma_start(out=xr[xs][:, :], in_=T["h1"][t * 128:(t + 1) * 128, :]), writes=[("xr", xs)], chan=cr[xs])
            norm_rows(P, xn[xs], junk, ssq[xs], rs[xs], epst, nb[xs], gB, 2, 512, ("xn", xs))
            for c in range(8):
                P.add("pe", lambda e, xs=xs, c=c: e.transpose(out=psT[xs][:, c * 128:(c + 1) * 128], in_=nb[xs][:, c * 128:(c + 1) * 128],
                                                              identity=ident[:, :]),
                      reads=[("nb", ("xn", xs)), "ident"], writes=[("psT", xs)] if c == 0 else [])
            P.last_w[("psT", xs)] = P.ops["pe"][-1]
            P.add("act", lambda e, xs=xs: e.copy(out=mT[xs][:, :, :], in_=psT[xs][:, :].rearrange("p (c t) -> p c t", c=8)),
                  reads=[("psT", xs)], writes=[("mT", xs)])

        def stage2(t):
            xs = t % 2
            for half in range(2):
                pb = (t * 2 + half) % 4
                for c in range(8):
                    P.add("pe", lambda e, pb=pb, c=c, xs=xs, half=half: e.matmul(
                        out=pso[pb][:, :], lhsT=mT[xs][:, c, :], rhs=wo[:, c, half * 512:(half + 1) * 512], start=(c == 0), stop=(c == 7)),
                        reads=[("mT", xs)] + (wkeys if t == 0 else []), writes=[("pso", pb)] if c == 0 else [])
                P.last_w[("pso", pb)] = P.ops["pe"][-1]
                P.add("dve", lambda e, pb=pb, xs=xs, half=half: e.tensor_tensor(
                    out=xr[xs][:, half * 512:(half + 1) * 512], in0=pso[pb][:, :], in1=xr[xs][:, half * 512:(half + 1) * 512], op=ALU.add),
                    reads=[("pso", pb), ("xr", xs)], writes=[("xr", xs)])
            P.add("sp", lambda e, xs=xs, t=t: e.dma_start(out=T["h1"][t * 128:(t + 1) * 128, :], in_=xr[xs][:, :]),
                  reads=[("xr", xs)], writes=[("xrst", xs)], chan=co[xs])

        stage1(0)
        for t in range(NT):
            if t + 1 < NT:
                stage1(t + 1)
            stage2(t)

    run_phase(nc, build)


def ple_phase(nc, T):
    def build(P, es):
        ident, identf = make_ident(P, nc, es)
        epst = sb(nc, es, "epst", [128, 1], F32)
        P.add("pool", lambda e: e.memset(epst[:, :], EPS), writes=["eps"])
        wg = sb(nc, es, "wg", [128, 8, D], BF16)
        wp = sb(nc, es, "wp", [128, 2, D], BF16)
        stage = [sb(nc, es, "lstg%d" % i, [128, D], F32) for i in range(2)]
        stch = [P.chan() for _ in range(2)]
        gB = sb(nc, es, "gB", [128, D], F32)
        gE = sb(nc, es, "gE", [128, D], F32)
        xn = [sb(nc, es, "xn%d" % i, [128, D], F32) for i in range(2)]
        pt = [sb(nc, es, "pt%d" % i, [128, 256], F32) for i in range(2)]
        pb16 = [sb(nc, es, "pb%d" % i, [128, 256], BF16) for i in range(2)]
        nb = [sb(nc, es, "nb%d" % i, [128, D], BF16) for i in range(2)]
        junk = sb(nc, es, "junk", [128, D], BF16)
        junk2 = sb(nc, es, "junk2", [128, D], BF16)
        ssq = [sb(nc, es, "ssq%d" % i, [128, 4], F32) for i in range(2)]
        rs = [sb(nc, es, "rs%d" % i, [128, 4], F32) for i in range(2)]
        mT = [sb(nc, es, "mT%d" % i, [128, 10, 128], BF16) for i in range(2)]
        gate = [sb(nc, es, "gate%d" % i, [128, D], F32) for i in range(2)]
        ev = [sb(nc, es, "ev%d" % i, [128, D], F32) for i in range(2)]
        psTa = ps(nc, es, "psTa", [128, 8, 128], BF16)
        psTb = ps(nc, es, "psTb", [128, 8, 128], BF16)
        psg = [ps(nc, es, "psg%d" % i, [128, 512], F32) for i in range(2)]
        pse = [ps(nc, es, "pse%d" % i, [128, 512], F32) for i in range(2)]
        cg = P.chan()
        cx = [P.chan() for _ in range(2)]
        cp = [P.chan() for _ in range(2)]
        co = [P.chan() for _ in range(2)]
        P.add("sp", lambda e: e.dma_start(out=gB[:, :], in_=T["ple_gate_norm"].partition_broadcast(128)), writes=["gB"], chan=cg)
        P.add("sp", lambda e: e.dma_start(out=gE[:, :], in_=T["ple_norm"].partition_broadcast(128)), writes=["gE"], chan=cg)
        cg.seal()
        wv = T["ple_w_gate"].rearrange("(c p) f -> p c f", p=128)
        wpv = T["ple_w_proj"].rearrange("(c p) f -> p c f", p=128)
        for c in range(10):
            s_ = c % 2
            src = wv[:, c, :] if c < 8 else wpv[:, c - 8, :]
            dstw = wg[:, c, :] if c < 8 else wp[:, c - 8, :]
            P.add("sp", lambda e, s_=s_, src=src: e.dma_start(out=stage[s_][:, :], in_=src), writes=[("stage", s_)], chan=stch[s_])
            P.add("dve" if c % 2 == 0 else "pool", lambda e, s_=s_, dstw=dstw: e.tensor_copy(out=dstw, in_=stage[s_][:, :]),
                  reads=[("stage", s_)], writes=[("w", c)])
        wkeys = [("w", c) for c in range(10)]
        def stage1(t):
            xs = t % 2
            P.add("sp", lambda e, xs=xs, t=t: e.dma_start(out=xn[xs][:, :], in_=T["h3"][t * 128:(t + 1) * 128, :]), writes=[("xn", xs)], chan=cx[xs])
            P.add("sp", lambda e, xs=xs, t=t: e.dma_start(out=pt[xs][:, :], in_=T["p"][t * 128:(t + 1) * 128, :]), writes=[("pt", xs)], chan=cp[xs])
            norm_rows(P, xn[xs], junk, ssq[xs], rs[xs], epst, nb[xs], gB, 1, D, ("xn", xs))
            P.add("pool", lambda e, xs=xs: e.tensor_copy(out=pb16[xs][:, :], in_=pt[xs][:, :]), reads=[("pt", xs)], writes=[("pb16", xs)])
            for c in range(10):
                src = nb[xs][:, c * 128:(c + 1) * 128] if c < 8 else pb16[xs][:, (c - 8) * 128:(c - 7) * 128]
                dstp = psTa[:, c, :] if c < 8 else psTb[:, c - 8, :]
                P.add("pe", lambda e, dstp=dstp, src=src: e.transpose(out=dstp, in_=src, identity=ident[:, :]),
                      reads=[("nb", ("xn", xs)), ("pb16", xs), "ident"], writes=["psTa" if c < 8 else "psTb"] if c in (0, 8) else [])
                if c == 7:
                    P.last_w["psTa"] = P.ops["pe"][-1]
            P.last_w["psTb"] = P.ops["pe"][-1]
            P.add("act", lambda e, xs=xs: e.copy(out=mT[xs][:, 0:8, :], in_=psTa[:, :, :]), reads=["psTa"], writes=[("mT", xs)])
            P.add("act", lambda e, xs=xs: e.copy(out=mT[xs][:, 8:10, :], in_=psTb[:, 0:2, :]), reads=["psTb"], writes=[("mT2", xs)])

        def stage2(t):
            xs = t % 2
            for half in range(2):
                hsl = slice(half * 512, (half + 1) * 512)
                for c in range(8):
                    P.add("pe", lambda e, c=c, xs=xs, half=half, hsl=hsl: e.matmul(
                        out=psg[half][:, :], lhsT=mT[xs][:, c, :], rhs=wg[:, c, hsl], start=(c == 0), stop=(c == 7)),
                        reads=[("mT", xs), ("mT2", xs)] + (wkeys if t == 0 else []), writes=[("psg", half)] if c == 0 else [])
                P.last_w[("psg", half)] = P.ops["pe"][-1]
                P.add("act", lambda e, xs=xs, half=half, hsl=hsl: e.activation(out=gate[xs][:, hsl], in_=psg[half][:, :], func=AF.Sigmoid),
                      reads=[("psg", half)], writes=[("gate", xs, half)])
                for c in range(2):
                    P.add("pe", lambda e, c=c, xs=xs, half=half, hsl=hsl: e.matmul(
                        out=pse[half][:, :], lhsT=mT[xs][:, 8 + c, :], rhs=wp[:, c, hsl], start=(c == 0), stop=(c == 1)),
                        reads=[("mT", xs), ("mT2", xs)] + (wkeys if t == 0 else []), writes=[("pse", half)] if c == 0 else [])
                P.last_w[("pse", half)] = P.ops["pe"][-1]
                P.add("act", lambda e, xs=xs, half=half, hsl=hsl: e.activation(
                    out=junk2[:, hsl], in_=pse[half][:, :], func=AF.Square, accum_out=ssq[xs][:, 2 + half:3 + half]),
                    reads=[("pse", half)], writes=[("junk2", half), ("ssqe", xs, half)])
            P.add("dve", lambda e, xs=xs: e.tensor_tensor(out=rs[xs][:, 2:3], in0=ssq[xs][:, 2:3], in1=ssq[xs][:, 3:4], op=ALU.add),
                  reads=[("ssqe", xs, 0), ("ssqe", xs, 1)], writes=[("rse", xs)])
            P.add("act", lambda e, xs=xs: e.activation(out=rs[xs][:, 2:3], in_=rs[xs][:, 2:3], func=AF.Sqrt, scale=1.0 / D, bias=epst[:, 0:1]),
                  reads=[("rse", xs), "eps"], writes=[("rse", xs)])
            P.add("dve", lambda e, xs=xs: e.reciprocal(out=rs[xs][:, 2:3], in_=rs[xs][:, 2:3]), reads=[("rse", xs)], writes=[("rse", xs)])
            for half in range(2):
                hsl = slice(half * 512, (half + 1) * 512)
                P.add("dve", lambda e, xs=xs, half=half, hsl=hsl: e.scalar_tensor_tensor(
                    out=ev[xs][:, hsl], in0=pse[half][:, :], scalar=rs[xs][:, 2:3], in1=gE[:, hsl], op0=ALU.mult, op1=ALU.mult),
                    reads=[("pse", half), ("rse", xs), "gE"], writes=[("ev", xs, half)])
                P.add("pool", lambda e, xs=xs, hsl=hsl: e.tensor_tensor(out=ev[xs][:, hsl], in0=ev[xs][:, hsl], in1=gate[xs][:, hsl], op=ALU.mult),
                      reads=[("ev", xs, half), ("gate", xs, half)], writes=[("ev", xs, half)])
                P.add("pool", lambda e, xs=xs, hsl=hsl: e.tensor_tensor(out=ev[xs][:, hsl], in0=ev[xs][:, hsl], in1=xn[xs][:, hsl], op=ALU.add),
                      reads=[("ev", xs, half), ("xn", xs)], writes=[("ev", xs, half)])
            P.add("sp", lambda e, xs=xs, t=t: e.dma_start(out=T["out"][t * 128:(t + 1) * 128, :], in_=ev[xs][:, :]),
                  reads=[("ev", xs, 0), ("ev", xs, 1)], chan=co[xs])


        stage1(0)
        for t in range(NT):
            if t + 1 < NT:
                stage1(t + 1)
            stage2(t)

    run_phase(nc, build)


def rope_tables_np():
    pos = np.arange(S, dtype=np.float32)
    inv = (np.float32(500000.0) ** (-np.arange(0, 16, 2, dtype=np.float32) / np.float32(16))).astype(np.float32)
    ang = (pos[:, None] * inv[None, :]).astype(np.float32)
    return np.cos(ang).astype(np.float32), np.sin(ang).astype(np.float32)


IN_SHAPES = dict(
    x=[S, D], p=[S, 256], ffn1_norm=[D], ffn1_wg=[D, DFF], ffn1_wu=[D, DFF], ffn1_wd=[DFF, D],
    mix_norm=[D], w_in=[D, 2848], b_forget=[8], q_norm_nsa=[64], k_norm_cmp=[64], k_norm_slc=[64], k_norm_win=[64],
    cmp_pos_k=[32, 64], cmp_pos_v=[32, 64], cmp_k_w1=[2048, 256], cmp_k_w2=[256, 64], cmp_v_w1=[2048, 256],
    cmp_v_w2=[256, 64], q_norm_fox=[64], k_norm_fox=[64], out_norm_nsa=[512], out_norm_fox=[512], w_out=[D, D],
    ffn2_norm=[D], ffn2_wg=[D, DFF], ffn2_wu=[D, DFF], ffn2_wd=[DFF, D], ple_gate_norm=[D], ple_w_gate=[D, D],
    ple_w_proj=[256, D], ple_norm=[D], rope_cos=[S, 8], rope_sin=[S, 8])


def build_nc(nph=99, debug=(), skip=()):
    nc = bass.Bass("TRN2", target_bir_lowering=False)
    T = {}
    for name, shape in IN_SHAPES.items():
        T[name] = nc.dram_tensor(name, shape, F32, kind="ExternalInput").ap()

    def scratch(name, shape, dt):
        kind = "ExternalOutput" if name in debug else "Internal"
        T[name] = nc.dram_tensor(name, shape, dt, kind=kind).ap()

    T["out"] = nc.dram_tensor("out", [S, D], F32, kind="ExternalOutput").ap()
    scratch("h1", [S, D], F32)
    scratch("QKT", [32, 64, S], BF16)
    scratch("V", [12, 128, NT, 72], BF16)
    scratch("G", [128, NT * 24], F32)
    scratch("LF", [128, NT * 8], F32)
    scratch("KCT", [2, 64, 256], BF16)
    scratch("VC", [2, 256, 65], BF16)
    scratch("OA", [S, D], F32)
    scratch("h3", [S, D], F32)
    if "DBGB" in debug:
        scratch("DBGB", [64, S], BF16)
    if "DBGT" in debug:
        scratch("DBGT", [3, 65, 512], F32)
    phases = [
        lambda: ffn_half_phase(nc, T, T["x"], T["x"], T["h1"], T["ffn1_norm"], T["ffn1_wg"], T["ffn1_wu"], T["ffn1_wd"], 0, 11, "f1a"),
        lambda: ffn_half_phase(nc, T, T["x"], T["h1"], T["h1"], T["ffn1_norm"], T["ffn1_wg"], T["ffn1_wu"], T["ffn1_wd"], 11, 11, "f1b"),
        lambda: proj_phase(nc, T),
        lambda: cmp_phase(nc, T),
        lambda: nsa_phase(nc, T),
        lambda: fox_phase(nc, T),
        lambda: out_phase(nc, T),
        lambda: ffn_half_phase(nc, T, T["h1"], T["h1"], T["h3"], T["ffn2_norm"], T["ffn2_wg"], T["ffn2_wu"], T["ffn2_wd"], 0, 11, "f2a"),
        lambda: ffn_half_phase(nc, T, T["h1"], T["h3"], T["h3"], T["ffn2_norm"], T["ffn2_wg"], T["ffn2_wu"], T["ffn2_wd"], 11, 11, "f2b"),
        lambda: ple_phase(nc, T),
    ]
    for k, ph in enumerate(phases[:nph]):
        if k not in skip:
            ph()
    return nc


def make_in_maps(inputs, cores=range(8)):
    cos, sin = rope_tables_np()
    in_maps = []
    for b in cores:
        m = {}
        for name in IN_SHAPES:
            if name == "x":
                a = inputs["x"][b]
            elif name == "p":
                a = inputs["p"][0, b]
            elif name == "rope_cos":
                a = cos
            elif name == "rope_sin":
                a = sin
            elif name == "w_in":
                a = inputs["w_in"][0][:, W_IN_PERM]
            else:
                a = inputs[name][0]
            m[name] = np.ascontiguousarray(a, dtype=np.float32)
        in_maps.append(m)
    return in_maps


def kernel(**inputs):
    nc = build_nc()
    res = run_bass_kernel_spmd(nc, make_in_maps(inputs), core_ids=list(range(8)))
    return np.stack([np.asarray(r["out"]) for r in res.results], axis=0)
```

```python
import numpy as np
from contextlib import ExitStack
import concourse.bass as bass
import concourse.mybir as mybir
from concourse.bass_utils import run_bass_kernel_spmd

F32 = mybir.dt.float32
BF16 = mybir.dt.bfloat16
AF = mybir.ActivationFunctionType
ALU = mybir.AluOpType
AX = mybir.AxisListType

S = 4096
D = 1024
DFF = 2816
NT = S // 128
NG = S // 512
EPS = 1e-6
SAME_ENGINE_SYNC = True
DBG = dict(kh=2, ng=NG, stage=5)
_UID = [0]


def _u(name):
    _UID[0] += 1
    return "%s_%d" % (name, _UID[0])

_OFF = dict(qa=0, kc=512, vc=640, ksl=768, vsl=896, kwn=1024, vwn=1152, ga=1280, qf=1304, kf=1816, vf=2328, fl=2840)
_SZ = dict(qa=512, kc=128, vc=128, ksl=128, vsl=128, kwn=128, vwn=128, ga=24, qf=512, kf=512, vf=512, fl=8)
_ORDER = ['kc', 'qa', 'ksl', 'kwn', 'qf', 'kf', 'vc', 'vsl', 'vwn', 'vf', 'ga', 'fl']
W_IN_PERM = np.concatenate([np.arange(_OFF[k], _OFF[k] + _SZ[k]) for k in _ORDER])


class Chan:
    def __init__(self, sem):
        self.sem = sem
        self.count = 0
        self.ops = []

    def seal(self):
        for op in self.ops:
            op.chanval = self.count


class Op:
    __slots__ = ("eng", "fn", "deps", "sig", "sigval", "chan", "chanval", "idx")


class Prog:
    ENGS = ("pe", "act", "dve", "pool", "sp")

    def __init__(self, nc, es):
        self.nc = nc
        self.es = es
        self.ops = {e: [] for e in self.ENGS}
        self.last_w = {}
        self.readers = {}
        self.engsem = {e: es.enter_context(nc.semaphore(_u("s_" + e))) for e in self.ENGS}
        self.chans = []

    def chan(self):
        c = Chan(self.es.enter_context(self.nc.semaphore(_u("c"))))
        self.chans.append(c)
        return c

    def add(self, eng, fn, reads=(), writes=(), chan=None):
        op = Op()
        op.eng = eng
        op.fn = fn
        op.sig = False
        op.sigval = 0
        op.chan = chan
        deps = []
        for r in reads:
            w = self.last_w.get(r)
            if w is not None:
                deps.append(w)
        for w in writes:
            lw = self.last_w.get(w)
            if lw is not None:
                deps.append(lw)
            deps.extend(self.readers.get(w, ()))
        best = {}
        for d in deps:
            k = ("c", id(d.chan)) if d.chan is not None else ("e", d.eng)
            if k not in best or best[k].idx < d.idx:
                best[k] = d
        op.deps = list(best.values())
        op.idx = len(self.ops[eng])
        for r in reads:
            self.readers.setdefault(r, []).append(op)
        for w in writes:
            self.last_w[w] = op
            self.readers[w] = []
        if chan is not None:
            chan.count += 16
            op.chanval = chan.count
            chan.ops.append(op)
        self.ops[eng].append(op)
        return op

    def emit(self, block):
        for e in self.ENGS:
            for op in self.ops[e]:
                for d in op.deps:
                    if d.chan is None and (d.eng != op.eng or SAME_ENGINE_SYNC):
                        d.sig = True
        for e in self.ENGS:
            c = 0
            for op in self.ops[e]:
                if op.sig and op.chan is None:
                    c += 1
                    op.sigval = c
        final = [(c.sem, c.count) for c in self.chans if c.count > 0]

        def mk(ename):
            ops = self.ops[ename]

            def body(eng):
                waited = {}
                if ename == "pool":
                    _FILL.clear()
                for op in ops:
                    need = []
                    for d in op.deps:
                        if d.chan is not None:
                            sem, val = d.chan.sem, d.chanval
                        elif d.eng != ename or SAME_ENGINE_SYNC:
                            sem, val = self.engsem[d.eng], d.sigval
                        else:
                            continue
                        k = id(sem)
                        if waited.get(k, 0) >= val:
                            continue
                        need.append((sem, val))
                        waited[k] = val
                    for sem, val in need[:-1]:
                        eng.wait_ge(sem, val)
                    ins = op.fn(eng)
                    if need:
                        ins._wait_ge(need[-1][0], need[-1][1])
                    if op.chan is not None:
                        ins.then_inc(op.chan.sem, 16)
                    elif op.sig:
                        ins.then_inc(self.engsem[ename], 1)
                if ename == "sp":
                    for sem, val in final:
                        eng.wait_ge(sem, val)
                if ename == "pool":
                    for r in _FILL.values():
                        eng.free_register(r)
                    _FILL.clear()
            return body

        block.tensor(mk("pe"))
        block.scalar(mk("act"))
        block.vector(mk("dve"))
        block.gpsimd(mk("pool"))
        block.sync(mk("sp"))


def run_phase(nc, build):
    with ExitStack() as es:
        P = Prog(nc, es)
        build(P, es)
        sems = list(P.engsem.values()) + [c.sem for c in P.chans]
        with nc.Block() as b0:
            def clr(e):
                for sm in sems:
                    e.sem_clear(sm)
            b0.sync(clr)
        with nc.Block() as block:
            P.emit(block)


def sb(nc, es, name, shape, dt):
    return es.enter_context(nc.sbuf_tensor(_u(name), shape, dt))


def ps(nc, es, name, shape, dt):
    return es.enter_context(nc.psum_tensor(_u(name), shape, dt))


def load_cast_rows(P, nc, es, dst, src_rows, ncols, chans, stage, key):
    n = len(src_rows)
    for k in range(n):
        s = k % len(stage)
        st = stage[s]
        ch = chans[s]
        src = src_rows[k]
        P.add("sp", lambda e, st=st, src=src: e.dma_start(out=st[:, 0:ncols], in_=src),
              writes=[("stage", s)], chan=ch)
        eng = "dve" if k % 2 == 0 else "pool"
        P.add(eng, lambda e, st=st, k=k: e.tensor_copy(out=dst[:, k, 0:ncols], in_=st[:, 0:ncols]),
              reads=[("stage", s)], writes=[(key, k)])


def ffn_half_phase(nc, T, src_norm, src_res, dst, gain, wg, wu, wd, f0, nfc, tagp):
    def build(P, es):
        FW = nfc * 128
        wg_s = sb(nc, es, "wg_s", [128, 8, FW], BF16)
        wu_s = sb(nc, es, "wu_s", [128, 8, FW], BF16)
        wd_s = sb(nc, es, "wd_s", [128, nfc, D], BF16)
        stage = [sb(nc, es, "stg%d" % i, [128, FW], F32) for i in range(3)]
        stch = [P.chan() for _ in range(3)]
        gB = sb(nc, es, "gB", [128, D], F32)
        ident = sb(nc, es, "ident", [128, 128], BF16)
        identf = sb(nc, es, "identf", [128, 128], F32)
        epst = sb(nc, es, "epst", [128, 1], F32)
        xn = [sb(nc, es, "xn%d" % i, [128, D], F32) for i in range(2)]
        xr = [sb(nc, es, "xr%d" % i, [128, D], F32) for i in range(2)]
        nb = [sb(nc, es, "nb%d" % i, [128, D], BF16) for i in range(4)]
        junk = sb(nc, es, "junk", [128, D], BF16)
        ssq = [sb(nc, es, "ssq%d" % i, [128, 4], F32) for i in range(2)]
        rs = [sb(nc, es, "rs%d" % i, [128, 4], F32) for i in range(2)]
        nT = [sb(nc, es, "nT%d" % i, [128, 8, 512], BF16) for i in range(2)]
        hT = sb(nc, es, "hT", [128, nfc, 512], BF16)
        sg = [sb(nc, es, "sg%d" % i, [128, 512], F32) for i in range(2)]
        psT = [ps(nc, es, "psT%d" % i, [128, D], BF16) for i in range(2)]
        psg = [ps(nc, es, "psg%d" % i, [128, 512], F32) for i in range(2)]
        psu = [ps(nc, es, "psu%d" % i, [128, 512], F32) for i in range(2)]
        pso = [ps(nc, es, "pso%d" % i, [128, 512], F32) for i in range(2)]
        cx = [P.chan() for _ in range(2)]
        cr = [P.chan() for _ in range(2)]
        co = [P.chan() for _ in range(2)]
        cg = P.chan()

        P.add("sp", lambda e: e.dma_start(out=gB[:, :], in_=gain.partition_broadcast(128)),
              writes=["gB"], chan=cg)
        P.add("pool", lambda e: e.memset(identf[:, :], 0.0), writes=["identf"])
        P.add("pool", lambda e: asel(e, out=identf[:, :], in_=identf[:, :], pattern=[[-1, 128]],
                                                compare_op=ALU.not_equal, fill=1.0, base=0,
                                                channel_multiplier=1),
              reads=["identf"], writes=["identf"])
        P.add("pool", lambda e: e.tensor_copy(out=ident[:, :], in_=identf[:, :]), reads=["identf"], writes=["ident"])
        P.add("pool", lambda e: e.memset(epst[:, :], EPS), writes=["eps"])

        wgv = wg.rearrange("(c p) f -> p c f", p=128)
        wuv = wu.rearrange("(c p) f -> p c f", p=128)
        load_cast_rows(P, nc, es, wg_s, [wgv[:, c, f0 * 128:f0 * 128 + FW] for c in range(8)], FW, stch, stage, "wg")
        load_cast_rows(P, nc, es, wu_s, [wuv[:, c, f0 * 128:f0 * 128 + FW] for c in range(8)], FW, stch, stage, "wu")
        load_cast_rows(P, nc, es, wd_s, [wd[(f0 + c) * 128:(f0 + c + 1) * 128, :] for c in range(nfc)], D, stch, stage, "wd")
        wkeys = [("wg", c) for c in range(8)] + [("wu", c) for c in range(8)]
        wdkeys = [("wd", c) for c in range(nfc)]

        def prep_group(g):
            sl = g % 2
            for k in range(4):
                t = 4 * g + k
                xs = t % 2
                P.add("sp", lambda e, xs=xs, t=t: e.dma_start(out=xn[xs][:, :], in_=src_norm[t * 128:(t + 1) * 128, :]),
                      writes=[("xn", xs)], chan=cx[xs])
                P.add("act", lambda e, xs=xs, sl=sl, k=k: e.activation(
                    out=junk[:, :], in_=xn[xs][:, :], func=AF.Square, accum_out=ssq[sl][:, k:k + 1]),
                    reads=[("xn", xs)], writes=["junk", ("ssq", sl, k)])
                P.add("act", lambda e, sl=sl, k=k: e.activation(out=rs[sl][:, k:k + 1], in_=ssq[sl][:, k:k + 1],
                                                                func=AF.Sqrt, scale=1.0 / D, bias=epst[:, 0:1]),
                      reads=[("ssq", sl, k), "eps"], writes=[("rs", sl, k)])
                P.add("dve", lambda e, sl=sl, k=k: e.reciprocal(out=rs[sl][:, k:k + 1], in_=rs[sl][:, k:k + 1]),
                      reads=[("rs", sl, k)], writes=[("rs", sl, k)])
                P.add("dve", lambda e, xs=xs, sl=sl, k=k: e.scalar_tensor_tensor(
                    out=nb[k][:, :], in0=xn[xs][:, :], scalar=rs[sl][:, k:k + 1], in1=gB[:, :],
                    op0=ALU.mult, op1=ALU.mult),
                    reads=[("xn", xs), ("rs", sl, k), "gB"], writes=[("nb", k)])

        def transposes(g):
            sl = g % 2
            for k in range(4):
                pb = k % 2
                for c in range(8):
                    P.add("pe", lambda e, pb=pb, k=k, c=c: e.transpose(
                        out=psT[pb][:, c * 128:(c + 1) * 128], in_=nb[k][:, c * 128:(c + 1) * 128], identity=ident[:, :]),
                        reads=[("nb", k), "ident"], writes=[("psT", pb)] if c == 0 else [])
                P.last_w[("psT", pb)] = P.ops["pe"][-1]
                P.add("act", lambda e, pb=pb, sl=sl, k=k: e.copy(
                    out=nT[sl][:, :, k * 128:(k + 1) * 128],
                    in_=psT[pb][:, :].rearrange("p (c t) -> p c t", c=8)),
                    reads=[("psT", pb)], writes=[("nT", sl, k)])

        def upgate(g):
            sl = g % 2
            for fc in range(nfc):
                b = fc % 2
                for (wt, pst, nm) in ((wg_s, psg, "psg"), (wu_s, psu, "psu")):
                    for c in range(8):
                        P.add("pe", lambda e, wt=wt, pst=pst, b=b, c=c, fc=fc, sl=sl: e.matmul(
                            out=pst[b][:, :], lhsT=wt[:, c, fc * 128:(fc + 1) * 128], rhs=nT[sl][:, c, :],
                            start=(c == 0), stop=(c == 7)),
                            reads=[("nT", sl, 0), ("nT", sl, 1), ("nT", sl, 2), ("nT", sl, 3)] + (wkeys if g == 0 else []),
                            writes=[(nm, b)] if c == 0 else [])
                    P.last_w[(nm, b)] = P.ops["pe"][-1]
                P.add("act", lambda e, b=b: e.activation(out=sg[b][:, :], in_=psg[b][:, :], func=AF.Silu),
                      reads=[("psg", b)], writes=[("sg", b)])
                P.add("dve", lambda e, b=b, fc=fc: e.tensor_tensor(out=hT[:, fc, :], in0=psu[b][:, :], in1=sg[b][:, :],
                                                                  op=ALU.mult),
                      reads=[("psu", b), ("sg", b)], writes=[("hT", fc)])

        def down(g):
            for k in range(4):
                t = 4 * g + k
                rsl = t % 2
                P.add("sp", lambda e, rsl=rsl, t=t: e.dma_start(out=xr[rsl][:, :], in_=src_res[t * 128:(t + 1) * 128, :]),
                      reads=[("dram", t)], writes=[("xr", rsl)], chan=cr[rsl])
                for half in range(2):
                    b = half
                    for fc in range(nfc):
                        P.add("pe", lambda e, b=b, fc=fc, k=k, half=half: e.matmul(
                            out=pso[b][:, :], lhsT=hT[:, fc, k * 128:(k + 1) * 128],
                            rhs=wd_s[:, fc, half * 512:(half + 1) * 512], start=(fc == 0), stop=(fc == nfc - 1)),
                            reads=[("hT", fc)] + (wdkeys if g == 0 else []),
                            writes=[("pso", b)] if fc == 0 else [])
                    P.last_w[("pso", b)] = P.ops["pe"][-1]
                    P.add("dve", lambda e, b=b, rsl=rsl, half=half: e.scalar_tensor_tensor(
                        out=xr[rsl][:, half * 512:(half + 1) * 512], in0=pso[b][:, :], scalar=0.5,
                        in1=xr[rsl][:, half * 512:(half + 1) * 512], op0=ALU.mult, op1=ALU.add),
                        reads=[("pso", b), ("xr", rsl)], writes=[("xr", rsl)])
                P.add("sp", lambda e, rsl=rsl, t=t: e.dma_start(out=dst[t * 128:(t + 1) * 128, :], in_=xr[rsl][:, :]),
                      reads=[("xr", rsl)], writes=[("dram", t)], chan=co[rsl])

        prep_group(0)
        transposes(0)
        for g in range(NG):
            if g + 1 < NG:
                prep_group(g + 1)
            upgate(g)
            if g + 1 < NG:
                transposes(g + 1)
            down(g)

    run_phase(nc, build)


def proj_phase(nc, T):
    def build(P, es):
        h1 = T["h1"]
        WIN = 2848
        win_s = sb(nc, es, "win_s", [128, 8, WIN], BF16)
        HW_ = WIN // 2
        stage = [sb(nc, es, "pstg%d" % i, [128, HW_], F32) for i in range(3)]
        stch = [P.chan() for _ in range(3)]
        gB = sb(nc, es, "gB", [128, D], F32)
        ident = sb(nc, es, "ident", [128, 128], BF16)
        identf = sb(nc, es, "identf", [128, 128], F32)
        epst = sb(nc, es, "epst", [128, 1], F32)
        g5 = sb(nc, es, "g5", [128, 5, 64], F32)
        GQ = sb(nc, es, "GQ", [128, 28, 64], F32)
        bfg = sb(nc, es, "bfg", [128, 8], F32)
        cosT = sb(nc, es, "cosT", [128, NT, 8], F32)
        sinT = sb(nc, es, "sinT", [128, NT, 8], F32)
        Gall = sb(nc, es, "Gall", [128, NT, 24], F32)
        LFall = sb(nc, es, "LFall", [128, NT, 8], F32)
        xn = [sb(nc, es, "xn%d" % i, [128, D], F32) for i in range(2)]
        nb = [sb(nc, es, "nb%d" % i, [128, D], BF16) for i in range(2)]
        junk = sb(nc, es, "junk", [128, D], BF16)
        ssq = sb(nc, es, "ssq", [128, 2], F32)
        rs = sb(nc, es, "rs", [128, 2], F32)
        aT = [sb(nc, es, "aT%d" % i, [128, 8, 128], BF16) for i in range(2)]
        qk = [sb(nc, es, "qk%d" % i, [128, 32, 64], F32) for i in range(2)]
        sq = [sb(nc, es, "sq%d" % i, [128, 32, 64], F32) for i in range(2)]
        hs = [sb(nc, es, "hs%d" % i, [128, 32], F32) for i in range(2)]
        rt = [sb(nc, es, "rt%d" % i, [128, 14, 8], F32) for i in range(4)]
        qkb = [sb(nc, es, "qkb%d" % i, [128, 2048], BF16) for i in range(2)]
        qkT = sb(nc, es, "qkT", [128, 16, 512], BF16)
        vst = [sb(nc, es, "vst%d" % i, [128, 12, 4, 72], BF16) for i in range(2)]
        psT = [ps(nc, es, "psT%d" % i, [128, D], BF16) for i in range(2)]
        pq = [ps(nc, es, "pq%d" % i, [128, 512], F32) for i in range(4)]
        psQ = [ps(nc, es, "psQ%d" % i, [128, 8, 128], BF16) for i in range(2)]
        cx = [P.chan() for _ in range(2)]
        cg = P.chan()
        cq = P.chan()
        cv = [P.chan() for _ in range(2)]
        cf = P.chan()

        P.add("sp", lambda e: e.dma_start(out=gB[:, :], in_=T["mix_norm"].partition_broadcast(128)), writes=["gB"], chan=cg)
        for i, nm in enumerate(("q_norm_nsa", "k_norm_slc", "k_norm_win", "q_norm_fox", "k_norm_fox")):
            P.add("sp", lambda e, i=i, nm=nm: e.dma_start(out=g5[:, i, :], in_=T[nm].partition_broadcast(128)),
                  writes=[("g5", i)], chan=cg)
        P.add("sp", lambda e: e.dma_start(out=bfg[:, :], in_=T["b_forget"].partition_broadcast(128)), writes=["bfg"], chan=cg)
        P.add("sp", lambda e: e.dma_start(out=cosT[:, :, :], in_=T["rope_cos"].rearrange("(t p) c -> p t c", p=128)),
              writes=["cosT"], chan=cg)
        P.add("sp", lambda e: e.dma_start(out=sinT[:, :, :], in_=T["rope_sin"].rearrange("(t p) c -> p t c", p=128)),
              writes=["sinT"], chan=cg)
        cg.seal()
        for (i, h0, nh) in ((0, 0, 8), (1, 8, 2), (2, 10, 2), (3, 12, 8), (4, 20, 8)):
            P.add("dve", lambda e, i=i, h0=h0, nh=nh: e.tensor_copy(
                out=GQ[:, h0:h0 + nh, :], in_=g5[:, i, :].unsqueeze(1).to_broadcast([128, nh, 64])),
                reads=[("g5", i)], writes=[("GQ", i)])
        gqk = [("GQ", i) for i in range(5)]
        P.add("pool", lambda e: e.memset(identf[:, :], 0.0), writes=["identf"])
        P.add("pool", lambda e: asel(e, out=identf[:, :], in_=identf[:, :], pattern=[[-1, 128]],
                                                compare_op=ALU.not_equal, fill=1.0, base=0, channel_multiplier=1),
              reads=["identf"], writes=["identf"])
        P.add("pool", lambda e: e.tensor_copy(out=ident[:, :], in_=identf[:, :]), reads=["identf"], writes=["ident"])
        P.add("pool", lambda e: e.memset(epst[:, :], EPS), writes=["eps"])
        wv = T["w_in"].rearrange("(c p) f -> p c f", p=128)
        for hf in range(2):
            for c in range(8):
                k = hf * 8 + c
                s = k % 3
                P.add("sp", lambda e, s=s, c=c, hf=hf: e.dma_start(out=stage[s][:, :], in_=wv[:, c, hf * HW_:(hf + 1) * HW_]),
                      writes=[("stage", s)], chan=stch[s])
                P.add("dve" if k % 2 == 0 else "pool", lambda e, s=s, c=c, hf=hf: e.tensor_copy(
                    out=win_s[:, c, hf * HW_:(hf + 1) * HW_], in_=stage[s][:, :]),
                    reads=[("stage", s)], writes=[("win", c, hf)])
        wkeys = [("win", c, hf) for c in range(8) for hf in range(2)]
        QKTv = T["QKT"].rearrange("(pr two) d s -> (two d) pr s", two=2)
        for i in range(2):
            P.add("pool", lambda e, i=i: e.memset(vst[i][:, :, :, 64:72], 1.0), writes=[("vst1", i)])
        CH = [(0, 512), (512, 512), (1024, 512), (1536, 512), (2048, 512), (2560, 288)]

        def stageA(t):
            xs = t % 2
            g, k = t // 4, t % 4
            P.add("sp", lambda e, xs=xs, t=t: e.dma_start(out=xn[xs][:, :], in_=h1[t * 128:(t + 1) * 128, :]),
                  writes=[("xn", xs)], chan=cx[xs])
            P.add("act", lambda e, xs=xs: e.activation(out=junk[:, :], in_=xn[xs][:, :], func=AF.Square,
                                                       accum_out=ssq[:, xs:xs + 1]),
                  reads=[("xn", xs)], writes=["junk", ("ssq", xs)])
            P.add("act", lambda e, xs=xs: e.activation(out=rs[:, xs:xs + 1], in_=ssq[:, xs:xs + 1], func=AF.Sqrt,
                                                       scale=1.0 / D, bias=epst[:, 0:1]),
                  reads=[("ssq", xs), "eps"], writes=[("rs", xs)])
            P.add("dve", lambda e, xs=xs: e.reciprocal(out=rs[:, xs:xs + 1], in_=rs[:, xs:xs + 1]),
                  reads=[("rs", xs)], writes=[("rs", xs)])
            P.add("dve", lambda e, xs=xs: e.scalar_tensor_tensor(
                out=nb[xs][:, :], in0=xn[xs][:, :], scalar=rs[:, xs:xs + 1], in1=gB[:, :], op0=ALU.mult, op1=ALU.mult),
                reads=[("xn", xs), ("rs", xs), "gB"], writes=[("nb", xs)])
            for c in range(8):
                P.add("pe", lambda e, xs=xs, c=c: e.transpose(
                    out=psT[xs][:, c * 128:(c + 1) * 128], in_=nb[xs][:, c * 128:(c + 1) * 128], identity=ident[:, :]),
                    reads=[("nb", xs), "ident"], writes=[("psT", xs)] if c == 0 else [])
            P.last_w[("psT", xs)] = P.ops["pe"][-1]
            P.add("act", lambda e, xs=xs: e.copy(out=aT[xs][:, :, :], in_=psT[xs][:, :].rearrange("p (c t) -> p c t", c=8)),
                  reads=[("psT", xs)], writes=[("aT", xs)])
            for ci, (c0, cw) in enumerate(CH):
                pb = (t * 6 + ci) % 4
                for c in range(8):
                    P.add("pe", lambda e, pb=pb, c=c, c0=c0, cw=cw, xs=xs: e.matmul(
                        out=pq[pb][:, 0:cw], lhsT=aT[xs][:, c, :], rhs=win_s[:, c, c0:c0 + cw],
                        start=(c == 0), stop=(c == 7)),
                        reads=[("aT", xs)] + (wkeys if t == 0 else []), writes=[("pq", pb)] if c == 0 else [])
                P.last_w[("pq", pb)] = P.ops["pe"][-1]
                if ci < 4:
                    P.add("act", lambda e, pb=pb, ci=ci, xs=xs: e.copy(
                        out=qk[xs][:, ci * 8:(ci + 1) * 8, :], in_=pq[pb][:, :].rearrange("p (h d) -> p h d", h=8)),
                        reads=[("pq", pb)], writes=[("qk", xs, ci)])
                    P.add("act", lambda e, pb=pb, ci=ci, xs=xs: e.activation(
                        out=sq[xs][:, ci * 8:(ci + 1) * 8, :], in_=pq[pb][:, :].rearrange("p (h d) -> p h d", h=8), func=AF.Square),
                        reads=[("pq", pb)], writes=[("sq", xs, ci)])
                elif ci == 4:
                    P.add("dve", lambda e, pb=pb, g=g, k=k: e.tensor_copy(
                        out=vst[g % 2][:, 0:8, k, 0:64], in_=pq[pb][:, 0:512].rearrange("p (h d) -> p h d", h=8)),
                        reads=[("pq", pb), ("vst1", g % 2)], writes=[("vst", g % 2, k, 0)])
                else:
                    P.add("dve", lambda e, pb=pb, g=g, k=k: e.tensor_copy(
                        out=vst[g % 2][:, 8:12, k, 0:64], in_=pq[pb][:, 0:256].rearrange("p (h d) -> p h d", h=4)),
                        reads=[("pq", pb), ("vst1", g % 2)], writes=[("vst", g % 2, k, 1)])
                    P.add("dve", lambda e, pb=pb, t=t: e.tensor_copy(out=Gall[:, t, :], in_=pq[pb][:, 256:280]),
                          reads=[("pq", pb)], writes=[("Gall", t)])
                    P.add("dve", lambda e, pb=pb, t=t: e.tensor_tensor(out=LFall[:, t, :], in0=pq[pb][:, 280:288], in1=bfg[:, :],
                                                                      op=ALU.add),
                          reads=[("pq", pb), "bfg"], writes=[("LFall", t)])
            if k == 3:
                P.add("sp", lambda e, g=g: e.dma_start(
                    out=T["V"].rearrange("h p t c -> p h (t c)")[:, :, 4 * g * 72:(4 * g + 4) * 72],
                    in_=vst[g % 2][:, :, :, :].rearrange("p h t c -> p h (t c)")),
                    reads=[("vst", g % 2, kk, j) for kk in range(4) for j in range(2)], chan=cv[g % 2])

        def stageB(t):
            xs = t % 2
            g, k = t // 4, t % 4
            P.add("dve", lambda e, xs=xs: e.tensor_reduce(out=hs[xs][:, :], in_=sq[xs][:, :, :], axis=AX.X, op=ALU.add),
                  reads=[("sq", xs, i) for i in range(4)], writes=[("hs", xs)])
            P.add("act", lambda e, xs=xs: e.activation(out=hs[xs][:, :], in_=hs[xs][:, :], func=AF.Sqrt,
                                                       scale=1.0 / 64, bias=epst[:, 0:1]),
                  reads=[("hs", xs), "eps"], writes=[("hs", xs)])
            P.add("dve", lambda e, xs=xs: e.reciprocal(out=hs[xs][:, :], in_=hs[xs][:, :]),
                  reads=[("hs", xs)], writes=[("hs", xs)])
            qkk = [("qk", xs, i) for i in range(4)]
            P.add("dve", lambda e, xs=xs: e.tensor_tensor(
                out=qk[xs][:, 2:30, :], in0=qk[xs][:, 2:30, :], in1=hs[xs][:, 2:30].unsqueeze(2).to_broadcast([128, 28, 64]),
                op=ALU.mult), reads=qkk + [("hs", xs)], writes=qkk)
            P.add("dve", lambda e, xs=xs: e.tensor_tensor(
                out=qk[xs][:, 2:30, :], in0=qk[xs][:, 2:30, :], in1=GQ[:, :, :], op=ALU.mult),
                reads=qkk + gqk, writes=qkk)
            cb = lambda tab, t=t: tab[:, t, :].unsqueeze(1).to_broadcast([128, 14, 8])
            x1 = lambda xs=xs: qk[xs][:, 0:14, 0:8]
            x2 = lambda xs=xs: qk[xs][:, 0:14, 8:16]
            for j, (src, tab) in enumerate(((x1, cosT), (x2, sinT), (x2, cosT), (x1, sinT))):
                P.add("pool", lambda e, j=j, src=src, tab=tab, cb=cb: e.tensor_tensor(
                    out=rt[j][:, :, :], in0=src(), in1=cb(tab), op=ALU.mult),
                    reads=qkk + ["cosT", "sinT"], writes=[("rt", j)])
            P.add("pool", lambda e, x1=x1: e.tensor_tensor(out=x1(), in0=rt[0][:, :, :], in1=rt[1][:, :, :], op=ALU.subtract),
                  reads=[("rt", 0), ("rt", 1)], writes=qkk)
            P.add("pool", lambda e, x2=x2: e.tensor_tensor(out=x2(), in0=rt[2][:, :, :], in1=rt[3][:, :, :], op=ALU.add),
                  reads=[("rt", 2), ("rt", 3)], writes=qkk)
            P.add("pool", lambda e, xs=xs: e.tensor_copy(out=qkb[xs][:, :], in_=qk[xs][:, :, :].rearrange("p h d -> p (h d)")),
                  reads=qkk, writes=[("qkb", xs)])
            for pr in range(16):
                hb = pr // 8
                P.add("pe", lambda e, pr=pr, hb=hb, xs=xs: e.transpose(
                    out=psQ[hb][:, pr % 8, :], in_=qkb[xs][:, pr * 128:(pr + 1) * 128], identity=ident[:, :]),
                    reads=[("qkb", xs), "ident"], writes=[("psQ", hb)] if pr % 8 == 0 else [])
                if pr % 8 == 7:
                    P.last_w[("psQ", hb)] = P.ops["pe"][-1]
                    P.add("act" if hb == 0 else "dve", (lambda e, hb=hb, k=k: e.copy(
                        out=qkT[:, hb * 8:(hb + 1) * 8, k * 128:(k + 1) * 128], in_=psQ[hb][:, :, :])) if hb == 0 else
                        (lambda e, hb=hb, k=k: e.tensor_copy(
                            out=qkT[:, hb * 8:(hb + 1) * 8, k * 128:(k + 1) * 128], in_=psQ[hb][:, :, :])),
                        reads=[("psQ", hb)], writes=[("qkT", k, hb)])
            if k == 3:
                P.add("sp", lambda e, g=g: e.dma_start(out=QKTv[:, :, g * 512:(g + 1) * 512], in_=qkT[:, :, :]),
                      reads=[("qkT", kk, hb) for kk in range(4) for hb in range(2)], chan=cq)

        stageA(0)
        for t in range(NT):
            if t + 1 < NT:
                stageA(t + 1)
            stageB(t)

        P.add("act", lambda e: e.activation(out=Gall[:, :, :], in_=Gall[:, :, :], func=AF.Sigmoid),
              reads=[("Gall", t) for t in range(NT)], writes=["GallF"])
        P.add("sp", lambda e: e.dma_start(out=T["G"], in_=Gall[:, :, :].rearrange("p t c -> p (t c)")),
              reads=["GallF"], chan=cf)
        P.add("act", lambda e: e.activation(out=LFall[:, :, :], in_=LFall[:, :, :], func=AF.Exp, scale=-1.0),
              reads=[("LFall", t) for t in range(NT)], writes=["LF1"])
        P.add("act", lambda e: e.activation(out=LFall[:, :, :], in_=LFall[:, :, :], func=AF.Ln, bias=1.0),
              reads=["LF1"], writes=["LF2"])
        P.add("dve", lambda e: e.tensor_scalar(out=LFall[:, :, :], in0=LFall[:, :, :], scalar1=-1.0, scalar2=None, op0=ALU.mult),
              reads=["LF2"], writes=["LF3"])
        P.add("sp", lambda e: e.dma_start(out=T["LF"], in_=LFall[:, :, :].rearrange("p t c -> p (t c)")),
              reads=["LF3"], chan=cf)

    run_phase(nc, build)


_FILL = {}


def asel(e, **kw):
    v = float(kw.pop("fill"))
    r = _FILL.get(v)
    if r is None:
        r = e.alloc_register()
        e.reg_mov(r, v)
        _FILL[v] = r
    return e.affine_select(fill=r, **kw)


def make_ident(P, nc, es):
    ident = sb(nc, es, "ident", [128, 128], BF16)
    identf = sb(nc, es, "identf", [128, 128], F32)
    P.add("pool", lambda e: e.memset(identf[:, :], 0.0), writes=["identf"])
    P.add("pool", lambda e: asel(e, out=identf[:, :], in_=identf[:, :], pattern=[[-1, 128]],
                                            compare_op=ALU.not_equal, fill=1.0, base=0, channel_multiplier=1),
          reads=["identf"], writes=["identf"])
    P.add("pool", lambda e: e.tensor_copy(out=ident[:, :], in_=identf[:, :]), reads=["identf"], writes=["ident"])
    return ident, identf


def cmp_phase(nc, T):
    def build(P, es):
        ident, identf = make_ident(P, nc, es)
        epst = sb(nc, es, "epst", [128, 1], F32)
        P.add("pool", lambda e: e.memset(epst[:, :], EPS), writes=["eps"])
        tok = sb(nc, es, "tok", [64, 4, S], BF16)
        w1s = [sb(nc, es, "w1s%d" % i, [64, 32, 256], BF16) for i in range(2)]
        stg = [sb(nc, es, "cstg%d" % i, [64, 32, 256], F32) for i in range(2)]
        w1f = [sb(nc, es, "w1f%d" % i, [128, 16, 256], F32) for i in range(2)]
        posr = sb(nc, es, "posr", [16, 2, 128], F32)
        posc = sb(nc, es, "posc", [128, 2, 16], F32)
        w2f = sb(nc, es, "w2f", [128, 2, 2, 64], F32)
        w2s = sb(nc, es, "w2s", [128, 2, 2, 64], BF16)
        biasT = sb(nc, es, "biasT", [128, 4], F32)
        gk = sb(nc, es, "gk", [128, 64], F32)
        hidT = [sb(nc, es, "hidT%d" % i, [128, 2, 256], BF16) for i in range(2)]
        ssq = sb(nc, es, "ssq", [128, 4], F32)
        junk = sb(nc, es, "junk", [128, 64], F32)
        kcb = [sb(nc, es, "kcb%d" % i, [128, 64], BF16) for i in range(2)]
        kcT = [sb(nc, es, "kcT%d" % i, [64, 256], BF16) for i in range(2)]
        vce = [sb(nc, es, "vce%d" % i, [128, 2, 65], BF16) for i in range(2)]
        psHf = [ps(nc, es, "psH%d" % i, [128, 512], F32) for i in range(2)]
        psH = [t[:, 0:256] for t in psHf]
        psOf = [ps(nc, es, "psO%d" % i, [128, 512], F32) for i in range(2)]
        psO = [t[:, 0:64] for t in psOf]
        psBf = ps(nc, es, "psB", [128, 512], F32)
        psB = psBf[:, 0:4]
        psPf = ps(nc, es, "psP", [128, 512], F32)
        psP = psPf[:, 0:32].rearrange("p (a b) -> p a b", a=2)
        psKf = ps(nc, es, "psK", [128, 1024], BF16)
        psK = psKf[0:64, 0:128]
        c0 = P.chan()
        c1 = [P.chan() for _ in range(2)]
        co = P.chan()

        for j, h in enumerate((0, 1, 30, 31)):
            P.add("sp", lambda e, j=j, h=h: e.dma_start(out=tok[:, j, :], in_=T["QKT"][h, :, :]), writes=[("tok", j)], chan=c0)
        P.add("sp", lambda e: e.dma_start(out=gk[:, :], in_=T["k_norm_cmp"].partition_broadcast(128)), writes=["gk"], chan=c0)
        for kv, nm in enumerate(("cmp_pos_k", "cmp_pos_v")):
            P.add("sp", lambda e, kv=kv, nm=nm: e.dma_start(
                out=posr[:, kv, :], in_=T[nm].rearrange("(c a) d -> c (a d)", a=2)), writes=[("posr", kv)], chan=c0)
        for kv, nm in enumerate(("cmp_k_w2", "cmp_v_w2")):
            P.add("sp", lambda e, kv=kv, nm=nm: e.dma_start(
                out=w2f[:, kv, :, :], in_=T[nm].rearrange("(c p) d -> p c d", p=128)), writes=[("w2f", kv)], chan=c0)
        for kv, nm in enumerate(("cmp_k_w1", "cmp_v_w1")):
            P.add("sp", lambda e, kv=kv, nm=nm: e.dma_start(
                out=w1f[kv][:, :, :], in_=T[nm].rearrange("(c p) h -> p c h", p=128)), writes=[("w1f", kv)], chan=c0)
        c0.seal()
        for kv, nm in enumerate(("cmp_k_w1", "cmp_v_w1")):
            P.add("sp", lambda e, kv=kv, nm=nm: e.dma_start(
                out=stg[kv][:, :, :], in_=T[nm].rearrange("(l d) h -> d l h", d=64)), writes=[("stg", kv)], chan=c1[kv])
            P.add("dve" if kv == 0 else "pool", lambda e, kv=kv: e.tensor_copy(out=w1s[kv][:, :, :], in_=stg[kv][:, :, :]),
                  reads=[("stg", kv)], writes=[("w1s", kv)])
        P.add("dve", lambda e: e.tensor_copy(out=w2s[:, :, :, :], in_=w2f[:, :, :, :]),
              reads=[("w2f", 0), ("w2f", 1)], writes=["w2s"])
        for kv in range(2):
            P.add("pe", lambda e, kv=kv: e.transpose(out=psP[:, kv, :], in_=posr[:, kv, :], identity=identf[0:16, 0:16]),
                  reads=[("posr", kv), "identf"], writes=[("psP", kv)])
        P.add("dve", lambda e: e.tensor_copy(out=posc[:, :, :], in_=psP),
              reads=[("psP", 0), ("psP", 1)], writes=["posc"])
        for kv in range(2):
            for hc in range(2):
                for c in range(16):
                    P.add("pe", lambda e, kv=kv, hc=hc, c=c: e.matmul(
                        out=psB[:, kv * 2 + hc:kv * 2 + hc + 1], lhsT=w1f[kv][:, c, hc * 128:(hc + 1) * 128],
                        rhs=posc[:, kv, c:c + 1], start=(c == 0), stop=(c == 15)),
                        reads=[("w1f", kv), "posc"], writes=["psB"] if (c == 0 and kv == 0 and hc == 0) else [])
        P.last_w["psB"] = P.ops["pe"][-1]
        P.add("dve", lambda e: e.tensor_copy(out=biasT[:, :], in_=psB), reads=["psB"], writes=["biasT"])
        for i in range(2):
            P.add("pool", lambda e, i=i: e.memset(kcb[i][:, :], 0.0), writes=[("kcb", i)])
            P.add("pool", lambda e, i=i: e.memset(vce[i][:, :, :], 0.0), writes=[("vce", i)])
            P.add("pool", lambda e, i=i: e.memset(vce[i][:, :, 64:65], 1.0), reads=[("vce", i)], writes=[("vce", i)])
            P.add("pool", lambda e, i=i: e.memset(hidT[i][:, :, :], 0.0), writes=[("hidT", i, 0), ("hidT", i, 1)])
        VCv = T["VC"].rearrange("h (c p) e -> h p c e", p=128)
        it = 0
        for kv in range(2):
            for head in range(2):
                sl = it % 2
                it += 1
                tv = tok[:, kv * 2 + head, :].rearrange("p (n r) -> p n r", r=16)
                for hc in range(2):
                    for l in range(32):
                        q, r = l // 16, l % 16
                        P.add("pe", lambda e, kv=kv, hc=hc, l=l, q=q, r=r, tv=tv: e.matmul(
                            out=psH[hc][:, 0:255], lhsT=w1s[kv][:, l, hc * 128:(hc + 1) * 128], rhs=tv[:, q:q + 255, r],
                            start=(l == 0), stop=(l == 31)),
                            reads=[("tok", kv * 2 + head), ("w1s", kv)], writes=[("psH", hc)] if l == 0 else [])
                    P.last_w[("psH", hc)] = P.ops["pe"][-1]
                    P.add("act", lambda e, kv=kv, hc=hc, sl=sl: e.activation(
                        out=hidT[sl][:, hc, 0:255], in_=psH[hc][:, 0:255], func=AF.Silu,
                        bias=biasT[:, kv * 2 + hc:kv * 2 + hc + 1]),
                        reads=[("psH", hc), "biasT"], writes=[("hidT", sl, hc)])
                for ci, (n0, nn) in enumerate(((0, 128), (128, 127))):
                    for hc in range(2):
                        P.add("pe", lambda e, kv=kv, hc=hc, sl=sl, ci=ci, n0=n0, nn=nn: e.matmul(
                            out=psO[ci][0:nn, :], lhsT=hidT[sl][:, hc, n0:n0 + nn], rhs=w2s[:, kv, hc, :],
                            start=(hc == 0), stop=(hc == 1)),
                            reads=[("hidT", sl, 0), ("hidT", sl, 1), "w2s"], writes=[("psO", ci)] if hc == 0 else [])
                    P.last_w[("psO", ci)] = P.ops["pe"][-1]
                    if kv == 0:
                        col = head * 2 + ci
                        P.add("act", lambda e, ci=ci, nn=nn, col=col: e.activation(
                            out=junk[0:nn, :], in_=psO[ci][0:nn, :], func=AF.Square, accum_out=ssq[0:nn, col:col + 1]),
                            reads=[("psO", ci)], writes=["junk", ("ssq", col)])
                        P.add("act", lambda e, nn=nn, col=col: e.activation(
                            out=ssq[0:nn, col:col + 1], in_=ssq[0:nn, col:col + 1], func=AF.Sqrt, scale=1.0 / 64,
                            bias=epst[0:nn, 0:1]), reads=[("ssq", col), "eps"], writes=[("ssq", col)])
                        P.add("dve", lambda e, nn=nn, col=col: e.reciprocal(out=ssq[0:nn, col:col + 1], in_=ssq[0:nn, col:col + 1]),
                              reads=[("ssq", col)], writes=[("ssq", col)])
                        P.add("dve", lambda e, ci=ci, nn=nn, col=col: e.scalar_tensor_tensor(
                            out=kcb[ci][0:nn, :], in0=psO[ci][0:nn, :], scalar=ssq[0:nn, col:col + 1], in1=gk[0:nn, :],
                            op0=ALU.mult, op1=ALU.mult), reads=[("psO", ci), ("ssq", col), "gk"], writes=[("kcb", ci)])
                        P.add("pe", lambda e, ci=ci: e.transpose(out=psK, in_=kcb[ci][:, :], identity=ident[:, :]),
                              reads=[("kcb", ci), "ident"], writes=["psK"])
                        P.add("act", lambda e, head=head, n0=n0: e.copy(out=kcT[head][:, n0:n0 + 128], in_=psK),
                              reads=["psK"], writes=[("kcT", head, n0)])
                    else:
                        P.add("dve", lambda e, ci=ci, nn=nn, head=head: e.tensor_copy(
                            out=vce[head][0:nn, ci, 0:64], in_=psO[ci][0:nn, :]), reads=[("psO", ci)], writes=[("vce", head)])
                if kv == 0:
                    P.add("sp", lambda e, head=head: e.dma_start(out=T["KCT"][head, :, :], in_=kcT[head][:, :]),
                          reads=[("kcT", head, 0), ("kcT", head, 128)], chan=co)
                else:
                    P.add("sp", lambda e, head=head: e.dma_start(out=VCv[head], in_=vce[head][:, :, :]),
                          reads=[("vce", head)], chan=co)

    run_phase(nc, build)


class UnitPipe:
    def __init__(self, P, psS, PT, depth=2):
        self.P, self.psS, self.PT, self.depth = P, psS, PT, depth
        self.q = []
        self.u = 0

    def push(self, lhsT, rhs, vlhsT, pacc, acc_key, first, last, mask, kdeps, bias=None, bkeys=(), post=None):
        P = self.P
        u = self.u
        self.u += 1
        sb_, pb = u % len(self.psS), u % len(self.PT)
        psS, PT = self.psS[sb_], self.PT[pb]
        P.add("pe", lambda e: e.matmul(out=psS[:, :], lhsT=lhsT, rhs=rhs, start=True, stop=True),
              reads=kdeps, writes=[("psS", sb_)])
        if bias is None:
            P.add("act", lambda e: e.activation(out=PT[:, :], in_=psS[:, :], func=AF.Exp, scale=0.125),
                  reads=[("psS", sb_)], writes=[("PT", pb)])
        else:
            P.add("act", lambda e: e.activation(out=PT[:, :], in_=psS[:, :], func=AF.Exp, scale=0.125, bias=bias),
                  reads=[("psS", sb_)] + list(bkeys), writes=[("PT", pb)])
        if mask is not None:
            base, cm, step = mask
            P.add("pool", lambda e: asel(e, out=PT[:, :], in_=PT[:, :], pattern=[[step, 512]], compare_op=ALU.is_ge,
                                         fill=0.0, base=base, channel_multiplier=cm), reads=[("PT", pb)], writes=[("PT", pb)])
        self.q.append((PT, pb, vlhsT, pacc, acc_key, first, last, kdeps, post))
        if len(self.q) > self.depth:
            self._pv()

    def _pv(self):
        P = self.P
        PT, pb, vlhsT, pacc, acc_key, first, last, kdeps, post = self.q.pop(0)
        P.add("pe", lambda e: e.matmul(out=pacc[0:65, :], lhsT=vlhsT, rhs=PT[:, :], start=first, stop=last),
              reads=[("PT", pb)] + list(kdeps), writes=[acc_key] if first else [])
        if last:
            P.last_w[acc_key] = P.ops["pe"][-1]
            if post is not None:
                post()

    def flush(self):
        while self.q:
            self._pv()


def nsa_phase(nc, T):
    BIG = 2048.0
    TINY = 1e-30

    def build(P, es):
        ident, identf = make_ident(P, nc, es)
        QB = sb(nc, es, "QB", [128, 4, S], BF16)
        KE = sb(nc, es, "KE", [128, S], BF16)
        KW = sb(nc, es, "KW", [128, S], BF16)
        KC = sb(nc, es, "KC", [128, 256], BF16)
        Vs = sb(nc, es, "Vs", [128, NT, 72], BF16)
        Vw = sb(nc, es, "Vw", [128, NT, 72], BF16)
        VCs = sb(nc, es, "VCs", [128, 2, 72], BF16)
        OVf = sb(nc, es, "OVf", [128, 2, 72], F32)
        OV = sb(nc, es, "OV", [128, 2, 72], BF16)
        Gs = sb(nc, es, "Gs", [128, NT, 24], F32)
        ET = [[sb(nc, es, "ET%d_%d" % (i, j), [128, 512], BF16) for j in range(2)] for i in range(2)]
        PT = [sb(nc, es, "PT%d" % i, [128, 512], BF16) for i in range(4)]
        OCs = sb(nc, es, "OCs", [65, 2, 4, 512], F32)
        OWs = sb(nc, es, "OWs", [65, 4, 512], F32)
        OSs = [sb(nc, es, "OSs%d" % i, [65, 512], F32) for i in range(2)]
        imp = sb(nc, es, "imp", [128, 4, 64], F32)
        impt = sb(nc, es, "impt", [128, 4, 64], F32)
        impm = [sb(nc, es, "impm%d" % i, [128, 64], F32) for i in range(2)]
        rd4 = sb(nc, es, "rd4", [128, 4], F32)
        m1 = sb(nc, es, "m1", [128, 8], F32)
        m2 = sb(nc, es, "m2", [128, 8], F32)
        tmp = sb(nc, es, "tmp", [128, 64], F32)
        thr = sb(nc, es, "thr", [128, 1], F32)
        BN = [sb(nc, es, "BN%d" % i, [128, 128], BF16) for i in range(4)]
        dn = [sb(nc, es, "dn%d" % i, [128, 3], F32) for i in range(2)]
        ost = [sb(nc, es, "ost%d" % i, [128, 4, 256], F32) for i in range(2)]
        psS = [ps(nc, es, "psS%d" % i, [128, 512], F32) for i in range(3)]
        psOC = ps(nc, es, "psOC", [128, 512], F32)
        psOS = ps(nc, es, "psOS", [128, 512], F32)
        psOW = ps(nc, es, "psOW", [128, 512], F32)
        psIB = ps(nc, es, "psIB", [128, 512], F32)
        psI = psIB[:, 0:260].rearrange("p (a b) -> p a b", a=4)
        psBT = psIB[:, 320:384].bitcast(BF16)
        psFb = ps(nc, es, "psFb", [128, 512], F32)
        psF = psFb[:, 0:195].rearrange("p (a b) -> p a b", a=3)
        c0 = P.chan()
        cks = [P.chan() for _ in range(2)]
        cst = [P.chan() for _ in range(2)]

        P.add("sp", lambda e: e.dma_start(out=Gs[:, :, :].rearrange("p t c -> p (t c)"), in_=T["G"]), writes=["Gs"], chan=c0)
        P.add("pool", lambda e: e.memset(KE[64:128, :], BIG), writes=["KEm"])
        P.add("pool", lambda e: asel(e, out=KE[64:128, :], in_=KE[64:128, :], pattern=[[1, S]], compare_op=ALU.is_ge,
                                                fill=0.0, base=0, channel_multiplier=-64), reads=["KEm"], writes=["KEm"])
        P.add("pool", lambda e: asel(e, out=KE[64:128, :], in_=KE[64:128, :], pattern=[[-1, S]], compare_op=ALU.is_ge,
                                                fill=0.0, base=63, channel_multiplier=64), reads=["KEm"], writes=["KEm"])
        P.add("pool", lambda e: e.memset(KW[64:128, :], 0.0), writes=["KW0"])
        P.add("pool", lambda e: e.memset(KC[64:128, :], 0.0), writes=["KC0"])
        for g in range(4):
            P.add("pool", lambda e, g=g: e.memset(QB[64:128, g, :], 0.0), writes=[("QB0", g)])
        P.add("pool", lambda e: e.memset(OVf[:, :, :], 1.0), writes=["OVf"])
        for nt in range(2):
            P.add("pool", lambda e, nt=nt: asel(e,
                out=OVf[:, nt, 0:64], in_=OVf[:, nt, 0:64], pattern=[[64, 64]], compare_op=ALU.is_ge, fill=0.0,
                base=63 - 2048 * nt, channel_multiplier=-16), reads=["OVf"], writes=["OVf"])
            P.add("pool", lambda e, nt=nt: asel(e,
                out=OVf[:, nt, 0:64], in_=OVf[:, nt, 0:64], pattern=[[-64, 64]], compare_op=ALU.is_ge, fill=0.0,
                base=2048 * nt + 31, channel_multiplier=16), reads=["OVf"], writes=["OVf"])
        P.add("pool", lambda e: e.tensor_copy(out=OV[:, :, :], in_=OVf[:, :, :]), reads=["OVf"], writes=["OV"])
        for i in range(4):
            P.add("pool", lambda e, i=i: e.memset(BN[i][:, 0:64], 0.0), writes=[("BN0", i)])
        OAv = T["OA"].rearrange("(t p) c -> p t c", p=128)

        def mask_ge(tile, base, cm, step):
            return lambda e: asel(e, out=tile[:, :], in_=tile[:, :], pattern=[[step, 512]], compare_op=ALU.is_ge,
                                             fill=0.0, base=base, channel_multiplier=cm)

        pipe = UnitPipe(P, psS, PT, depth=2)

        for kh in range(DBG['kh']):
            ck = cks[kh]
            for g in range(4):
                P.add("sp", lambda e, g=g, kh=kh: e.dma_start(out=QB[0:64, g, :], in_=T["QKT"][2 + 4 * kh + g, :, :]),
                      writes=[("QBq", g)], chan=ck)
            P.add("sp", lambda e, kh=kh: e.dma_start(out=KE[0:64, :], in_=T["QKT"][10 + kh, :, :]), writes=["KEk"], chan=ck)
            P.add("sp", lambda e, kh=kh: e.dma_start(out=KW[0:64, :], in_=T["QKT"][12 + kh, :, :]), writes=["KW"], chan=ck)
            P.add("sp", lambda e, kh=kh: e.dma_start(out=KC[0:64, :], in_=T["KCT"][kh, :, :]), writes=["KC"], chan=ck)
            P.add("sp", lambda e, kh=kh: e.dma_start(out=Vs[:, :, :], in_=T["V"][kh]), writes=["Vs"], chan=ck)
            P.add("sp", lambda e, kh=kh: e.dma_start(out=Vw[:, :, :], in_=T["V"][2 + kh]), writes=["Vw"], chan=ck)
            P.add("sp", lambda e, kh=kh: e.dma_start(out=VCs[:, :, 0:65], in_=T["VC"][kh].rearrange("(c p) e -> p c e", p=128)),
                  writes=["VCs"], chan=ck)
            ck.seal()
            def emit_AB1(i):
                qsl = slice(i * 512, (i + 1) * 512)
                nts = [0] if i < 4 else [0, 1]

                def s1(g):
                    for nt in nts:
                        u = pipe.u
                        pipe.u += 1
                        sb_ = u % 3
                        et = ET[g % 2][nt]
                        P.add("pe", lambda e, nt=nt, g=g, sb_=sb_, qsl=qsl: e.matmul(
                            out=psS[sb_][:, :], lhsT=KC[:, nt * 128:(nt + 1) * 128], rhs=QB[:, g, qsl], start=True, stop=True),
                            reads=["KC", "KC0", ("QBq", g), ("QB0", g)] + ([("QBm", tt) for tt in range(4)] if i > 0 or kh > 0 else []), writes=[("psS", sb_)])
                        P.add("act", lambda e, et=et, sb_=sb_: e.activation(out=et[:, :], in_=psS[sb_][:, :], func=AF.Exp, scale=0.125),
                              reads=[("psS", sb_)], writes=[("ET", g % 2, nt)])
                        P.add("pool", mask_ge(et, 512 * i - 2048 * nt - 31, -16, 1), reads=[("ET", g % 2, nt)], writes=[("ET", g % 2, nt)])

                def s2(g):
                    for j, nt in enumerate(nts):
                        P.add("pe", lambda e, nt=nt, j=j, g=g: e.matmul(
                            out=psOC[0:65, :], lhsT=VCs[:, nt, 0:65], rhs=ET[g % 2][nt][:, :], start=(j == 0), stop=(j == len(nts) - 1)),
                            reads=[("ET", g % 2, nt), "VCs"], writes=["psOC"] if j == 0 else [])
                    P.last_w["psOC"] = P.ops["pe"][-1]
                    P.add("act", lambda e, g=g, i=i: e.copy(out=OCs[:, i % 2, g, :], in_=psOC[0:65, :]), reads=["psOC"], writes=[("OCs", i % 2, g)])
                    for tt in range(4):
                        for j, nt in enumerate(nts):
                            P.add("pe", lambda e, nt=nt, j=j, tt=tt, g=g: e.matmul(
                                out=psI[:, tt, :], lhsT=ET[g % 2][nt][:, tt * 128:(tt + 1) * 128], rhs=OV[:, nt, 0:65],
                                start=(j == 0), stop=(j == len(nts) - 1)),
                                reads=[("ET", g % 2, nt), "OV"], writes=["psI"] if (j == 0 and tt == 0) else [])
                    P.last_w["psI"] = P.ops["pe"][-1]
                    P.add("dve", lambda e: e.tensor_scalar(out=rd4[:, :], in0=psI[:, :, 64], scalar1=TINY, scalar2=None, op0=ALU.max),
                          reads=["psI"], writes=["rd4"])
                    P.add("dve", lambda e: e.reciprocal(out=rd4[:, :], in_=rd4[:, :]), reads=["rd4"], writes=["rd4"])
                    if g == 0:
                        P.add("dve", lambda e: e.tensor_tensor(
                            out=imp[:, :, :], in0=psI[:, :, 0:64], in1=rd4[:, :].unsqueeze(2).to_broadcast([128, 4, 64]), op=ALU.mult),
                            reads=["psI", "rd4"], writes=["imp"])
                    else:
                        P.add("dve", lambda e: e.tensor_tensor(
                            out=impt[:, :, :], in0=psI[:, :, 0:64], in1=rd4[:, :].unsqueeze(2).to_broadcast([128, 4, 64]), op=ALU.mult),
                            reads=["psI", "rd4"], writes=["impt"])
                        P.add("dve", lambda e: e.tensor_tensor(out=imp[:, :, :], in0=imp[:, :, :], in1=impt[:, :, :], op=ALU.add),
                              reads=["imp", "impt"], writes=["imp"])

                s1(0)
                for g in range(4):
                    if g < 3:
                        s1(g + 1)
                    s2(g)
                for tt in range(4):
                    bs = tt % 2
                    t0 = 512 * i + 128 * tt
                    P.add("pool", lambda e, tt=tt, bs=bs, t0=t0: asel(e,
                        out=impm[bs][:, :], in_=imp[:, tt, :], pattern=[[-64, 64]], compare_op=ALU.is_ge, fill=1.0e6,
                        base=t0 - 128, channel_multiplier=1), reads=["imp"], writes=[("impm", bs)])
                    P.add("pool", lambda e, bs=bs, t0=t0: asel(e,
                        out=impm[bs][:, :], in_=impm[bs][:, :], pattern=[[-64, 64]], compare_op=ALU.is_ge, fill=-1.0,
                        base=t0, channel_multiplier=1), reads=[("impm", bs)], writes=[("impm", bs)])
                    P.add("pool", lambda e, bs=bs: e.memset(impm[bs][:, 0:1], 1.0e6), reads=[("impm", bs)], writes=[("impm", bs)])
                    P.add("dve", lambda e, bs=bs: e.max(out=m1[:, :], in_=impm[bs][:, :]), reads=[("impm", bs)], writes=["m1"])
                    P.add("dve", lambda e, bs=bs: e.match_replace(out=tmp[:, :], in_to_replace=m1[:, :], in_values=impm[bs][:, :],
                                                                  imm_value=-2.0), reads=[("impm", bs), "m1"], writes=["tmp"])
                    P.add("dve", lambda e: e.max(out=m2[:, :], in_=tmp[:, :]), reads=["tmp"], writes=["m2"])
                    P.add("dve", lambda e: e.tensor_scalar(out=thr[:, :], in0=m2[:, 7:8], scalar1=0.0, scalar2=None, op0=ALU.max),
                          reads=["m2"], writes=["thr"])
                    P.add("dve", lambda e, bs=bs, tt=tt: e.tensor_scalar(
                        out=BN[tt][:, 64:128], in0=impm[bs][:, :], scalar1=thr[:, 0:1], scalar2=1.0, op0=ALU.is_ge, op1=ALU.subtract),
                        reads=[("impm", bs), "thr", ("BN0", tt)], writes=[("BN", tt)])

            for i in range(DBG['ng']):
                qsl = slice(i * 512, (i + 1) * 512)
                if i == 0:
                    emit_AB1(0)
                for g in range(4):
                    kts = list(range(max(0, 4 * i - 4), 4 * i + 4))
                    for j, kt in enumerate(kts):
                        if kt >= 4 * i:
                            ms = (-128 * (kt - 4 * i), -1, 1)
                        else:
                            ms = (128 * (kt - 4 * i + 4) - 1, 1, -1)
                        post = (lambda g=g: P.add("dve", lambda e: e.tensor_copy(out=OWs[:, g, :], in_=psOW[0:65, :]),
                                                  reads=["psOW"], writes=[("OWs", g)]))
                        pipe.push(KW[:, kt * 128:(kt + 1) * 128], QB[:, g, qsl], Vw[:, kt, 0:65], psOW, "psOW",
                                  j == 0, j == len(kts) - 1, ms, ["KW", "KW0", ("QBq", g), ("QB0", g), "Vw"], post=post)
                for tt in range(4):
                    t0 = 512 * i + 128 * tt
                    P.add("pe", lambda e, tt=tt: e.transpose(out=psBT, in_=BN[tt][:, :], identity=ident[:, :]),
                          reads=[("BN", tt), ("BN0", tt), "ident"], writes=["psBT"])
                    P.add("act", lambda e, t0=t0: e.copy(out=QB[64:128, :, t0:t0 + 128],
                                                         in_=psBT[64:128].unsqueeze(1).to_broadcast([64, 4, 128])),
                          reads=["psBT"] + [("QB0", g_) for g_ in range(4)], writes=[("QBm", tt)])

                def finalize(g):
                    osl = g % 2
                    hd = kh * 4 + g
                    for tt in range(4):
                        tile_i = 4 * i + tt
                        ds = tt % 2
                        tsl = slice(tt * 128, (tt + 1) * 128)
                        for b, (src, key) in enumerate(((OCs[:, i % 2, g, tsl], ("OCs", i % 2, g)), (OSs[osl][:, tsl], ("OSs", osl)),
                                                         (OWs[:, g, tsl], ("OWs", g)))):
                            P.add("pe", lambda e, b=b, src=src: e.transpose(out=psF[:, b, :], in_=src, identity=identf[0:65, 0:65]),
                                  reads=[key, "identf"], writes=["psF"] if b == 0 else [])
                        P.last_w["psF"] = P.ops["pe"][-1]
                        P.add("dve", lambda e, ds=ds: e.tensor_scalar(out=dn[ds][:, :], in0=psF[:, :, 64], scalar1=TINY, scalar2=None,
                                                                      op0=ALU.max), reads=["psF"], writes=[("dn", ds)])
                        P.add("dve", lambda e, ds=ds: e.reciprocal(out=dn[ds][:, :], in_=dn[ds][:, :]), reads=[("dn", ds)], writes=[("dn", ds)])
                        P.add("dve", lambda e, ds=ds, tile_i=tile_i, hd=hd: e.tensor_tensor(
                            out=dn[ds][:, :], in0=dn[ds][:, :], in1=Gs[:, tile_i, hd * 3:hd * 3 + 3], op=ALU.mult),
                            reads=[("dn", ds), "Gs"], writes=[("dn", ds)])
                        oo = ost[i % 2][:, tt, g * 64:(g + 1) * 64]
                        P.add("dve", lambda e, ds=ds, oo=oo: e.tensor_scalar(out=oo, in0=psF[:, 0, 0:64], scalar1=dn[ds][:, 0:1],
                                                                             scalar2=None, op0=ALU.mult),
                              reads=["psF", ("dn", ds)], writes=[("ost", i % 2, tt, g)])
                        for b in (1, 2):
                            P.add("dve", lambda e, ds=ds, oo=oo, b=b: e.scalar_tensor_tensor(
                                out=oo, in0=psF[:, b, 0:64], scalar=dn[ds][:, b:b + 1], in1=oo, op0=ALU.mult, op1=ALU.add),
                                reads=["psF", ("dn", ds), ("ost", i % 2, tt, g)], writes=[("ost", i % 2, tt, g)])

                pending = []
                for g in range(4):
                    kts = list(range(0, 4 * i + 4))
                    osl = g % 2
                    for j, kt in enumerate(kts):
                        ms = (-128 * (kt - 4 * i), -1, 1) if kt >= 4 * i else None

                        def post(g=g, osl=osl):
                            P.add("dve", lambda e: e.tensor_copy(out=OSs[osl][:, :], in_=psOS[0:65, :]), reads=["psOS"], writes=[("OSs", osl)])
                            pending.append(g)
                        pipe.push(KE[:, kt * 128:(kt + 1) * 128], QB[:, g, qsl], Vs[:, kt, 0:65], psOS, "psOS",
                                  j == 0, j == len(kts) - 1, ms,
                                  ["KEk", "KEm", ("QBq", g), "Vs"] + [("QBm", tt) for tt in range(4)], post=post)
                        if j == 3 and pending:
                            finalize(pending.pop(0))
                    if g == 1 and i + 1 < DBG['ng']:
                        emit_AB1(i + 1)
                pipe.flush()
                while pending:
                    finalize(pending.pop(0))
                if "DBGB" in T and kh == 0:
                    P.add("sp", lambda e, qsl=qsl: e.dma_start(out=T["DBGB"][:, qsl], in_=QB[64:128, 0, qsl]),
                          reads=[("QBm", tt) for tt in range(4)], chan=c0)
                P.add("sp", lambda e, i=i, kh=kh: e.dma_start(out=OAv[:, 4 * i:4 * i + 4, kh * 256:(kh + 1) * 256], in_=ost[i % 2][:, :, :]),
                      reads=[("ost", i % 2, tt, g) for tt in range(4) for g in range(4)], chan=cst[i % 2])

    run_phase(nc, build)


def fox_phase(nc, T):
    TINY = 1e-30

    def build(P, es):
        ident, identf = make_ident(P, nc, es)
        QT = [sb(nc, es, "QT%d" % i, [128, S], BF16) for i in range(2)]
        KT = [sb(nc, es, "KT%d" % i, [128, S], BF16) for i in range(2)]
        Vf = [sb(nc, es, "Vf%d" % i, [128, NT, 72], BF16) for i in range(2)]
        lf = sb(nc, es, "lf", [128, NT, 8], F32)
        U = sb(nc, es, "U", [128, 128], F32)
        ONES = sb(nc, es, "ONES", [128, 128], F32)
        ones32 = sb(nc, es, "ones32", [128, NT], F32)
        cin = sb(nc, es, "cin", [128, NT, 8], F32)
        tot = sb(nc, es, "tot", [128, NT, 8], F32)
        incl = sb(nc, es, "incl", [128, NT, 8], F32)
        call = sb(nc, es, "call", [128, NT, 8], F32)
        biasT = sb(nc, es, "biasT", [128, NG, 8, NT], F32)
        PT = [sb(nc, es, "PT%d" % i, [128, 512], BF16) for i in range(4)]
        OFs = [sb(nc, es, "OFs%d" % i, [65, 512], F32) for i in range(2)]
        dn = [sb(nc, es, "dn%d" % i, [128, 1], F32) for i in range(2)]
        ostf = [sb(nc, es, "ostf%d" % i, [128, 4, 64], F32) for i in range(2)]
        psS = [ps(nc, es, "psS%d" % i, [128, 512], F32) for i in range(3)]
        psO = [ps(nc, es, "psO%d" % i, [128, 512], F32) for i in range(2)]
        psFF = [ps(nc, es, "psFF%d" % i, [128, 512], F32) for i in range(2)]
        psF = [psFF[0][:, 0:65], psFF[1][:, 0:65]]
        psC = psS[0][:, 0:NT * 8]
        psTt = psS[1][:, 0:NT * 8]
        c0 = P.chan()
        ckh = [P.chan() for _ in range(2)]
        cst = [P.chan() for _ in range(2)]
        OAv = T["OA"].rearrange("(t p) c -> p t c", p=128)

        P.add("sp", lambda e: e.dma_start(out=lf[:, :, :].rearrange("p t c -> p (t c)"), in_=T["LF"]), writes=["lf"], chan=c0)
        P.add("pool", lambda e: e.memset(U[:, :], 1.0), writes=["U"])
        P.add("pool", lambda e: asel(e, out=U[:, :], in_=U[:, :], pattern=[[1, 128]], compare_op=ALU.is_ge, fill=0.0,
                                     base=0, channel_multiplier=-1), reads=["U"], writes=["U"])
        P.add("pool", lambda e: e.memset(ONES[:, :], 1.0), writes=["ONES"])
        for i in range(2):
            P.add("pool", lambda e, i=i: e.memset(QT[i][64:128, :], 0.0), writes=[("QT0", i)])
            P.add("pool", lambda e, i=i: e.memset(KT[i][64:128, :], 0.0), writes=[("KT0", i)])
        P.add("pool", lambda e: e.memset(ones32[:, :], 1.0), writes=["ones32"])
        lff = lf[:, :, :].rearrange("p t c -> p (t c)")
        P.add("pe", lambda e: e.matmul(out=psC, lhsT=U[:, :], rhs=lff, start=True, stop=True), reads=["U", "lf"], writes=[("psS", 0)])
        P.add("pe", lambda e: e.matmul(out=psTt, lhsT=ONES[:, :], rhs=lff, start=True, stop=True), reads=["ONES", "lf"], writes=[("psS", 1)])
        P.add("dve", lambda e: e.tensor_copy(out=cin[:, :, :].rearrange("p t c -> p (t c)"), in_=psC), reads=[("psS", 0)], writes=["cin"])
        P.add("dve", lambda e: e.tensor_copy(out=tot[:, :, :].rearrange("p t c -> p (t c)"), in_=psTt), reads=[("psS", 1)], writes=["tot"])
        for h in range(8):
            P.add("dve", lambda e, h=h: e.tensor_tensor_scan(out=incl[:, :, h], data0=ones32[:, :], data1=tot[:, :, h], initial=0.0,
                                                             op0=ALU.mult, op1=ALU.add), reads=["tot", "ones32"], writes=[("incl", h)])
        inck = [("incl", h) for h in range(8)]
        P.add("dve", lambda e: e.tensor_tensor(out=call[:, :, :], in0=incl[:, :, :], in1=tot[:, :, :], op=ALU.subtract),
              reads=inck + ["tot"], writes=["call"])
        P.add("dve", lambda e: e.tensor_tensor(out=call[:, :, :], in0=call[:, :, :], in1=cin[:, :, :], op=ALU.add),
              reads=["call", "cin"], writes=["call"])
        for i in range(NG):
            for h in range(8):
                P.add("dve", lambda e, i=i, h=h: e.tensor_scalar(
                    out=biasT[:, i, h, :], in0=call[:, :, h], scalar1=-1.0, scalar2=incl[:, 4 * i + 1, h:h + 1],
                    op0=ALU.mult, op1=ALU.add), reads=["call"] + inck, writes=[("biasT", i, h)])
        pipe = UnitPipe(P, psS, PT, depth=2)
        fi = 0
        pending = []

        def finalize(h, i, ob):
            for tt in range(4):
                fb = tt % 2
                P.add("pe", lambda e, fb=fb, ob=ob, tt=tt: e.transpose(
                    out=psF[fb], in_=OFs[ob][:, tt * 128:(tt + 1) * 128], identity=identf[0:65, 0:65]),
                    reads=[("OFs", ob), "identf"], writes=[("psF", fb)])
                P.add("dve", lambda e, fb=fb: e.tensor_scalar(out=dn[fb][:, :], in0=psF[fb][:, 64:65], scalar1=TINY, scalar2=None,
                                                              op0=ALU.max), reads=[("psF", fb)], writes=[("dn", fb)])
                P.add("dve", lambda e, fb=fb: e.reciprocal(out=dn[fb][:, :], in_=dn[fb][:, :]), reads=[("dn", fb)], writes=[("dn", fb)])
                P.add("dve", lambda e, fb=fb, ob=ob, tt=tt: e.tensor_scalar(
                    out=ostf[ob][:, tt, :], in0=psF[fb][:, 0:64], scalar1=dn[fb][:, 0:1], scalar2=None, op0=ALU.mult),
                    reads=[("psF", fb), ("dn", fb)], writes=[("ostf", ob, tt)])
            P.add("sp", lambda e, i=i, h=h, ob=ob: e.dma_start(
                out=OAv[:, 4 * i:4 * i + 4, 512 + 64 * h:512 + 64 * (h + 1)], in_=ostf[ob][:, :, :]),
                reads=[("ostf", ob, tt) for tt in range(4)], chan=cst[ob])

        for h in range(DBG.get('fh', 8)):
            hs_ = h % 2
            ck = ckh[hs_]
            P.add("sp", lambda e, h=h, hs_=hs_: e.dma_start(out=QT[hs_][0:64, :], in_=T["QKT"][14 + h, :, :]), writes=[("QT", hs_)], chan=ck)
            P.add("sp", lambda e, h=h, hs_=hs_: e.dma_start(out=KT[hs_][0:64, :], in_=T["QKT"][22 + h, :, :]), writes=[("KT", hs_)], chan=ck)
            P.add("sp", lambda e, h=h, hs_=hs_: e.dma_start(out=Vf[hs_][:, :, :], in_=T["V"][4 + h]), writes=[("Vf", hs_)], chan=ck)
            for op in ck.ops[-3:]:
                op.chanval = ck.count
            for i in range(NG):
                qsl = slice(i * 512, (i + 1) * 512)
                ob = fi % 2
                fi += 1
                nk = 4 * i + 4
                for kt in range(nk):
                    ms = (-128 * (kt - 4 * i), -1, 1) if kt >= 4 * i else None

                    def post(h=h, i=i, ob=ob):
                        P.add("dve", lambda e: e.tensor_copy(out=OFs[ob][:, :], in_=psO[ob][0:65, :]), reads=[("psO", ob)], writes=[("OFs", ob)])
                        pending.append((h, i, ob))
                    pipe.push(KT[hs_][:, kt * 128:(kt + 1) * 128], QT[hs_][:, qsl], Vf[hs_][:, kt, 0:65], psO[ob], ("psO", ob),
                              kt == 0, kt == nk - 1, ms, [("KT", hs_), ("QT", hs_), ("Vf", hs_), ("QT0", hs_), ("KT0", hs_)],
                              bias=biasT[:, i, h, kt:kt + 1], bkeys=[("biasT", i, h)], post=post)
                    if pending and (kt == 3 or DBG.get('fox_now', 0)):
                        finalize(*pending.pop(0))
        pipe.flush()
        while pending:
            finalize(*pending.pop(0))

    run_phase(nc, build)


def norm_rows(P, src, junk, ssq, rs, epst, nb, gB, nparts, width, key):
    for j in range(nparts):
        cs = slice(j * width, (j + 1) * width)
        P.add("act", lambda e, cs=cs, j=j: e.activation(out=junk[:, cs], in_=src[:, cs], func=AF.Square, accum_out=ssq[:, j:j + 1]),
              reads=[key], writes=["junk", ("ssq", key, j)])
    P.add("act", lambda e: e.activation(out=rs[:, 0:nparts], in_=ssq[:, 0:nparts], func=AF.Sqrt, scale=1.0 / width, bias=epst[:, 0:1]),
          reads=[("ssq", key, j) for j in range(nparts)] + ["eps"], writes=[("rs", key)])
    P.add("dve", lambda e: e.reciprocal(out=rs[:, 0:nparts], in_=rs[:, 0:nparts]), reads=[("rs", key)], writes=[("rs", key)])
    for j in range(nparts):
        cs = slice(j * width, (j + 1) * width)
        P.add("dve", lambda e, cs=cs, j=j: e.scalar_tensor_tensor(out=nb[:, cs], in0=src[:, cs], scalar=rs[:, j:j + 1], in1=gB[:, cs],
                                                                  op0=ALU.mult, op1=ALU.mult),
              reads=[key, ("rs", key), "gB", "gB2"], writes=[("nb", key)])


def out_phase(nc, T):
    def build(P, es):
        ident, identf = make_ident(P, nc, es)
        epst = sb(nc, es, "epst", [128, 1], F32)
        P.add("pool", lambda e: e.memset(epst[:, :], EPS), writes=["eps"])
        wo = sb(nc, es, "wo", [128, 8, D], BF16)
        stage = [sb(nc, es, "ostg%d" % i, [128, D], F32) for i in range(2)]
        stch = [P.chan() for _ in range(2)]
        gB = sb(nc, es, "gB", [128, D], F32)
        xn = [sb(nc, es, "xn%d" % i, [128, D], F32) for i in range(2)]
        xr = [sb(nc, es, "xr%d" % i, [128, D], F32) for i in range(2)]
        nb = [sb(nc, es, "nb%d" % i, [128, D], BF16) for i in range(2)]
        junk = sb(nc, es, "junk", [128, D], BF16)
        ssq = [sb(nc, es, "ssq%d" % i, [128, 2], F32) for i in range(2)]
        rs = [sb(nc, es, "rs%d" % i, [128, 2], F32) for i in range(2)]
        mT = [sb(nc, es, "mT%d" % i, [128, 8, 128], BF16) for i in range(2)]
        psT = [ps(nc, es, "psT%d" % i, [128, D], BF16) for i in range(2)]
        pso = [ps(nc, es, "pso%d" % i, [128, 512], F32) for i in range(4)]
        cg = P.chan()
        cx = [P.chan() for _ in range(2)]
        cr = [P.chan() for _ in range(2)]
        co = [P.chan() for _ in range(2)]
        P.add("sp", lambda e: e.dma_start(out=gB[:, 0:512], in_=T["out_norm_nsa"].partition_broadcast(128)), writes=["gB"], chan=cg)
        P.add("sp", lambda e: e.dma_start(out=gB[:, 512:1024], in_=T["out_norm_fox"].partition_broadcast(128)), writes=["gB2"], chan=cg)
        cg.seal()
        wv = T["w_out"].rearrange("(c p) f -> p c f", p=128)
        for c in range(8):
            s_ = c % 2
            P.add("sp", lambda e, s_=s_, c=c: e.dma_start(out=stage[s_][:, :], in_=wv[:, c, :]), writes=[("stage", s_)], chan=stch[s_])
            P.add("dve" if c % 2 == 0 else "pool", lambda e, s_=s_, c=c: e.tensor_copy(out=wo[:, c, :], in_=stage[s_][:, :]),
                  reads=[("stage", s_)], writes=[("wo", c)])
        wkeys = [("wo", c) for c in range(8)]
        def stage1(t):
            xs = t % 2
            P.add("sp", lambda e, xs=xs, t=t: e.dma_start(out=xn[xs][:, :], in_=T["OA"][t * 128:(t + 1) * 128, :]), writes=[("xn", xs)], chan=cx[xs])
            P.add("sp", lambda e, xs=xs, t=t: e.dma_start(out=xr[xs][:, :], in_=T["h1"][t * 128:(t + 1) * 128, :]), writes=[("xr", xs)], chan=cr[xs])
            norm_rows(P, xn[xs], junk, ssq[xs], rs[xs], epst, nb[xs], gB, 2, 512, ("xn", xs))
            for c in range(8):
                P.add("pe", lambda e, xs=xs, c=c: e.transpose(out=psT[xs][:, c * 128:(c + 1) * 128], in_=nb[xs][:, c * 128:(c + 1) * 128],
                                                              identity=ident[:, :]),
                      reads=[("nb", ("xn", xs)), "ident"], writes=[("psT", xs)] if c == 0 else [])
            P.last_w[("psT", xs)] = P.ops["pe"][-1]
            P.add("act", lambda e, xs=xs: e.copy(out=mT[xs][:, :, :], in_=psT[xs][:, :].rearrange("p (c t) -> p c t", c=8)),
                  reads=[("psT", xs)], writes=[("mT", xs)])

        def stage2(t):
            xs = t % 2
            for half in range(2):
                pb = (t * 2 + half) % 4
                for c in range(8):
                    P.add("pe", lambda e, pb=pb, c=c, xs=xs, half=half: e.matmul(
                        out=pso[pb][:, :], lhsT=mT[xs][:, c, :], rhs=wo[:, c, half * 512:(half + 1) * 512], start=(c == 0), stop=(c == 7)),
                        reads=[("mT", xs)] + (wkeys if t == 0 else []), writes=[("pso", pb)] if c == 0 else [])
                P.last_w[("pso", pb)] = P.ops["pe"][-1]
                P.add("dve", lambda e, pb=pb, xs=xs, half=half: e.tensor_tensor(
                    out=xr[xs][:, half * 512:(half + 1) * 512], in0=pso[pb][:, :], in1=xr[xs][:, half * 512:(half + 1) * 512], op=ALU.add),
                    reads=[("pso", pb), ("xr", xs)], writes=[("xr", xs)])
            P.add("sp", lambda e, xs=xs, t=t: e.dma_start(out=T["h1"][t * 128:(t + 1) * 128, :], in_=xr[xs][:, :]),
                  reads=[("xr", xs)], writes=[("xrst", xs)], chan=co[xs])

        stage1(0)
        for t in range(NT):
            if t + 1 < NT:
                stage1(t + 1)
            stage2(t)

    run_phase(nc, build)


def ple_phase(nc, T):
    def build(P, es):
        ident, identf = make_ident(P, nc, es)
        epst = sb(nc, es, "epst", [128, 1], F32)
        P.add("pool", lambda e: e.memset(epst[:, :], EPS), writes=["eps"])
        wg = sb(nc, es, "wg", [128, 8, D], BF16)
        wp = sb(nc, es, "wp", [128, 2, D], BF16)
        stage = [sb(nc, es, "lstg%d" % i, [128, D], F32) for i in range(2)]
        stch = [P.chan() for _ in range(2)]
        gB = sb(nc, es, "gB", [128, D], F32)
        gE = sb(nc, es, "gE", [128, D], F32)
        xn = [sb(nc, es, "xn%d" % i, [128, D], F32) for i in range(2)]
        pt = [sb(nc, es, "pt%d" % i, [128, 256], F32) for i in range(2)]
        pb16 = [sb(nc, es, "pb%d" % i, [128, 256], BF16) for i in range(2)]
        nb = [sb(nc, es, "nb%d" % i, [128, D], BF16) for i in range(2)]
        junk = sb(nc, es, "junk", [128, D], BF16)
        junk2 = sb(nc, es, "junk2", [128, D], BF16)
        ssq = [sb(nc, es, "ssq%d" % i, [128, 4], F32) for i in range(2)]
        rs = [sb(nc, es, "rs%d" % i, [128, 4], F32) for i in range(2)]
        mT = [sb(nc, es, "mT%d" % i, [128, 10, 128], BF16) for i in range(2)]
        gate = [sb(nc, es, "gate%d" % i, [128, D], F32) for i in range(2)]
        ev = [sb(nc, es, "ev%d" % i, [128, D], F32) for i in range(2)]
        psTa = ps(nc, es, "psTa", [128, 8, 128], BF16)
        psTb = ps(nc, es, "psTb", [128, 8, 128], BF16)
        psg = [ps(nc, es, "psg%d" % i, [128, 512], F32) for i in range(2)]
        pse = [ps(nc, es, "pse%d" % i, [128, 512], F32) for i in range(2)]
        cg = P.chan()
        cx = [P.chan() for _ in range(2)]
        cp = [P.chan() for _ in range(2)]
        co = [P.chan() for _ in range(2)]
        P.add("sp", lambda e: e.dma_start(out=gB[:, :], in_=T["ple_gate_norm"].partition_broadcast(128)), writes=["gB"], chan=cg)
        P.add("sp", lambda e: e.dma_start(out=gE[:, :], in_=T["ple_norm"].partition_broadcast(128)), writes=["gE"], chan=cg)
        cg.seal()
        wv = T["ple_w_gate"].rearrange("(c p) f -> p c f", p=128)
        wpv = T["ple_w_proj"].rearrange("(c p) f -> p c f", p=128)
        for c in range(10):
            s_ = c % 2
            src = wv[:, c, :] if c < 8 else wpv[:, c - 8, :]
            dstw = wg[:, c, :] if c < 8 else wp[:, c - 8, :]
            P.add("sp", lambda e, s_=s_, src=src: e.dma_start(out=stage[s_][:, :], in_=src), writes=[("stage", s_)], chan=stch[s_])
            P.add("dve" if c % 2 == 0 else "pool", lambda e, s_=s_, dstw=dstw: e.tensor_copy(out=dstw, in_=stage[s_][:, :]),
                  reads=[("stage", s_)], writes=[("w", c)])
        wkeys = [("w", c) for c in range(10)]
        def stage1(t):
            xs = t % 2
            P.add("sp", lambda e, xs=xs, t=t: e.dma_start(out=xn[xs][:, :], in_=T["h3"][t * 128:(t + 1) * 128, :]), writes=[("xn", xs)], chan=cx[xs])
            P.add("sp", lambda e, xs=xs, t=t: e.dma_start(out=pt[xs][:, :], in_=T["p"][t * 128:(t + 1) * 128, :]), writes=[("pt", xs)], chan=cp[xs])
            norm_rows(P, xn[xs], junk, ssq[xs], rs[xs], epst, nb[xs], gB, 1, D, ("xn", xs))
            P.add("pool", lambda e, xs=xs: e.tensor_copy(out=pb16[xs][:, :], in_=pt[xs][:, :]), reads=[("pt", xs)], writes=[("pb16", xs)])
            for c in range(10):
                src = nb[xs][:, c * 128:(c + 1) * 128] if c < 8 else pb16[xs][:, (c - 8) * 128:(c - 7) * 128]
                dstp = psTa[:, c, :] if c < 8 else psTb[:, c - 8, :]
                P.add("pe", lambda e, dstp=dstp, src=src: e.transpose(out=dstp, in_=src, identity=ident[:, :]),
                      reads=[("nb", ("xn", xs)), ("pb16", xs), "ident"], writes=["psTa" if c < 8 else "psTb"] if c in (0, 8) else [])
                if c == 7:
                    P.last_w["psTa"] = P.ops["pe"][-1]
            P.last_w["psTb"] = P.ops["pe"][-1]
            P.add("act", lambda e, xs=xs: e.copy(out=mT[xs][:, 0:8, :], in_=psTa[:, :, :]), reads=["psTa"], writes=[("mT", xs)])
            P.add("act", lambda e, xs=xs: e.copy(out=mT[xs][:, 8:10, :], in_=psTb[:, 0:2, :]), reads=["psTb"], writes=[("mT2", xs)])

        def stage2(t):
            xs = t % 2
            for half in range(2):
                hsl = slice(half * 512, (half + 1) * 512)
                for c in range(8):
                    P.add("pe", lambda e, c=c, xs=xs, half=half, hsl=hsl: e.matmul(
                        out=psg[half][:, :], lhsT=mT[xs][:, c, :], rhs=wg[:, c, hsl], start=(c == 0), stop=(c == 7)),
                        reads=[("mT", xs), ("mT2", xs)] + (wkeys if t == 0 else []), writes=[("psg", half)] if c == 0 else [])
                P.last_w[("psg", half)] = P.ops["pe"][-1]
                P.add("act", lambda e, xs=xs, half=half, hsl=hsl: e.activation(out=gate[xs][:, hsl], in_=psg[half][:, :], func=AF.Sigmoid),
                      reads=[("psg", half)], writes=[("gate", xs, half)])
                for c in range(2):
                    P.add("pe", lambda e, c=c, xs=xs, half=half, hsl=hsl: e.matmul(
                        out=pse[half][:, :], lhsT=mT[xs][:, 8 + c, :], rhs=wp[:, c, hsl], start=(c == 0), stop=(c == 1)),
                        reads=[("mT", xs), ("mT2", xs)] + (wkeys if t == 0 else []), writes=[("pse", half)] if c == 0 else [])
                P.last_w[("pse", half)] = P.ops["pe"][-1]
                P.add("act", lambda e, xs=xs, half=half, hsl=hsl: e.activation(
                    out=junk2[:, hsl], in_=pse[half][:, :], func=AF.Square, accum_out=ssq[xs][:, 2 + half:3 + half]),
                    reads=[("pse", half)], writes=[("junk2", half), ("ssqe", xs, half)])
            P.add("dve", lambda e, xs=xs: e.tensor_tensor(out=rs[xs][:, 2:3], in0=ssq[xs][:, 2:3], in1=ssq[xs][:, 3:4], op=ALU.add),
                  reads=[("ssqe", xs, 0), ("ssqe", xs, 1)], writes=[("rse", xs)])
            P.add("act", lambda e, xs=xs: e.activation(out=rs[xs][:, 2:3], in_=rs[xs][:, 2:3], func=AF.Sqrt, scale=1.0 / D, bias=epst[:, 0:1]),
                  reads=[("rse", xs), "eps"], writes=[("rse", xs)])
            P.add("dve", lambda e, xs=xs: e.reciprocal(out=rs[xs][:, 2:3], in_=rs[xs][:, 2:3]), reads=[("rse", xs)], writes=[("rse", xs)])
            for half in range(2):
                hsl = slice(half * 512, (half + 1) * 512)
                P.add("dve", lambda e, xs=xs, half=half, hsl=hsl: e.scalar_tensor_tensor(
                    out=ev[xs][:, hsl], in0=pse[half][:, :], scalar=rs[xs][:, 2:3], in1=gE[:, hsl], op0=ALU.mult, op1=ALU.mult),
                    reads=[("pse", half), ("rse", xs), "gE"], writes=[("ev", xs, half)])
                P.add("pool", lambda e, xs=xs, hsl=hsl: e.tensor_tensor(out=ev[xs][:, hsl], in0=ev[xs][:, hsl], in1=gate[xs][:, hsl], op=ALU.mult),
                      reads=[("ev", xs, half), ("gate", xs, half)], writes=[("ev", xs, half)])
                P.add("pool", lambda e, xs=xs, hsl=hsl: e.tensor_tensor(out=ev[xs][:, hsl], in0=ev[xs][:, hsl], in1=xn[xs][:, hsl], op=ALU.add),
                      reads=[("ev", xs, half), ("xn", xs)], writes=[("ev", xs, half)])
            P.add("sp", lambda e, xs=xs, t=t: e.dma_start(out=T["out"][t * 128:(t + 1) * 128, :], in_=ev[xs][:, :]),
                  reads=[("ev", xs, 0), ("ev", xs, 1)], chan=co[xs])


        stage1(0)
        for t in range(NT):
            if t + 1 < NT:
                stage1(t + 1)
            stage2(t)

    run_phase(nc, build)


def rope_tables_np():
    pos = np.arange(S, dtype=np.float32)
    inv = (np.float32(500000.0) ** (-np.arange(0, 16, 2, dtype=np.float32) / np.float32(16))).astype(np.float32)
    ang = (pos[:, None] * inv[None, :]).astype(np.float32)
    return np.cos(ang).astype(np.float32), np.sin(ang).astype(np.float32)


IN_SHAPES = dict(
    x=[S, D], p=[S, 256], ffn1_norm=[D], ffn1_wg=[D, DFF], ffn1_wu=[D, DFF], ffn1_wd=[DFF, D],
    mix_norm=[D], w_in=[D, 2848], b_forget=[8], q_norm_nsa=[64], k_norm_cmp=[64], k_norm_slc=[64], k_norm_win=[64],
    cmp_pos_k=[32, 64], cmp_pos_v=[32, 64], cmp_k_w1=[2048, 256], cmp_k_w2=[256, 64], cmp_v_w1=[2048, 256],
    cmp_v_w2=[256, 64], q_norm_fox=[64], k_norm_fox=[64], out_norm_nsa=[512], out_norm_fox=[512], w_out=[D, D],
    ffn2_norm=[D], ffn2_wg=[D, DFF], ffn2_wu=[D, DFF], ffn2_wd=[DFF, D], ple_gate_norm=[D], ple_w_gate=[D, D],
    ple_w_proj=[256, D], ple_norm=[D], rope_cos=[S, 8], rope_sin=[S, 8])


def build_nc(nph=99, debug=(), skip=()):
    nc = bass.Bass("TRN2", target_bir_lowering=False)
    T = {}
    for name, shape in IN_SHAPES.items():
        T[name] = nc.dram_tensor(name, shape, F32, kind="ExternalInput").ap()

    def scratch(name, shape, dt):
        kind = "ExternalOutput" if name in debug else "Internal"
        T[name] = nc.dram_tensor(name, shape, dt, kind=kind).ap()

    T["out"] = nc.dram_tensor("out", [S, D], F32, kind="ExternalOutput").ap()
    scratch("h1", [S, D], F32)
    scratch("QKT", [32, 64, S], BF16)
    scratch("V", [12, 128, NT, 72], BF16)
    scratch("G", [128, NT * 24], F32)
    scratch("LF", [128, NT * 8], F32)
    scratch("KCT", [2, 64, 256], BF16)
    scratch("VC", [2, 256, 65], BF16)
    scratch("OA", [S, D], F32)
    scratch("h3", [S, D], F32)
    if "DBGB" in debug:
        scratch("DBGB", [64, S], BF16)
    if "DBGT" in debug:
        scratch("DBGT", [3, 65, 512], F32)
    phases = [
        lambda: ffn_half_phase(nc, T, T["x"], T["x"], T["h1"], T["ffn1_norm"], T["ffn1_wg"], T["ffn1_wu"], T["ffn1_wd"], 0, 11, "f1a"),
        lambda: ffn_half_phase(nc, T, T["x"], T["h1"], T["h1"], T["ffn1_norm"], T["ffn1_wg"], T["ffn1_wu"], T["ffn1_wd"], 11, 11, "f1b"),
        lambda: proj_phase(nc, T),
        lambda: cmp_phase(nc, T),
        lambda: nsa_phase(nc, T),
        lambda: fox_phase(nc, T),
        lambda: out_phase(nc, T),
        lambda: ffn_half_phase(nc, T, T["h1"], T["h1"], T["h3"], T["ffn2_norm"], T["ffn2_wg"], T["ffn2_wu"], T["ffn2_wd"], 0, 11, "f2a"),
        lambda: ffn_half_phase(nc, T, T["h1"], T["h3"], T["h3"], T["ffn2_norm"], T["ffn2_wg"], T["ffn2_wu"], T["ffn2_wd"], 11, 11, "f2b"),
        lambda: ple_phase(nc, T),
    ]
    for k, ph in enumerate(phases[:nph]):
        if k not in skip:
            ph()
    return nc


def make_in_maps(inputs, cores=range(8)):
    cos, sin = rope_tables_np()
    in_maps = []
    for b in cores:
        m = {}
        for name in IN_SHAPES:
            if name == "x":
                a = inputs["x"][b]
            elif name == "p":
                a = inputs["p"][0, b]
            elif name == "rope_cos":
                a = cos
            elif name == "rope_sin":
                a = sin
            elif name == "w_in":
                a = inputs["w_in"][0][:, W_IN_PERM]
            else:
                a = inputs[name][0]
            m[name] = np.ascontiguousarray(a, dtype=np.float32)
        in_maps.append(m)
    return in_maps


def kernel(**inputs):
    nc = build_nc()
    res = run_bass_kernel_spmd(nc, make_in_maps(inputs), core_ids=list(range(8)))
    return np.stack([np.asarray(r["out"]) for r in res.results], axis=0)
```

```python
import numpy as np
from contextlib import ExitStack
import concourse.bass as bass
import concourse.mybir as mybir
from concourse.bass_utils import run_bass_kernel_spmd

F32 = mybir.dt.float32
BF16 = mybir.dt.bfloat16
AF = mybir.ActivationFunctionType
ALU = mybir.AluOpType
AX = mybir.AxisListType

S = 4096
D = 1024
DFF = 2816
NT = S // 128
NG = S // 512
EPS = 1e-6
SAME_ENGINE_SYNC = True
DBG = dict(kh=2, ng=NG, stage=5)
_UID = [0]


def _u(name):
    _UID[0] += 1
    return "%s_%d" % (name, _UID[0])

_OFF = dict(qa=0, kc=512, vc=640, ksl=768, vsl=896, kwn=1024, vwn=1152, ga=1280, qf=1304, kf=1816, vf=2328, fl=2840)
_SZ = dict(qa=512, kc=128, vc=128, ksl=128, vsl=128, kwn=128, vwn=128, ga=24, qf=512, kf=512, vf=512, fl=8)
_ORDER = ['kc', 'qa', 'ksl', 'kwn', 'qf', 'kf', 'vc', 'vsl', 'vwn', 'vf', 'ga', 'fl']
W_IN_PERM = np.concatenate([np.arange(_OFF[k], _OFF[k] + _SZ[k]) for k in _ORDER])


class Chan:
    def __init__(self, sem):
        self.sem = sem
        self.count = 0
        self.ops = []

    def seal(self):
        for op in self.ops:
            op.chanval = self.count


class Op:
    __slots__ = ("eng", "fn", "deps", "sig", "sigval", "chan", "chanval", "idx")


class Prog:
    ENGS = ("pe", "act", "dve", "pool", "sp")

    def __init__(self, nc, es):
        self.nc = nc
        self.es = es
        self.ops = {e: [] for e in self.ENGS}
        self.last_w = {}
        self.readers = {}
        self.engsem = {e: es.enter_context(nc.semaphore(_u("s_" + e))) for e in self.ENGS}
        self.chans = []

    def chan(self):
        c = Chan(self.es.enter_context(self.nc.semaphore(_u("c"))))
        self.chans.append(c)
        return c

    def add(self, eng, fn, reads=(), writes=(), chan=None):
        op = Op()
        op.eng = eng
        op.fn = fn
        op.sig = False
        op.sigval = 0
        op.chan = chan
        deps = []
        for r in reads:
            w = self.last_w.get(r)
            if w is not None:
                deps.append(w)
        for w in writes:
            lw = self.last_w.get(w)
            if lw is not None:
                deps.append(lw)
            deps.extend(self.readers.get(w, ()))
        best = {}
        for d in deps:
            k = ("c", id(d.chan)) if d.chan is not None else ("e", d.eng)
            if k not in best or best[k].idx < d.idx:
                best[k] = d
        op.deps = list(best.values())
        op.idx = len(self.ops[eng])
        for r in reads:
            self.readers.setdefault(r, []).append(op)
        for w in writes:
            self.last_w[w] = op
            self.readers[w] = []
        if chan is not None:
            chan.count += 16
            op.chanval = chan.count
            chan.ops.append(op)
        self.ops[eng].append(op)
        return op

    def emit(self, block):
        for e in self.ENGS:
            for op in self.ops[e]:
                for d in op.deps:
                    if d.chan is None and (d.eng != op.eng or SAME_ENGINE_SYNC):
                        d.sig = True
        for e in self.ENGS:
            c = 0
            for op in self.ops[e]:
                if op.sig and op.chan is None:
                    c += 1
                    op.sigval = c
        final = [(c.sem, c.count) for c in self.chans if c.count > 0]

        def mk(ename):
            ops = self.ops[ename]

            def body(eng):
                waited = {}
                if ename == "pool":
                    _FILL.clear()
                for op in ops:
                    need = []
                    for d in op.deps:
                        if d.chan is not None:
                            sem, val = d.chan.sem, d.chanval
                        elif d.eng != ename or SAME_ENGINE_SYNC:
                            sem, val = self.engsem[d.eng], d.sigval
                        else:
                            continue
                        k = id(sem)
                        if waited.get(k, 0) >= val:
                            continue
                        need.append((sem, val))
                        waited[k] = val
                    for sem, val in need[:-1]:
                        eng.wait_ge(sem, val)
                    ins = op.fn(eng)
                    if need:
                        ins._wait_ge(need[-1][0], need[-1][1])
                    if op.chan is not None:
                        ins.then_inc(op.chan.sem, 16)
                    elif op.sig:
                        ins.then_inc(self.engsem[ename], 1)
                if ename == "sp":
                    for sem, val in final:
                        eng.wait_ge(sem, val)
                if ename == "pool":
                    for r in _FILL.values():
                        eng.free_register(r)
                    _FILL.clear()
            return body

        block.tensor(mk("pe"))
        block.scalar(mk("act"))
        block.vector(mk("dve"))
        block.gpsimd(mk("pool"))
        block.sync(mk("sp"))


def run_phase(nc, build):
    with ExitStack() as es:
        P = Prog(nc, es)
        build(P, es)
        sems = list(P.engsem.values()) + [c.sem for c in P.chans]
        with nc.Block() as b0:
            def clr(e):
                for sm in sems:
                    e.sem_clear(sm)
            b0.sync(clr)
        with nc.Block() as block:
            P.emit(block)


def sb(nc, es, name, shape, dt):
    return es.enter_context(nc.sbuf_tensor(_u(name), shape, dt))


def ps(nc, es, name, shape, dt):
    return es.enter_context(nc.psum_tensor(_u(name), shape, dt))


def load_cast_rows(P, nc, es, dst, src_rows, ncols, chans, stage, key):
    n = len(src_rows)
    for k in range(n):
        s = k % len(stage)
        st = stage[s]
        ch = chans[s]
        src = src_rows[k]
        P.add("sp", lambda e, st=st, src=src: e.dma_start(out=st[:, 0:ncols], in_=src),
              writes=[("stage", s)], chan=ch)
        eng = "dve" if k % 2 == 0 else "pool"
        P.add(eng, lambda e, st=st, k=k: e.tensor_copy(out=dst[:, k, 0:ncols], in_=st[:, 0:ncols]),
              reads=[("stage", s)], writes=[(key, k)])


def ffn_half_phase(nc, T, src_norm, src_res, dst, gain, wg, wu, wd, f0, nfc, tagp):
    def build(P, es):
        FW = nfc * 128
        wg_s = sb(nc, es, "wg_s", [128, 8, FW], BF16)
        wu_s = sb(nc, es, "wu_s", [128, 8, FW], BF16)
        wd_s = sb(nc, es, "wd_s", [128, nfc, D], BF16)
        stage = [sb(nc, es, "stg%d" % i, [128, FW], F32) for i in range(3)]
        stch = [P.chan() for _ in range(3)]
        gB = sb(nc, es, "gB", [128, D], F32)
        ident = sb(nc, es, "ident", [128, 128], BF16)
        identf = sb(nc, es, "identf", [128, 128], F32)
        epst = sb(nc, es, "epst", [128, 1], F32)
        xn = [sb(nc, es, "xn%d" % i, [128, D], F32) for i in range(2)]
        xr = [sb(nc, es, "xr%d" % i, [128, D], F32) for i in range(2)]
        nb = [sb(nc, es, "nb%d" % i, [128, D], BF16) for i in range(4)]
        junk = sb(nc, es, "junk", [128, D], BF16)
        ssq = [sb(nc, es, "ssq%d" % i, [128, 4], F32) for i in range(2)]
        rs = [sb(nc, es, "rs%d" % i, [128, 4], F32) for i in range(2)]
        nT = [sb(nc, es, "nT%d" % i, [128, 8, 512], BF16) for i in range(2)]
        hT = sb(nc, es, "hT", [128, nfc, 512], BF16)
        sg = [sb(nc, es, "sg%d" % i, [128, 512], F32) for i in range(2)]
        psT = [ps(nc, es, "psT%d" % i, [128, D], BF16) for i in range(2)]
        psg = [ps(nc, es, "psg%d" % i, [128, 512], F32) for i in range(2)]
        psu = [ps(nc, es, "psu%d" % i, [128, 512], F32) for i in range(2)]
        pso = [ps(nc, es, "pso%d" % i, [128, 512], F32) for i in range(2)]
        cx = [P.chan() for _ in range(2)]
        cr = [P.chan() for _ in range(2)]
        co = [P.chan() for _ in range(2)]
        cg = P.chan()

        P.add("sp", lambda e: e.dma_start(out=gB[:, :], in_=gain.partition_broadcast(128)),
              writes=["gB"], chan=cg)
        P.add("pool", lambda e: e.memset(identf[:, :], 0.0), writes=["identf"])
        P.add("pool", lambda e: asel(e, out=identf[:, :], in_=identf[:, :], pattern=[[-1, 128]],
                                                compare_op=ALU.not_equal, fill=1.0, base=0,
                                                channel_multiplier=1),
              reads=["identf"], writes=["identf"])
        P.add("pool", lambda e: e.tensor_copy(out=ident[:, :], in_=identf[:, :]), reads=["identf"], writes=["ident"])
        P.add("pool", lambda e: e.memset(epst[:, :], EPS), writes=["eps"])

        wgv = wg.rearrange("(c p) f -> p c f", p=128)
        wuv = wu.rearrange("(c p) f -> p c f", p=128)
        load_cast_rows(P, nc, es, wg_s, [wgv[:, c, f0 * 128:f0 * 128 + FW] for c in range(8)], FW, stch, stage, "wg")
        load_cast_rows(P, nc, es, wu_s, [wuv[:, c, f0 * 128:f0 * 128 + FW] for c in range(8)], FW, stch, stage, "wu")
        load_cast_rows(P, nc, es, wd_s, [wd[(f0 + c) * 128:(f0 + c + 1) * 128, :] for c in range(nfc)], D, stch, stage, "wd")
        wkeys = [("wg", c) for c in range(8)] + [("wu", c) for c in range(8)]
        wdkeys = [("wd", c) for c in range(nfc)]

        def prep_group(g):
            sl = g % 2
            for k in range(4):
                t = 4 * g + k
                xs = t % 2
                P.add("sp", lambda e, xs=xs, t=t: e.dma_start(out=xn[xs][:, :], in_=src_norm[t * 128:(t + 1) * 128, :]),
                      writes=[("xn", xs)], chan=cx[xs])
                P.add("act", lambda e, xs=xs, sl=sl, k=k: e.activation(
                    out=junk[:, :], in_=xn[xs][:, :], func=AF.Square, accum_out=ssq[sl][:, k:k + 1]),
                    reads=[("xn", xs)], writes=["junk", ("ssq", sl, k)])
                P.add("act", lambda e, sl=sl, k=k: e.activation(out=rs[sl][:, k:k + 1], in_=ssq[sl][:, k:k + 1],
                                                                func=AF.Sqrt, scale=1.0 / D, bias=epst[:, 0:1]),
                      reads=[("ssq", sl, k), "eps"], writes=[("rs", sl, k)])
                P.add("dve", lambda e, sl=sl, k=k: e.reciprocal(out=rs[sl][:, k:k + 1], in_=rs[sl][:, k:k + 1]),
                      reads=[("rs", sl, k)], writes=[("rs", sl, k)])
                P.add("dve", lambda e, xs=xs, sl=sl, k=k: e.scalar_tensor_tensor(
                    out=nb[k][:, :], in0=xn[xs][:, :], scalar=rs[sl][:, k:k + 1], in1=gB[:, :],
                    op0=ALU.mult, op1=ALU.mult),
                    reads=[("xn", xs), ("rs", sl, k), "gB"], writes=[("nb", k)])

        def transposes(g):
            sl = g % 2
            for k in range(4):
                pb = k % 2
                for c in range(8):
                    P.add("pe", lambda e, pb=pb, k=k, c=c: e.transpose(
                        out=psT[pb][:, c * 128:(c + 1) * 128], in_=nb[k][:, c * 128:(c + 1) * 128], identity=ident[:, :]),
                        reads=[("nb", k), "ident"], writes=[("psT", pb)] if c == 0 else [])
                P.last_w[("psT", pb)] = P.ops["pe"][-1]
                P.add("act", lambda e, pb=pb, sl=sl, k=k: e.copy(
                    out=nT[sl][:, :, k * 128:(k + 1) * 128],
                    in_=psT[pb][:, :].rearrange("p (c t) -> p c t", c=8)),
                    reads=[("psT", pb)], writes=[("nT", sl, k)])

        def upgate(g):
            sl = g % 2
            for fc in range(nfc):
                b = fc % 2
                for (wt, pst, nm) in ((wg_s, psg, "psg"), (wu_s, psu, "psu")):
                    for c in range(8):
                        P.add("pe", lambda e, wt=wt, pst=pst, b=b, c=c, fc=fc, sl=sl: e.matmul(
                            out=pst[b][:, :], lhsT=wt[:, c, fc * 128:(fc + 1) * 128], rhs=nT[sl][:, c, :],
                            start=(c == 0), stop=(c == 7)),
                            reads=[("nT", sl, 0), ("nT", sl, 1), ("nT", sl, 2), ("nT", sl, 3)] + (wkeys if g == 0 else []),
                            writes=[(nm, b)] if c == 0 else [])
                    P.last_w[(nm, b)] = P.ops["pe"][-1]
                P.add("act", lambda e, b=b: e.activation(out=sg[b][:, :], in_=psg[b][:, :], func=AF.Silu),
                      reads=[("psg", b)], writes=[("sg", b)])
                P.add("dve", lambda e, b=b, fc=fc: e.tensor_tensor(out=hT[:, fc, :], in0=psu[b][:, :], in1=sg[b][:, :],
                                                                  op=ALU.mult),
                      reads=[("psu", b), ("sg", b)], writes=[("hT", fc)])

        def down(g):
            for k in range(4):
                t = 4 * g + k
                rsl = t % 2
                P.add("sp", lambda e, rsl=rsl, t=t: e.dma_start(out=xr[rsl][:, :], in_=src_res[t * 128:(t + 1) * 128, :]),
                      reads=[("dram", t)], writes=[("xr", rsl)], chan=cr[rsl])
                for half in range(2):
                    b = half
                    for fc in range(nfc):
                        P.add("pe", lambda e, b=b, fc=fc, k=k, half=half: e.matmul(
                            out=pso[b][:, :], lhsT=hT[:, fc, k * 128:(k + 1) * 128],
                            rhs=wd_s[:, fc, half * 512:(half + 1) * 512], start=(fc == 0), stop=(fc == nfc - 1)),
                            reads=[("hT", fc)] + (wdkeys if g == 0 else []),
                            writes=[("pso", b)] if fc == 0 else [])
                    P.last_w[("pso", b)] = P.ops["pe"][-1]
                    P.add("dve", lambda e, b=b, rsl=rsl, half=half: e.scalar_tensor_tensor(
                        out=xr[rsl][:, half * 512:(half + 1) * 512], in0=pso[b][:, :], scalar=0.5,
                        in1=xr[rsl][:, half * 512:(half + 1) * 512], op0=ALU.mult, op1=ALU.add),
                        reads=[("pso", b), ("xr", rsl)], writes=[("xr", rsl)])
                P.add("sp", lambda e, rsl=rsl, t=t: e.dma_start(out=dst[t * 128:(t + 1) * 128, :], in_=xr[rsl][:, :]),
                      reads=[("xr", rsl)], writes=[("dram", t)], chan=co[rsl])

        prep_group(0)
        transposes(0)
        for g in range(NG):
            if g + 1 < NG:
                prep_group(g + 1)
            upgate(g)
            if g + 1 < NG:
                transposes(g + 1)
            down(g)

    run_phase(nc, build)


def proj_phase(nc, T):
    def build(P, es):
        h1 = T["h1"]
        WIN = 2848
        win_s = sb(nc, es, "win_s", [128, 8, WIN], BF16)
        HW_ = WIN // 2
        stage = [sb(nc, es, "pstg%d" % i, [128, HW_], F32) for i in range(3)]
        stch = [P.chan() for _ in range(3)]
        gB = sb(nc, es, "gB", [128, D], F32)
        ident = sb(nc, es, "ident", [128, 128], BF16)
        identf = sb(nc, es, "identf", [128, 128], F32)
        epst = sb(nc, es, "epst", [128, 1], F32)
        g5 = sb(nc, es, "g5", [128, 5, 64], F32)
        GQ = sb(nc, es, "GQ", [128, 28, 64], F32)
        bfg = sb(nc, es, "bfg", [128, 8], F32)
        cosT = sb(nc, es, "cosT", [128, NT, 8], F32)
        sinT = sb(nc, es, "sinT", [128, NT, 8], F32)
        Gall = sb(nc, es, "Gall", [128, NT, 24], F32)
        LFall = sb(nc, es, "LFall", [128, NT, 8], F32)
        xn = [sb(nc, es, "xn%d" % i, [128, D], F32) for i in range(2)]
        nb = [sb(nc, es, "nb%d" % i, [128, D], BF16) for i in range(2)]
        junk = sb(nc, es, "junk", [128, D], BF16)
        ssq = sb(nc, es, "ssq", [128, 2], F32)
        rs = sb(nc, es, "rs", [128, 2], F32)
        aT = [sb(nc, es, "aT%d" % i, [128, 8, 128], BF16) for i in range(2)]
        qk = [sb(nc, es, "qk%d" % i, [128, 32, 64], F32) for i in range(2)]
        sq = [sb(nc, es, "sq%d" % i, [128, 32, 64], F32) for i in range(2)]
        hs = [sb(nc, es, "hs%d" % i, [128, 32], F32) for i in range(2)]
        rt = [sb(nc, es, "rt%d" % i, [128, 14, 8], F32) for i in range(4)]
        qkb = [sb(nc, es, "qkb%d" % i, [128, 2048], BF16) for i in range(2)]
        qkT = sb(nc, es, "qkT", [128, 16, 512], BF16)
        vst = [sb(nc, es, "vst%d" % i, [128, 12, 4, 72], BF16) for i in range(2)]
        psT = [ps(nc, es, "psT%d" % i, [128, D], BF16) for i in range(2)]
        pq = [ps(nc, es, "pq%d" % i, [128, 512], F32) for i in range(4)]
        psQ = [ps(nc, es, "psQ%d" % i, [128, 8, 128], BF16) for i in range(2)]
        cx = [P.chan() for _ in range(2)]
        cg = P.chan()
        cq = P.chan()
        cv = [P.chan() for _ in range(2)]
        cf = P.chan()

        P.add("sp", lambda e: e.dma_start(out=gB[:, :], in_=T["mix_norm"].partition_broadcast(128)), writes=["gB"], chan=cg)
        for i, nm in enumerate(("q_norm_nsa", "k_norm_slc", "k_norm_win", "q_norm_fox", "k_norm_fox")):
            P.add("sp", lambda e, i=i, nm=nm: e.dma_start(out=g5[:, i, :], in_=T[nm].partition_broadcast(128)),
                  writes=[("g5", i)], chan=cg)
        P.add("sp", lambda e: e.dma_start(out=bfg[:, :], in_=T["b_forget"].partition_broadcast(128)), writes=["bfg"], chan=cg)
        P.add("sp", lambda e: e.dma_start(out=cosT[:, :, :], in_=T["rope_cos"].rearrange("(t p) c -> p t c", p=128)),
              writes=["cosT"], chan=cg)
        P.add("sp", lambda e: e.dma_start(out=sinT[:, :, :], in_=T["rope_sin"].rearrange("(t p) c -> p t c", p=128)),
              writes=["sinT"], chan=cg)
        cg.seal()
        for (i, h0, nh) in ((0, 0, 8), (1, 8, 2), (2, 10, 2), (3, 12, 8), (4, 20, 8)):
            P.add("dve", lambda e, i=i, h0=h0, nh=nh: e.tensor_copy(
                out=GQ[:, h0:h0 + nh, :], in_=g5[:, i, :].unsqueeze(1).to_broadcast([128, nh, 64])),
                reads=[("g5", i)], writes=[("GQ", i)])
        gqk = [("GQ", i) for i in range(5)]
        P.add("pool", lambda e: e.memset(identf[:, :], 0.0), writes=["identf"])
        P.add("pool", lambda e: asel(e, out=identf[:, :], in_=identf[:, :], pattern=[[-1, 128]],
                                                compare_op=ALU.not_equal, fill=1.0, base=0, channel_multiplier=1),
              reads=["identf"], writes=["identf"])
        P.add("pool", lambda e: e.tensor_copy(out=ident[:, :], in_=identf[:, :]), reads=["identf"], writes=["ident"])
        P.add("pool", lambda e: e.memset(epst[:, :], EPS), writes=["eps"])
        wv = T["w_in"].rearrange("(c p) f -> p c f", p=128)
        for hf in range(2):
            for c in range(8):
                k = hf * 8 + c
                s = k % 3
                P.add("sp", lambda e, s=s, c=c, hf=hf: e.dma_start(out=stage[s][:, :], in_=wv[:, c, hf * HW_:(hf + 1) * HW_]),
                      writes=[("stage", s)], chan=stch[s])
                P.add("dve" if k % 2 == 0 else "pool", lambda e, s=s, c=c, hf=hf: e.tensor_copy(
                    out=win_s[:, c, hf * HW_:(hf + 1) * HW_], in_=stage[s][:, :]),
                    reads=[("stage", s)], writes=[("win", c, hf)])
        wkeys = [("win", c, hf) for c in range(8) for hf in range(2)]
        QKTv = T["QKT"].rearrange("(pr two) d s -> (two d) pr s", two=2)
        for i in range(2):
            P.add("pool", lambda e, i=i: e.memset(vst[i][:, :, :, 64:72], 1.0), writes=[("vst1", i)])
        CH = [(0, 512), (512, 512), (1024, 512), (1536, 512), (2048, 512), (2560, 288)]

        def stageA(t):
            xs = t % 2
            g, k = t // 4, t % 4
            P.add("sp", lambda e, xs=xs, t=t: e.dma_start(out=xn[xs][:, :], in_=h1[t * 128:(t + 1) * 128, :]),
                  writes=[("xn", xs)], chan=cx[xs])
            P.add("act", lambda e, xs=xs: e.activation(out=junk[:, :], in_=xn[xs][:, :], func=AF.Square,
                                                       accum_out=ssq[:, xs:xs + 1]),
                  reads=[("xn", xs)], writes=["junk", ("ssq", xs)])
            P.add("act", lambda e, xs=xs: e.activation(out=rs[:, xs:xs + 1], in_=ssq[:, xs:xs + 1], func=AF.Sqrt,
                                                       scale=1.0 / D, bias=epst[:, 0:1]),
                  reads=[("ssq", xs), "eps"], writes=[("rs", xs)])
            P.add("dve", lambda e, xs=xs: e.reciprocal(out=rs[:, xs:xs + 1], in_=rs[:, xs:xs + 1]),
                  reads=[("rs", xs)], writes=[("rs", xs)])
            P.add("dve", lambda e, xs=xs: e.scalar_tensor_tensor(
                out=nb[xs][:, :], in0=xn[xs][:, :], scalar=rs[:, xs:xs + 1], in1=gB[:, :], op0=ALU.mult, op1=ALU.mult),
                reads=[("xn", xs), ("rs", xs), "gB"], writes=[("nb", xs)])
            for c in range(8):
                P.add("pe", lambda e, xs=xs, c=c: e.transpose(
                    out=psT[xs][:, c * 128:(c + 1) * 128], in_=nb[xs][:, c * 128:(c + 1) * 128], identity=ident[:, :]),
                    reads=[("nb", xs), "ident"], writes=[("psT", xs)] if c == 0 else [])
            P.last_w[("psT", xs)] = P.ops["pe"][-1]
            P.add("act", lambda e, xs=xs: e.copy(out=aT[xs][:, :, :], in_=psT[xs][:, :].rearrange("p (c t) -> p c t", c=8)),
                  reads=[("psT", xs)], writes=[("aT", xs)])
            for ci, (c0, cw) in enumerate(CH):
                pb = (t * 6 + ci) % 4
                for c in range(8):
                    P.add("pe", lambda e, pb=pb, c=c, c0=c0, cw=cw, xs=xs: e.matmul(
                        out=pq[pb][:, 0:cw], lhsT=aT[xs][:, c, :], rhs=win_s[:, c, c0:c0 + cw],
                        start=(c == 0), stop=(c == 7)),
                        reads=[("aT", xs)] + (wkeys if t == 0 else []), writes=[("pq", pb)] if c == 0 else [])
                P.last_w[("pq", pb)] = P.ops["pe"][-1]
                if ci < 4:
                    P.add("act", lambda e, pb=pb, ci=ci, xs=xs: e.copy(
                        out=qk[xs][:, ci * 8:(ci + 1) * 8, :], in_=pq[pb][:, :].rearrange("p (h d) -> p h d", h=8)),
                        reads=[("pq", pb)], writes=[("qk", xs, ci)])
                    P.add("act", lambda e, pb=pb, ci=ci, xs=xs: e.activation(
                        out=sq[xs][:, ci * 8:(ci + 1) * 8, :], in_=pq[pb][:, :].rearrange("p (h d) -> p h d", h=8), func=AF.Square),
                        reads=[("pq", pb)], writes=[("sq", xs, ci)])
                elif ci == 4:
                    P.add("dve", lambda e, pb=pb, g=g, k=k: e.tensor_copy(
                        out=vst[g % 2][:, 0:8, k, 0:64], in_=pq[pb][:, 0:512].rearrange("p (h d) -> p h d", h=8)),
                        reads=[("pq", pb), ("vst1", g % 2)], writes=[("vst", g % 2, k, 0)])
                else:
                    P.add("dve", lambda e, pb=pb, g=g, k=k: e.tensor_copy(
                        out=vst[g % 2][:, 8:12, k, 0:64], in_=pq[pb][:, 0:256].rearrange("p (h d) -> p h d", h=4)),
                        reads=[("pq", pb), ("vst1", g % 2)], writes=[("vst", g % 2, k, 1)])
                    P.add("dve", lambda e, pb=pb, t=t: e.tensor_copy(out=Gall[:, t, :], in_=pq[pb][:, 256:280]),
                          reads=[("pq", pb)], writes=[("Gall", t)])
                    P.add("dve", lambda e, pb=pb, t=t: e.tensor_tensor(out=LFall[:, t, :], in0=pq[pb][:, 280:288], in1=bfg[:, :],
                                                                      op=ALU.add),
                          reads=[("pq", pb), "bfg"], writes=[("LFall", t)])
            if k == 3:
                P.add("sp", lambda e, g=g: e.dma_start(
                    out=T["V"].rearrange("h p t c -> p h (t c)")[:, :, 4 * g * 72:(4 * g + 4) * 72],
                    in_=vst[g % 2][:, :, :, :].rearrange("p h t c -> p h (t c)")),
                    reads=[("vst", g % 2, kk, j) for kk in range(4) for j in range(2)], chan=cv[g % 2])

        def stageB(t):
            xs = t % 2
            g, k = t // 4, t % 4
            P.add("dve", lambda e, xs=xs: e.tensor_reduce(out=hs[xs][:, :], in_=sq[xs][:, :, :], axis=AX.X, op=ALU.add),
                  reads=[("sq", xs, i) for i in range(4)], writes=[("hs", xs)])
            P.add("act", lambda e, xs=xs: e.activation(out=hs[xs][:, :], in_=hs[xs][:, :], func=AF.Sqrt,
                                                       scale=1.0 / 64, bias=epst[:, 0:1]),
                  reads=[("hs", xs), "eps"], writes=[("hs", xs)])
            P.add("dve", lambda e, xs=xs: e.reciprocal(out=hs[xs][:, :], in_=hs[xs][:, :]),
                  reads=[("hs", xs)], writes=[("hs", xs)])
            qkk = [("qk", xs, i) for i in range(4)]
            P.add("dve", lambda e, xs=xs: e.tensor_tensor(
                out=qk[xs][:, 2:30, :], in0=qk[xs][:, 2:30, :], in1=hs[xs][:, 2:30].unsqueeze(2).to_broadcast([128, 28, 64]),
                op=ALU.mult), reads=qkk + [("hs", xs)], writes=qkk)
            P.add("dve", lambda e, xs=xs: e.tensor_tensor(
                out=qk[xs][:, 2:30, :], in0=qk[xs][:, 2:30, :], in1=GQ[:, :, :], op=ALU.mult),
                reads=qkk + gqk, writes=qkk)
            cb = lambda tab, t=t: tab[:, t, :].unsqueeze(1).to_broadcast([128, 14, 8])
            x1 = lambda xs=xs: qk[xs][:, 0:14, 0:8]
            x2 = lambda xs=xs: qk[xs][:, 0:14, 8:16]
            for j, (src, tab) in enumerate(((x1, cosT), (x2, sinT), (x2, cosT), (x1, sinT))):
                P.add("pool", lambda e, j=j, src=src, tab=tab, cb=cb: e.tensor_tensor(
                    out=rt[j][:, :, :], in0=src(), in1=cb(tab), op=ALU.mult),
                    reads=qkk + ["cosT", "sinT"], writes=[("rt", j)])
            P.add("pool", lambda e, x1=x1: e.tensor_tensor(out=x1(), in0=rt[0][:, :, :], in1=rt[1][:, :, :], op=ALU.subtract),
                  reads=[("rt", 0), ("rt", 1)], writes=qkk)
            P.add("pool", lambda e, x2=x2: e.tensor_tensor(out=x2(), in0=rt[2][:, :, :], in1=rt[3][:, :, :], op=ALU.add),
                  reads=[("rt", 2), ("rt", 3)], writes=qkk)
            P.add("pool", lambda e, xs=xs: e.tensor_copy(out=qkb[xs][:, :], in_=qk[xs][:, :, :].rearrange("p h d -> p (h d)")),
                  reads=qkk, writes=[("qkb", xs)])
            for pr in range(16):
                hb = pr // 8
                P.add("pe", lambda e, pr=pr, hb=hb, xs=xs: e.transpose(
                    out=psQ[hb][:, pr % 8, :], in_=qkb[xs][:, pr * 128:(pr + 1) * 128], identity=ident[:, :]),
                    reads=[("qkb", xs), "ident"], writes=[("psQ", hb)] if pr % 8 == 0 else [])
                if pr % 8 == 7:
                    P.last_w[("psQ", hb)] = P.ops["pe"][-1]
                    P.add("act" if hb == 0 else "dve", (lambda e, hb=hb, k=k: e.copy(
                        out=qkT[:, hb * 8:(hb + 1) * 8, k * 128:(k + 1) * 128], in_=psQ[hb][:, :, :])) if hb == 0 else
                        (lambda e, hb=hb, k=k: e.tensor_copy(
                            out=qkT[:, hb * 8:(hb + 1) * 8, k * 128:(k + 1) * 128], in_=psQ[hb][:, :, :])),
                        reads=[("psQ", hb)], writes=[("qkT", k, hb)])
            if k == 3:
                P.add("sp", lambda e, g=g: e.dma_start(out=QKTv[:, :, g * 512:(g + 1) * 512], in_=qkT[:, :, :]),
                      reads=[("qkT", kk, hb) for kk in range(4) for hb in range(2)], chan=cq)

        stageA(0)
        for t in range(NT):
            if t + 1 < NT:
                stageA(t + 1)
            stageB(t)

        P.add("act", lambda e: e.activation(out=Gall[:, :, :], in_=Gall[:, :, :], func=AF.Sigmoid),
              reads=[("Gall", t) for t in range(NT)], writes=["GallF"])
        P.add("sp", lambda e: e.dma_start(out=T["G"], in_=Gall[:, :, :].rearrange("p t c -> p (t c)")),
              reads=["GallF"], chan=cf)
        P.add("act", lambda e: e.activation(out=LFall[:, :, :], in_=LFall[:, :, :], func=AF.Exp, scale=-1.0),
              reads=[("LFall", t) for t in range(NT)], writes=["LF1"])
        P.add("act", lambda e: e.activation(out=LFall[:, :, :], in_=LFall[:, :, :], func=AF.Ln, bias=1.0),
              reads=["LF1"], writes=["LF2"])
        P.add("dve", lambda e: e.tensor_scalar(out=LFall[:, :, :], in0=LFall[:, :, :], scalar1=-1.0, scalar2=None, op0=ALU.mult),
              reads=["LF2"], writes=["LF3"])
        P.add("sp", lambda e: e.dma_start(out=T["LF"], in_=LFall[:, :, :].rearrange("p t c -> p (t c)")),
              reads=["LF3"], chan=cf)

    run_phase(nc, build)


_FILL = {}


def asel(e, **kw):
    v = float(kw.pop("fill"))
    r = _FILL.get(v)
    if r is None:
        r = e.alloc_register()
        e.reg_mov(r, v)
        _FILL[v] = r
    return e.affine_select(fill=r, **kw)


def make_ident(P, nc, es):
    ident = sb(nc, es, "ident", [128, 128], BF16)
    identf = sb(nc, es, "identf", [128, 128], F32)
    P.add("pool", lambda e: e.memset(identf[:, :], 0.0), writes=["identf"])
    P.add("pool", lambda e: asel(e, out=identf[:, :], in_=identf[:, :], pattern=[[-1, 128]],
                                            compare_op=ALU.not_equal, fill=1.0, base=0, channel_multiplier=1),
          reads=["identf"], writes=["identf"])
    P.add("pool", lambda e: e.tensor_copy(out=ident[:, :], in_=identf[:, :]), reads=["identf"], writes=["ident"])
    return ident, identf


def cmp_phase(nc, T):
    def build(P, es):
        ident, identf = make_ident(P, nc, es)
        epst = sb(nc, es, "epst", [128, 1], F32)
        P.add("pool", lambda e: e.memset(epst[:, :], EPS), writes=["eps"])
        tok = sb(nc, es, "tok", [64, 4, S], BF16)
        w1s = [sb(nc, es, "w1s%d" % i, [64, 32, 256], BF16) for i in range(2)]
        stg = [sb(nc, es, "cstg%d" % i, [64, 32, 256], F32) for i in range(2)]
        w1f = [sb(nc, es, "w1f%d" % i, [128, 16, 256], F32) for i in range(2)]
        posr = sb(nc, es, "posr", [16, 2, 128], F32)
        posc = sb(nc, es, "posc", [128, 2, 16], F32)
        w2f = sb(nc, es, "w2f", [128, 2, 2, 64], F32)
        w2s = sb(nc, es, "w2s", [128, 2, 2, 64], BF16)
        biasT = sb(nc, es, "biasT", [128, 4], F32)
        gk = sb(nc, es, "gk", [128, 64], F32)
        hidT = [sb(nc, es, "hidT%d" % i, [128, 2, 256], BF16) for i in range(2)]
        ssq = sb(nc, es, "ssq", [128, 4], F32)
        junk = sb(nc, es, "junk", [128, 64], F32)
        kcb = [sb(nc, es, "kcb%d" % i, [128, 64], BF16) for i in range(2)]
        kcT = [sb(nc, es, "kcT%d" % i, [64, 256], BF16) for i in range(2)]
        vce = [sb(nc, es, "vce%d" % i, [128, 2, 65], BF16) for i in range(2)]
        psHf = [ps(nc, es, "psH%d" % i, [128, 512], F32) for i in range(2)]
        psH = [t[:, 0:256] for t in psHf]
        psOf = [ps(nc, es, "psO%d" % i, [128, 512], F32) for i in range(2)]
        psO = [t[:, 0:64] for t in psOf]
        psBf = ps(nc, es, "psB", [128, 512], F32)
        psB = psBf[:, 0:4]
        psPf = ps(nc, es, "psP", [128, 512], F32)
        psP = psPf[:, 0:32].rearrange("p (a b) -> p a b", a=2)
        psKf = ps(nc, es, "psK", [128, 1024], BF16)
        psK = psKf[0:64, 0:128]
        c0 = P.chan()
        c1 = [P.chan() for _ in range(2)]
        co = P.chan()

        for j, h in enumerate((0, 1, 30, 31)):
            P.add("sp", lambda e, j=j, h=h: e.dma_start(out=tok[:, j, :], in_=T["QKT"][h, :, :]), writes=[("tok", j)], chan=c0)
        P.add("sp", lambda e: e.dma_start(out=gk[:, :], in_=T["k_norm_cmp"].partition_broadcast(128)), writes=["gk"], chan=c0)
        for kv, nm in enumerate(("cmp_pos_k", "cmp_pos_v")):
            P.add("sp", lambda e, kv=kv, nm=nm: e.dma_start(
                out=posr[:, kv, :], in_=T[nm].rearrange("(c a) d -> c (a d)", a=2)), writes=[("posr", kv)], chan=c0)
        for kv, nm in enumerate(("cmp_k_w2", "cmp_v_w2")):
            P.add("sp", lambda e, kv=kv, nm=nm: e.dma_start(
                out=w2f[:, kv, :, :], in_=T[nm].rearrange("(c p) d -> p c d", p=128)), writes=[("w2f", kv)], chan=c0)
        for kv, nm in enumerate(("cmp_k_w1", "cmp_v_w1")):
            P.add("sp", lambda e, kv=kv, nm=nm: e.dma_start(
                out=w1f[kv][:, :, :], in_=T[nm].rearrange("(c p) h -> p c h", p=128)), writes=[("w1f", kv)], chan=c0)
        c0.seal()
        for kv, nm in enumerate(("cmp_k_w1", "cmp_v_w1")):
            P.add("sp", lambda e, kv=kv, nm=nm: e.dma_start(
                out=stg[kv][:, :, :], in_=T[nm].rearrange("(l d) h -> d l h", d=64)), writes=[("stg", kv)], chan=c1[kv])
            P.add("dve" if kv == 0 else "pool", lambda e, kv=kv: e.tensor_copy(out=w1s[kv][:, :, :], in_=stg[kv][:, :, :]),
                  reads=[("stg", kv)], writes=[("w1s", kv)])
        P.add("dve", lambda e: e.tensor_copy(out=w2s[:, :, :, :], in_=w2f[:, :, :, :]),
              reads=[("w2f", 0), ("w2f", 1)], writes=["w2s"])
        for kv in range(2):
            P.add("pe", lambda e, kv=kv: e.transpose(out=psP[:, kv, :], in_=posr[:, kv, :], identity=identf[0:16, 0:16]),
                  reads=[("posr", kv), "identf"], writes=[("psP", kv)])
        P.add("dve", lambda e: e.tensor_copy(out=posc[:, :, :], in_=psP),
              reads=[("psP", 0), ("psP", 1)], writes=["posc"])
        for kv in range(2):
            for hc in range(2):
                for c in range(16):
                    P.add("pe", lambda e, kv=kv, hc=hc, c=c: e.matmul(
                        out=psB[:, kv * 2 + hc:kv * 2 + hc + 1], lhsT=w1f[kv][:, c, hc * 128:(hc + 1) * 128],
                        rhs=posc[:, kv, c:c + 1], start=(c == 0), stop=(c == 15)),
                        reads=[("w1f", kv), "posc"], writes=["psB"] if (c == 0 and kv == 0 and hc == 0) else [])
        P.last_w["psB"] = P.ops["pe"][-1]
        P.add("dve", lambda e: e.tensor_copy(out=biasT[:, :], in_=psB), reads=["psB"], writes=["biasT"])
        for i in range(2):
            P.add("pool", lambda e, i=i: e.memset(kcb[i][:, :], 0.0), writes=[("kcb", i)])
            P.add("pool", lambda e, i=i: e.memset(vce[i][:, :, :], 0.0), writes=[("vce", i)])
            P.add("pool", lambda e, i=i: e.memset(vce[i][:, :, 64:65], 1.0), reads=[("vce", i)], writes=[("vce", i)])
            P.add("pool", lambda e, i=i: e.memset(hidT[i][:, :, :], 0.0), writes=[("hidT", i, 0), ("hidT", i, 1)])
        VCv = T["VC"].rearrange("h (c p) e -> h p c e", p=128)
        it = 0
        for kv in range(2):
            for head in range(2):
                sl = it % 2
                it += 1
                tv = tok[:, kv * 2 + head, :].rearrange("p (n r) -> p n r", r=16)
                for hc in range(2):
                    for l in range(32):
                        q, r = l // 16, l % 16
                        P.add("pe", lambda e, kv=kv, hc=hc, l=l, q=q, r=r, tv=tv: e.matmul(
                            out=psH[hc][:, 0:255], lhsT=w1s[kv][:, l, hc * 128:(hc + 1) * 128], rhs=tv[:, q:q + 255, r],
                            start=(l == 0), stop=(l == 31)),
                            reads=[("tok", kv * 2 + head), ("w1s", kv)], writes=[("psH", hc)] if l == 0 else [])
                    P.last_w[("psH", hc)] = P.ops["pe"][-1]
                    P.add("act", lambda e, kv=kv, hc=hc, sl=sl: e.activation(
                        out=hidT[sl][:, hc, 0:255], in_=psH[hc][:, 0:255], func=AF.Silu,
                        bias=biasT[:, kv * 2 + hc:kv * 2 + hc + 1]),
                        reads=[("psH", hc), "biasT"], writes=[("hidT", sl, hc)])
                for ci, (n0, nn) in enumerate(((0, 128), (128, 127))):
                    for hc in range(2):
                        P.add("pe", lambda e, kv=kv, hc=hc, sl=sl, ci=ci, n0=n0, nn=nn: e.matmul(
                            out=psO[ci][0:nn, :], lhsT=hidT[sl][:, hc, n0:n0 + nn], rhs=w2s[:, kv, hc, :],
                            start=(hc == 0), stop=(hc == 1)),
                            reads=[("hidT", sl, 0), ("hidT", sl, 1), "w2s"], writes=[("psO", ci)] if hc == 0 else [])
                    P.last_w[("psO", ci)] = P.ops["pe"][-1]
                    if kv == 0:
                        col = head * 2 + ci
                        P.add("act", lambda e, ci=ci, nn=nn, col=col: e.activation(
                            out=junk[0:nn, :], in_=psO[ci][0:nn, :], func=AF.Square, accum_out=ssq[0:nn, col:col + 1]),
                            reads=[("psO", ci)], writes=["junk", ("ssq", col)])
                        P.add("act", lambda e, nn=nn, col=col: e.activation(
                            out=ssq[0:nn, col:col + 1], in_=ssq[0:nn, col:col + 1], func=AF.Sqrt, scale=1.0 / 64,
                            bias=epst[0:nn, 0:1]), reads=[("ssq", col), "eps"], writes=[("ssq", col)])
                        P.add("dve", lambda e, nn=nn, col=col: e.reciprocal(out=ssq[0:nn, col:col + 1], in_=ssq[0:nn, col:col + 1]),
                              reads=[("ssq", col)], writes=[("ssq", col)])
                        P.add("dve", lambda e, ci=ci, nn=nn, col=col: e.scalar_tensor_tensor(
                            out=kcb[ci][0:nn, :], in0=psO[ci][0:nn, :], scalar=ssq[0:nn, col:col + 1], in1=gk[0:nn, :],
                            op0=ALU.mult, op1=ALU.mult), reads=[("psO", ci), ("ssq", col), "gk"], writes=[("kcb", ci)])
                        P.add("pe", lambda e, ci=ci: e.transpose(out=psK, in_=kcb[ci][:, :], identity=ident[:, :]),
                              reads=[("kcb", ci), "ident"], writes=["psK"])
                        P.add("act", lambda e, head=head, n0=n0: e.copy(out=kcT[head][:, n0:n0 + 128], in_=psK),
                              reads=["psK"], writes=[("kcT", head, n0)])
                    else:
                        P.add("dve", lambda e, ci=ci, nn=nn, head=head: e.tensor_copy(
                            out=vce[head][0:nn, ci, 0:64], in_=psO[ci][0:nn, :]), reads=[("psO", ci)], writes=[("vce", head)])
                if kv == 0:
                    P.add("sp", lambda e, head=head: e.dma_start(out=T["KCT"][head, :, :], in_=kcT[head][:, :]),
                          reads=[("kcT", head, 0), ("kcT", head, 128)], chan=co)
                else:
                    P.add("sp", lambda e, head=head: e.dma_start(out=VCv[head], in_=vce[head][:, :, :]),
                          reads=[("vce", head)], chan=co)

    run_phase(nc, build)


class UnitPipe:
    def __init__(self, P, psS, PT, depth=2):
        self.P, self.psS, self.PT, self.depth = P, psS, PT, depth
        self.q = []
        self.u = 0

    def push(self, lhsT, rhs, vlhsT, pacc, acc_key, first, last, mask, kdeps, bias=None, bkeys=(), post=None, cols=(0, 512)):
        P = self.P
        u = self.u
        self.u += 1
        sb_, pb = u % len(self.psS), u % len(self.PT)
        psS, PT = self.psS[sb_], self.PT[pb]
        c0, c1 = cols
        assert not first or (c0, c1) == (0, 512)
        rhs = rhs[:, c0:c1]
        P.add("pe", lambda e: e.matmul(out=psS[:, c0:c1], lhsT=lhsT, rhs=rhs, start=True, stop=True),
              reads=kdeps, writes=[("psS", sb_)])
        if bias is None:
            P.add("act", lambda e: e.activation(out=PT[:, c0:c1], in_=psS[:, c0:c1], func=AF.Exp, scale=0.125),
                  reads=[("psS", sb_)], writes=[("PT", pb)])
        else:
            P.add("act", lambda e: e.activation(out=PT[:, c0:c1], in_=psS[:, c0:c1], func=AF.Exp, scale=0.125, bias=bias),
                  reads=[("psS", sb_)] + list(bkeys), writes=[("PT", pb)])
        if mask is not None:
            base, cm, step = mask
            P.add("pool", lambda e: asel(e, out=PT[:, c0:c1], in_=PT[:, c0:c1], pattern=[[step, c1 - c0]], compare_op=ALU.is_ge,
                                         fill=0.0, base=base + step * c0, channel_multiplier=cm), reads=[("PT", pb)], writes=[("PT", pb)])
        self.q.append((PT, pb, vlhsT, pacc, acc_key, first, last, kdeps, post, cols))
        if len(self.q) > self.depth:
            self._pv()

    def _pv(self):
        P = self.P
        PT, pb, vlhsT, pacc, acc_key, first, last, kdeps, post, (c0, c1) = self.q.pop(0)
        P.add("pe", lambda e: e.matmul(out=pacc[0:65, c0:c1], lhsT=vlhsT, rhs=PT[:, c0:c1], start=first, stop=last),
              reads=[("PT", pb)] + list(kdeps), writes=[acc_key] if first else [])
        if last:
            P.last_w[acc_key] = P.ops["pe"][-1]
            if post is not None:
                post()

    def flush(self):
        while self.q:
            self._pv()


def nsa_phase(nc, T):
    BIG = 2048.0
    TINY = 1e-30

    def build(P, es):
        ident, identf = make_ident(P, nc, es)
        QB = sb(nc, es, "QB", [128, 4, S], BF16)
        KE = sb(nc, es, "KE", [128, S], BF16)
        KW = sb(nc, es, "KW", [128, S], BF16)
        KC = sb(nc, es, "KC", [128, 256], BF16)
        Vs = sb(nc, es, "Vs", [128, NT, 72], BF16)
        Vw = sb(nc, es, "Vw", [128, NT, 72], BF16)
        VCs = sb(nc, es, "VCs", [128, 2, 72], BF16)
        OVf = sb(nc, es, "OVf", [128, 2, 72], F32)
        OV = sb(nc, es, "OV", [128, 2, 72], BF16)
        Gs = sb(nc, es, "Gs", [128, NT, 24], F32)
        ET = [[sb(nc, es, "ET%d_%d" % (i, j), [128, 512], BF16) for j in range(2)] for i in range(2)]
        PT = [sb(nc, es, "PT%d" % i, [128, 512], BF16) for i in range(4)]
        OCs = sb(nc, es, "OCs", [65, 2, 4, 512], F32)
        OWs = sb(nc, es, "OWs", [65, 4, 512], F32)
        OSs = [sb(nc, es, "OSs%d" % i, [65, 512], F32) for i in range(2)]
        imp = sb(nc, es, "imp", [128, 4, 64], F32)
        impt = sb(nc, es, "impt", [128, 4, 64], F32)
        impm = [sb(nc, es, "impm%d" % i, [128, 64], F32) for i in range(2)]
        rd4 = sb(nc, es, "rd4", [128, 4], F32)
        m1 = sb(nc, es, "m1", [128, 8], F32)
        m2 = sb(nc, es, "m2", [128, 8], F32)
        tmp = sb(nc, es, "tmp", [128, 64], F32)
        thr = sb(nc, es, "thr", [128, 1], F32)
        BN = [sb(nc, es, "BN%d" % i, [128, 128], BF16) for i in range(4)]
        dn = [sb(nc, es, "dn%d" % i, [128, 3], F32) for i in range(2)]
        ost = [sb(nc, es, "ost%d" % i, [128, 4, 256], F32) for i in range(2)]
        psS = [ps(nc, es, "psS%d" % i, [128, 512], F32) for i in range(3)]
        psOC = ps(nc, es, "psOC", [128, 512], F32)
        psOS = ps(nc, es, "psOS", [128, 512], F32)
        psOW = ps(nc, es, "psOW", [128, 512], F32)
        psIB = ps(nc, es, "psIB", [128, 512], F32)
        psI = psIB[:, 0:260].rearrange("p (a b) -> p a b", a=4)
        psBT = psIB[:, 320:384].bitcast(BF16)
        psFb = ps(nc, es, "psFb", [128, 512], F32)
        psF = psFb[:, 0:195].rearrange("p (a b) -> p a b", a=3)
        c0 = P.chan()
        cks = [P.chan() for _ in range(2)]
        cst = [P.chan() for _ in range(2)]

        P.add("sp", lambda e: e.dma_start(out=Gs[:, :, :].rearrange("p t c -> p (t c)"), in_=T["G"]), writes=["Gs"], chan=c0)
        P.add("pool", lambda e: e.memset(KE[64:128, :], BIG), writes=["KEm"])
        P.add("pool", lambda e: asel(e, out=KE[64:128, :], in_=KE[64:128, :], pattern=[[1, S]], compare_op=ALU.is_ge,
                                                fill=0.0, base=0, channel_multiplier=-64), reads=["KEm"], writes=["KEm"])
        P.add("pool", lambda e: asel(e, out=KE[64:128, :], in_=KE[64:128, :], pattern=[[-1, S]], compare_op=ALU.is_ge,
                                                fill=0.0, base=63, channel_multiplier=64), reads=["KEm"], writes=["KEm"])
        P.add("pool", lambda e: e.memset(KW[64:128, :], 0.0), writes=["KW0"])
        P.add("pool", lambda e: e.memset(KC[64:128, :], 0.0), writes=["KC0"])
        for g in range(4):
            P.add("pool", lambda e, g=g: e.memset(QB[64:128, g, :], 0.0), writes=[("QB0", g)])
        P.add("pool", lambda e: e.memset(OVf[:, :, :], 1.0), writes=["OVf"])
        for nt in range(2):
            P.add("pool", lambda e, nt=nt: asel(e,
                out=OVf[:, nt, 0:64], in_=OVf[:, nt, 0:64], pattern=[[64, 64]], compare_op=ALU.is_ge, fill=0.0,
                base=63 - 2048 * nt, channel_multiplier=-16), reads=["OVf"], writes=["OVf"])
            P.add("pool", lambda e, nt=nt: asel(e,
                out=OVf[:, nt, 0:64], in_=OVf[:, nt, 0:64], pattern=[[-64, 64]], compare_op=ALU.is_ge, fill=0.0,
                base=2048 * nt + 31, channel_multiplier=16), reads=["OVf"], writes=["OVf"])
        P.add("pool", lambda e: e.tensor_copy(out=OV[:, :, :], in_=OVf[:, :, :]), reads=["OVf"], writes=["OV"])
        for i in range(4):
            P.add("pool", lambda e, i=i: e.memset(BN[i][:, 0:64], 0.0), writes=[("BN0", i)])
        OAv = T["OA"].rearrange("(t p) c -> p t c", p=128)

        def mask_ge(tile, base, cm, step):
            return lambda e: asel(e, out=tile[:, :], in_=tile[:, :], pattern=[[step, 512]], compare_op=ALU.is_ge,
                                             fill=0.0, base=base, channel_multiplier=cm)

        pipe = UnitPipe(P, psS, PT, depth=2)

        for kh in range(DBG['kh']):
            ck = cks[kh]
            for g in range(4):
                P.add("sp", lambda e, g=g, kh=kh: e.dma_start(out=QB[0:64, g, :], in_=T["QKT"][2 + 4 * kh + g, :, :]),
                      writes=[("QBq", g)], chan=ck)
            P.add("sp", lambda e, kh=kh: e.dma_start(out=KE[0:64, :], in_=T["QKT"][10 + kh, :, :]), writes=["KEk"], chan=ck)
            P.add("sp", lambda e, kh=kh: e.dma_start(out=KW[0:64, :], in_=T["QKT"][12 + kh, :, :]), writes=["KW"], chan=ck)
            P.add("sp", lambda e, kh=kh: e.dma_start(out=KC[0:64, :], in_=T["KCT"][kh, :, :]), writes=["KC"], chan=ck)
            P.add("sp", lambda e, kh=kh: e.dma_start(out=Vs[:, :, :], in_=T["V"][kh]), writes=["Vs"], chan=ck)
            P.add("sp", lambda e, kh=kh: e.dma_start(out=Vw[:, :, :], in_=T["V"][2 + kh]), writes=["Vw"], chan=ck)
            P.add("sp", lambda e, kh=kh: e.dma_start(out=VCs[:, :, 0:65], in_=T["VC"][kh].rearrange("(c p) e -> p c e", p=128)),
                  writes=["VCs"], chan=ck)
            ck.seal()
            def emit_AB1(i):
                qsl = slice(i * 512, (i + 1) * 512)
                nts = [0] if i < 4 else [0, 1]

                def s1(g):
                    for nt in nts:
                        u = pipe.u
                        pipe.u += 1
                        sb_ = u % 3
                        et = ET[g % 2][nt]
                        P.add("pe", lambda e, nt=nt, g=g, sb_=sb_, qsl=qsl: e.matmul(
                            out=psS[sb_][:, :], lhsT=KC[:, nt * 128:(nt + 1) * 128], rhs=QB[:, g, qsl], start=True, stop=True),
                            reads=["KC", "KC0", ("QBq", g), ("QB0", g)] + ([("QBm", tt) for tt in range(4)] if i > 0 or kh > 0 else []), writes=[("psS", sb_)])
                        P.add("act", lambda e, et=et, sb_=sb_: e.activation(out=et[:, :], in_=psS[sb_][:, :], func=AF.Exp, scale=0.125),
                              reads=[("psS", sb_)], writes=[("ET", g % 2, nt)])
                        P.add("pool", mask_ge(et, 512 * i - 2048 * nt - 31, -16, 1), reads=[("ET", g % 2, nt)], writes=[("ET", g % 2, nt)])

                def s2(g):
                    for j, nt in enumerate(nts):
                        P.add("pe", lambda e, nt=nt, j=j, g=g: e.matmul(
                            out=psOC[0:65, :], lhsT=VCs[:, nt, 0:65], rhs=ET[g % 2][nt][:, :], start=(j == 0), stop=(j == len(nts) - 1)),
                            reads=[("ET", g % 2, nt), "VCs"], writes=["psOC"] if j == 0 else [])
                    P.last_w["psOC"] = P.ops["pe"][-1]
                    P.add("act", lambda e, g=g, i=i: e.copy(out=OCs[:, i % 2, g, :], in_=psOC[0:65, :]), reads=["psOC"], writes=[("OCs", i % 2, g)])
                    for tt in range(4):
                        for j, nt in enumerate(nts):
                            P.add("pe", lambda e, nt=nt, j=j, tt=tt, g=g: e.matmul(
                                out=psI[:, tt, :], lhsT=ET[g % 2][nt][:, tt * 128:(tt + 1) * 128], rhs=OV[:, nt, 0:65],
                                start=(j == 0), stop=(j == len(nts) - 1)),
                                reads=[("ET", g % 2, nt), "OV"], writes=["psI"] if (j == 0 and tt == 0) else [])
                    P.last_w["psI"] = P.ops["pe"][-1]
                    P.add("dve", lambda e: e.tensor_scalar(out=rd4[:, :], in0=psI[:, :, 64], scalar1=TINY, scalar2=None, op0=ALU.max),
                          reads=["psI"], writes=["rd4"])
                    P.add("dve", lambda e: e.reciprocal(out=rd4[:, :], in_=rd4[:, :]), reads=["rd4"], writes=["rd4"])
                    if g == 0:
                        P.add("dve", lambda e: e.tensor_tensor(
                            out=imp[:, :, :], in0=psI[:, :, 0:64], in1=rd4[:, :].unsqueeze(2).to_broadcast([128, 4, 64]), op=ALU.mult),
                            reads=["psI", "rd4"], writes=["imp"])
                    else:
                        P.add("dve", lambda e: e.tensor_tensor(
                            out=impt[:, :, :], in0=psI[:, :, 0:64], in1=rd4[:, :].unsqueeze(2).to_broadcast([128, 4, 64]), op=ALU.mult),
                            reads=["psI", "rd4"], writes=["impt"])
                        P.add("dve", lambda e: e.tensor_tensor(out=imp[:, :, :], in0=imp[:, :, :], in1=impt[:, :, :], op=ALU.add),
                              reads=["imp", "impt"], writes=["imp"])

                s1(0)
                for g in range(4):
                    if g < 3:
                        s1(g + 1)
                    s2(g)
                for tt in range(4):
                    bs = tt % 2
                    t0 = 512 * i + 128 * tt
                    P.add("pool", lambda e, tt=tt, bs=bs, t0=t0: asel(e,
                        out=impm[bs][:, :], in_=imp[:, tt, :], pattern=[[-64, 64]], compare_op=ALU.is_ge, fill=1.0e6,
                        base=t0 - 128, channel_multiplier=1), reads=["imp"], writes=[("impm", bs)])
                    P.add("pool", lambda e, bs=bs, t0=t0: asel(e,
                        out=impm[bs][:, :], in_=impm[bs][:, :], pattern=[[-64, 64]], compare_op=ALU.is_ge, fill=-1.0,
                        base=t0, channel_multiplier=1), reads=[("impm", bs)], writes=[("impm", bs)])
                    P.add("pool", lambda e, bs=bs: e.memset(impm[bs][:, 0:1], 1.0e6), reads=[("impm", bs)], writes=[("impm", bs)])
                    P.add("dve", lambda e, bs=bs: e.max(out=m1[:, :], in_=impm[bs][:, :]), reads=[("impm", bs)], writes=["m1"])
                    P.add("dve", lambda e, bs=bs: e.match_replace(out=tmp[:, :], in_to_replace=m1[:, :], in_values=impm[bs][:, :],
                                                                  imm_value=-2.0), reads=[("impm", bs), "m1"], writes=["tmp"])
                    P.add("dve", lambda e: e.max(out=m2[:, :], in_=tmp[:, :]), reads=["tmp"], writes=["m2"])
                    P.add("dve", lambda e: e.tensor_scalar(out=thr[:, :], in0=m2[:, 7:8], scalar1=0.0, scalar2=None, op0=ALU.max),
                          reads=["m2"], writes=["thr"])
                    P.add("dve", lambda e, bs=bs, tt=tt: e.tensor_scalar(
                        out=BN[tt][:, 64:128], in0=impm[bs][:, :], scalar1=thr[:, 0:1], scalar2=1.0, op0=ALU.is_ge, op1=ALU.subtract),
                        reads=[("impm", bs), "thr", ("BN0", tt)], writes=[("BN", tt)])

            for i in range(DBG['ng']):
                qsl = slice(i * 512, (i + 1) * 512)
                if i == 0:
                    emit_AB1(0)
                for g in range(4):
                    kts = list(range(4 * i, 4 * i + 4)) + list(range(max(0, 4 * i - 4), 4 * i))
                    for j, kt in enumerate(kts):
                        if kt >= 4 * i:
                            ms = (-128 * (kt - 4 * i), -1, 1)
                            cols = (128 * (kt - 4 * i), 512)
                        else:
                            ms = (128 * (kt - 4 * i + 4) - 1, 1, -1)
                            cols = (0, 128 * (kt - 4 * i + 4) + 128)
                        post = (lambda g=g: P.add("dve", lambda e: e.tensor_copy(out=OWs[:, g, :], in_=psOW[0:65, :]),
                                                  reads=["psOW"], writes=[("OWs", g)]))
                        pipe.push(KW[:, kt * 128:(kt + 1) * 128], QB[:, g, qsl], Vw[:, kt, 0:65], psOW, "psOW",
                                  j == 0, j == len(kts) - 1, ms, ["KW", "KW0", ("QBq", g), ("QB0", g), "Vw"], post=post, cols=cols)
                for tt in range(4):
                    t0 = 512 * i + 128 * tt
                    P.add("pe", lambda e, tt=tt: e.transpose(out=psBT, in_=BN[tt][:, :], identity=ident[:, :]),
                          reads=[("BN", tt), ("BN0", tt), "ident"], writes=["psBT"])
                    P.add("act", lambda e, t0=t0: e.copy(out=QB[64:128, :, t0:t0 + 128],
                                                         in_=psBT[64:128].unsqueeze(1).to_broadcast([64, 4, 128])),
                          reads=["psBT"] + [("QB0", g_) for g_ in range(4)], writes=[("QBm", tt)])

                def finalize(g):
                    osl = g % 2
                    hd = kh * 4 + g
                    for tt in range(4):
                        tile_i = 4 * i + tt
                        ds = tt % 2
                        tsl = slice(tt * 128, (tt + 1) * 128)
                        for b, (src, key) in enumerate(((OCs[:, i % 2, g, tsl], ("OCs", i % 2, g)), (OSs[osl][:, tsl], ("OSs", osl)),
                                                         (OWs[:, g, tsl], ("OWs", g)))):
                            P.add("pe", lambda e, b=b, src=src: e.transpose(out=psF[:, b, :], in_=src, identity=identf[0:65, 0:65]),
                                  reads=[key, "identf"], writes=["psF"] if b == 0 else [])
                        P.last_w["psF"] = P.ops["pe"][-1]
                        P.add("dve", lambda e, ds=ds: e.tensor_scalar(out=dn[ds][:, :], in0=psF[:, :, 64], scalar1=TINY, scalar2=None,
                                                                      op0=ALU.max), reads=["psF"], writes=[("dn", ds)])
                        P.add("dve", lambda e, ds=ds: e.reciprocal(out=dn[ds][:, :], in_=dn[ds][:, :]), reads=[("dn", ds)], writes=[("dn", ds)])
                        P.add("dve", lambda e, ds=ds, tile_i=tile_i, hd=hd: e.tensor_tensor(
                            out=dn[ds][:, :], in0=dn[ds][:, :], in1=Gs[:, tile_i, hd * 3:hd * 3 + 3], op=ALU.mult),
                            reads=[("dn", ds), "Gs"], writes=[("dn", ds)])
                        oo = ost[i % 2][:, tt, g * 64:(g + 1) * 64]
                        P.add("dve", lambda e, ds=ds, oo=oo: e.tensor_scalar(out=oo, in0=psF[:, 0, 0:64], scalar1=dn[ds][:, 0:1],
                                                                             scalar2=None, op0=ALU.mult),
                              reads=["psF", ("dn", ds)], writes=[("ost", i % 2, tt, g)])
                        for b in (1, 2):
                            P.add("dve", lambda e, ds=ds, oo=oo, b=b: e.scalar_tensor_tensor(
                                out=oo, in0=psF[:, b, 0:64], scalar=dn[ds][:, b:b + 1], in1=oo, op0=ALU.mult, op1=ALU.add),
                                reads=["psF", ("dn", ds), ("ost", i % 2, tt, g)], writes=[("ost", i % 2, tt, g)])

                pending = []
                for g in range(4):
                    kts = list(range(0, 4 * i + 4))
                    osl = g % 2
                    for j, kt in enumerate(kts):
                        ms = (-128 * (kt - 4 * i), -1, 1) if kt >= 4 * i else None
                        cols = (128 * (kt - 4 * i), 512) if kt >= 4 * i else (0, 512)

                        def post(g=g, osl=osl):
                            P.add("dve", lambda e: e.tensor_copy(out=OSs[osl][:, :], in_=psOS[0:65, :]), reads=["psOS"], writes=[("OSs", osl)])
                            pending.append(g)
                        pipe.push(KE[:, kt * 128:(kt + 1) * 128], QB[:, g, qsl], Vs[:, kt, 0:65], psOS, "psOS",
                                  j == 0, j == len(kts) - 1, ms,
                                  ["KEk", "KEm", ("QBq", g), "Vs"] + [("QBm", tt) for tt in range(4)], post=post, cols=cols)
                        if j == 3 and pending:
                            finalize(pending.pop(0))
                    if g == 1 and i + 1 < DBG['ng']:
                        emit_AB1(i + 1)
                pipe.flush()
                while pending:
                    finalize(pending.pop(0))
                if "DBGB" in T and kh == 0:
                    P.add("sp", lambda e, qsl=qsl: e.dma_start(out=T["DBGB"][:, qsl], in_=QB[64:128, 0, qsl]),
                          reads=[("QBm", tt) for tt in range(4)], chan=c0)
                P.add("sp", lambda e, i=i, kh=kh: e.dma_start(out=OAv[:, 4 * i:4 * i + 4, kh * 256:(kh + 1) * 256], in_=ost[i % 2][:, :, :]),
                      reads=[("ost", i % 2, tt, g) for tt in range(4) for g in range(4)], chan=cst[i % 2])

    run_phase(nc, build)


def fox_phase(nc, T):
    TINY = 1e-30

    def build(P, es):
        ident, identf = make_ident(P, nc, es)
        QT = [sb(nc, es, "QT%d" % i, [128, S], BF16) for i in range(2)]
        KT = [sb(nc, es, "KT%d" % i, [128, S], BF16) for i in range(2)]
        Vf = [sb(nc, es, "Vf%d" % i, [128, NT, 72], BF16) for i in range(2)]
        lf = sb(nc, es, "lf", [128, NT, 8], F32)
        U = sb(nc, es, "U", [128, 128], F32)
        ONES = sb(nc, es, "ONES", [128, 128], F32)
        ones32 = sb(nc, es, "ones32", [128, NT], F32)
        cin = sb(nc, es, "cin", [128, NT, 8], F32)
        tot = sb(nc, es, "tot", [128, NT, 8], F32)
        incl = sb(nc, es, "incl", [128, NT, 8], F32)
        call = sb(nc, es, "call", [128, NT, 8], F32)
        biasT = sb(nc, es, "biasT", [128, NG, 8, NT], F32)
        PT = [sb(nc, es, "PT%d" % i, [128, 512], BF16) for i in range(4)]
        OFs = [sb(nc, es, "OFs%d" % i, [65, 512], F32) for i in range(2)]
        dn = [sb(nc, es, "dn%d" % i, [128, 1], F32) for i in range(2)]
        ostf = [sb(nc, es, "ostf%d" % i, [128, 4, 64], F32) for i in range(2)]
        psS = [ps(nc, es, "psS%d" % i, [128, 512], F32) for i in range(3)]
        psO = [ps(nc, es, "psO%d" % i, [128, 512], F32) for i in range(2)]
        psFF = [ps(nc, es, "psFF%d" % i, [128, 512], F32) for i in range(2)]
        psF = [psFF[0][:, 0:65], psFF[1][:, 0:65]]
        psC = psS[0][:, 0:NT * 8]
        psTt = psS[1][:, 0:NT * 8]
        c0 = P.chan()
        ckh = [P.chan() for _ in range(2)]
        cst = [P.chan() for _ in range(2)]
        OAv = T["OA"].rearrange("(t p) c -> p t c", p=128)

        P.add("sp", lambda e: e.dma_start(out=lf[:, :, :].rearrange("p t c -> p (t c)"), in_=T["LF"]), writes=["lf"], chan=c0)
        P.add("pool", lambda e: e.memset(U[:, :], 1.0), writes=["U"])
        P.add("pool", lambda e: asel(e, out=U[:, :], in_=U[:, :], pattern=[[1, 128]], compare_op=ALU.is_ge, fill=0.0,
                                     base=0, channel_multiplier=-1), reads=["U"], writes=["U"])
        P.add("pool", lambda e: e.memset(ONES[:, :], 1.0), writes=["ONES"])
        for i in range(2):
            P.add("pool", lambda e, i=i: e.memset(QT[i][64:128, :], 0.0), writes=[("QT0", i)])
            P.add("pool", lambda e, i=i: e.memset(KT[i][64:128, :], 0.0), writes=[("KT0", i)])
        P.add("pool", lambda e: e.memset(ones32[:, :], 1.0), writes=["ones32"])
        lff = lf[:, :, :].rearrange("p t c -> p (t c)")
        P.add("pe", lambda e: e.matmul(out=psC, lhsT=U[:, :], rhs=lff, start=True, stop=True), reads=["U", "lf"], writes=[("psS", 0)])
        P.add("pe", lambda e: e.matmul(out=psTt, lhsT=ONES[:, :], rhs=lff, start=True, stop=True), reads=["ONES", "lf"], writes=[("psS", 1)])
        P.add("dve", lambda e: e.tensor_copy(out=cin[:, :, :].rearrange("p t c -> p (t c)"), in_=psC), reads=[("psS", 0)], writes=["cin"])
        P.add("dve", lambda e: e.tensor_copy(out=tot[:, :, :].rearrange("p t c -> p (t c)"), in_=psTt), reads=[("psS", 1)], writes=["tot"])
        for h in range(8):
            P.add("dve", lambda e, h=h: e.tensor_tensor_scan(out=incl[:, :, h], data0=ones32[:, :], data1=tot[:, :, h], initial=0.0,
                                                             op0=ALU.mult, op1=ALU.add), reads=["tot", "ones32"], writes=[("incl", h)])
        inck = [("incl", h) for h in range(8)]
        P.add("dve", lambda e: e.tensor_tensor(out=call[:, :, :], in0=incl[:, :, :], in1=tot[:, :, :], op=ALU.subtract),
              reads=inck + ["tot"], writes=["call"])
        P.add("dve", lambda e: e.tensor_tensor(out=call[:, :, :], in0=call[:, :, :], in1=cin[:, :, :], op=ALU.add),
              reads=["call", "cin"], writes=["call"])
        for i in range(NG):
            for h in range(8):
                P.add("dve", lambda e, i=i, h=h: e.tensor_scalar(
                    out=biasT[:, i, h, :], in0=call[:, :, h], scalar1=-1.0, scalar2=incl[:, 4 * i + 1, h:h + 1],
                    op0=ALU.mult, op1=ALU.add), reads=["call"] + inck, writes=[("biasT", i, h)])
        pipe = UnitPipe(P, psS, PT, depth=2)
        fi = 0
        pending = []

        def finalize(h, i, ob):
            for tt in range(4):
                fb = tt % 2
                P.add("pe", lambda e, fb=fb, ob=ob, tt=tt: e.transpose(
                    out=psF[fb], in_=OFs[ob][:, tt * 128:(tt + 1) * 128], identity=identf[0:65, 0:65]),
                    reads=[("OFs", ob), "identf"], writes=[("psF", fb)])
                P.add("dve", lambda e, fb=fb: e.tensor_scalar(out=dn[fb][:, :], in0=psF[fb][:, 64:65], scalar1=TINY, scalar2=None,
                                                              op0=ALU.max), reads=[("psF", fb)], writes=[("dn", fb)])
                P.add("dve", lambda e, fb=fb: e.reciprocal(out=dn[fb][:, :], in_=dn[fb][:, :]), reads=[("dn", fb)], writes=[("dn", fb)])
                P.add("dve", lambda e, fb=fb, ob=ob, tt=tt: e.tensor_scalar(
                    out=ostf[ob][:, tt, :], in0=psF[fb][:, 0:64], scalar1=dn[fb][:, 0:1], scalar2=None, op0=ALU.mult),
                    reads=[("psF", fb), ("dn", fb)], writes=[("ostf", ob, tt)])
            P.add("sp", lambda e, i=i, h=h, ob=ob: e.dma_start(
                out=OAv[:, 4 * i:4 * i + 4, 512 + 64 * h:512 + 64 * (h + 1)], in_=ostf[ob][:, :, :]),
                reads=[("ostf", ob, tt) for tt in range(4)], chan=cst[ob])

        for h in range(DBG.get('fh', 8)):
            hs_ = h % 2
            ck = ckh[hs_]
            P.add("sp", lambda e, h=h, hs_=hs_: e.dma_start(out=QT[hs_][0:64, :], in_=T["QKT"][14 + h, :, :]), writes=[("QT", hs_)], chan=ck)
            P.add("sp", lambda e, h=h, hs_=hs_: e.dma_start(out=KT[hs_][0:64, :], in_=T["QKT"][22 + h, :, :]), writes=[("KT", hs_)], chan=ck)
            P.add("sp", lambda e, h=h, hs_=hs_: e.dma_start(out=Vf[hs_][:, :, :], in_=T["V"][4 + h]), writes=[("Vf", hs_)], chan=ck)
            for op in ck.ops[-3:]:
                op.chanval = ck.count
            for i in range(NG):
                qsl = slice(i * 512, (i + 1) * 512)
                ob = fi % 2
                fi += 1
                nk = 4 * i + 4
                for kt in range(nk):
                    ms = (-128 * (kt - 4 * i), -1, 1) if kt >= 4 * i else None
                    cols = (128 * (kt - 4 * i), 512) if kt >= 4 * i else (0, 512)

                    def post(h=h, i=i, ob=ob):
                        P.add("dve", lambda e: e.tensor_copy(out=OFs[ob][:, :], in_=psO[ob][0:65, :]), reads=[("psO", ob)], writes=[("OFs", ob)])
                        pending.append((h, i, ob))
                    pipe.push(KT[hs_][:, kt * 128:(kt + 1) * 128], QT[hs_][:, qsl], Vf[hs_][:, kt, 0:65], psO[ob], ("psO", ob),
                              kt == 0, kt == nk - 1, ms, [("KT", hs_), ("QT", hs_), ("Vf", hs_), ("QT0", hs_), ("KT0", hs_)],
                              bias=biasT[:, i, h, kt:kt + 1], bkeys=[("biasT", i, h)], post=post, cols=cols)
                    if pending and (kt == 3 or DBG.get('fox_now', 0)):
                        finalize(*pending.pop(0))
        pipe.flush()
        while pending:
            finalize(*pending.pop(0))

    run_phase(nc, build)


def norm_rows(P, src, junk, ssq, rs, epst, nb, gB, nparts, width, key):
    for j in range(nparts):
        cs = slice(j * width, (j + 1) * width)
        P.add("act", lambda e, cs=cs, j=j: e.activation(out=junk[:, cs], in_=src[:, cs], func=AF.Square, accum_out=ssq[:, j:j + 1]),
              reads=[key], writes=["junk", ("ssq", key, j)])
    P.add("act", lambda e: e.activation(out=rs[:, 0:nparts], in_=ssq[:, 0:nparts], func=AF.Sqrt, scale=1.0 / width, bias=epst[:, 0:1]),
          reads=[("ssq", key, j) for j in range(nparts)] + ["eps"], writes=[("rs", key)])
    P.add("dve", lambda e: e.reciprocal(out=rs[:, 0:nparts], in_=rs[:, 0:nparts]), reads=[("rs", key)], writes=[("rs", key)])
    for j in range(nparts):
        cs = slice(j * width, (j + 1) * width)
        P.add("dve", lambda e, cs=cs, j=j: e.scalar_tensor_tensor(out=nb[:, cs], in0=src[:, cs], scalar=rs[:, j:j + 1], in1=gB[:, cs],
                                                                  op0=ALU.mult, op1=ALU.mult),
              reads=[key, ("rs", key), "gB", "gB2"], writes=[("nb", key)])


def out_phase(nc, T):
    def build(P, es):
        ident, identf = make_ident(P, nc, es)
        epst = sb(nc, es, "epst", [128, 1], F32)
        P.add("pool", lambda e: e.memset(epst[:, :], EPS), writes=["eps"])
        wo = sb(nc, es, "wo", [128, 8, D], BF16)
        stage = [sb(nc, es, "ostg%d" % i, [128, D], F32) for i in range(2)]
        stch = [P.chan() for _ in range(2)]
        gB = sb(nc, es, "gB", [128, D], F32)
        xn = [sb(nc, es, "xn%d" % i, [128, D], F32) for i in range(2)]
        xr = [sb(nc, es, "xr%d" % i, [128, D], F32) for i in range(2)]
        nb = [sb(nc, es, "nb%d" % i, [128, D], BF16) for i in range(2)]
        junk = sb(nc, es, "junk", [128, D], BF16)
        ssq = [sb(nc, es, "ssq%d" % i, [128, 2], F32) for i in range(2)]
        rs = [sb(nc, es, "rs%d" % i, [128, 2], F32) for i in range(2)]
        mT = [sb(nc, es, "mT%d" % i, [128, 8, 128], BF16) for i in range(2)]
        psT = [ps(nc, es, "psT%d" % i, [128, D], BF16) for i in range(2)]
        pso = [ps(nc, es, "pso%d" % i, [128, 512], F32) for i in range(4)]
        cg = P.chan()
        cx = [P.chan() for _ in range(2)]
        cr = [P.chan() for _ in range(2)]
        co = [P.chan() for _ in range(2)]
        P.add("sp", lambda e: e.dma_start(out=gB[:, 0:512], in_=T["out_norm_nsa"].partition_broadcast(128)), writes=["gB"], chan=cg)
        P.add("sp", lambda e: e.dma_start(out=gB[:, 512:1024], in_=T["out_norm_fox"].partition_broadcast(128)), writes=["gB2"], chan=cg)
        cg.seal()
        wv = T["w_out"].rearrange("(c p) f -> p c f", p=128)
        for c in range(8):
            s_ = c % 2
            P.add("sp", lambda e, s_=s_, c=c: e.dma_start(out=stage[s_][:, :], in_=wv[:, c, :]), writes=[("stage", s_)], chan=stch[s_])
            P.add("dve" if c % 2 == 0 else "pool", lambda e, s_=s_, c=c: e.tensor_copy(out=wo[:, c, :], in_=stage[s_][:, :]),
                  reads=[("stage", s_)], writes=[("wo", c)])
        wkeys = [("wo", c) for c in range(8)]
        def stage1(t):
            xs = t % 2
            P.add("sp", lambda e, xs=xs, t=t: e.dma_start(out=xn[xs][:, :], in_=T["OA"][t * 128:(t + 1) * 128, :]), writes=[("xn", xs)], chan=cx[xs])
            P.add("sp", lambda e, xs=xs, t=t: e.dma_start(out=xr[xs][:, :], in_=T["h1"][t * 128:(t + 1) * 128, :]), writes=[("xr", xs)], chan=cr[xs])
            norm_rows(P, xn[xs], junk, ssq[xs], rs[xs], epst, nb[xs], gB, 2, 512, ("xn", xs))
            for c in range(8):
                P.add("pe", lambda e, xs=xs, c=c: e.transpose(out=psT[xs][:, c * 128:(c + 1) * 128], in_=nb[xs][:, c * 128:(c + 1) * 128],
                                                              identity=ident[:, :]),
                      reads=[("nb", ("xn", xs)), "ident"], writes=[("psT", xs)] if c == 0 else [])
            P.last_w[("psT", xs)] = P.ops["pe"][-1]
            P.add("act", lambda e, xs=xs: e.copy(out=mT[xs][:, :, :], in_=psT[xs][:, :].rearrange("p (c t) -> p c t", c=8)),
                  reads=[("psT", xs)], writes=[("mT", xs)])

        def stage2(t):
            xs = t % 2
            for half in range(2):
                pb = (t * 2 + half) % 4
                for c in range(8):
                    P.add("pe", lambda e, pb=pb, c=c, xs=xs, half=half: e.matmul(
                        out=pso[pb][:, :], lhsT=mT[xs][:, c, :], rhs=wo[:, c, half * 512:(half + 1) * 512], start=(c == 0), stop=(c == 7)),
                        reads=[("mT", xs)] + (wkeys if t == 0 else []), writes=[("pso", pb)] if c == 0 else [])
                P.last_w[("pso", pb)] = P.ops["pe"][-1]
                P.add("dve", lambda e, pb=pb, xs=xs, half=half: e.tensor_tensor(
                    out=xr[xs][:, half * 512:(half + 1) * 512], in0=pso[pb][:, :], in1=xr[xs][:, half * 512:(half + 1) * 512], op=ALU.add),
                    reads=[("pso", pb), ("xr", xs)], writes=[("xr", xs)])
            P.add("sp", lambda e, xs=xs, t=t: e.dma_start(out=T["h1"][t * 128:(t + 1) * 128, :], in_=xr[xs][:, :]),
                  reads=[("xr", xs)], writes=[("xrst", xs)], chan=co[xs])

        stage1(0)
        for t in range(NT):
            if t + 1 < NT:
                stage1(t + 1)
            stage2(t)

    run_phase(nc, build)


def ple_phase(nc, T):
    def build(P, es):
        ident, identf = make_ident(P, nc, es)
        epst = sb(nc, es, "epst", [128, 1], F32)
        P.add("pool", lambda e: e.memset(epst[:, :], EPS), writes=["eps"])
        wg = sb(nc, es, "wg", [128, 8, D], BF16)
        wp = sb(nc, es, "wp", [128, 2, D], BF16)
        stage = [sb(nc, es, "lstg%d" % i, [128, D], F32) for i in range(2)]
        stch = [P.chan() for _ in range(2)]
        gB = sb(nc, es, "gB", [128, D], F32)
        gE = sb(nc, es, "gE", [128, D], F32)
        xn = [sb(nc, es, "xn%d" % i, [128, D], F32) for i in range(2)]
        pt = [sb(nc, es, "pt%d" % i, [128, 256], F32) for i in range(2)]
        pb16 = [sb(nc, es, "pb%d" % i, [128, 256], BF16) for i in range(2)]
        nb = [sb(nc, es, "nb%d" % i, [128, D], BF16) for i in range(2)]
        junk = sb(nc, es, "junk", [128, D], BF16)
        junk2 = sb(nc, es, "junk2", [128, D], BF16)
        ssq = [sb(nc, es, "ssq%d" % i, [128, 4], F32) for i in range(2)]
        rs = [sb(nc, es, "rs%d" % i, [128, 4], F32) for i in range(2)]
        mT = [sb(nc, es, "mT%d" % i, [128, 10, 128], BF16) for i in range(2)]
        gate = [sb(nc, es, "gate%d" % i, [128, D], F32) for i in range(2)]
        ev = [sb(nc, es, "ev%d" % i, [128, D], F32) for i in range(2)]
        psTa = ps(nc, es, "psTa", [128, 8, 128], BF16)
        psTb = ps(nc, es, "psTb", [128, 8, 128], BF16)
        psg = [ps(nc, es, "psg%d" % i, [128, 512], F32) for i in range(2)]
        pse = [ps(nc, es, "pse%d" % i, [128, 512], F32) for i in range(2)]
        cg = P.chan()
        cx = [P.chan() for _ in range(2)]
        cp = [P.chan() for _ in range(2)]
        co = [P.chan() for _ in range(2)]
        P.add("sp", lambda e: e.dma_start(out=gB[:, :], in_=T["ple_gate_norm"].partition_broadcast(128)), writes=["gB"], chan=cg)
        P.add("sp", lambda e: e.dma_start(out=gE[:, :], in_=T["ple_norm"].partition_broadcast(128)), writes=["gE"], chan=cg)
        cg.seal()
        wv = T["ple_w_gate"].rearrange("(c p) f -> p c f", p=128)
        wpv = T["ple_w_proj"].rearrange("(c p) f -> p c f", p=128)
        for c in range(10):
            s_ = c % 2
            src = wv[:, c, :] if c < 8 else wpv[:, c - 8, :]
            dstw = wg[:, c, :] if c < 8 else wp[:, c - 8, :]
            P.add("sp", lambda e, s_=s_, src=src: e.dma_start(out=stage[s_][:, :], in_=src), writes=[("stage", s_)], chan=stch[s_])
            P.add("dve" if c % 2 == 0 else "pool", lambda e, s_=s_, dstw=dstw: e.tensor_copy(out=dstw, in_=stage[s_][:, :]),
                  reads=[("stage", s_)], writes=[("w", c)])
        wkeys = [("w", c) for c in range(10)]
        def stage1(t):
            xs = t % 2
            P.add("sp", lambda e, xs=xs, t=t: e.dma_start(out=xn[xs][:, :], in_=T["h3"][t * 128:(t + 1) * 128, :]), writes=[("xn", xs)], chan=cx[xs])
            P.add("sp", lambda e, xs=xs, t=t: e.dma_start(out=pt[xs][:, :], in_=T["p"][t * 128:(t + 1) * 128, :]), writes=[("pt", xs)], chan=cp[xs])
            norm_rows(P, xn[xs], junk, ssq[xs], rs[xs], epst, nb[xs], gB, 1, D, ("xn", xs))
            P.add("pool", lambda e, xs=xs: e.tensor_copy(out=pb16[xs][:, :], in_=pt[xs][:, :]), reads=[("pt", xs)], writes=[("pb16", xs)])
            for c in range(10):
                src = nb[xs][:, c * 128:(c + 1) * 128] if c < 8 else pb16[xs][:, (c - 8) * 128:(c - 7) * 128]
                dstp = psTa[:, c, :] if c < 8 else psTb[:, c - 8, :]
                P.add("pe", lambda e, dstp=dstp, src=src: e.transpose(out=dstp, in_=src, identity=ident[:, :]),
                      reads=[("nb", ("xn", xs)), ("pb16", xs), "ident"], writes=["psTa" if c < 8 else "psTb"] if c in (0, 8) else [])
                if c == 7:
                    P.last_w["psTa"] = P.ops["pe"][-1]
            P.last_w["psTb"] = P.ops["pe"][-1]
            P.add("act", lambda e, xs=xs: e.copy(out=mT[xs][:, 0:8, :], in_=psTa[:, :, :]), reads=["psTa"], writes=[("mT", xs)])
            P.add("act", lambda e, xs=xs: e.copy(out=mT[xs][:, 8:10, :], in_=psTb[:, 0:2, :]), reads=["psTb"], writes=[("mT2", xs)])

        def stage2(t):
            xs = t % 2
            for half in range(2):
                hsl = slice(half * 512, (half + 1) * 512)
                for c in range(8):
                    P.add("pe", lambda e, c=c, xs=xs, half=half, hsl=hsl: e.matmul(
                        out=psg[half][:, :], lhsT=mT[xs][:, c, :], rhs=wg[:, c, hsl], start=(c == 0), stop=(c == 7)),
                        reads=[("mT", xs), ("mT2", xs)] + (wkeys if t == 0 else []), writes=[("psg", half)] if c == 0 else [])
                P.last_w[("psg", half)] = P.ops["pe"][-1]
                P.add("act", lambda e, xs=xs, half=half, hsl=hsl: e.activation(out=gate[xs][:, hsl], in_=psg[half][:, :], func=AF.Sigmoid),
                      reads=[("psg", half)], writes=[("gate", xs, half)])
                for c in range(2):
                    P.add("pe", lambda e, c=c, xs=xs, half=half, hsl=hsl: e.matmul(
                        out=pse[half][:, :], lhsT=mT[xs][:, 8 + c, :], rhs=wp[:, c, hsl], start=(c == 0), stop=(c == 1)),
                        reads=[("mT", xs), ("mT2", xs)] + (wkeys if t == 0 else []), writes=[("pse", half)] if c == 0 else [])
                P.last_w[("pse", half)] = P.ops["pe"][-1]
                P.add("act", lambda e, xs=xs, half=half, hsl=hsl: e.activation(
                    out=junk2[:, hsl], in_=pse[half][:, :], func=AF.Square, accum_out=ssq[xs][:, 2 + half:3 + half]),
                    reads=[("pse", half)], writes=[("junk2", half), ("ssqe", xs, half)])
            P.add("dve", lambda e, xs=xs: e.tensor_tensor(out=rs[xs][:, 2:3], in0=ssq[xs][:, 2:3], in1=ssq[xs][:, 3:4], op=ALU.add),
                  reads=[("ssqe", xs, 0), ("ssqe", xs, 1)], writes=[("rse", xs)])
            P.add("act", lambda e, xs=xs: e.activation(out=rs[xs][:, 2:3], in_=rs[xs][:, 2:3], func=AF.Sqrt, scale=1.0 / D, bias=epst[:, 0:1]),
                  reads=[("rse", xs), "eps"], writes=[("rse", xs)])
            P.add("dve", lambda e, xs=xs: e.reciprocal(out=rs[xs][:, 2:3], in_=rs[xs][:, 2:3]), reads=[("rse", xs)], writes=[("rse", xs)])
            for half in range(2):
                hsl = slice(half * 512, (half + 1) * 512)
                P.add("dve", lambda e, xs=xs, half=half, hsl=hsl: e.scalar_tensor_tensor(
                    out=ev[xs][:, hsl], in0=pse[half][:, :], scalar=rs[xs][:, 2:3], in1=gE[:, hsl], op0=ALU.mult, op1=ALU.mult),
                    reads=[("pse", half), ("rse", xs), "gE"], writes=[("ev", xs, half)])
                P.add("pool", lambda e, xs=xs, hsl=hsl: e.tensor_tensor(out=ev[xs][:, hsl], in0=ev[xs][:, hsl], in1=gate[xs][:, hsl], op=ALU.mult),
                      reads=[("ev", xs, half), ("gate", xs, half)], writes=[("ev", xs, half)])
                P.add("pool", lambda e, xs=xs, hsl=hsl: e.tensor_tensor(out=ev[xs][:, hsl], in0=ev[xs][:, hsl], in1=xn[xs][:, hsl], op=ALU.add),
                      reads=[("ev", xs, half), ("xn", xs)], writes=[("ev", xs, half)])
            P.add("sp", lambda e, xs=xs, t=t: e.dma_start(out=T["out"][t * 128:(t + 1) * 128, :], in_=ev[xs][:, :]),
                  reads=[("ev", xs, 0), ("ev", xs, 1)], chan=co[xs])


        stage1(0)
        for t in range(NT):
            if t + 1 < NT:
                stage1(t + 1)
            stage2(t)

    run_phase(nc, build)


def rope_tables_np():
    pos = np.arange(S, dtype=np.float32)
    inv = (np.float32(500000.0) ** (-np.arange(0, 16, 2, dtype=np.float32) / np.float32(16))).astype(np.float32)
    ang = (pos[:, None] * inv[None, :]).astype(np.float32)
    return np.cos(ang).astype(np.float32), np.sin(ang).astype(np.float32)


IN_SHAPES = dict(
    x=[S, D], p=[S, 256], ffn1_norm=[D], ffn1_wg=[D, DFF], ffn1_wu=[D, DFF], ffn1_wd=[DFF, D],
    mix_norm=[D], w_in=[D, 2848], b_forget=[8], q_norm_nsa=[64], k_norm_cmp=[64], k_norm_slc=[64], k_norm_win=[64],
    cmp_pos_k=[32, 64], cmp_pos_v=[32, 64], cmp_k_w1=[2048, 256], cmp_k_w2=[256, 64], cmp_v_w1=[2048, 256],
    cmp_v_w2=[256, 64], q_norm_fox=[64], k_norm_fox=[64], out_norm_nsa=[512], out_norm_fox=[512], w_out=[D, D],
    ffn2_norm=[D], ffn2_wg=[D, DFF], ffn2_wu=[D, DFF], ffn2_wd=[DFF, D], ple_gate_norm=[D], ple_w_gate=[D, D],
    ple_w_proj=[256, D], ple_norm=[D], rope_cos=[S, 8], rope_sin=[S, 8])


def build_nc(nph=99, debug=(), skip=()):
    nc = bass.Bass("TRN2", target_bir_lowering=False)
    T = {}
    for name, shape in IN_SHAPES.items():
        T[name] = nc.dram_tensor(name, shape, F32, kind="ExternalInput").ap()

    def scratch(name, shape, dt):
        kind = "ExternalOutput" if name in debug else "Internal"
        T[name] = nc.dram_tensor(name, shape, dt, kind=kind).ap()

    T["out"] = nc.dram_tensor("out", [S, D], F32, kind="ExternalOutput").ap()
    scratch("h1", [S, D], F32)
    scratch("QKT", [32, 64, S], BF16)
    scratch("V", [12, 128, NT, 72], BF16)
    scratch("G", [128, NT * 24], F32)
    scratch("LF", [128, NT * 8], F32)
    scratch("KCT", [2, 64, 256], BF16)
    scratch("VC", [2, 256, 65], BF16)
    scratch("OA", [S, D], F32)
    scratch("h3", [S, D], F32)
    if "DBGB" in debug:
        scratch("DBGB", [64, S], BF16)
    if "DBGT" in debug:
        scratch("DBGT", [3, 65, 512], F32)
    phases = [
        lambda: ffn_half_phase(nc, T, T["x"], T["x"], T["h1"], T["ffn1_norm"], T["ffn1_wg"], T["ffn1_wu"], T["ffn1_wd"], 0, 11, "f1a"),
        lambda: ffn_half_phase(nc, T, T["x"], T["h1"], T["h1"], T["ffn1_norm"], T["ffn1_wg"], T["ffn1_wu"], T["ffn1_wd"], 11, 11, "f1b"),
        lambda: proj_phase(nc, T),
        lambda: cmp_phase(nc, T),
        lambda: nsa_phase(nc, T),
        lambda: fox_phase(nc, T),
        lambda: out_phase(nc, T),
        lambda: ffn_half_phase(nc, T, T["h1"], T["h1"], T["h3"], T["ffn2_norm"], T["ffn2_wg"], T["ffn2_wu"], T["ffn2_wd"], 0, 11, "f2a"),
        lambda: ffn_half_phase(nc, T, T["h1"], T["h3"], T["h3"], T["ffn2_norm"], T["ffn2_wg"], T["ffn2_wu"], T["ffn2_wd"], 11, 11, "f2b"),
        lambda: ple_phase(nc, T),
    ]
    for k, ph in enumerate(phases[:nph]):
        if k not in skip:
            ph()
    return nc


def make_in_maps(inputs, cores=range(8)):
    cos, sin = rope_tables_np()
    in_maps = []
    for b in cores:
        m = {}
        for name in IN_SHAPES:
            if name == "x":
                a = inputs["x"][b]
            elif name == "p":
                a = inputs["p"][0, b]
            elif name == "rope_cos":
                a = cos
            elif name == "rope_sin":
                a = sin
            elif name == "w_in":
                a = inputs["w_in"][0][:, W_IN_PERM]
            else:
                a = inputs[name][0]
            m[name] = np.ascontiguousarray(a, dtype=np.float32)
        in_maps.append(m)
    return in_maps


def kernel(**inputs):
    nc = build_nc()
    res = run_bass_kernel_spmd(nc, make_in_maps(inputs), core_ids=list(range(8)))
    return np.stack([np.asarray(r["out"]) for r in res.results], axis=0)
```

```python
import numpy as np
from contextlib import ExitStack
import concourse.bass as bass
import concourse.mybir as mybir
from concourse.bass_utils import run_bass_kernel_spmd

F32 = mybir.dt.float32
BF16 = mybir.dt.bfloat16
AF = mybir.ActivationFunctionType
ALU = mybir.AluOpType
AX = mybir.AxisListType

S = 4096
D = 1024
DFF = 2816
NT = S // 128
NG = S // 512
EPS = 1e-6
SAME_ENGINE_SYNC = True
DBG = dict(kh=2, ng=NG, stage=5)
_UID = [0]


def _u(name):
    _UID[0] += 1
    return "%s_%d" % (name, _UID[0])

_OFF = dict(qa=0, kc=512, vc=640, ksl=768, vsl=896, kwn=1024, vwn=1152, ga=1280, qf=1304, kf=1816, vf=2328, fl=2840)
_SZ = dict(qa=512, kc=128, vc=128, ksl=128, vsl=128, kwn=128, vwn=128, ga=24, qf=512, kf=512, vf=512, fl=8)
_ORDER = ['kc', 'qa', 'ksl', 'kwn', 'qf', 'kf', 'vc', 'vsl', 'vwn', 'vf', 'ga', 'fl']
W_IN_PERM = np.concatenate([np.arange(_OFF[k], _OFF[k] + _SZ[k]) for k in _ORDER])


class Chan:
    def __init__(self, sem):
        self.sem = sem
        self.count = 0
        self.ops = []

    def seal(self):
        for op in self.ops:
            op.chanval = self.count


class Op:
    __slots__ = ("eng", "fn", "deps", "sig", "sigval", "chan", "chanval", "idx")


class Prog:
    ENGS = ("pe", "act", "dve", "pool", "sp")

    def __init__(self, nc, es):
        self.nc = nc
        self.es = es
        self.ops = {e: [] for e in self.ENGS}
        self.last_w = {}
        self.readers = {}
        self.engsem = {e: es.enter_context(nc.semaphore(_u("s_" + e))) for e in self.ENGS}
        self.chans = []

    def chan(self):
        c = Chan(self.es.enter_context(self.nc.semaphore(_u("c"))))
        self.chans.append(c)
        return c

    def add(self, eng, fn, reads=(), writes=(), chan=None):
        op = Op()
        op.eng = eng
        op.fn = fn
        op.sig = False
        op.sigval = 0
        op.chan = chan
        deps = []
        for r in reads:
            w = self.last_w.get(r)
            if w is not None:
                deps.append(w)
        for w in writes:
            lw = self.last_w.get(w)
            if lw is not None:
                deps.append(lw)
            deps.extend(self.readers.get(w, ()))
        best = {}
        for d in deps:
            k = ("c", id(d.chan)) if d.chan is not None else ("e", d.eng)
            if k not in best or best[k].idx < d.idx:
                best[k] = d
        op.deps = list(best.values())
        op.idx = len(self.ops[eng])
        for r in reads:
            self.readers.setdefault(r, []).append(op)
        for w in writes:
            self.last_w[w] = op
            self.readers[w] = []
        if chan is not None:
            chan.count += 16
            op.chanval = chan.count
            chan.ops.append(op)
        self.ops[eng].append(op)
        return op

    def emit(self, block):
        for e in self.ENGS:
            for op in self.ops[e]:
                for d in op.deps:
                    if d.chan is None and (d.eng != op.eng or SAME_ENGINE_SYNC):
                        d.sig = True
        for e in self.ENGS:
            c = 0
            for op in self.ops[e]:
                if op.sig and op.chan is None:
                    c += 1
                    op.sigval = c
        final = [(c.sem, c.count) for c in self.chans if c.count > 0]

        def mk(ename):
            ops = self.ops[ename]

            def body(eng):
                waited = {}
                if ename == "pool":
                    _FILL.clear()
                for op in ops:
                    need = []
                    for d in op.deps:
                        if d.chan is not None:
                            sem, val = d.chan.sem, d.chanval
                        elif d.eng != ename or SAME_ENGINE_SYNC:
                            sem, val = self.engsem[d.eng], d.sigval
                        else:
                            continue
                        k = id(sem)
                        if waited.get(k, 0) >= val:
                            continue
                        need.append((sem, val))
                        waited[k] = val
                    for sem, val in need[:-1]:
                        eng.wait_ge(sem, val)
                    ins = op.fn(eng)
                    if need:
                        ins._wait_ge(need[-1][0], need[-1][1])
                    if op.chan is not None:
                        ins.then_inc(op.chan.sem, 16)
                    elif op.sig:
                        ins.then_inc(self.engsem[ename], 1)
                if ename == "sp":
                    for sem, val in final:
                        eng.wait_ge(sem, val)
                if ename == "pool":
                    for r in _FILL.values():
                        eng.free_register(r)
                    _FILL.clear()
            return body

        block.tensor(mk("pe"))
        block.scalar(mk("act"))
        block.vector(mk("dve"))
        block.gpsimd(mk("pool"))
        block.sync(mk("sp"))


def run_phase(nc, build):
    with ExitStack() as es:
        P = Prog(nc, es)
        build(P, es)
        sems = list(P.engsem.values()) + [c.sem for c in P.chans]
        with nc.Block() as b0:
            def clr(e):
                for sm in sems:
                    e.sem_clear(sm)
            b0.sync(clr)
        with nc.Block() as block:
            P.emit(block)


def sb(nc, es, name, shape, dt):
    return es.enter_context(nc.sbuf_tensor(_u(name), shape, dt))


def ps(nc, es, name, shape, dt):
    return es.enter_context(nc.psum_tensor(_u(name), shape, dt))


def load_cast_rows(P, nc, es, dst, src_rows, ncols, chans, stage, key):
    n = len(src_rows)
    for k in range(n):
        s = k % len(stage)
        st = stage[s]
        ch = chans[s]
        src = src_rows[k]
        P.add("sp", lambda e, st=st, src=src: e.dma_start(out=st[:, 0:ncols], in_=src),
              writes=[("stage", s)], chan=ch)
        eng = "dve" if k % 2 == 0 else "pool"
        P.add(eng, lambda e, st=st, k=k: e.tensor_copy(out=dst[:, k, 0:ncols], in_=st[:, 0:ncols]),
              reads=[("stage", s)], writes=[(key, k)])


def ffn_half_phase(nc, T, src_norm, src_res, dst, gain, wg, wu, wd, f0, nfc, tagp):
    def build(P, es):
        FW = nfc * 128
        wg_s = sb(nc, es, "wg_s", [128, 8, FW], BF16)
        wu_s = sb(nc, es, "wu_s", [128, 8, FW], BF16)
        wd_s = sb(nc, es, "wd_s", [128, nfc, D], BF16)
        stage = [sb(nc, es, "stg%d" % i, [128, FW], F32) for i in range(3)]
        stch = [P.chan() for _ in range(3)]
        gB = sb(nc, es, "gB", [128, D], F32)
        ident = sb(nc, es, "ident", [128, 128], BF16)
        identf = sb(nc, es, "identf", [128, 128], F32)
        epst = sb(nc, es, "epst", [128, 1], F32)
        xn = [sb(nc, es, "xn%d" % i, [128, D], F32) for i in range(2)]
        xr = [sb(nc, es, "xr%d" % i, [128, D], F32) for i in range(2)]
        nb = [sb(nc, es, "nb%d" % i, [128, D], BF16) for i in range(4)]
        junk = sb(nc, es, "junk", [128, D], BF16)
        ssq = [sb(nc, es, "ssq%d" % i, [128, 4], F32) for i in range(2)]
        rs = [sb(nc, es, "rs%d" % i, [128, 4], F32) for i in range(2)]
        nT = [sb(nc, es, "nT%d" % i, [128, 8, 512], BF16) for i in range(2)]
        hT = sb(nc, es, "hT", [128, nfc, 512], BF16)
        sg = [sb(nc, es, "sg%d" % i, [128, 512], F32) for i in range(2)]
        psT = [ps(nc, es, "psT%d" % i, [128, D], BF16) for i in range(2)]
        psg = [ps(nc, es, "psg%d" % i, [128, 512], F32) for i in range(2)]
        psu = [ps(nc, es, "psu%d" % i, [128, 512], F32) for i in range(2)]
        pso = [ps(nc, es, "pso%d" % i, [128, 512], F32) for i in range(2)]
        cx = [P.chan() for _ in range(2)]
        cr = [P.chan() for _ in range(2)]
        co = [P.chan() for _ in range(2)]
        cg = P.chan()

        P.add("sp", lambda e: e.dma_start(out=gB[:, :], in_=gain.partition_broadcast(128)),
              writes=["gB"], chan=cg)
        P.add("pool", lambda e: e.memset(identf[:, :], 0.0), writes=["identf"])
        P.add("pool", lambda e: asel(e, out=identf[:, :], in_=identf[:, :], pattern=[[-1, 128]],
                                                compare_op=ALU.not_equal, fill=1.0, base=0,
                                                channel_multiplier=1),
              reads=["identf"], writes=["identf"])
        P.add("pool", lambda e: e.tensor_copy(out=ident[:, :], in_=identf[:, :]), reads=["identf"], writes=["ident"])
        P.add("pool", lambda e: e.memset(epst[:, :], EPS), writes=["eps"])

        wgv = wg.rearrange("(c p) f -> p c f", p=128)
        wuv = wu.rearrange("(c p) f -> p c f", p=128)
        load_cast_rows(P, nc, es, wg_s, [wgv[:, c, f0 * 128:f0 * 128 + FW] for c in range(8)], FW, stch, stage, "wg")
        load_cast_rows(P, nc, es, wu_s, [wuv[:, c, f0 * 128:f0 * 128 + FW] for c in range(8)], FW, stch, stage, "wu")
        load_cast_rows(P, nc, es, wd_s, [wd[(f0 + c) * 128:(f0 + c + 1) * 128, :] for c in range(nfc)], D, stch, stage, "wd")
        wkeys = [("wg", c) for c in range(8)] + [("wu", c) for c in range(8)]
        wdkeys = [("wd", c) for c in range(nfc)]

        def prep_group(g):
            sl = g % 2
            for k in range(4):
                t = 4 * g + k
                xs = t % 2
                P.add("sp", lambda e, xs=xs, t=t: e.dma_start(out=xn[xs][:, :], in_=src_norm[t * 128:(t + 1) * 128, :]),
                      writes=[("xn", xs)], chan=cx[xs])
                P.add("act", lambda e, xs=xs, sl=sl, k=k: e.activation(
                    out=junk[:, :], in_=xn[xs][:, :], func=AF.Square, accum_out=ssq[sl][:, k:k + 1]),
                    reads=[("xn", xs)], writes=["junk", ("ssq", sl, k)])
                P.add("act", lambda e, sl=sl, k=k: e.activation(out=rs[sl][:, k:k + 1], in_=ssq[sl][:, k:k + 1],
                                                                func=AF.Sqrt, scale=1.0 / D, bias=epst[:, 0:1]),
                      reads=[("ssq", sl, k), "eps"], writes=[("rs", sl, k)])
                P.add("dve", lambda e, sl=sl, k=k: e.reciprocal(out=rs[sl][:, k:k + 1], in_=rs[sl][:, k:k + 1]),
                      reads=[("rs", sl, k)], writes=[("rs", sl, k)])
                P.add("dve", lambda e, xs=xs, sl=sl, k=k: e.scalar_tensor_tensor(
                    out=nb[k][:, :], in0=xn[xs][:, :], scalar=rs[sl][:, k:k + 1], in1=gB[:, :],
                    op0=ALU.mult, op1=ALU.mult),
                    reads=[("xn", xs), ("rs", sl, k), "gB"], writes=[("nb", k)])

        def transposes(g):
            sl = g % 2
            for k in range(4):
                pb = k % 2
                for c in range(8):
                    P.add("pe", lambda e, pb=pb, k=k, c=c: e.transpose(
                        out=psT[pb][:, c * 128:(c + 1) * 128], in_=nb[k][:, c * 128:(c + 1) * 128], identity=ident[:, :]),
                        reads=[("nb", k), "ident"], writes=[("psT", pb)] if c == 0 else [])
                P.last_w[("psT", pb)] = P.ops["pe"][-1]
                P.add("act", lambda e, pb=pb, sl=sl, k=k: e.copy(
                    out=nT[sl][:, :, k * 128:(k + 1) * 128],
                    in_=psT[pb][:, :].rearrange("p (c t) -> p c t", c=8)),
                    reads=[("psT", pb)], writes=[("nT", sl, k)])

        def upgate(g):
            sl = g % 2
            for fc in range(nfc):
                b = fc % 2
                for (wt, pst, nm) in ((wg_s, psg, "psg"), (wu_s, psu, "psu")):
                    for c in range(8):
                        P.add("pe", lambda e, wt=wt, pst=pst, b=b, c=c, fc=fc, sl=sl: e.matmul(
                            out=pst[b][:, :], lhsT=wt[:, c, fc * 128:(fc + 1) * 128], rhs=nT[sl][:, c, :],
                            start=(c == 0), stop=(c == 7)),
                            reads=[("nT", sl, 0), ("nT", sl, 1), ("nT", sl, 2), ("nT", sl, 3)] + (wkeys if g == 0 else []),
                            writes=[(nm, b)] if c == 0 else [])
                    P.last_w[(nm, b)] = P.ops["pe"][-1]
                P.add("act", lambda e, b=b: e.activation(out=sg[b][:, :], in_=psg[b][:, :], func=AF.Silu),
                      reads=[("psg", b)], writes=[("sg", b)])
                P.add("dve", lambda e, b=b, fc=fc: e.tensor_tensor(out=hT[:, fc, :], in0=psu[b][:, :], in1=sg[b][:, :],
                                                                  op=ALU.mult),
                      reads=[("psu", b), ("sg", b)], writes=[("hT", fc)])

        def down(g):
            for k in range(4):
                t = 4 * g + k
                rsl = t % 2
                P.add("sp", lambda e, rsl=rsl, t=t: e.dma_start(out=xr[rsl][:, :], in_=src_res[t * 128:(t + 1) * 128, :]),
                      reads=[("dram", t)], writes=[("xr", rsl)], chan=cr[rsl])
                for half in range(2):
                    b = half
                    for fc in range(nfc):
                        P.add("pe", lambda e, b=b, fc=fc, k=k, half=half: e.matmul(
                            out=pso[b][:, :], lhsT=hT[:, fc, k * 128:(k + 1) * 128],
                            rhs=wd_s[:, fc, half * 512:(half + 1) * 512], start=(fc == 0), stop=(fc == nfc - 1)),
                            reads=[("hT", fc)] + (wdkeys if g == 0 else []),
                            writes=[("pso", b)] if fc == 0 else [])
                    P.last_w[("pso", b)] = P.ops["pe"][-1]
                    P.add("dve", lambda e, b=b, rsl=rsl, half=half: e.scalar_tensor_tensor(
                        out=xr[rsl][:, half * 512:(half + 1) * 512], in0=pso[b][:, :], scalar=0.5,
                        in1=xr[rsl][:, half * 512:(half + 1) * 512], op0=ALU.mult, op1=ALU.add),
                        reads=[("pso", b), ("xr", rsl)], writes=[("xr", rsl)])
                P.add("sp", lambda e, rsl=rsl, t=t: e.dma_start(out=dst[t * 128:(t + 1) * 128, :], in_=xr[rsl][:, :]),
                      reads=[("xr", rsl)], writes=[("dram", t)], chan=co[rsl])

        prep_group(0)
        transposes(0)
        for g in range(NG):
            if g + 1 < NG:
                prep_group(g + 1)
            upgate(g)
            if g + 1 < NG:
                transposes(g + 1)
            down(g)

    run_phase(nc, build)


def proj_phase(nc, T):
    def build(P, es):
        h1 = T["h1"]
        WIN = 2848
        win_s = sb(nc, es, "win_s", [128, 8, WIN], BF16)
        HW_ = WIN // 2
        stage = [sb(nc, es, "pstg%d" % i, [128, HW_], F32) for i in range(3)]
        stch = [P.chan() for _ in range(3)]
        gB = sb(nc, es, "gB", [128, D], F32)
        ident = sb(nc, es, "ident", [128, 128], BF16)
        identf = sb(nc, es, "identf", [128, 128], F32)
        epst = sb(nc, es, "epst", [128, 1], F32)
        g5 = sb(nc, es, "g5", [128, 5, 64], F32)
        GQ = sb(nc, es, "GQ", [128, 28, 64], F32)
        bfg = sb(nc, es, "bfg", [128, 8], F32)
        cosT = sb(nc, es, "cosT", [128, NT, 8], F32)
        sinT = sb(nc, es, "sinT", [128, NT, 8], F32)
        Gall = sb(nc, es, "Gall", [128, NT, 24], F32)
        LFall = sb(nc, es, "LFall", [128, NT, 8], F32)
        xn = [sb(nc, es, "xn%d" % i, [128, D], F32) for i in range(2)]
        nb = [sb(nc, es, "nb%d" % i, [128, D], BF16) for i in range(2)]
        junk = sb(nc, es, "junk", [128, D], BF16)
        ssq = sb(nc, es, "ssq", [128, 2], F32)
        rs = sb(nc, es, "rs", [128, 2], F32)
        aT = [sb(nc, es, "aT%d" % i, [128, 8, 128], BF16) for i in range(2)]
        qk = [sb(nc, es, "qk%d" % i, [128, 32, 64], F32) for i in range(2)]
        sq = [sb(nc, es, "sq%d" % i, [128, 32, 64], F32) for i in range(2)]
        hs = [sb(nc, es, "hs%d" % i, [128, 32], F32) for i in range(2)]
        rt = [sb(nc, es, "rt%d" % i, [128, 14, 8], F32) for i in range(4)]
        qkb = [sb(nc, es, "qkb%d" % i, [128, 2048], BF16) for i in range(2)]
        qkT = sb(nc, es, "qkT", [128, 16, 512], BF16)
        vst = [sb(nc, es, "vst%d" % i, [128, 12, 4, 72], BF16) for i in range(2)]
        psT = [ps(nc, es, "psT%d" % i, [128, D], BF16) for i in range(2)]
        pq = [ps(nc, es, "pq%d" % i, [128, 512], F32) for i in range(4)]
        psQ = [ps(nc, es, "psQ%d" % i, [128, 8, 128], BF16) for i in range(2)]
        cx = [P.chan() for _ in range(2)]
        cg = P.chan()
        cq = P.chan()
        cv = [P.chan() for _ in range(2)]
        cf = P.chan()

        P.add("sp", lambda e: e.dma_start(out=gB[:, :], in_=T["mix_norm"].partition_broadcast(128)), writes=["gB"], chan=cg)
        for i, nm in enumerate(("q_norm_nsa", "k_norm_slc", "k_norm_win", "q_norm_fox", "k_norm_fox")):
            P.add("sp", lambda e, i=i, nm=nm: e.dma_start(out=g5[:, i, :], in_=T[nm].partition_broadcast(128)),
                  writes=[("g5", i)], chan=cg)
        P.add("sp", lambda e: e.dma_start(out=bfg[:, :], in_=T["b_forget"].partition_broadcast(128)), writes=["bfg"], chan=cg)
        P.add("sp", lambda e: e.dma_start(out=cosT[:, :, :], in_=T["rope_cos"].rearrange("(t p) c -> p t c", p=128)),
              writes=["cosT"], chan=cg)
        P.add("sp", lambda e: e.dma_start(out=sinT[:, :, :], in_=T["rope_sin"].rearrange("(t p) c -> p t c", p=128)),
              writes=["sinT"], chan=cg)
        cg.seal()
        for (i, h0, nh) in ((0, 0, 8), (1, 8, 2), (2, 10, 2), (3, 12, 8), (4, 20, 8)):
            P.add("dve", lambda e, i=i, h0=h0, nh=nh: e.tensor_copy(
                out=GQ[:, h0:h0 + nh, :], in_=g5[:, i, :].unsqueeze(1).to_broadcast([128, nh, 64])),
                reads=[("g5", i)], writes=[("GQ", i)])
        gqk = [("GQ", i) for i in range(5)]
        P.add("pool", lambda e: e.memset(identf[:, :], 0.0), writes=["identf"])
        P.add("pool", lambda e: asel(e, out=identf[:, :], in_=identf[:, :], pattern=[[-1, 128]],
                                                compare_op=ALU.not_equal, fill=1.0, base=0, channel_multiplier=1),
              reads=["identf"], writes=["identf"])
        P.add("pool", lambda e: e.tensor_copy(out=ident[:, :], in_=identf[:, :]), reads=["identf"], writes=["ident"])
        P.add("pool", lambda e: e.memset(epst[:, :], EPS), writes=["eps"])
        wv = T["w_in"].rearrange("(c p) f -> p c f", p=128)
        for hf in range(2):
            for c in range(8):
                k = hf * 8 + c
                s = k % 3
                P.add("sp", lambda e, s=s, c=c, hf=hf: e.dma_start(out=stage[s][:, :], in_=wv[:, c, hf * HW_:(hf + 1) * HW_]),
                      writes=[("stage", s)], chan=stch[s])
                P.add("dve" if k % 2 == 0 else "pool", lambda e, s=s, c=c, hf=hf: e.tensor_copy(
                    out=win_s[:, c, hf * HW_:(hf + 1) * HW_], in_=stage[s][:, :]),
                    reads=[("stage", s)], writes=[("win", c, hf)])
        wkeys = [("win", c, hf) for c in range(8) for hf in range(2)]
        QKTv = T["QKT"].rearrange("(pr two) d s -> (two d) pr s", two=2)
        for i in range(2):
            P.add("pool", lambda e, i=i: e.memset(vst[i][:, :, :, 64:72], 1.0), writes=[("vst1", i)])
        CH = [(0, 512), (512, 512), (1024, 512), (1536, 512), (2048, 512), (2560, 288)]

        def stageA(t):
            xs = t % 2
            g, k = t // 4, t % 4
            P.add("sp", lambda e, xs=xs, t=t: e.dma_start(out=xn[xs][:, :], in_=h1[t * 128:(t + 1) * 128, :]),
                  writes=[("xn", xs)], chan=cx[xs])
            P.add("act", lambda e, xs=xs: e.activation(out=junk[:, :], in_=xn[xs][:, :], func=AF.Square,
                                                       accum_out=ssq[:, xs:xs + 1]),
                  reads=[("xn", xs)], writes=["junk", ("ssq", xs)])
            P.add("act", lambda e, xs=xs: e.activation(out=rs[:, xs:xs + 1], in_=ssq[:, xs:xs + 1], func=AF.Sqrt,
                                                       scale=1.0 / D, bias=epst[:, 0:1]),
                  reads=[("ssq", xs), "eps"], writes=[("rs", xs)])
            P.add("dve", lambda e, xs=xs: e.reciprocal(out=rs[:, xs:xs + 1], in_=rs[:, xs:xs + 1]),
                  reads=[("rs", xs)], writes=[("rs", xs)])
            P.add("dve", lambda e, xs=xs: e.scalar_tensor_tensor(
                out=nb[xs][:, :], in0=xn[xs][:, :], scalar=rs[:, xs:xs + 1], in1=gB[:, :], op0=ALU.mult, op1=ALU.mult),
                reads=[("xn", xs), ("rs", xs), "gB"], writes=[("nb", xs)])
            for c in range(8):
                P.add("pe", lambda e, xs=xs, c=c: e.transpose(
                    out=psT[xs][:, c * 128:(c + 1) * 128], in_=nb[xs][:, c * 128:(c + 1) * 128], identity=ident[:, :]),
                    reads=[("nb", xs), "ident"], writes=[("psT", xs)] if c == 0 else [])
            P.last_w[("psT", xs)] = P.ops["pe"][-1]
            P.add("act", lambda e, xs=xs: e.copy(out=aT[xs][:, :, :], in_=psT[xs][:, :].rearrange("p (c t) -> p c t", c=8)),
                  reads=[("psT", xs)], writes=[("aT", xs)])
            for ci, (c0, cw) in enumerate(CH):
                pb = (t * 6 + ci) % 4
                for c in range(8):
                    P.add("pe", lambda e, pb=pb, c=c, c0=c0, cw=cw, xs=xs: e.matmul(
                        out=pq[pb][:, 0:cw], lhsT=aT[xs][:, c, :], rhs=win_s[:, c, c0:c0 + cw],
                        start=(c == 0), stop=(c == 7)),
                        reads=[("aT", xs)] + (wkeys if t == 0 else []), writes=[("pq", pb)] if c == 0 else [])
                P.last_w[("pq", pb)] = P.ops["pe"][-1]
                if ci < 4:
                    P.add("act", lambda e, pb=pb, ci=ci, xs=xs: e.copy(
                        out=qk[xs][:, ci * 8:(ci + 1) * 8, :], in_=pq[pb][:, :].rearrange("p (h d) -> p h d", h=8)),
                        reads=[("pq", pb)], writes=[("qk", xs, ci)])
                    P.add("act", lambda e, pb=pb, ci=ci, xs=xs: e.activation(
                        out=sq[xs][:, ci * 8:(ci + 1) * 8, :], in_=pq[pb][:, :].rearrange("p (h d) -> p h d", h=8), func=AF.Square),
                        reads=[("pq", pb)], writes=[("sq", xs, ci)])
                elif ci == 4:
                    P.add("dve", lambda e, pb=pb, g=g, k=k: e.tensor_copy(
                        out=vst[g % 2][:, 0:8, k, 0:64], in_=pq[pb][:, 0:512].rearrange("p (h d) -> p h d", h=8)),
                        reads=[("pq", pb), ("vst1", g % 2)], writes=[("vst", g % 2, k, 0)])
                else:
                    P.add("dve", lambda e, pb=pb, g=g, k=k: e.tensor_copy(
                        out=vst[g % 2][:, 8:12, k, 0:64], in_=pq[pb][:, 0:256].rearrange("p (h d) -> p h d", h=4)),
                        reads=[("pq", pb), ("vst1", g % 2)], writes=[("vst", g % 2, k, 1)])
                    P.add("dve", lambda e, pb=pb, t=t: e.tensor_copy(out=Gall[:, t, :], in_=pq[pb][:, 256:280]),
                          reads=[("pq", pb)], writes=[("Gall", t)])
                    P.add("dve", lambda e, pb=pb, t=t: e.tensor_tensor(out=LFall[:, t, :], in0=pq[pb][:, 280:288], in1=bfg[:, :],
                                                                      op=ALU.add),
                          reads=[("pq", pb), "bfg"], writes=[("LFall", t)])
            if k == 3:
                P.add("sp", lambda e, g=g: e.dma_start(
                    out=T["V"].rearrange("h p t c -> p h (t c)")[:, :, 4 * g * 72:(4 * g + 4) * 72],
                    in_=vst[g % 2][:, :, :, :].rearrange("p h t c -> p h (t c)")),
                    reads=[("vst", g % 2, kk, j) for kk in range(4) for j in range(2)], chan=cv[g % 2])

        def stageB(t):
            xs = t % 2
            g, k = t // 4, t % 4
            P.add("dve", lambda e, xs=xs: e.tensor_reduce(out=hs[xs][:, :], in_=sq[xs][:, :, :], axis=AX.X, op=ALU.add),
                  reads=[("sq", xs, i) for i in range(4)], writes=[("hs", xs)])
            P.add("act", lambda e, xs=xs: e.activation(out=hs[xs][:, :], in_=hs[xs][:, :], func=AF.Sqrt,
                                                       scale=1.0 / 64, bias=epst[:, 0:1]),
                  reads=[("hs", xs), "eps"], writes=[("hs", xs)])
            P.add("dve", lambda e, xs=xs: e.reciprocal(out=hs[xs][:, :], in_=hs[xs][:, :]),
                  reads=[("hs", xs)], writes=[("hs", xs)])
            qkk = [("qk", xs, i) for i in range(4)]
            P.add("dve", lambda e, xs=xs: e.tensor_tensor(
                out=qk[xs][:, 2:30, :], in0=qk[xs][:, 2:30, :], in1=hs[xs][:, 2:30].unsqueeze(2).to_broadcast([128, 28, 64]),
                op=ALU.mult), reads=qkk + [("hs", xs)], writes=qkk)
            P.add("dve", lambda e, xs=xs: e.tensor_tensor(
                out=qk[xs][:, 2:30, :], in0=qk[xs][:, 2:30, :], in1=GQ[:, :, :], op=ALU.mult),
                reads=qkk + gqk, writes=qkk)
            cb = lambda tab, t=t: tab[:, t, :].unsqueeze(1).to_broadcast([128, 14, 8])
            x1 = lambda xs=xs: qk[xs][:, 0:14, 0:8]
            x2 = lambda xs=xs: qk[xs][:, 0:14, 8:16]
            for j, (src, tab) in enumerate(((x1, cosT), (x2, sinT), (x2, cosT), (x1, sinT))):
                P.add("pool", lambda e, j=j, src=src, tab=tab, cb=cb: e.tensor_tensor(
                    out=rt[j][:, :, :], in0=src(), in1=cb(tab), op=ALU.mult),
                    reads=qkk + ["cosT", "sinT"], writes=[("rt", j)])
            P.add("pool", lambda e, x1=x1: e.tensor_tensor(out=x1(), in0=rt[0][:, :, :], in1=rt[1][:, :, :], op=ALU.subtract),
                  reads=[("rt", 0), ("rt", 1)], writes=qkk)
            P.add("pool", lambda e, x2=x2: e.tensor_tensor(out=x2(), in0=rt[2][:, :, :], in1=rt[3][:, :, :], op=ALU.add),
                  reads=[("rt", 2), ("rt", 3)], writes=qkk)
            P.add("pool", lambda e, xs=xs: e.tensor_copy(out=qkb[xs][:, :], in_=qk[xs][:, :, :].rearrange("p h d -> p (h d)")),
                  reads=qkk, writes=[("qkb", xs)])
            for pr in range(16):
                hb = pr // 8
                P.add("pe", lambda e, pr=pr, hb=hb, xs=xs: e.transpose(
                    out=psQ[hb][:, pr % 8, :], in_=qkb[xs][:, pr * 128:(pr + 1) * 128], identity=ident[:, :]),
                    reads=[("qkb", xs), "ident"], writes=[("psQ", hb)] if pr % 8 == 0 else [])
                if pr % 8 == 7:
                    P.last_w[("psQ", hb)] = P.ops["pe"][-1]
                    P.add("act" if hb == 0 else "dve", (lambda e, hb=hb, k=k: e.copy(
                        out=qkT[:, hb * 8:(hb + 1) * 8, k * 128:(k + 1) * 128], in_=psQ[hb][:, :, :])) if hb == 0 else
                        (lambda e, hb=hb, k=k: e.tensor_copy(
                            out=qkT[:, hb * 8:(hb + 1) * 8, k * 128:(k + 1) * 128], in_=psQ[hb][:, :, :])),
                        reads=[("psQ", hb)], writes=[("qkT", k, hb)])
            if k == 3:
                P.add("sp", lambda e, g=g: e.dma_start(out=QKTv[:, :, g * 512:(g + 1) * 512], in_=qkT[:, :, :]),
                      reads=[("qkT", kk, hb) for kk in range(4) for hb in range(2)], chan=cq)

        stageA(0)
        for t in range(NT):
            if t + 1 < NT:
                stageA(t + 1)
            stageB(t)

        P.add("act", lambda e: e.activation(out=Gall[:, :, :], in_=Gall[:, :, :], func=AF.Sigmoid),
              reads=[("Gall", t) for t in range(NT)], writes=["GallF"])
        P.add("sp", lambda e: e.dma_start(out=T["G"], in_=Gall[:, :, :].rearrange("p t c -> p (t c)")),
              reads=["GallF"], chan=cf)
        P.add("act", lambda e: e.activation(out=LFall[:, :, :], in_=LFall[:, :, :], func=AF.Exp, scale=-1.0),
              reads=[("LFall", t) for t in range(NT)], writes=["LF1"])
        P.add("act", lambda e: e.activation(out=LFall[:, :, :], in_=LFall[:, :, :], func=AF.Ln, bias=1.0),
              reads=["LF1"], writes=["LF2"])
        P.add("dve", lambda e: e.tensor_scalar(out=LFall[:, :, :], in0=LFall[:, :, :], scalar1=-1.0, scalar2=None, op0=ALU.mult),
              reads=["LF2"], writes=["LF3"])
        P.add("sp", lambda e: e.dma_start(out=T["LF"], in_=LFall[:, :, :].rearrange("p t c -> p (t c)")),
              reads=["LF3"], chan=cf)

    run_phase(nc, build)


_FILL = {}


def asel(e, **kw):
    v = float(kw.pop("fill"))
    r = _FILL.get(v)
    if r is None:
        r = e.alloc_register()
        e.reg_mov(r, v)
        _FILL[v] = r
    return e.affine_select(fill=r, **kw)


def make_ident(P, nc, es):
    ident = sb(nc, es, "ident", [128, 128], BF16)
    identf = sb(nc, es, "identf", [128, 128], F32)
    P.add("pool", lambda e: e.memset(identf[:, :], 0.0), writes=["identf"])
    P.add("pool", lambda e: asel(e, out=identf[:, :], in_=identf[:, :], pattern=[[-1, 128]],
                                            compare_op=ALU.not_equal, fill=1.0, base=0, channel_multiplier=1),
          reads=["identf"], writes=["identf"])
    P.add("pool", lambda e: e.tensor_copy(out=ident[:, :], in_=identf[:, :]), reads=["identf"], writes=["ident"])
    return ident, identf


def cmp_phase(nc, T):
    def build(P, es):
        ident, identf = make_ident(P, nc, es)
        epst = sb(nc, es, "epst", [128, 1], F32)
        P.add("pool", lambda e: e.memset(epst[:, :], EPS), writes=["eps"])
        tok = sb(nc, es, "tok", [64, 4, S], BF16)
        w1s = [sb(nc, es, "w1s%d" % i, [64, 32, 256], BF16) for i in range(2)]
        stg = [sb(nc, es, "cstg%d" % i, [64, 32, 256], F32) for i in range(2)]
        w1f = [sb(nc, es, "w1f%d" % i, [128, 16, 256], F32) for i in range(2)]
        posr = sb(nc, es, "posr", [16, 2, 128], F32)
        posc = sb(nc, es, "posc", [128, 2, 16], F32)
        w2f = sb(nc, es, "w2f", [128, 2, 2, 64], F32)
        w2s = sb(nc, es, "w2s", [128, 2, 2, 64], BF16)
        biasT = sb(nc, es, "biasT", [128, 4], F32)
        gk = sb(nc, es, "gk", [128, 64], F32)
        hidT = [sb(nc, es, "hidT%d" % i, [128, 2, 256], BF16) for i in range(2)]
        ssq = sb(nc, es, "ssq", [128, 4], F32)
        junk = sb(nc, es, "junk", [128, 64], F32)
        kcb = [sb(nc, es, "kcb%d" % i, [128, 64], BF16) for i in range(2)]
        kcT = [sb(nc, es, "kcT%d" % i, [64, 256], BF16) for i in range(2)]
        vce = [sb(nc, es, "vce%d" % i, [128, 2, 65], BF16) for i in range(2)]
        psHf = [ps(nc, es, "psH%d" % i, [128, 512], F32) for i in range(2)]
        psH = [t[:, 0:256] for t in psHf]
        psOf = [ps(nc, es, "psO%d" % i, [128, 512], F32) for i in range(2)]
        psO = [t[:, 0:64] for t in psOf]
        psBf = ps(nc, es, "psB", [128, 512], F32)
        psB = psBf[:, 0:4]
        psPf = ps(nc, es, "psP", [128, 512], F32)
        psP = psPf[:, 0:32].rearrange("p (a b) -> p a b", a=2)
        psKf = ps(nc, es, "psK", [128, 1024], BF16)
        psK = psKf[0:64, 0:128]
        c0 = P.chan()
        c1 = [P.chan() for _ in range(2)]
        co = P.chan()

        for j, h in enumerate((0, 1, 30, 31)):
            P.add("sp", lambda e, j=j, h=h: e.dma_start(out=tok[:, j, :], in_=T["QKT"][h, :, :]), writes=[("tok", j)], chan=c0)
        P.add("sp", lambda e: e.dma_start(out=gk[:, :], in_=T["k_norm_cmp"].partition_broadcast(128)), writes=["gk"], chan=c0)
        for kv, nm in enumerate(("cmp_pos_k", "cmp_pos_v")):
            P.add("sp", lambda e, kv=kv, nm=nm: e.dma_start(
                out=posr[:, kv, :], in_=T[nm].rearrange("(c a) d -> c (a d)", a=2)), writes=[("posr", kv)], chan=c0)
        for kv, nm in enumerate(("cmp_k_w2", "cmp_v_w2")):
            P.add("sp", lambda e, kv=kv, nm=nm: e.dma_start(
                out=w2f[:, kv, :, :], in_=T[nm].rearrange("(c p) d -> p c d", p=128)), writes=[("w2f", kv)], chan=c0)
        for kv, nm in enumerate(("cmp_k_w1", "cmp_v_w1")):
            P.add("sp", lambda e, kv=kv, nm=nm: e.dma_start(
                out=w1f[kv][:, :, :], in_=T[nm].rearrange("(c p) h -> p c h", p=128)), writes=[("w1f", kv)], chan=c0)
        c0.seal()
        for kv, nm in enumerate(("cmp_k_w1", "cmp_v_w1")):
            P.add("sp", lambda e, kv=kv, nm=nm: e.dma_start(
                out=stg[kv][:, :, :], in_=T[nm].rearrange("(l d) h -> d l h", d=64)), writes=[("stg", kv)], chan=c1[kv])
            P.add("dve" if kv == 0 else "pool", lambda e, kv=kv: e.tensor_copy(out=w1s[kv][:, :, :], in_=stg[kv][:, :, :]),
                  reads=[("stg", kv)], writes=[("w1s", kv)])
        P.add("dve", lambda e: e.tensor_copy(out=w2s[:, :, :, :], in_=w2f[:, :, :, :]),
              reads=[("w2f", 0), ("w2f", 1)], writes=["w2s"])
        for kv in range(2):
            P.add("pe", lambda e, kv=kv: e.transpose(out=psP[:, kv, :], in_=posr[:, kv, :], identity=identf[0:16, 0:16]),
                  reads=[("posr", kv), "identf"], writes=[("psP", kv)])
        P.add("dve", lambda e: e.tensor_copy(out=posc[:, :, :], in_=psP),
              reads=[("psP", 0), ("psP", 1)], writes=["posc"])
        for kv in range(2):
            for hc in range(2):
                for c in range(16):
                    P.add("pe", lambda e, kv=kv, hc=hc, c=c: e.matmul(
                        out=psB[:, kv * 2 + hc:kv * 2 + hc + 1], lhsT=w1f[kv][:, c, hc * 128:(hc + 1) * 128],
                        rhs=posc[:, kv, c:c + 1], start=(c == 0), stop=(c == 15)),
                        reads=[("w1f", kv), "posc"], writes=["psB"] if (c == 0 and kv == 0 and hc == 0) else [])
        P.last_w["psB"] = P.ops["pe"][-1]
        P.add("dve", lambda e: e.tensor_copy(out=biasT[:, :], in_=psB), reads=["psB"], writes=["biasT"])
        for i in range(2):
            P.add("pool", lambda e, i=i: e.memset(kcb[i][:, :], 0.0), writes=[("kcb", i)])
            P.add("pool", lambda e, i=i: e.memset(vce[i][:, :, :], 0.0), writes=[("vce", i)])
            P.add("pool", lambda e, i=i: e.memset(vce[i][:, :, 64:65], 1.0), reads=[("vce", i)], writes=[("vce", i)])
            P.add("pool", lambda e, i=i: e.memset(hidT[i][:, :, :], 0.0), writes=[("hidT", i, 0), ("hidT", i, 1)])
        VCv = T["VC"].rearrange("h (c p) e -> h p c e", p=128)
        it = 0
        for kv in range(2):
            for head in range(2):
                sl = it % 2
                it += 1
                tv = tok[:, kv * 2 + head, :].rearrange("p (n r) -> p n r", r=16)
                for hc in range(2):
                    for l in range(32):
                        q, r = l // 16, l % 16
                        P.add("pe", lambda e, kv=kv, hc=hc, l=l, q=q, r=r, tv=tv: e.matmul(
                            out=psH[hc][:, 0:255], lhsT=w1s[kv][:, l, hc * 128:(hc + 1) * 128], rhs=tv[:, q:q + 255, r],
                            start=(l == 0), stop=(l == 31)),
                            reads=[("tok", kv * 2 + head), ("w1s", kv)], writes=[("psH", hc)] if l == 0 else [])
                    P.last_w[("psH", hc)] = P.ops["pe"][-1]
                    P.add("act", lambda e, kv=kv, hc=hc, sl=sl: e.activation(
                        out=hidT[sl][:, hc, 0:255], in_=psH[hc][:, 0:255], func=AF.Silu,
                        bias=biasT[:, kv * 2 + hc:kv * 2 + hc + 1]),
                        reads=[("psH", hc), "biasT"], writes=[("hidT", sl, hc)])
                for ci, (n0, nn) in enumerate(((0, 128), (128, 127))):
                    for hc in range(2):
                        P.add("pe", lambda e, kv=kv, hc=hc, sl=sl, ci=ci, n0=n0, nn=nn: e.matmul(
                            out=psO[ci][0:nn, :], lhsT=hidT[sl][:, hc, n0:n0 + nn], rhs=w2s[:, kv, hc, :],
                            start=(hc == 0), stop=(hc == 1)),
                            reads=[("hidT", sl, 0), ("hidT", sl, 1), "w2s"], writes=[("psO", ci)] if hc == 0 else [])
                    P.last_w[("psO", ci)] = P.ops["pe"][-1]
                    if kv == 0:
                        col = head * 2 + ci
                        P.add("act", lambda e, ci=ci, nn=nn, col=col: e.activation(
                            out=junk[0:nn, :], in_=psO[ci][0:nn, :], func=AF.Square, accum_out=ssq[0:nn, col:col + 1]),
                            reads=[("psO", ci)], writes=["junk", ("ssq", col)])
                        P.add("act", lambda e, nn=nn, col=col: e.activation(
                            out=ssq[0:nn, col:col + 1], in_=ssq[0:nn, col:col + 1], func=AF.Sqrt, scale=1.0 / 64,
                            bias=epst[0:nn, 0:1]), reads=[("ssq", col), "eps"], writes=[("ssq", col)])
                        P.add("dve", lambda e, nn=nn, col=col: e.reciprocal(out=ssq[0:nn, col:col + 1], in_=ssq[0:nn, col:col + 1]),
                              reads=[("ssq", col)], writes=[("ssq", col)])
                        P.add("dve", lambda e, ci=ci, nn=nn, col=col: e.scalar_tensor_tensor(
                            out=kcb[ci][0:nn, :], in0=psO[ci][0:nn, :], scalar=ssq[0:nn, col:col + 1], in1=gk[0:nn, :],
                            op0=ALU.mult, op1=ALU.mult), reads=[("psO", ci), ("ssq", col), "gk"], writes=[("kcb", ci)])
                        P.add("pe", lambda e, ci=ci: e.transpose(out=psK, in_=kcb[ci][:, :], identity=ident[:, :]),
                              reads=[("kcb", ci), "ident"], writes=["psK"])
                        P.add("act", lambda e, head=head, n0=n0: e.copy(out=kcT[head][:, n0:n0 + 128], in_=psK),
                              reads=["psK"], writes=[("kcT", head, n0)])
                    else:
                        P.add("dve", lambda e, ci=ci, nn=nn, head=head: e.tensor_copy(
                            out=vce[head][0:nn, ci, 0:64], in_=psO[ci][0:nn, :]), reads=[("psO", ci)], writes=[("vce", head)])
                if kv == 0:
                    P.add("sp", lambda e, head=head: e.dma_start(out=T["KCT"][head, :, :], in_=kcT[head][:, :]),
                          reads=[("kcT", head, 0), ("kcT", head, 128)], chan=co)
                else:
                    P.add("sp", lambda e, head=head: e.dma_start(out=VCv[head], in_=vce[head][:, :, :]),
                          reads=[("vce", head)], chan=co)

    run_phase(nc, build)


class UnitPipe:
    def __init__(self, P, psS, PT, depth=2):
        self.P, self.psS, self.PT, self.depth = P, psS, PT, depth
        self.q = []
        self.u = 0

    def push(self, lhsT, rhs, vlhsT, pacc, acc_key, first, last, mask, kdeps, bias=None, bkeys=(), post=None, cols=(0, 512)):
        P = self.P
        u = self.u
        self.u += 1
        sb_, pb = u % len(self.psS), u % len(self.PT)
        psS, PT = self.psS[sb_], self.PT[pb]
        c0, c1 = cols
        assert not first or (c0, c1) == (0, 512)
        rhs = rhs[:, c0:c1]
        P.add("pe", lambda e: e.matmul(out=psS[:, c0:c1], lhsT=lhsT, rhs=rhs, start=True, stop=True),
              reads=kdeps, writes=[("psS", sb_)])
        if bias is None:
            P.add("act", lambda e: e.activation(out=PT[:, c0:c1], in_=psS[:, c0:c1], func=AF.Exp, scale=0.125),
                  reads=[("psS", sb_)], writes=[("PT", pb)])
        else:
            P.add("act", lambda e: e.activation(out=PT[:, c0:c1], in_=psS[:, c0:c1], func=AF.Exp, scale=0.125, bias=bias),
                  reads=[("psS", sb_)] + list(bkeys), writes=[("PT", pb)])
        if mask is not None:
            base, cm, step = mask
            P.add("pool", lambda e: asel(e, out=PT[:, c0:c1], in_=PT[:, c0:c1], pattern=[[step, c1 - c0]], compare_op=ALU.is_ge,
                                         fill=0.0, base=base + step * c0, channel_multiplier=cm), reads=[("PT", pb)], writes=[("PT", pb)])
        self.q.append((PT, pb, vlhsT, pacc, acc_key, first, last, kdeps, post, cols))
        if len(self.q) > self.depth:
            self._pv()

    def _pv(self):
        P = self.P
        PT, pb, vlhsT, pacc, acc_key, first, last, kdeps, post, (c0, c1) = self.q.pop(0)
        P.add("pe", lambda e: e.matmul(out=pacc[0:65, c0:c1], lhsT=vlhsT, rhs=PT[:, c0:c1], start=first, stop=last),
              reads=[("PT", pb)] + list(kdeps), writes=[acc_key] if first else [])
        if last:
            P.last_w[acc_key] = P.ops["pe"][-1]
            if post is not None:
                post()

    def flush(self):
        while self.q:
            self._pv()


def nsa_phase(nc, T):
    BIG = 2048.0
    TINY = 1e-30

    def build(P, es):
        ident, identf = make_ident(P, nc, es)
        QB = sb(nc, es, "QB", [128, 4, S], BF16)
        KE = sb(nc, es, "KE", [128, S], BF16)
        KW = sb(nc, es, "KW", [128, S], BF16)
        KC = sb(nc, es, "KC", [128, 256], BF16)
        Vs = sb(nc, es, "Vs", [128, NT, 72], BF16)
        Vw = sb(nc, es, "Vw", [128, NT, 72], BF16)
        VCs = sb(nc, es, "VCs", [128, 2, 72], BF16)
        OVf = sb(nc, es, "OVf", [128, 2, 72], F32)
        OV = sb(nc, es, "OV", [128, 2, 72], BF16)
        Gs = sb(nc, es, "Gs", [128, NT, 24], F32)
        ET = [[sb(nc, es, "ET%d_%d" % (i, j), [128, 512], BF16) for j in range(2)] for i in range(2)]
        PT = [sb(nc, es, "PT%d" % i, [128, 512], BF16) for i in range(4)]
        OCs = sb(nc, es, "OCs", [65, 2, 4, 512], F32)
        OWs = sb(nc, es, "OWs", [65, 4, 512], F32)
        OSs = [sb(nc, es, "OSs%d" % i, [65, 512], F32) for i in range(2)]
        imp = sb(nc, es, "imp", [128, 4, 64], F32)
        impt = sb(nc, es, "impt", [128, 4, 64], F32)
        impm = [sb(nc, es, "impm%d" % i, [128, 64], F32) for i in range(2)]
        rd4 = sb(nc, es, "rd4", [128, 4], F32)
        m1 = sb(nc, es, "m1", [128, 8], F32)
        m2 = sb(nc, es, "m2", [128, 8], F32)
        tmp = sb(nc, es, "tmp", [128, 64], F32)
        thr = sb(nc, es, "thr", [128, 1], F32)
        BN = [sb(nc, es, "BN%d" % i, [128, 128], BF16) for i in range(4)]
        dn = [sb(nc, es, "dn%d" % i, [128, 3], F32) for i in range(2)]
        ost = [sb(nc, es, "ost%d" % i, [128, 4, 256], F32) for i in range(2)]
        psS = [ps(nc, es, "psS%d" % i, [128, 512], F32) for i in range(3)]
        psOC = ps(nc, es, "psOC", [128, 512], F32)
        psOS = ps(nc, es, "psOS", [128, 512], F32)
        psOW = ps(nc, es, "psOW", [128, 512], F32)
        psIB = ps(nc, es, "psIB", [128, 512], F32)
        psI = psIB[:, 0:260].rearrange("p (a b) -> p a b", a=4)
        psBT = psIB[:, 320:384].bitcast(BF16)
        psFb = ps(nc, es, "psFb", [128, 512], F32)
        psF = psFb[:, 0:195].rearrange("p (a b) -> p a b", a=3)
        c0 = P.chan()
        cks = [P.chan() for _ in range(2)]
        cst = [P.chan() for _ in range(2)]

        P.add("sp", lambda e: e.dma_start(out=Gs[:, :, :].rearrange("p t c -> p (t c)"), in_=T["G"]), writes=["Gs"], chan=c0)
        P.add("pool", lambda e: e.memset(KE[64:128, :], BIG), writes=["KEm"])
        P.add("pool", lambda e: asel(e, out=KE[64:128, :], in_=KE[64:128, :], pattern=[[1, S]], compare_op=ALU.is_ge,
                                                fill=0.0, base=0, channel_multiplier=-64), reads=["KEm"], writes=["KEm"])
        P.add("pool", lambda e: asel(e, out=KE[64:128, :], in_=KE[64:128, :], pattern=[[-1, S]], compare_op=ALU.is_ge,
                                                fill=0.0, base=63, channel_multiplier=64), reads=["KEm"], writes=["KEm"])
        P.add("pool", lambda e: e.memset(KW[64:128, :], 0.0), writes=["KW0"])
        P.add("pool", lambda e: e.memset(KC[64:128, :], 0.0), writes=["KC0"])
        for g in range(4):
            P.add("pool", lambda e, g=g: e.memset(QB[64:128, g, :], 0.0), writes=[("QB0", g)])
        P.add("pool", lambda e: e.memset(OVf[:, :, :], 1.0), writes=["OVf"])
        for nt in range(2):
            P.add("pool", lambda e, nt=nt: asel(e,
                out=OVf[:, nt, 0:64], in_=OVf[:, nt, 0:64], pattern=[[64, 64]], compare_op=ALU.is_ge, fill=0.0,
                base=63 - 2048 * nt, channel_multiplier=-16), reads=["OVf"], writes=["OVf"])
            P.add("pool", lambda e, nt=nt: asel(e,
                out=OVf[:, nt, 0:64], in_=OVf[:, nt, 0:64], pattern=[[-64, 64]], compare_op=ALU.is_ge, fill=0.0,
                base=2048 * nt + 31, channel_multiplier=16), reads=["OVf"], writes=["OVf"])
        P.add("pool", lambda e: e.tensor_copy(out=OV[:, :, :], in_=OVf[:, :, :]), reads=["OVf"], writes=["OV"])
        for i in range(4):
            P.add("pool", lambda e, i=i: e.memset(BN[i][:, 0:64], 0.0), writes=[("BN0", i)])
        OAv = T["OA"].rearrange("(t p) c -> p t c", p=128)

        def mask_ge(tile, base, cm, step):
            return lambda e: asel(e, out=tile[:, :], in_=tile[:, :], pattern=[[step, 512]], compare_op=ALU.is_ge,
                                             fill=0.0, base=base, channel_multiplier=cm)

        pipe = UnitPipe(P, psS, PT, depth=2)

        for kh in range(DBG['kh']):
            ck = cks[kh]
            for g in range(4):
                P.add("sp", lambda e, g=g, kh=kh: e.dma_start(out=QB[0:64, g, :], in_=T["QKT"][2 + 4 * kh + g, :, :]),
                      writes=[("QBq", g)], chan=ck)
            P.add("sp", lambda e, kh=kh: e.dma_start(out=KE[0:64, :], in_=T["QKT"][10 + kh, :, :]), writes=["KEk"], chan=ck)
            P.add("sp", lambda e, kh=kh: e.dma_start(out=KW[0:64, :], in_=T["QKT"][12 + kh, :, :]), writes=["KW"], chan=ck)
            P.add("sp", lambda e, kh=kh: e.dma_start(out=KC[0:64, :], in_=T["KCT"][kh, :, :]), writes=["KC"], chan=ck)
            P.add("sp", lambda e, kh=kh: e.dma_start(out=Vs[:, :, :], in_=T["V"][kh]), writes=["Vs"], chan=ck)
            P.add("sp", lambda e, kh=kh: e.dma_start(out=Vw[:, :, :], in_=T["V"][2 + kh]), writes=["Vw"], chan=ck)
            P.add("sp", lambda e, kh=kh: e.dma_start(out=VCs[:, :, 0:65], in_=T["VC"][kh].rearrange("(c p) e -> p c e", p=128)),
                  writes=["VCs"], chan=ck)
            ck.seal()
            def emit_AB1(i):
                qsl = slice(i * 512, (i + 1) * 512)
                nts = [0] if i < 4 else [0, 1]

                def s1(g):
                    for nt in nts:
                        u = pipe.u
                        pipe.u += 1
                        sb_ = u % 3
                        et = ET[g % 2][nt]
                        P.add("pe", lambda e, nt=nt, g=g, sb_=sb_, qsl=qsl: e.matmul(
                            out=psS[sb_][:, :], lhsT=KC[:, nt * 128:(nt + 1) * 128], rhs=QB[:, g, qsl], start=True, stop=True),
                            reads=["KC", "KC0", ("QBq", g), ("QB0", g)] + ([("QBm", tt) for tt in range(4)] if i > 0 or kh > 0 else []), writes=[("psS", sb_)])
                        P.add("act", lambda e, et=et, sb_=sb_: e.activation(out=et[:, :], in_=psS[sb_][:, :], func=AF.Exp, scale=0.125),
                              reads=[("psS", sb_)], writes=[("ET", g % 2, nt)])
                        P.add("pool", mask_ge(et, 512 * i - 2048 * nt - 31, -16, 1), reads=[("ET", g % 2, nt)], writes=[("ET", g % 2, nt)])

                def s2(g):
                    for j, nt in enumerate(nts):
                        P.add("pe", lambda e, nt=nt, j=j, g=g: e.matmul(
                            out=psOC[0:65, :], lhsT=VCs[:, nt, 0:65], rhs=ET[g % 2][nt][:, :], start=(j == 0), stop=(j == len(nts) - 1)),
                            reads=[("ET", g % 2, nt), "VCs"], writes=["psOC"] if j == 0 else [])
                    P.last_w["psOC"] = P.ops["pe"][-1]
                    P.add("act", lambda e, g=g, i=i: e.copy(out=OCs[:, i % 2, g, :], in_=psOC[0:65, :]), reads=["psOC"], writes=[("OCs", i % 2, g)])
                    for tt in range(4):
                        for j, nt in enumerate(nts):
                            P.add("pe", lambda e, nt=nt, j=j, tt=tt, g=g: e.matmul(
                                out=psI[:, tt, :], lhsT=ET[g % 2][nt][:, tt * 128:(tt + 1) * 128], rhs=OV[:, nt, 0:65],
                                start=(j == 0), stop=(j == len(nts) - 1)),
                                reads=[("ET", g % 2, nt), "OV"], writes=["psI"] if (j == 0 and tt == 0) else [])
                    P.last_w["psI"] = P.ops["pe"][-1]
                    P.add("dve", lambda e: e.tensor_scalar(out=rd4[:, :], in0=psI[:, :, 64], scalar1=TINY, scalar2=None, op0=ALU.max),
                          reads=["psI"], writes=["rd4"])
                    P.add("dve", lambda e: e.reciprocal(out=rd4[:, :], in_=rd4[:, :]), reads=["rd4"], writes=["rd4"])
                    if g == 0:
                        P.add("dve", lambda e: e.tensor_tensor(
                            out=imp[:, :, :], in0=psI[:, :, 0:64], in1=rd4[:, :].unsqueeze(2).to_broadcast([128, 4, 64]), op=ALU.mult),
                            reads=["psI", "rd4"], writes=["imp"])
                    else:
                        P.add("dve", lambda e: e.tensor_tensor(
                            out=impt[:, :, :], in0=psI[:, :, 0:64], in1=rd4[:, :].unsqueeze(2).to_broadcast([128, 4, 64]), op=ALU.mult),
                            reads=["psI", "rd4"], writes=["impt"])
                        P.add("dve", lambda e: e.tensor_tensor(out=imp[:, :, :], in0=imp[:, :, :], in1=impt[:, :, :], op=ALU.add),
                              reads=["imp", "impt"], writes=["imp"])

                s1(0)
                for g in range(4):
                    if g < 3:
                        s1(g + 1)
                    s2(g)
                for tt in range(4):
                    bs = tt % 2
                    t0 = 512 * i + 128 * tt
                    P.add("pool", lambda e, tt=tt, bs=bs, t0=t0: asel(e,
                        out=impm[bs][:, :], in_=imp[:, tt, :], pattern=[[-64, 64]], compare_op=ALU.is_ge, fill=1.0e6,
                        base=t0 - 128, channel_multiplier=1), reads=["imp"], writes=[("impm", bs)])
                    P.add("pool", lambda e, bs=bs, t0=t0: asel(e,
                        out=impm[bs][:, :], in_=impm[bs][:, :], pattern=[[-64, 64]], compare_op=ALU.is_ge, fill=-1.0,
                        base=t0, channel_multiplier=1), reads=[("impm", bs)], writes=[("impm", bs)])
                    P.add("pool", lambda e, bs=bs: e.memset(impm[bs][:, 0:1], 1.0e6), reads=[("impm", bs)], writes=[("impm", bs)])
                    P.add("dve", lambda e, bs=bs: e.max(out=m1[:, :], in_=impm[bs][:, :]), reads=[("impm", bs)], writes=["m1"])
                    P.add("dve", lambda e, bs=bs: e.match_replace(out=tmp[:, :], in_to_replace=m1[:, :], in_values=impm[bs][:, :],
                                                                  imm_value=-2.0), reads=[("impm", bs), "m1"], writes=["tmp"])
                    P.add("dve", lambda e: e.max(out=m2[:, :], in_=tmp[:, :]), reads=["tmp"], writes=["m2"])
                    P.add("dve", lambda e: e.tensor_scalar(out=thr[:, :], in0=m2[:, 7:8], scalar1=0.0, scalar2=None, op0=ALU.max),
                          reads=["m2"], writes=["thr"])
                    P.add("dve", lambda e, bs=bs, tt=tt: e.tensor_scalar(
                        out=BN[tt][:, 64:128], in0=impm[bs][:, :], scalar1=thr[:, 0:1], scalar2=1.0, op0=ALU.is_ge, op1=ALU.subtract),
                        reads=[("impm", bs), "thr", ("BN0", tt)], writes=[("BN", tt)])

            for i in range(DBG['ng']):
                qsl = slice(i * 512, (i + 1) * 512)
                if i == 0:
                    emit_AB1(0)
                for g in range(4):
                    kts = list(range(4 * i, 4 * i + 4)) + list(range(max(0, 4 * i - 4), 4 * i))
                    for j, kt in enumerate(kts):
                        if kt >= 4 * i:
                            ms = (-128 * (kt - 4 * i), -1, 1)
                            cols = (128 * (kt - 4 * i), 512)
                        else:
                            ms = (128 * (kt - 4 * i + 4) - 1, 1, -1)
                            cols = (0, 128 * (kt - 4 * i + 4) + 128)
                        post = (lambda g=g: P.add("dve", lambda e: e.tensor_copy(out=OWs[:, g, :], in_=psOW[0:65, :]),
                                                  reads=["psOW"], writes=[("OWs", g)]))
                        pipe.push(KW[:, kt * 128:(kt + 1) * 128], QB[:, g, qsl], Vw[:, kt, 0:65], psOW, "psOW",
                                  j == 0, j == len(kts) - 1, ms, ["KW", "KW0", ("QBq", g), ("QB0", g), "Vw"], post=post, cols=cols)
                for tt in range(4):
                    t0 = 512 * i + 128 * tt
                    P.add("pe", lambda e, tt=tt: e.transpose(out=psBT, in_=BN[tt][:, :], identity=ident[:, :]),
                          reads=[("BN", tt), ("BN0", tt), "ident"], writes=["psBT"])
                    P.add("act", lambda e, t0=t0: e.copy(out=QB[64:128, :, t0:t0 + 128],
                                                         in_=psBT[64:128].unsqueeze(1).to_broadcast([64, 4, 128])),
                          reads=["psBT"] + [("QB0", g_) for g_ in range(4)], writes=[("QBm", tt)])

                def finalize(g):
                    osl = g % 2
                    hd = kh * 4 + g
                    for tt in range(4):
                        tile_i = 4 * i + tt
                        ds = tt % 2
                        tsl = slice(tt * 128, (tt + 1) * 128)
                        for b, (src, key) in enumerate(((OCs[:, i % 2, g, tsl], ("OCs", i % 2, g)), (OSs[osl][:, tsl], ("OSs", osl)),
                                                         (OWs[:, g, tsl], ("OWs", g)))):
                            P.add("pe", lambda e, b=b, src=src: e.transpose(out=psF[:, b, :], in_=src, identity=identf[0:65, 0:65]),
                                  reads=[key, "identf"], writes=["psF"] if b == 0 else [])
                        P.last_w["psF"] = P.ops["pe"][-1]
                        P.add("dve", lambda e, ds=ds: e.tensor_scalar(out=dn[ds][:, :], in0=psF[:, :, 64], scalar1=TINY, scalar2=None,
                                                                      op0=ALU.max), reads=["psF"], writes=[("dn", ds)])
                        P.add("dve", lambda e, ds=ds: e.reciprocal(out=dn[ds][:, :], in_=dn[ds][:, :]), reads=[("dn", ds)], writes=[("dn", ds)])
                        P.add("dve", lambda e, ds=ds, tile_i=tile_i, hd=hd: e.tensor_tensor(
                            out=dn[ds][:, :], in0=dn[ds][:, :], in1=Gs[:, tile_i, hd * 3:hd * 3 + 3], op=ALU.mult),
                            reads=[("dn", ds), "Gs"], writes=[("dn", ds)])
                        oo = ost[i % 2][:, tt, g * 64:(g + 1) * 64]
                        P.add("dve", lambda e, ds=ds, oo=oo: e.tensor_scalar(out=oo, in0=psF[:, 0, 0:64], scalar1=dn[ds][:, 0:1],
                                                                             scalar2=None, op0=ALU.mult),
                              reads=["psF", ("dn", ds)], writes=[("ost", i % 2, tt, g)])
                        for b in (1, 2):
                            P.add("dve", lambda e, ds=ds, oo=oo, b=b: e.scalar_tensor_tensor(
                                out=oo, in0=psF[:, b, 0:64], scalar=dn[ds][:, b:b + 1], in1=oo, op0=ALU.mult, op1=ALU.add),
                                reads=["psF", ("dn", ds), ("ost", i % 2, tt, g)], writes=[("ost", i % 2, tt, g)])

                pending = []
                for g in range(4):
                    kts = list(range(0, 4 * i + 4))
                    osl = g % 2
                    for j, kt in enumerate(kts):
                        ms = (-128 * (kt - 4 * i), -1, 1) if kt >= 4 * i else None
                        cols = (128 * (kt - 4 * i), 512) if kt >= 4 * i else (0, 512)

                        def post(g=g, osl=osl):
                            P.add("dve", lambda e: e.tensor_copy(out=OSs[osl][:, :], in_=psOS[0:65, :]), reads=["psOS"], writes=[("OSs", osl)])
                            pending.append(g)
                        pipe.push(KE[:, kt * 128:(kt + 1) * 128], QB[:, g, qsl], Vs[:, kt, 0:65], psOS, "psOS",
                                  j == 0, j == len(kts) - 1, ms,
                                  ["KEk", "KEm", ("QBq", g), "Vs"] + [("QBm", tt) for tt in range(4)], post=post, cols=cols)
                        if j == 3 and pending:
                            finalize(pending.pop(0))
                    if g == 1 and i + 1 < DBG['ng']:
                        emit_AB1(i + 1)
                pipe.flush()
                while pending:
                    finalize(pending.pop(0))
                if "DBGB" in T and kh == 0:
                    P.add("sp", lambda e, qsl=qsl: e.dma_start(out=T["DBGB"][:, qsl], in_=QB[64:128, 0, qsl]),
                          reads=[("QBm", tt) for tt in range(4)], chan=c0)
                P.add("sp", lambda e, i=i, kh=kh: e.dma_start(out=OAv[:, 4 * i:4 * i + 4, kh * 256:(kh + 1) * 256], in_=ost[i % 2][:, :, :]),
                      reads=[("ost", i % 2, tt, g) for tt in range(4) for g in range(4)], chan=cst[i % 2])

    run_phase(nc, build)


def fox_phase(nc, T):
    TINY = 1e-30

    def build(P, es):
        ident, identf = make_ident(P, nc, es)
        QT = [sb(nc, es, "QT%d" % i, [128, S], BF16) for i in range(2)]
        KT = [sb(nc, es, "KT%d" % i, [128, S], BF16) for i in range(2)]
        Vf = [sb(nc, es, "Vf%d" % i, [128, NT, 72], BF16) for i in range(2)]
        lf = sb(nc, es, "lf", [128, NT, 8], F32)
        U = sb(nc, es, "U", [128, 128], F32)
        ONES = sb(nc, es, "ONES", [128, 128], F32)
        ones32 = sb(nc, es, "ones32", [128, NT], F32)
        cin = sb(nc, es, "cin", [128, NT, 8], F32)
        tot = sb(nc, es, "tot", [128, NT, 8], F32)
        incl = sb(nc, es, "incl", [128, NT, 8], F32)
        call = sb(nc, es, "call", [128, NT, 8], F32)
        biasT = sb(nc, es, "biasT", [128, NG, 8, NT], F32)
        PT = [sb(nc, es, "PT%d" % i, [128, 512], BF16) for i in range(4)]
        OFs = [sb(nc, es, "OFs%d" % i, [65, 512], F32) for i in range(2)]
        dn = [sb(nc, es, "dn%d" % i, [128, 1], F32) for i in range(2)]
        ostf = [sb(nc, es, "ostf%d" % i, [128, 4, 64], F32) for i in range(2)]
        psS = [ps(nc, es, "psS%d" % i, [128, 512], F32) for i in range(3)]
        psO = [ps(nc, es, "psO%d" % i, [128, 512], F32) for i in range(2)]
        psFF = [ps(nc, es, "psFF%d" % i, [128, 512], F32) for i in range(2)]
        psF = [psFF[0][:, 0:65], psFF[1][:, 0:65]]
        psC = psS[0][:, 0:NT * 8]
        psTt = psS[1][:, 0:NT * 8]
        c0 = P.chan()
        ckh = [P.chan() for _ in range(2)]
        cst = [P.chan() for _ in range(2)]
        OAv = T["OA"].rearrange("(t p) c -> p t c", p=128)

        P.add("sp", lambda e: e.dma_start(out=lf[:, :, :].rearrange("p t c -> p (t c)"), in_=T["LF"]), writes=["lf"], chan=c0)
        P.add("pool", lambda e: e.memset(U[:, :], 1.0), writes=["U"])
        P.add("pool", lambda e: asel(e, out=U[:, :], in_=U[:, :], pattern=[[1, 128]], compare_op=ALU.is_ge, fill=0.0,
                                     base=0, channel_multiplier=-1), reads=["U"], writes=["U"])
        P.add("pool", lambda e: e.memset(ONES[:, :], 1.0), writes=["ONES"])
        for i in range(2):
            P.add("pool", lambda e, i=i: e.memset(QT[i][64:128, :], 0.0), writes=[("QT0", i)])
            P.add("pool", lambda e, i=i: e.memset(KT[i][64:128, :], 0.0), writes=[("KT0", i)])
        P.add("pool", lambda e: e.memset(ones32[:, :], 1.0), writes=["ones32"])
        lff = lf[:, :, :].rearrange("p t c -> p (t c)")
        P.add("pe", lambda e: e.matmul(out=psC, lhsT=U[:, :], rhs=lff, start=True, stop=True), reads=["U", "lf"], writes=[("psS", 0)])
        P.add("pe", lambda e: e.matmul(out=psTt, lhsT=ONES[:, :], rhs=lff, start=True, stop=True), reads=["ONES", "lf"], writes=[("psS", 1)])
        P.add("dve", lambda e: e.tensor_copy(out=cin[:, :, :].rearrange("p t c -> p (t c)"), in_=psC), reads=[("psS", 0)], writes=["cin"])
        P.add("dve", lambda e: e.tensor_copy(out=tot[:, :, :].rearrange("p t c -> p (t c)"), in_=psTt), reads=[("psS", 1)], writes=["tot"])
        for h in range(8):
            P.add("dve", lambda e, h=h: e.tensor_tensor_scan(out=incl[:, :, h], data0=ones32[:, :], data1=tot[:, :, h], initial=0.0,
                                                             op0=ALU.mult, op1=ALU.add), reads=["tot", "ones32"], writes=[("incl", h)])
        inck = [("incl", h) for h in range(8)]
        P.add("dve", lambda e: e.tensor_tensor(out=call[:, :, :], in0=incl[:, :, :], in1=tot[:, :, :], op=ALU.subtract),
              reads=inck + ["tot"], writes=["call"])
        P.add("dve", lambda e: e.tensor_tensor(out=call[:, :, :], in0=call[:, :, :], in1=cin[:, :, :], op=ALU.add),
              reads=["call", "cin"], writes=["call"])
        for i in range(NG):
            for h in range(8):
                P.add("dve", lambda e, i=i, h=h: e.tensor_scalar(
                    out=biasT[:, i, h, :], in0=call[:, :, h], scalar1=-1.0, scalar2=incl[:, 4 * i + 1, h:h + 1],
                    op0=ALU.mult, op1=ALU.add), reads=["call"] + inck, writes=[("biasT", i, h)])
        pipe = UnitPipe(P, psS, PT, depth=2)
        fi = 0
        pending = []

        def finalize(h, i, ob):
            for tt in range(4):
                fb = tt % 2
                P.add("pe", lambda e, fb=fb, ob=ob, tt=tt: e.transpose(
                    out=psF[fb], in_=OFs[ob][:, tt * 128:(tt + 1) * 128], identity=identf[0:65, 0:65]),
                    reads=[("OFs", ob), "identf"], writes=[("psF", fb)])
                P.add("dve", lambda e, fb=fb: e.tensor_scalar(out=dn[fb][:, :], in0=psF[fb][:, 64:65], scalar1=TINY, scalar2=None,
                                                              op0=ALU.max), reads=[("psF", fb)], writes=[("dn", fb)])
                P.add("dve", lambda e, fb=fb: e.reciprocal(out=dn[fb][:, :], in_=dn[fb][:, :]), reads=[("dn", fb)], writes=[("dn", fb)])
                P.add("dve", lambda e, fb=fb, ob=ob, tt=tt: e.tensor_scalar(
                    out=ostf[ob][:, tt, :], in0=psF[fb][:, 0:64], scalar1=dn[fb][:, 0:1], scalar2=None, op0=ALU.mult),
                    reads=[("psF", fb), ("dn", fb)], writes=[("ostf", ob, tt)])
            P.add("sp", lambda e, i=i, h=h, ob=ob: e.dma_start(
                out=OAv[:, 4 * i:4 * i + 4, 512 + 64 * h:512 + 64 * (h + 1)], in_=ostf[ob][:, :, :]),
                reads=[("ostf", ob, tt) for tt in range(4)], chan=cst[ob])

        for h in range(DBG.get('fh', 8)):
            hs_ = h % 2
            ck = ckh[hs_]
            P.add("sp", lambda e, h=h, hs_=hs_: e.dma_start(out=QT[hs_][0:64, :], in_=T["QKT"][14 + h, :, :]), writes=[("QT", hs_)], chan=ck)
            P.add("sp", lambda e, h=h, hs_=hs_: e.dma_start(out=KT[hs_][0:64, :], in_=T["QKT"][22 + h, :, :]), writes=[("KT", hs_)], chan=ck)
            P.add("sp", lambda e, h=h, hs_=hs_: e.dma_start(out=Vf[hs_][:, :, :], in_=T["V"][4 + h]), writes=[("Vf", hs_)], chan=ck)
            for op in ck.ops[-3:]:
                op.chanval = ck.count
            for i in range(NG):
                qsl = slice(i * 512, (i + 1) * 512)
                ob = fi % 2
                fi += 1
                nk = 4 * i + 4
                for kt in range(nk):
                    ms = (-128 * (kt - 4 * i), -1, 1) if kt >= 4 * i else None
                    cols = (128 * (kt - 4 * i), 512) if kt >= 4 * i else (0, 512)

                    def post(h=h, i=i, ob=ob):
                        P.add("dve", lambda e: e.tensor_copy(out=OFs[ob][:, :], in_=psO[ob][0:65, :]), reads=[("psO", ob)], writes=[("OFs", ob)])
                        pending.append((h, i, ob))
                    pipe.push(KT[hs_][:, kt * 128:(kt + 1) * 128], QT[hs_][:, qsl], Vf[hs_][:, kt, 0:65], psO[ob], ("psO", ob),
                              kt == 0, kt == nk - 1, ms, [("KT", hs_), ("QT", hs_), ("Vf", hs_), ("QT0", hs_), ("KT0", hs_)],
                              bias=biasT[:, i, h, kt:kt + 1], bkeys=[("biasT", i, h)], post=post, cols=cols)
                    if pending and (kt == 3 or DBG.get('fox_now', 0)):
                        finalize(*pending.pop(0))
        pipe.flush()
        while pending:
            finalize(*pending.pop(0))

    run_phase(nc, build)


def norm_rows(P, src, junk, ssq, rs, epst, nb, gB, nparts, width, key):
    for j in range(nparts):
        cs = slice(j * width, (j + 1) * width)
        P.add("act", lambda e, cs=cs, j=j: e.activation(out=junk[:, cs], in_=src[:, cs], func=AF.Square, accum_out=ssq[:, j:j + 1]),
              reads=[key], writes=["junk", ("ssq", key, j)])
    P.add("act", lambda e: e.activation(out=rs[:, 0:nparts], in_=ssq[:, 0:nparts], func=AF.Sqrt, scale=1.0 / width, bias=epst[:, 0:1]),
          reads=[("ssq", key, j) for j in range(nparts)] + ["eps"], writes=[("rs", key)])
    P.add("dve", lambda e: e.reciprocal(out=rs[:, 0:nparts], in_=rs[:, 0:nparts]), reads=[("rs", key)], writes=[("rs", key)])
    for j in range(nparts):
        cs = slice(j * width, (j + 1) * width)
        P.add("dve", lambda e, cs=cs, j=j: e.scalar_tensor_tensor(out=nb[:, cs], in0=src[:, cs], scalar=rs[:, j:j + 1], in1=gB[:, cs],
                                                                  op0=ALU.mult, op1=ALU.mult),
              reads=[key, ("rs", key), "gB", "gB2"], writes=[("nb", key)])


def out_phase(nc, T):
    def build(P, es):
        ident, identf = make_ident(P, nc, es)
        epst = sb(nc, es, "epst", [128, 1], F32)
        P.add("pool", lambda e: e.memset(epst[:, :], EPS), writes=["eps"])
        wo = sb(nc, es, "wo", [128, 8, D], BF16)
        stage = [sb(nc, es, "ostg%d" % i, [128, D], F32) for i in range(2)]
        stch = [P.chan() for _ in range(2)]
        gB = sb(nc, es, "gB", [128, D], F32)
        xn = [sb(nc, es, "xn%d" % i, [128, D], F32) for i in range(2)]
        xr = [sb(nc, es, "xr%d" % i, [128, D], F32) for i in range(2)]
        nb = [sb(nc, es, "nb%d" % i, [128, D], BF16) for i in range(2)]
        junk = sb(nc, es, "junk", [128, D], BF16)
        ssq = [sb(nc, es, "ssq%d" % i, [128, 2], F32) for i in range(2)]
        rs = [sb(nc, es, "rs%d" % i, [128, 2], F32) for i in range(2)]
        mT = [sb(nc, es, "mT%d" % i, [128, 8, 128], BF16) for i in range(2)]
        psT = [ps(nc, es, "psT%d" % i, [128, D], BF16) for i in range(2)]
        pso = [ps(nc, es, "pso%d" % i, [128, 512], F32) for i in range(4)]
        cg = P.chan()
        cx = [P.chan() for _ in range(2)]
        cr = [P.chan() for _ in range(2)]
        co = [P.chan() for _ in range(2)]
        P.add("sp", lambda e: e.dma_start(out=gB[:, 0:512], in_=T["out_norm_nsa"].partition_broadcast(128)), writes=["gB"], chan=cg)
        P.add("sp", lambda e: e.dma_start(out=gB[:, 512:1024], in_=T["out_norm_fox"].partition_broadcast(128)), writes=["gB2"], chan=cg)
        cg.seal()
        wv = T["w_out"].rearrange("(c p) f -> p c f", p=128)
        for c in range(8):
            s_ = c % 2
            P.add("sp", lambda e, s_=s_, c=c: e.dma_start(out=stage[s_][:, :], in_=wv[:, c, :]), writes=[("stage", s_)], chan=stch[s_])
            P.add("dve" if c % 2 == 0 else "pool", lambda e, s_=s_, c=c: e.tensor_copy(out=wo[:, c, :], in_=stage[s_][:, :]),
                  reads=[("stage", s_)], writes=[("wo", c)])
        wkeys = [("wo", c) for c in range(8)]
        def stage1(t):
            xs = t % 2
            P.add("sp", lambda e, xs=xs, t=t: e.dma_start(out=xn[xs][:, :], in_=T["OA"][t * 128:(t + 1) * 128, :]), writes=[("xn", xs)], chan=cx[xs])
            P.add("sp", lambda e, xs=xs, t=t: e.dma_start(out=xr[xs][:, :], in_=T["h1"][t * 128:(t + 1) * 128, :]), writes=[("xr", xs)], chan=cr[xs])
            norm_rows(P, xn[xs], junk, ssq[xs], rs[xs], epst, nb[xs], gB, 2, 512, ("xn", xs))
            for c in range(8):
                P.add("pe", lambda e, xs=xs, c=c: e.transpose(out=psT[xs][:, c * 128:(c + 1) * 128], in_=nb[xs][:, c * 128:(c + 1) * 128],
                                                              identity=ident[:, :]),
                      reads=[("nb", ("xn", xs)), "ident"], writes=[("psT", xs)] if c == 0 else [])
            P.last_w[("psT", xs)] = P.ops["pe"][-1]
            P.add("act", lambda e, xs=xs: e.copy(out=mT[xs][:, :, :], in_=psT[xs][:, :].rearrange("p (c t) -> p c t", c=8)),
                  reads=[("psT", xs)], writes=[("mT", xs)])

        def stage2(t):
            xs = t % 2
            for half in range(2):
                pb = (t * 2 + half) % 4
                for c in range(8):
                    P.add("pe", lambda e, pb=pb, c=c, xs=xs, half=half: e.matmul(
                        out=pso[pb][:, :], lhsT=mT[xs][:, c, :], rhs=wo[:, c, half * 512:(half + 1) * 512], start=(c == 0), stop=(c == 7)),
                        reads=[("mT", xs)] + (wkeys if t == 0 else []), writes=[("pso", pb)] if c == 0 else [])
                P.last_w[("pso", pb)] = P.ops["pe"][-1]
                P.add("dve", lambda e, pb=pb, xs=xs, half=half: e.tensor_tensor(
                    out=xr[xs][:, half * 512:(half + 1) * 512], in0=pso[pb][:, :], in1=xr[xs][:, half * 512:(half + 1) * 512], op=ALU.add),
                    reads=[("pso", pb), ("xr", xs)], writes=[("xr", xs)])
            P.add("sp", lambda e, xs=xs, t=t: e.dma_start(out=T["h1"][t * 128:(t + 1) * 128, :], in_=xr[xs][:, :]),
                  reads=[("xr", xs)], writes=[("xrst", xs)], chan=co[xs])

        stage1(0)
        for t in range(NT):
            if t + 1 < NT:
                stage1(t + 1)
            stage2(t)

    run_phase(nc, build)


def ple_phase(nc, T):
    def build(P, es):
        ident, identf = make_ident(P, nc, es)
        epst = sb(nc, es, "epst", [128, 1], F32)
        P.add("pool", lambda e: e.memset(epst[:, :], EPS), writes=["eps"])
        wg = sb(nc, es, "wg", [128, 8, D], BF16)
        wp = sb(nc, es, "wp", [128, 2, D], BF16)
        stage = [sb(nc, es, "lstg%d" % i, [128, D], F32) for i in range(2)]
        stch = [P.chan() for _ in range(2)]
        gB = sb(nc, es, "gB", [128, D], F32)
        gE = sb(nc, es, "gE", [128, D], F32)
        xn = [sb(nc, es, "xn%d" % i, [128, D], F32) for i in range(2)]
        pt = [sb(nc, es, "pt%d" % i, [128, 256], F32) for i in range(2)]
        pb16 = [sb(nc, es, "pb%d" % i, [128, 256], BF16) for i in range(2)]
        nb = [sb(nc, es, "nb%d" % i, [128, D], BF16) for i in range(2)]
        junk = sb(nc, es, "junk", [128, D], BF16)
        junk2 = sb(nc, es, "junk2", [128, D], BF16)
        ssq = [sb(nc, es, "ssq%d" % i, [128, 4], F32) for i in range(2)]
        rs = [sb(nc, es, "rs%d" % i, [128, 4], F32) for i in range(2)]
        mT = [sb(nc, es, "mT%d" % i, [128, 10, 128], BF16) for i in range(2)]
        gate = [sb(nc, es, "gate%d" % i, [128, D], F32) for i in range(2)]
        ev = [sb(nc, es, "ev%d" % i, [128, D], F32) for i in range(2)]
        psTa = ps(nc, es, "psTa", [128, 8, 128], BF16)
        psTb = ps(nc, es, "psTb", [128, 8, 128], BF16)
        psg = [ps(nc, es, "psg%d" % i, [128, 512], F32) for i in range(2)]
        pse = [ps(nc, es, "pse%d" % i, [128, 512], F32) for i in range(2)]
        cg = P.chan()
        cx = [P.chan() for _ in range(2)]
        cp = [P.chan() for _ in range(2)]
        co = [P.chan() for _ in range(2)]
        P.add("sp", lambda e: e.dma_start(out=gB[:, :], in_=T["ple_gate_norm"].partition_broadcast(128)), writes=["gB"], chan=cg)
        P.add("sp", lambda e: e.dma_start(out=gE[:, :], in_=T["ple_norm"].partition_broadcast(128)), writes=["gE"], chan=cg)
        cg.seal()
        wv = T["ple_w_gate"].rearrange("(c p) f -> p c f", p=128)
        wpv = T["ple_w_proj"].rearrange("(c p) f -> p c f", p=128)
        for c in range(10):
            s_ = c % 2
            src = wv[:, c, :] if c < 8 else wpv[:, c - 8, :]
            dstw = wg[:, c, :] if c < 8 else wp[:, c - 8, :]
            P.add("sp", lambda e, s_=s_, src=src: e.dma_start(out=stage[s_][:, :], in_=src), writes=[("stage", s_)], chan=stch[s_])
            P.add("dve" if c % 2 == 0 else "pool", lambda e, s_=s_, dstw=dstw: e.tensor_copy(out=dstw, in_=stage[s_][:, :]),
                  reads=[("stage", s_)], writes=[("w", c)])
        wkeys = [("w", c) for c in range(10)]
        def stage1(t):
            xs = t % 2
            P.add("sp", lambda e, xs=xs, t=t: e.dma_start(out=xn[xs][:, :], in_=T["h3"][t * 128:(t + 1) * 128, :]), writes=[("xn", xs)], chan=cx[xs])
            P.add("sp", lambda e, xs=xs, t=t: e.dma_start(out=pt[xs][:, :], in_=T["p"][t * 128:(t + 1) * 128, :]), writes=[("pt", xs)], chan=cp[xs])
            norm_rows(P, xn[xs], junk, ssq[xs], rs[xs], epst, nb[xs], gB, 1, D, ("xn", xs))
            P.add("pool", lambda e, xs=xs: e.tensor_copy(out=pb16[xs][:, :], in_=pt[xs][:, :]), reads=[("pt", xs)], writes=[("pb16", xs)])
            for c in range(10):
                src = nb[xs][:, c * 128:(c + 1) * 128] if c < 8 else pb16[xs][:, (c - 8) * 128:(c - 7) * 128]
                dstp = psTa[:, c, :] if c < 8 else psTb[:, c - 8, :]
                P.add("pe", lambda e, dstp=dstp, src=src: e.transpose(out=dstp, in_=src, identity=ident[:, :]),
                      reads=[("nb", ("xn", xs)), ("pb16", xs), "ident"], writes=["psTa" if c < 8 else "psTb"] if c in (0, 8) else [])
                if c == 7:
                    P.last_w["psTa"] = P.ops["pe"][-1]
            P.last_w["psTb"] = P.ops["pe"][-1]
            P.add("act", lambda e, xs=xs: e.copy(out=mT[xs][:, 0:8, :], in_=psTa[:, :, :]), reads=["psTa"], writes=[("mT", xs)])
            P.add("act", lambda e, xs=xs: e.copy(out=mT[xs][:, 8:10, :], in_=psTb[:, 0:2, :]), reads=["psTb"], writes=[("mT2", xs)])

        def stage2(t):
            xs = t % 2
            for half in range(2):
                hsl = slice(half * 512, (half + 1) * 512)
                for c in range(8):
                    P.add("pe", lambda e, c=c, xs=xs, half=half, hsl=hsl: e.matmul(
                        out=psg[half][:, :], lhsT=mT[xs][:, c, :], rhs=wg[:, c, hsl], start=(c == 0), stop=(c == 7)),
                        reads=[("mT", xs), ("mT2", xs)] + (wkeys if t == 0 else []), writes=[("psg", half)] if c == 0 else [])
                P.last_w[("psg", half)] = P.ops["pe"][-1]
                P.add("act", lambda e, xs=xs, half=half, hsl=hsl: e.activation(out=gate[xs][:, hsl], in_=psg[half][:, :], func=AF.Sigmoid),
                      reads=[("psg", half)], writes=[("gate", xs, half)])
                for c in range(2):
                    P.add("pe", lambda e, c=c, xs=xs, half=half, hsl=hsl: e.matmul(
                        out=pse[half][:, :], lhsT=mT[xs][:, 8 + c, :], rhs=wp[:, c, hsl], start=(c == 0), stop=(c == 1)),
                        reads=[("mT", xs), ("mT2", xs)] + (wkeys if t == 0 else []), writes=[("pse", half)] if c == 0 else [])
                P.last_w[("pse", half)] = P.ops["pe"][-1]
                P.add("act", lambda e, xs=xs, half=half, hsl=hsl: e.activation(
                    out=junk2[:, hsl], in_=pse[half][:, :], func=AF.Square, accum_out=ssq[xs][:, 2 + half:3 + half]),
                    reads=[("pse", half)], writes=[("junk2", half), ("ssqe", xs, half)])
            P.add("dve", lambda e, xs=xs: e.tensor_tensor(out=rs[xs][:, 2:3], in0=ssq[xs][:, 2:3], in1=ssq[xs][:, 3:4], op=ALU.add),
                  reads=[("ssqe", xs, 0), ("ssqe", xs, 1)], writes=[("rse", xs)])
            P.add("act", lambda e, xs=xs: e.activation(out=rs[xs][:, 2:3], in_=rs[xs][:, 2:3], func=AF.Sqrt, scale=1.0 / D, bias=epst[:, 0:1]),
                  reads=[("rse", xs), "eps"], writes=[("rse", xs)])
            P.add("dve", lambda e, xs=xs: e.reciprocal(out=rs[xs][:, 2:3], in_=rs[xs][:, 2:3]), reads=[("rse", xs)], writes=[("rse", xs)])
            for half in range(2):
                hsl = slice(half * 512, (half + 1) * 512)
                P.add("dve", lambda e, xs=xs, half=half, hsl=hsl: e.scalar_tensor_tensor(
                    out=ev[xs][:, hsl], in0=pse[half][:, :], scalar=rs[xs][:, 2:3], in1=gE[:, hsl], op0=ALU.mult, op1=ALU.mult),
                    reads=[("pse", half), ("rse", xs), "gE"], writes=[("ev", xs, half)])
                P.add("dve", lambda e, xs=xs, hsl=hsl: e.tensor_tensor(out=ev[xs][:, hsl], in0=ev[xs][:, hsl], in1=gate[xs][:, hsl], op=ALU.mult),
                      reads=[("ev", xs, half), ("gate", xs, half)], writes=[("ev", xs, half)])
                P.add("dve", lambda e, xs=xs, hsl=hsl: e.tensor_tensor(out=ev[xs][:, hsl], in0=ev[xs][:, hsl], in1=xn[xs][:, hsl], op=ALU.add),
                      reads=[("ev", xs, half), ("xn", xs)], writes=[("ev", xs, half)])
            P.add("sp", lambda e, xs=xs, t=t: e.dma_start(out=T["out"][t * 128:(t + 1) * 128, :], in_=ev[xs][:, :]),
                  reads=[("ev", xs, 0), ("ev", xs, 1)], chan=co[xs])


        stage1(0)
        for t in range(NT):
            if t + 1 < NT:
                stage1(t + 1)
            stage2(t)

    run_phase(nc, build)


def rope_tables_np():
    pos = np.arange(S, dtype=np.float32)
    inv = (np.float32(500000.0) ** (-np.arange(0, 16, 2, dtype=np.float32) / np.float32(16))).astype(np.float32)
    ang = (pos[:, None] * inv[None, :]).astype(np.float32)
    return np.cos(ang).astype(np.float32), np.sin(ang).astype(np.float32)


IN_SHAPES = dict(
    x=[S, D], p=[S, 256], ffn1_norm=[D], ffn1_wg=[D, DFF], ffn1_wu=[D, DFF], ffn1_wd=[DFF, D],
    mix_norm=[D], w_in=[D, 2848], b_forget=[8], q_norm_nsa=[64], k_norm_cmp=[64], k_norm_slc=[64], k_norm_win=[64],
    cmp_pos_k=[32, 64], cmp_pos_v=[32, 64], cmp_k_w1=[2048, 256], cmp_k_w2=[256, 64], cmp_v_w1=[2048, 256],
    cmp_v_w2=[256, 64], q_norm_fox=[64], k_norm_fox=[64], out_norm_nsa=[512], out_norm_fox=[512], w_out=[D, D],
    ffn2_norm=[D], ffn2_wg=[D, DFF], ffn2_wu=[D, DFF], ffn2_wd=[DFF, D], ple_gate_norm=[D], ple_w_gate=[D, D],
    ple_w_proj=[256, D], ple_norm=[D], rope_cos=[S, 8], rope_sin=[S, 8])


def build_nc(nph=99, debug=(), skip=()):
    nc = bass.Bass("TRN2", target_bir_lowering=False)
    T = {}
    for name, shape in IN_SHAPES.items():
        T[name] = nc.dram_tensor(name, shape, F32, kind="ExternalInput").ap()

    def scratch(name, shape, dt):
        kind = "ExternalOutput" if name in debug else "Internal"
        T[name] = nc.dram_tensor(name, shape, dt, kind=kind).ap()

    T["out"] = nc.dram_tensor("out", [S, D], F32, kind="ExternalOutput").ap()
    scratch("h1", [S, D], F32)
    scratch("QKT", [32, 64, S], BF16)
    scratch("V", [12, 128, NT, 72], BF16)
    scratch("G", [128, NT * 24], F32)
    scratch("LF", [128, NT * 8], F32)
    scratch("KCT", [2, 64, 256], BF16)
    scratch("VC", [2, 256, 65], BF16)
    scratch("OA", [S, D], F32)
    scratch("h3", [S, D], F32)
    if "DBGB" in debug:
        scratch("DBGB", [64, S], BF16)
    if "DBGT" in debug:
        scratch("DBGT", [3, 65, 512], F32)
    phases = [
        lambda: ffn_half_phase(nc, T, T["x"], T["x"], T["h1"], T["ffn1_norm"], T["ffn1_wg"], T["ffn1_wu"], T["ffn1_wd"], 0, 11, "f1a"),
        lambda: ffn_half_phase(nc, T, T["x"], T["h1"], T["h1"], T["ffn1_norm"], T["ffn1_wg"], T["ffn1_wu"], T["ffn1_wd"], 11, 11, "f1b"),
        lambda: proj_phase(nc, T),
        lambda: cmp_phase(nc, T),
        lambda: nsa_phase(nc, T),
        lambda: fox_phase(nc, T),
        lambda: out_phase(nc, T),
        lambda: ffn_half_phase(nc, T, T["h1"], T["h1"], T["h3"], T["ffn2_norm"], T["ffn2_wg"], T["ffn2_wu"], T["ffn2_wd"], 0, 11, "f2a"),
        lambda: ffn_half_phase(nc, T, T["h1"], T["h3"], T["h3"], T["ffn2_norm"], T["ffn2_wg"], T["ffn2_wu"], T["ffn2_wd"], 11, 11, "f2b"),
        lambda: ple_phase(nc, T),
    ]
    for k, ph in enumerate(phases[:nph]):
        if k not in skip:
            ph()
    return nc


def make_in_maps(inputs, cores=range(8)):
    cos, sin = rope_tables_np()
    in_maps = []
    for b in cores:
        m = {}
        for name in IN_SHAPES:
            if name == "x":
                a = inputs["x"][b]
            elif name == "p":
                a = inputs["p"][0, b]
            elif name == "rope_cos":
                a = cos
            elif name == "rope_sin":
                a = sin
            elif name == "w_in":
                a = inputs["w_in"][0][:, W_IN_PERM]
            else:
                a = inputs[name][0]
            m[name] = np.ascontiguousarray(a, dtype=np.float32)
        in_maps.append(m)
    return in_maps


def kernel(**inputs):
    nc = build_nc()
    res = run_bass_kernel_spmd(nc, make_in_maps(inputs), core_ids=list(range(8)))
    return np.stack([np.asarray(r["out"]) for r in res.results], axis=0)
```
